# Optimizing a Trainium2 kernel written in Bass

```python
import jax, jax.numpy as jnp
from jax import lax
import numpy as np

D_MODEL = 1024
BATCH = 2
SEQ = 16384
DEPTH = 4

BLOCK = 128
N_META = 16
PAD = BLOCK - N_META
EPS = 1e-6
HEAD_DIM = 64
SWA_HEADS = 8
SWA_KV_HEADS = 2
SWA_GROUP = SWA_HEADS // SWA_KV_HEADS
WINDOW = 128
SB_HEADS = 4
SB_SUB = 64
GLA_HEADS = 4
GLA_DK = 32
GLA_DV = 64
GLA_GATE_RANK = 16
GLA_TAU = 16.0
GLA_CHUNK = 16
SWA_WIDTH = SWA_HEADS * HEAD_DIM
SB_WIDTH = SB_HEADS * HEAD_DIM
GLA_WIDTH = GLA_HEADS * GLA_DV
MIX_WIDTH = SWA_WIDTH + SB_WIDTH + GLA_WIDTH
SWA_KV_WIDTH = SWA_KV_HEADS * HEAD_DIM
GLA_K_WIDTH = GLA_HEADS * GLA_DK
IN_COLS = (SWA_WIDTH + 2 * SWA_KV_WIDTH) + 3 * SB_WIDTH + (2 * GLA_K_WIDTH + GLA_WIDTH + GLA_GATE_RANK + GLA_WIDTH)
PEER_HEADS = 8
N_KEYS = 64
N_EXPERTS = N_KEYS * N_KEYS
PEER_DK = 256
PEER_HALF = PEER_DK // 2
PEER_TOPK = 16

kernel_name = "hymba_swa_stickbreak_gla_peer"


def rms_norm(x, g):
    xf = x.astype(jnp.float32)
    y = xf * lax.rsqrt(jnp.mean(xf * xf, axis=-1, keepdims=True) + EPS)
    return (y * g.astype(jnp.float32)).astype(x.dtype)


def sliding_window_attention(q, k, v, sinks):
    b, l = q.shape[:2]
    nb = l // BLOCK
    qb = q.reshape(b, nb, BLOCK, SWA_KV_HEADS, SWA_GROUP, HEAD_DIM)

    def with_prev(t):
        t = t.reshape(b, nb, BLOCK, SWA_KV_HEADS, HEAD_DIM)
        prev = jnp.concatenate([jnp.zeros_like(t[:, :1]), t[:, :-1]], axis=1)
        return jnp.concatenate([prev, t], axis=2)

    kk, vv = with_prev(k), with_prev(v)
    s = jnp.einsum('bnqhgd,bnkhd->bnhgqk', qb, kk).astype(jnp.float32) * (HEAD_DIM ** -0.5)
    blk = jnp.arange(nb)[:, None, None]
    qpos = blk * BLOCK + jnp.arange(BLOCK)[None, :, None]
    kpos = (blk - 1) * BLOCK + jnp.arange(2 * BLOCK)[None, None, :]
    diff = qpos - kpos
    mask = (diff >= 0) & (diff < WINDOW) & (kpos >= PAD)
    s = jnp.where(mask[None, :, None, None], s, -jnp.inf)
    sink = jnp.broadcast_to(sinks.astype(jnp.float32).reshape(1, 1, SWA_KV_HEADS, SWA_GROUP, 1, 1), s.shape[:-1] + (1,))
    p = jax.nn.softmax(jnp.concatenate([s, sink], axis=-1), axis=-1)[..., :-1]
    o = jnp.einsum('bnhgqk,bnkhd->bnqhgd', p.astype(v.dtype), vv)
    return o.reshape(b, l, SWA_HEADS, HEAD_DIM)


def stick_breaking_attention(q, k, v):
    b, l = q.shape[:2]
    nb = l // BLOCK
    tri = (jnp.arange(SB_SUB)[:, None] > jnp.arange(SB_SUB)[None, :]).astype(jnp.float32)
    outs = []
    for n in range(nb):
        nk = (n + 1) * BLOCK
        ns = nk // SB_SUB
        qb = q[:, n * BLOCK:(n + 1) * BLOCK]
        kb, vb = k[:, :nk], v[:, :nk]
        z = jnp.einsum('bqhd,bkhd->bhqk', qb, kb).astype(jnp.float32) * (HEAD_DIM ** -0.5)
        qpos = n * BLOCK + jnp.arange(BLOCK)
        kpos = jnp.arange(nk)
        mask = (kpos[None, :] < qpos[:, None]) & (kpos[None, :] >= PAD)
        log_keep = jnp.where(mask, jax.nn.log_sigmoid(-z), 0.0).reshape(b, SB_HEADS, BLOCK, ns, SB_SUB)
        within = jnp.einsum('bhqnj,js->bhqns', log_keep, tri)
        tot = jnp.sum(log_keep, axis=-1)
        suffix = jnp.cumsum(tot[..., ::-1], axis=-1)[..., ::-1] - tot
        later = (within + suffix[..., None]).reshape(b, SB_HEADS, BLOCK, nk)
        a = jnp.where(mask, jnp.exp(jax.nn.log_sigmoid(z) + later), 0.0)
        outs.append(jnp.einsum('bhqk,bkhd->bqhd', a.astype(v.dtype), vb))
    return jnp.concatenate(outs, axis=1)


def gated_linear_attention(q, k, v, log_a):
    b, l = q.shape[:2]
    nc = l // GLA_CHUNK

    def chunks(t):
        return t.reshape(b, nc, GLA_CHUNK, GLA_HEADS, -1).transpose(0, 1, 3, 2, 4).astype(jnp.float32)

    qc = chunks(q) * (GLA_DK ** -0.5)
    kc, vc = chunks(k), chunks(v)
    cum = jnp.cumsum(chunks(log_a), axis=3)
    tri = jnp.tril(jnp.ones((GLA_CHUNK, GLA_CHUNK), dtype=bool))
    rel = cum[..., :, None, :] - cum[..., None, :, :]
    decay = jnp.exp(jnp.where(tri[:, :, None], rel, -jnp.inf))
    scores = jnp.einsum('bnhtd,bnhsd,bnhtsd->bnhts', qc, kc, decay)
    o_intra = jnp.einsum('bnhts,bnhsv->bnhtv', scores, vc)
    last = cum[..., -1:, :]
    q_in = qc * jnp.exp(cum)
    k_in = kc * jnp.exp(last - cum)
    chunk_decay = jnp.exp(last[..., 0, :])

    def step(state, inp):
        q_i, k_i, v_i, d_i = inp
        out = jnp.einsum('bhtd,bhdv->bhtv', q_i, state)
        state = state * d_i[..., None] + jnp.einsum('bhsd,bhsv->bhdv', k_i, v_i)
        return state, out

    s0 = jnp.zeros((b, GLA_HEADS, GLA_DK, GLA_DV), jnp.float32)
    xs = (jnp.moveaxis(q_in, 1, 0), jnp.moveaxis(k_in, 1, 0), jnp.moveaxis(vc, 1, 0), jnp.moveaxis(chunk_decay, 1, 0))
    _, o_inter = lax.scan(step, s0, xs)
    o = o_intra + jnp.moveaxis(o_inter, 0, 1)
    return o.transpose(0, 1, 3, 2, 4).reshape(b, l, GLA_HEADS, GLA_DV)


def hybrid_mixer(xn, w_in, sinks, gla_w2, gla_b, g_swa, g_sb, g_gla, w_out):
    b, l, _ = xn.shape
    proj = xn @ w_in
    sizes = [SWA_WIDTH, SWA_KV_WIDTH, SWA_KV_WIDTH, SB_WIDTH, SB_WIDTH, SB_WIDTH,
             GLA_K_WIDTH, GLA_K_WIDTH, GLA_WIDTH, GLA_GATE_RANK, GLA_WIDTH]
    splits = [int(c) for c in np.cumsum(sizes)[:-1]]
    q_a, k_a, v_a, q_b, k_b, v_b, q_c, k_c, v_c, gate_lr, r_c = jnp.split(proj, splits, axis=-1)
    valid = (jnp.arange(l) >= PAD)[None, :, None]
    o_a = sliding_window_attention(q_a.reshape(b, l, SWA_HEADS, HEAD_DIM),
                                   k_a.reshape(b, l, SWA_KV_HEADS, HEAD_DIM),
                                   v_a.reshape(b, l, SWA_KV_HEADS, HEAD_DIM), sinks)
    o_a = rms_norm(o_a, g_swa.reshape(SWA_HEADS, HEAD_DIM))
    o_b = stick_breaking_attention(q_b.reshape(b, l, SB_HEADS, HEAD_DIM),
                                   k_b.reshape(b, l, SB_HEADS, HEAD_DIM),
                                   v_b.reshape(b, l, SB_HEADS, HEAD_DIM))
    o_b = rms_norm(o_b, g_sb.reshape(SB_HEADS, HEAD_DIM))
    log_a = jax.nn.log_sigmoid((gate_lr @ gla_w2 + gla_b).astype(jnp.float32)) / GLA_TAU
    k_c = jnp.where(valid, k_c, 0)
    o_c = gated_linear_attention(q_c.reshape(b, l, GLA_HEADS, GLA_DK),
                                 k_c.reshape(b, l, GLA_HEADS, GLA_DK),
                                 v_c.reshape(b, l, GLA_HEADS, GLA_DV),
                                 log_a.reshape(b, l, GLA_HEADS, GLA_DK)).astype(xn.dtype)
    o_c = rms_norm(o_c, g_gla.reshape(GLA_HEADS, GLA_DV)) * jax.nn.silu(r_c).reshape(b, l, GLA_HEADS, GLA_DV)
    mixed = jnp.concatenate([o_a.reshape(b, l, SWA_WIDTH), o_b.reshape(b, l, SB_WIDTH),
                             o_c.reshape(b, l, GLA_WIDTH)], axis=-1)
    return mixed @ w_out


def peer_ffn(xn, wq, sub_keys, u, v):
    b, l, d = xn.shape
    xt = xn.reshape(-1, d)
    t = xt.shape[0]
    q = (xt @ wq).reshape(t, PEER_HEADS, 2, PEER_HALF)
    s = jnp.einsum('thpd,hpnd->thpn', q, sub_keys).astype(jnp.float32)
    s_top, i_top = lax.top_k(s, PEER_TOPK)
    cand_s = (s_top[:, :, 0, :, None] + s_top[:, :, 1, None, :]).reshape(t, PEER_HEADS, PEER_TOPK * PEER_TOPK)
    cand_i = (i_top[:, :, 0, :, None] * N_KEYS + i_top[:, :, 1, None, :]).reshape(t, PEER_HEADS, PEER_TOPK * PEER_TOPK)
    best_s, best_j = lax.top_k(cand_s, PEER_TOPK)
    idx = jnp.take_along_axis(cand_i, best_j, axis=-1).reshape(t, PEER_HEADS * PEER_TOPK)
    g = jax.nn.softmax(best_s, axis=-1).reshape(t, PEER_HEADS * PEER_TOPK)
    h_all = jnp.einsum('td,nd->tn', xt, u)
    h_sel = jnp.take_along_axis(h_all, idx, axis=-1).astype(jnp.float32)
    a = jax.nn.gelu(h_sel, approximate=False) * g
    w = jnp.zeros((t, N_EXPERTS), jnp.float32).at[jnp.arange(t)[:, None], idx].add(a)
    return (w.astype(xt.dtype) @ v).reshape(b, l, d)


def setup_inputs(seed: int = 0) -> dict:
    key = jax.random.key(seed)
    ks = jax.random.split(key, 18)
    nrm = jax.random.normal
    f32 = jnp.float32
    return {
        "x": nrm(ks[0], (BATCH, SEQ, D_MODEL), f32),
        "meta_tokens": nrm(ks[1], (N_META, D_MODEL), f32),
        "attn_norm": 1.0 + 0.02 * nrm(ks[2], (DEPTH, D_MODEL), f32),
        "w_in": nrm(ks[3], (DEPTH, D_MODEL, IN_COLS), f32) * D_MODEL ** -0.5,
        "attn_sinks": 0.5 * nrm(ks[4], (DEPTH, SWA_HEADS), f32),
        "gla_gate_w2": nrm(ks[5], (DEPTH, GLA_GATE_RANK, GLA_K_WIDTH), f32) * GLA_GATE_RANK ** -0.5,
        "gla_gate_b": 0.1 * nrm(ks[6], (DEPTH, GLA_K_WIDTH), f32),
        "swa_out_norm": 1.0 + 0.02 * nrm(ks[7], (DEPTH, SWA_WIDTH), f32),
        "sb_out_norm": 1.0 + 0.02 * nrm(ks[8], (DEPTH, SB_WIDTH), f32),
        "gla_out_norm": 1.0 + 0.02 * nrm(ks[9], (DEPTH, GLA_WIDTH), f32),
        "w_out": nrm(ks[10], (DEPTH, MIX_WIDTH, D_MODEL), f32) * MIX_WIDTH ** -0.5,
        "ffn_norm": 1.0 + 0.02 * nrm(ks[11], (DEPTH, D_MODEL), f32),
        "peer_w_q": nrm(ks[12], (DEPTH, D_MODEL, PEER_HEADS * PEER_DK), f32) * D_MODEL ** -0.5,
        "peer_sub_keys": nrm(ks[13], (DEPTH, PEER_HEADS, 2, N_KEYS, PEER_HALF), f32) * PEER_HALF ** -0.5,
        "peer_u": nrm(ks[14], (DEPTH, N_EXPERTS, D_MODEL), f32) * D_MODEL ** -0.5,
        "peer_v": nrm(ks[15], (DEPTH, N_EXPERTS, D_MODEL), f32) * PEER_TOPK ** -0.5,
        "final_norm": 1.0 + 0.02 * nrm(ks[16], (D_MODEL,), f32),
    }


def reference(x, meta_tokens, attn_norm, w_in, attn_sinks, gla_gate_w2, gla_gate_b, swa_out_norm,
              sb_out_norm, gla_out_norm, w_out, ffn_norm, peer_w_q, peer_sub_keys, peer_u, peer_v, final_norm):
    b = x.shape[0]
    pad = jnp.zeros((b, PAD, D_MODEL), x.dtype)
    meta = jnp.broadcast_to(meta_tokens[None].astype(x.dtype), (b, N_META, D_MODEL))
    h = jnp.concatenate([pad, meta, x], axis=1)
    for i in range(DEPTH):
        h = h + hybrid_mixer(rms_norm(h, attn_norm[i]), w_in[i], attn_sinks[i], gla_gate_w2[i], gla_gate_b[i],
                             swa_out_norm[i], sb_out_norm[i], gla_out_norm[i], w_out[i])
        h = h + peer_ffn(rms_norm(h, ffn_norm[i]), peer_w_q[i], peer_sub_keys[i], peer_u[i], peer_v[i])
    return rms_norm(h[:, BLOCK:], final_norm)
```

```python
import numpy as np
from contextlib import ExitStack
import ml_dtypes
import concourse.bass as bass
import concourse.mybir as mybir
from concourse.bass_utils import run_bass_kernel_spmd

F32 = mybir.dt.float32
BF16 = mybir.dt.bfloat16
AF = mybir.ActivationFunctionType
ALU = mybir.AluOpType
AX = mybir.AxisListType

D = 1024
IN_COLS = 2320
EPS = 1e-6
NEG = -30000.0
NCORES = 8


class KB:
    ENGS = ("pe", "act", "dve", "pool", "sp")
    NDMA = 8

    def __init__(self, nc, stack):
        self.nc = nc
        self._stack = stack
        self.q = {e: [] for e in self.ENGS}
        self.cnt = {e: 0 for e in self.ENGS}
        self.sem = {e: stack.enter_context(nc.semaphore("s_" + e)) for e in self.ENGS}
        self.dsem = {e: [stack.enter_context(nc.semaphore("d_%s%d" % (e, i))) for i in range(self.NDMA)]
                     for e in ("sp", "pool")}
        self.dcnt = {e: [0] * self.NDMA for e in self.dsem}
        self.drot = {e: 0 for e in self.dsem}
        self.seen = {e: {} for e in self.ENGS}
        self.lastw = {}
        self.readers = {}
        self.pending_noinc = {e: False for e in self.ENGS}
        self.excl = set()

    def _deps(self, eng, reads, writes):
        toks = []
        for k in list(reads) + list(writes):
            t = self.lastw.get(k)
            if t is not None:
                toks.append(t)
        for k in writes:
            toks.extend(self.readers.get(k, {}).values())
        waits = {}
        for (sem, val, src) in toks:
            if src == "pe" and eng == "pe":
                continue
            sid = id(sem)
            if self.seen[eng].get(sid, 0) >= val:
                continue
            if sid not in waits or waits[sid][1] < val:
                waits[sid] = (sem, val)
        for sid, (sem, val) in waits.items():
            self.seen[eng][sid] = val
        return list(waits.values())

    def _record(self, rkey, tok, reads, writes):
        for k in writes:
            self.lastw[k] = tok
            self.readers[k] = {}
        for k in reads:
            self.readers.setdefault(k, {})[rkey] = tok

    def op(self, eng, fn, reads=(), writes=(), inc=True):
        ex = [k for k in reads if k in self.excl and k not in writes]
        if ex:
            writes = list(writes) + ex
        waits = self._deps(eng, reads, writes)
        sem = self.sem[eng]
        if inc:
            self.cnt[eng] += 1
            tok = (sem, self.cnt[eng], eng)
            self.pending_noinc[eng] = False
        else:
            tok = (sem, self.cnt[eng] + 1, eng)
            self.pending_noinc[eng] = True
        self.q[eng].append((waits, fn, (sem, 1) if inc else None))
        self._record(eng, tok, reads, writes)

    def dma(self, eng, out, in_, reads=(), writes=()):
        waits = self._deps(eng, reads, writes)
        r = self.drot[eng]
        self.drot[eng] = (r + 1) % self.NDMA
        sem = self.dsem[eng][r]
        prev = self.dcnt[eng][r]
        if prev > 0 and self.seen[eng].get(id(sem), 0) < prev:
            waits.append((sem, prev))
            self.seen[eng][id(sem)] = prev
        self.dcnt[eng][r] += 16
        tok = (sem, self.dcnt[eng][r], "dma")
        self.q[eng].append((waits, lambda e, o=out, i=in_: e.dma_start(out=o, in_=i), (sem, 16)))
        self._record(("dma", id(sem)), tok, reads, writes)

    def coll(self, kind, op, groups, in_, out, reads, writes):
        waits = self._deps("pool", reads, writes)
        if not hasattr(self, "csem"):
            self.csem = self._stack.enter_context(self.nc.semaphore("s_cc"))
            self.ccnt = 0
        if self.ccnt > 0 and self.seen["pool"].get(id(self.csem), 0) < self.ccnt:
            waits.append((self.csem, self.ccnt))
            self.seen["pool"][id(self.csem)] = self.ccnt
        self.ccnt += 1
        tok = (self.csem, self.ccnt, "dma")
        self.q["pool"].append((waits, lambda e, k=kind, o=op, g=groups, i=in_, u=out:
                               e.collective_compute(k, o, replica_groups=g, ins=[i.opt()], outs=[u.opt()]), (self.csem, 1)))
        self._record(("dma", id(self.csem)), tok, reads, writes)

    def wait_all(self, eng, keys):
        waits = self._deps(eng, keys, ())
        self.q[eng].append((waits, None, None))

    def barrier(self):
        for e in self.ENGS:
            waits = []
            for f in self.ENGS:
                if f != e and self.cnt[f] > 0 and self.seen[e].get(id(self.sem[f]), 0) < self.cnt[f]:
                    waits.append((self.sem[f], self.cnt[f]))
                    self.seen[e][id(self.sem[f])] = self.cnt[f]
            for q in self.dsem:
                for r in range(self.NDMA):
                    s, v = self.dsem[q][r], self.dcnt[q][r]
                    if v > 0 and self.seen[e].get(id(s), 0) < v:
                        waits.append((s, v))
                        self.seen[e][id(s)] = v
            if hasattr(self, "csem") and self.ccnt > 0 and self.seen[e].get(id(self.csem), 0) < self.ccnt:
                waits.append((self.csem, self.ccnt))
                self.seen[e][id(self.csem)] = self.ccnt
            self.q[e].append((waits, None, None))

    def emit(self):
        nc = self.nc
        for e in self.ENGS:
            assert not self.pending_noinc[e], "trailing non-inc op on " + e
        qs = self.q
        self.q = {e: [] for e in self.ENGS}
        with nc.Block() as block:
            def run(engname):
                def body(e):
                    for waits, fn, inc in qs[engname]:
                        for sem, val in waits:
                            e.wait_ge(sem, val)
                        if fn is None:
                            continue
                        ins = fn(e)
                        if inc is not None:
                            ins.then_inc(inc[0], inc[1])
                return body
            block.tensor(run("pe"))
            block.scalar(run("act"))
            block.vector(run("dve"))
            block.gpsimd(run("pool"))
            block.sync(run("sp"))

    def act(self, out, in_, func, r, w, eng="act", **kw):
        self.op(eng, lambda e, o=out, i=in_, f=func, k=kw: e.activation(out=o, in_=i, func=f, **k), r, w)

    def tt(self, out, in0, in1, op, r, w, eng="dve"):
        self.op(eng, lambda e, o=out, a=in0, b=in1, p=op: e.tensor_tensor(out=o, in0=a, in1=b, op=p), r, w)

    def ts(self, out, in0, s1, op0, r, w, s2=None, op1=None, eng="dve"):
        if op1 is None:
            self.op(eng, lambda e, o=out, a=in0, x=s1, p=op0: e.tensor_scalar(out=o, in0=a, scalar1=x, scalar2=None, op0=p), r, w)
        else:
            self.op(eng, lambda e, o=out, a=in0, x=s1, y=s2, p=op0, q=op1: e.tensor_scalar(out=o, in0=a, scalar1=x, scalar2=y, op0=p, op1=q), r, w)

    def stt(self, out, in0, scalar, in1, op0, op1, r, w, **kw):
        self.op("dve", lambda e, o=out, a=in0, s=scalar, b=in1, p=op0, q=op1, k=kw:
                e.scalar_tensor_tensor(out=o, in0=a, scalar=s, in1=b, op0=p, op1=q, **k), r, w)

    def cp(self, out, in_, r, w, eng="dve"):
        if eng == "act":
            self.op("act", lambda e, o=out, i=in_: e.activation(out=o, in_=i, func=AF.Copy), r, w)
        else:
            self.op(eng, lambda e, o=out, i=in_: e.tensor_copy(out=o, in_=i), r, w)

    def mm(self, out, lhsT, rhs, start, stop, r, w, inc=True):
        self.op("pe", lambda e, o=out, l=lhsT, x=rhs, s=start, t=stop: e.matmul(o, lhsT=l, rhs=x, start=s, stop=t), r, w, inc=inc)

    def tr(self, out, in_, ident, r, w, inc=True):
        self.op("pe", lambda e, o=out, i=in_, d=ident: e.transpose(o, i, d), r, w, inc=inc)

    def memset(self, ap, val, w, eng="pool"):
        self.op(eng, lambda e, a=ap, v=val: e.memset(a, v), (), w)

    def asel(self, ap, cmp, fill, base, cm, pattern, key):
        self.op("pool", lambda e, a=ap, c=cmp, f=fill, b=base, m=cm, p=pattern:
                e.affine_select(out=a, in_=a, compare_op=c, fill=f, base=b, pattern=p, channel_multiplier=m), [key], [key])


def _ident(kb, T, name="ident"):
    idf = T(name + "_f", [128, 128], F32)
    idb = T(name + "_b", [128, 128], BF16)
    kb.memset(idf[:], 0.0, [name + "_f"])
    kb.asel(idf[:], ALU.not_equal, 1.0, 0, 1, [[-1, 128]], name + "_f")
    kb.cp(idb[:], idf[:], [name + "_f"], [name + "_b"], eng="pool")
    return idf, idb


def _rstd(kb, src, junk, ss, n, rkeys, pfx):
    kb.act(junk, src, AF.Square, rkeys, [pfx + "junk", pfx + "ss"], accum_out=ss)
    kb.ts(ss, ss, 1.0 / n, ALU.mult, [pfx + "ss"], [pfx + "ss"], s2=EPS, op1=ALU.add)
    kb.act(ss, ss, AF.Sqrt, [pfx + "ss"], [pfx + "ss"])
    kb.op("dve", lambda e, a=ss: e.reciprocal(out=a, in_=a), [pfx + "ss"], [pfx + "ss"])


def build_p1(NT):
    nc = bass.Bass("TRN2", target_bir_lowering=False)
    h = nc.dram_tensor("h", [NT * 128, D], F32, kind="ExternalInput").ap()
    w = nc.dram_tensor("w", [D, IN_COLS], F32, kind="ExternalInput").ap()
    g = nc.dram_tensor("g", [128, 8], F32, kind="ExternalInput").ap()
    proj = nc.dram_tensor("proj", [NT * 128, IN_COLS], BF16, kind="ExternalOutput").ap()
    with ExitStack() as st:
        kb = KB(nc, st)
        T = lambda name, shape, dt=F32: st.enter_context(nc.sbuf_tensor(name, shape, dt))
        ps = [st.enter_context(nc.psum_tensor("ps%d" % i, [128, 512], F32)) for i in range(8)]
        idf, idb = _ident(kb, T)
        gt = T("gt", [128, 8])
        kb.dma("sp", gt[:], g, writes=["gt"])
        Wg = T("Wg", [128, 8, IN_COLS], BF16)
        stage = [T("stage%d" % i, [128, IN_COLS]) for i in range(2)]
        for k in range(8):
            sk = "stage%d" % (k % 2)
            kb.dma("sp", stage[k % 2][:], w[k * 128:(k + 1) * 128, :], writes=[sk])
            kb.ts(Wg[:, k, :], stage[k % 2][:], gt[:, k:k + 1], ALU.mult, [sk, "gt"], ["Wg%d" % k])
        ht = [T("ht%d" % i, [128, D]) for i in range(2)]
        junk = T("junk", [128, D], BF16)
        ss = T("ss", [128, 1])
        xn = T("xn", [128, D], BF16)
        xnT = T("xnT", [128, D], BF16)
        pr = [T("pr%d" % i, [128, IN_COLS], BF16) for i in range(2)]
        pT = ps[0][:].bitcast(BF16)
        cgs = [(c0, min(512, IN_COLS - c0)) for c0 in range(0, IN_COLS, 512)]
        wkeys = ["Wg%d" % k for k in range(8)]
        for i in range(NT):
            hk = "ht%d" % (i % 2)
            hb = ht[i % 2]
            kb.dma("sp", hb[:], h[i * 128:(i + 1) * 128, :], writes=[hk])
            _rstd(kb, hb[:], junk[:], ss[:], D, [hk], "a")
            kb.ts(xn[:], hb[:], ss[:, 0:1], ALU.mult, [hk, "ass"], ["xn"])
            for k in range(8):
                kb.tr(pT[:, k * 128:(k + 1) * 128], xn[:, k * 128:(k + 1) * 128], idb[:], ["xn", "ident_b"], ["pT"], inc=(k == 7))
            kb.cp(xnT[:], pT, ["pT"], ["xnT"], eng="act")
            prk = "pr%d" % (i % 2)
            for ci, (c0, cw) in enumerate(cgs):
                pk = "pp%d" % (ci % 2)
                pp = ps[1 + ci % 2]
                for k in range(8):
                    kb.mm(pp[:, 0:cw], xnT[:, k * 128:(k + 1) * 128], Wg[:, k, c0:c0 + cw], k == 0, k == 7,
                          ["xnT", wkeys[k]], [pk], inc=(k == 7))
                kb.cp(pr[i % 2][:, c0:c0 + cw], pp[:, 0:cw], [pk], [prk], eng=("act" if ci % 2 else "dve"))
            kb.dma("pool", proj[i * 128:(i + 1) * 128, :], pr[i % 2][:], reads=[prk], writes=["out"])
        kb.wait_all("pool", ["out"])
        kb.emit()
    return nc


def build_p2(Lp, parts=(1, 1, 1)):
    NCH = Lp // 512
    NB = Lp // 128
    nc = bass.Bass("TRN2", target_bir_lowering=False)
    IN = lambda n, s, dt=BF16: nc.dram_tensor(n, s, dt, kind="ExternalInput").ap()
    qaT = IN("qaT", [128, Lp]); kaT = IN("kaT", [128, Lp]); va = IN("va", [Lp, 64])
    qbT = IN("qbT", [64, Lp]); kbT = IN("kbT", [64, Lp]); vb = IN("vb", [Lp, 64])
    qcT = IN("qcT", [32, Lp]); kcT = IN("kcT", [32, Lp]); vc = IN("vc", [Lp, 64])
    glrT = IN("glrT", [16, Lp])
    w2 = IN("w2", [16, 32], F32); gb = IN("gb", [32, 1], F32); sinks = IN("sinks", [128, 2], F32)
    oa = nc.dram_tensor("oa", [Lp, 128], F32, kind="ExternalOutput").ap()
    obT = nc.dram_tensor("obT", [64, Lp], F32, kind="ExternalOutput").ap()
    oc = nc.dram_tensor("oc", [Lp, 64], F32, kind="ExternalOutput").ap()
    with ExitStack() as st:
        kb = KB(nc, st)
        T = lambda name, shape, dt=F32: st.enter_context(nc.sbuf_tensor(name, shape, dt))
        ps = [st.enter_context(nc.psum_tensor("ps%d" % i, [128, 512], F32)) for i in range(8)]
        idf, idb = _ident(kb, T)
        m_gen = T("m_gen", [128, 256]); m_n0 = T("m_n0", [128, 256]); m_n1 = T("m_n1", [128, 256])
        for m, nm, extra in ((m_gen, "m_gen", None), (m_n0, "m_n0", -240), (m_n1, "m_n1", -112)):
            kb.memset(m[:], 0.0, [nm])
            kb.asel(m[:], ALU.is_ge, NEG, -1, -1, [[1, 256]], nm)
            kb.asel(m[:], ALU.is_ge, NEG, 128, 1, [[-1, 256]], nm)
            if extra is not None:
                kb.asel(m[:], ALU.is_ge, NEG, extra, 0, [[1, 256]], nm)
        sbm_f = T("sbm_f", [128, 512])
        sbm = [T("sbm%d" % d, [128, 512], BF16) for d in range(4)]
        for d in range(4):
            kb.memset(sbm_f[:], 1.0, ["sbm_f"])
            kb.asel(sbm_f[:], ALU.is_ge, 0.0, -1 - 128 * d, -1, [[1, 512]], "sbm_f")
            kb.cp(sbm[d][:], sbm_f[:], ["sbm_f"], ["sbm%d" % d], eng="pool")
        ntri_f = T("ntri_f", [128, 128]); ntri = T("ntri", [128, 128], BF16); nones = T("nones", [128, 128], BF16)
        kb.memset(ntri_f[:], -1.0, ["ntri_f"])
        kb.asel(ntri_f[:], ALU.is_ge, 0.0, 0, 1, [[-1, 128]], "ntri_f")
        kb.cp(ntri[:], ntri_f[:], ["ntri_f"], ["ntri"], eng="pool")
        kb.memset(nones[:], -1.0, ["nones"])
        mle = T("mle", [128, 128])
        kb.memset(mle[:], 1.0, ["mle"])
        kb.asel(mle[:], ALU.is_ge, 0.0, 0, -1, [[1, 128]], "mle")
        rmask = T("rmask", [32, 512])
        kb.memset(rmask[:], 1.0, ["rmask"])
        for b in range(4):
            kb.memset(rmask[:, b * 128:b * 128 + 1], 0.0, ["rmask"])
        w2f = T("w2f", [16, 32]); w2b = T("w2b", [16, 32], BF16); gbt = T("gbt", [32, 1]); sk_t = T("sk_t", [128, 2])
        kb.dma("sp", w2f[:], w2, writes=["w2f"]); kb.dma("sp", gbt[:], gb, writes=["gbt"]); kb.dma("sp", sk_t[:], sinks, writes=["sk"])
        kb.cp(w2b[:], w2f[:], ["w2f"], ["w2b"])
        kb.ts(gbt[:], gbt[:], -1.0, ALU.mult, ["gbt"], ["gbt"])
        KbT = T("KbT", [64, Lp], BF16); Vb = T("Vb", [128, NB, 64], BF16)
        kb.dma("sp", KbT[:], kbT, writes=["KbT"])
        for n0 in range(0, NB, 16):
            n1 = min(NB, n0 + 16)
            kb.dma("sp", Vb[:, n0:n1, :], vb[n0 * 128:n1 * 128, :].rearrange("(n p) d -> p n d", p=128), writes=["Vb"])
        S = T("S", [32, 64]); Sb = T("Sb", [32, 64], BF16)
        kb.memset(S[:], 0.0, ["S"]); kb.memset(Sb[:], 0.0, ["Sb"])
        qa_c = [T("qa_c%d" % i, [128, 512], BF16) for i in range(2)]
        ka_c = [T("ka_c%d" % i, [128, 640], BF16) for i in range(2)]
        va_c = [T("va_c%d" % i, [128, 5, 64], BF16) for i in range(2)]
        qb_c = [T("qb_c%d" % i, [64, 512], BF16) for i in range(2)]
        qs_c = [T("qs_c%d" % i, [64, 512], BF16) for i in range(2)]
        qc_c = [T("qc_c%d" % i, [32, 512], BF16) for i in range(2)]
        kc_c = [T("kc_c%d" % i, [32, 512], BF16) for i in range(2)]
        vc_c = [T("vc_c%d" % i, [128, 4, 64], BF16) for i in range(2)]
        gl_c = [T("gl_c%d" % i, [16, 512], BF16) for i in range(2)]
        sm = T("sm", [128, 256]); pexp = T("pexp", [128, 256], BF16); pTs = T("pTs", [128, 256], BF16)
        st8 = T("st8", [128, 8]); oa_t = [T("oa_t%d" % i, [128, 128]) for i in range(2)]
        ge = T("ge", [32, 512]); gsp = T("gsp", [32, 512]); gcs = T("gcs", [32, 512])
        geq = T("geq", [32, 512]); gek = T("gek", [32, 512])
        qt = T("qt", [32, 512], BF16); kt = T("kt", [32, 512], BF16)
        scb = T("scb", [128, 128], BF16); ktm = T("ktm", [128, 32], BF16); oc_t = [T("oc_t%d" % i, [128, 64]) for i in range(2)]
        stmp = T("stmp", [32, 64])
        e_t = [T("e_t%d" % i, [128, 512]) for i in range(2)]
        sp_t = [T("sp_t%d" % i, [128, 512], BF16) for i in range(2)]
        a_t = [T("a_t%d" % i, [128, 512], BF16) for i in range(2)]
        R = T("R", [128, 512], BF16)
        ob_t = [T("ob_t%d" % i, [64, 512]) for i in range(2)]
        pz = [ps[0], ps[1]]; pc = [ps[2], ps[3]]; pO = ps[4]
        pA = ps[5]; pX = ps[6]; pB = ps[7]
        pT_bf = pB[:].bitcast(BF16)

        def load_chunk(c):
            i = c % 2
            c0 = c * 512
            kb.dma("sp", qa_c[i][:], qaT[:, c0:c0 + 512], writes=["qa_c%d" % i])
            if c == 0:
                kb.memset(ka_c[i][:, 0:128], 0.0, ["ka_c%d" % i])
                kb.memset(va_c[i][:, 0, :], 0.0, ["va_c%d" % i])
                kb.dma("sp", ka_c[i][:, 128:640], kaT[:, 0:512], writes=["ka_c%d" % i])
                kb.dma("sp", va_c[i][:, 1:5, :], va[0:512, :].rearrange("(n p) d -> p n d", p=128), writes=["va_c%d" % i])
            else:
                kb.dma("sp", ka_c[i][:], kaT[:, c0 - 128:c0 + 512], writes=["ka_c%d" % i])
                kb.dma("sp", va_c[i][:], va[c0 - 128:c0 + 512, :].rearrange("(n p) d -> p n d", p=128), writes=["va_c%d" % i])
            kb.dma("sp", qb_c[i][:], qbT[:, c0:c0 + 512], writes=["qb_c%d" % i])
            kb.dma("sp", qc_c[i][:], qcT[:, c0:c0 + 512], writes=["qc_c%d" % i])
            kb.dma("sp", kc_c[i][:], kcT[:, c0:c0 + 512], writes=["kc_c%d" % i])
            kb.dma("sp", vc_c[i][:], vc[c0:c0 + 512, :].rearrange("(n p) d -> p n d", p=128), writes=["vc_c%d" % i])
            kb.dma("sp", gl_c[i][:], glrT[:, c0:c0 + 512], writes=["gl_c%d" % i])

        def _gla(c, i):
            kb.mm(pX[0:32, :], w2b[:], gl_c[i][:], True, True, ["w2b", "gl_c%d" % i], ["pX"])
            kb.act(ge[:], pX[0:32, :], AF.Exp, ["pX", "gbt"], ["ge"], bias=gbt[:, 0:1], scale=-1.0)
            kb.act(gsp[:], ge[:], AF.Ln, ["ge"], ["gsp"], bias=1.0, scale=1.0)
            kb.op("dve", lambda e: e.tensor_tensor_scan(out=gcs[:], data0=rmask[:], data1=gsp[:], initial=0.0, op0=ALU.mult, op1=ALU.add),
                  ["rmask", "gsp"], ["gcs"])
            kb.act(geq[:], gcs[:], AF.Exp, ["gcs"], ["geq"], scale=-1.0 / 16.0)
            kb.act(gek[:], gcs[:], AF.Exp, ["gcs"], ["gek"], scale=1.0 / 16.0)
            kb.stt(qt[:], qc_c[i][:], 32.0 ** -0.5, geq[:], ALU.mult, ALU.mult, ["qc_c%d" % i, "geq"], ["qt"])
            kb.tt(kt[:], kc_c[i][:], gek[:], ALU.mult, ["kc_c%d" % i, "gek"], ["kt"])
            for blk in range(4):
                n = 4 * c + blk
                bs = slice(blk * 128, (blk + 1) * 128)
                kb.mm(pA[:, 384:512], kt[:, bs], qt[:, bs], True, True, ["kt", "qt"], ["pA"])
                kb.tt(scb[:], pA[:, 384:512], mle[:], ALU.mult, ["pA", "mle"], ["scb"])
                kb.tr(pT_bf[:, 512:544], kt[:, bs], idb[0:32, 0:32], ["kt", "ident_b"], ["pB"])
                kb.cp(ktm[:], pT_bf[:, 512:544], ["pB"], ["ktm"], eng="act")
                kb.mm(pA[:, 320:384], scb[:], vc_c[i][:, blk, :], True, False, ["scb", "vc_c%d" % i], ["pA"], inc=False)
                kb.mm(pA[:, 320:384], qt[:, bs], Sb[:], False, True, ["qt", "Sb"], ["pA"])
                ock = "oc_t%d" % (n % 2)
                kb.cp(oc_t[n % 2][:], pA[:, 320:384], ["pA"], [ock], eng="act")
                kb.dma("pool", oc[n * 128:(n + 1) * 128, :], oc_t[n % 2][:], reads=[ock], writes=["oc"])
                kb.mm(pB[0:32, 384:448], ktm[:], vc_c[i][:, blk, :], True, True, ["ktm", "vc_c%d" % i], ["pB"])
                kb.tt(stmp[:], pB[0:32, 384:448], S[:], ALU.add, ["pB", "S"], ["stmp"])
                kb.ts(S[:], stmp[:], geq[:, blk * 128 + 127:blk * 128 + 128], ALU.mult, ["stmp", "geq"], ["S"])
                kb.cp(Sb[:], S[:], ["S"], ["Sb"])

        def _sb(c, i):
            kb.ts(qs_c[i][:], qb_c[i][:], 0.125, ALU.mult, ["qb_c%d" % i], ["qs_c%d" % i])
            nkb = 4 * c + 4
            for it in range(nkb):
                kblk = nkb - 1 - it
                j = it % 2
                dg = kblk - 4 * c
                first, last = (it == 0), (kblk == 0)
                ksl = KbT[:, kblk * 128:(kblk + 1) * 128]
                kb.mm(pz[j][:], ksl, qs_c[i][:], True, True, ["KbT", "qs_c%d" % i], ["pz%d" % j])
                kb.act(e_t[j][:], pz[j][:], AF.Exp, ["pz%d" % j], ["e_t%d" % j])
                kb.act(sp_t[j][:], e_t[j][:], AF.Ln, ["e_t%d" % j], ["sp_t%d" % j], bias=1.0, scale=1.0)
                if dg >= 0:
                    kb.tt(sp_t[j][:], sp_t[j][:], sbm[dg][:], ALU.mult, ["sp_t%d" % j, "sbm%d" % dg], ["sp_t%d" % j])
                kb.mm(pc[j][:], ntri[:], sp_t[j][:], True, False, ["ntri", "sp_t%d" % j], ["pc%d" % j], inc=False)
                if not first:
                    kb.mm(pc[j][:], nones[:], R[:], False, False, ["nones", "R"], ["pc%d" % j], inc=False)
                kb.mm(pc[j][:], ksl, qs_c[i][:], False, True, ["KbT", "qs_c%d" % i], ["pc%d" % j])
                if not last:
                    if first:
                        kb.cp(R[:], sp_t[j][:], ["sp_t%d" % j], ["R"], eng="pool")
                    else:
                        kb.tt(R[:], R[:], sp_t[j][:], ALU.add, ["R", "sp_t%d" % j], ["R"], eng="pool")
                kb.act(a_t[j][:], pc[j][:], AF.Exp, ["pc%d" % j], ["a_t%d" % j])
                if dg >= 0:
                    kb.tt(a_t[j][:], a_t[j][:], sbm[dg][:], ALU.mult, ["a_t%d" % j, "sbm%d" % dg], ["a_t%d" % j])
                kb.mm(pO[0:64, :], Vb[:, kblk, :], a_t[j][:], first, last, ["Vb", "a_t%d" % j], ["pO"], inc=True)
            kb.cp(ob_t[i][:], pO[0:64, :], ["pO"], ["ob_t%d" % i])
            kb.dma("pool", obT[:, c * 512:(c + 1) * 512], ob_t[i][:], reads=["ob_t%d" % i], writes=["ob"])

        load_chunk(0)
        for c in range(NCH):
            i = c % 2
            if c + 1 < NCH:
                load_chunk(c + 1)
            for blk in (range(4) if parts[0] else []):
                n = 4 * c + blk
                msk = m_n0 if n == 0 else (m_n1 if n == 1 else m_gen)
                mk = "m_n0" if n == 0 else ("m_n1" if n == 1 else "m_gen")
                ok = "oa_t%d" % (n % 2)
                for hh in range(2):
                    hs = slice(hh * 64, (hh + 1) * 64)
                    kb.mm(pA[:, 0:256], qa_c[i][hs, blk * 128:(blk + 1) * 128], ka_c[i][hs, blk * 128:blk * 128 + 256],
                          True, True, ["qa_c%d" % i, "ka_c%d" % i], ["pA"])
                    kb.stt(sm[:], pA[:, 0:256], 0.125, msk[:], ALU.mult, ALU.add, ["pA", mk], ["sm"])
                    kb.op("dve", lambda e: e.tensor_reduce(out=st8[:, 0:1], in_=sm[:], axis=AX.X, op=ALU.max), ["sm"], ["st8"])
                    kb.tt(st8[:, 0:1], st8[:, 0:1], sk_t[:, hh:hh + 1], ALU.max, ["st8", "sk"], ["st8"])
                    kb.ts(st8[:, 1:2], st8[:, 0:1], -1.0, ALU.mult, ["st8"], ["st8"])
                    kb.act(pexp[:], sm[:], AF.Exp, ["sm", "st8"], ["pexp", "st8"], bias=st8[:, 1:2], scale=1.0, accum_out=st8[:, 2:3])
                    kb.act(st8[:, 3:4], sk_t[:, hh:hh + 1], AF.Exp, ["sk", "st8"], ["st8"], bias=st8[:, 1:2], scale=1.0)
                    kb.tt(st8[:, 4:5], st8[:, 2:3], st8[:, 3:4], ALU.add, ["st8"], ["st8"])
                    kb.op("dve", lambda e: e.reciprocal(out=st8[:, 5:6], in_=st8[:, 4:5]), ["st8"], ["st8"])
                    kb.tr(pT_bf[:, 0:128], pexp[:, 0:128], idb[:], ["pexp", "ident_b"], ["pB"], inc=False)
                    kb.tr(pT_bf[:, 128:256], pexp[:, 128:256], idb[:], ["pexp", "ident_b"], ["pB"])
                    kb.cp(pTs[:], pT_bf[:, 0:256], ["pB"], ["pTs"], eng="act")
                    kb.mm(pA[:, 256:320], pTs[:, 0:128], va_c[i][:, blk, :], True, False, ["pTs", "va_c%d" % i], ["pA"], inc=False)
                    kb.mm(pA[:, 256:320], pTs[:, 128:256], va_c[i][:, blk + 1, :], False, True, ["pTs", "va_c%d" % i], ["pA"])
                    kb.ts(oa_t[n % 2][:, hs], pA[:, 256:320], st8[:, 5:6], ALU.mult, ["pA", "st8"], [ok])
                kb.dma("pool", oa[n * 128:(n + 1) * 128, :], oa_t[n % 2][:], reads=[ok], writes=["oa"])
            if parts[1]:
              _gla(c, i)
            if parts[2]:
              _sb(c, i)
        kb.wait_all("pool", ["oa", "oc", "ob"])
        kb.emit()
    return nc


def build_p3(NT, final):
    nc = bass.Bass("TRN2", target_bir_lowering=False)
    IN = lambda n, s, dt=F32: nc.dram_tensor(n, s, dt, kind="ExternalInput").ap()
    h = IN("h", [NT * 128, D]); o = IN("o", [NT * 128, D]); rc = IN("rc", [NT * 128, 256], BF16)
    gmix = IN("gmix", [128, D]); wout = IN("wout", [D, D]); gffn = IN("gffn", [128, 8])
    wq = IN("wq", [D, 2048]); ksub = IN("ksub", [128, 16, 64]); uT = IN("uT", [D, 4096]); v = IN("v", [4096, D])
    gfin = IN("gfin", [128, D])
    hout = nc.dram_tensor("hout", [NT * 128, D], F32, kind="ExternalOutput").ap()
    h1_d = nc.dram_tensor("h1_d", [NT * 128, D], F32, kind="Internal").ap()
    sc_d = nc.dram_tensor("sc_d", [NT * 128, D], F32, kind="Internal").ap()
    tb_d = nc.dram_tensor("tb_d", [NT * 128, 16], F32, kind="Internal").ap()
    xT_d = nc.dram_tensor("xT_d", [NT * 128, D], BF16, kind="Internal").ap()
    with ExitStack() as st:
        kb = KB(nc, st)
        ps = [st.enter_context(nc.psum_tensor("ps%d" % i, [128, 512], F32)) for i in range(8)]
        T0 = lambda name, shape, dt=F32: st.enter_context(nc.sbuf_tensor(name, shape, dt))
        idf, idb = _ident(kb, T0)
        gft = T0("gft", [128, 8])
        kb.dma("sp", gft[:], gffn, writes=["gft"])
        stage = [T0("stage%d" % i, [128, 1024]) for i in range(2)]
        scnt = [0]

        def load_w(dst, src, rows_k, cols, key, scale_col=None):
            for k in range(rows_k):
                for c0 in range(0, cols, 1024):
                    cw = min(1024, cols - c0)
                    si = scnt[0] % 2
                    scnt[0] += 1
                    sk = "stage%d" % si
                    kb.dma("sp", stage[si][:, 0:cw], src[k * 128:(k + 1) * 128, c0:c0 + cw], writes=[sk])
                    if scale_col:
                        kb.ts(dst[:, k, c0:c0 + cw], stage[si][:, 0:cw], gft[:, k:k + 1], ALU.mult, [sk, "gft"], [key])
                    else:
                        kb.cp(dst[:, k, c0:c0 + cw], stage[si][:, 0:cw], [sk], [key], eng="pool")

        with ExitStack() as sa:
            T = lambda name, shape, dt=F32: sa.enter_context(nc.sbuf_tensor(name, shape, dt))
            Wo = T("Wo", [128, 8, D], BF16); Wq = T("Wq", [128, 8, 2048], BF16); Ks = T("Ks", [128, 16, 64], BF16)
            gm = T("gm", [128, D]); ksf = T("ksf", [128, 16, 64])
            kb.dma("sp", gm[:], gmix, writes=["gm"])
            kb.dma("sp", ksf[:], ksub, writes=["ksf"])
            kb.cp(Ks[:], ksf[:], ["ksf"], ["Ks"])
            load_w(Wo, wout, 8, D, "Wo")
            load_w(Wq, wq, 8, 2048, "Wq", scale_col=True)
            ht = [T("ht%d" % i, [128, D]) for i in range(2)]
            ot = [T("ot%d" % i, [128, D]) for i in range(2)]
            rct = [T("rct%d" % i, [128, 256], BF16) for i in range(2)]
            sq = T("sq", [128, D]); m1 = T("m1", [128, D]); sil = T("sil", [128, 256])
            s16 = T("s16", [128, 16]); mix = T("mix", [128, D], BF16); mixT = T("mixT", [128, D], BF16)
            h1 = T("h1", [128, D]); junk = T("junk", [128, D], BF16); ss = T("ss", [128, 1])
            xn = T("xn", [128, D], BF16); xnT = T("xnT", [128, D], BF16)
            qT = T("qT", [128, 16, 128], BF16); sct = T("sct", [128, D])
            t1 = T("t1", [128, 8, 16]); t2 = T("t2", [128, 8, 16]); wk = T("wk", [128, 256])
            cand = T("cand", [128, 8, 256]); c8a = T("c8a", [128, 8, 8]); c8b = T("c8b", [128, 8, 8])
            csh = T("csh", [128, 8, 256]); ec = T("ec", [128, 8, 256]); mk8 = T("mk8", [128, 8, 256])
            Z = T("Z", [128, 8]); tb = T("tb", [128, 16])
            pT = ps[0][:].bitcast(BF16)
            for i in range(NT):
                b = i % 2
                rs = slice(i * 128, (i + 1) * 128)
                kb.dma("sp", ht[b][:], h[rs, :], writes=["ht%d" % b])
                kb.dma("sp", ot[b][:], o[rs, :], writes=["ot%d" % b])
                kb.dma("sp", rct[b][:], rc[rs, :], writes=["rct%d" % b])
                kb.act(sq[:], ot[b][:], AF.Square, ["ot%d" % b], ["sq"])
                kb.op("dve", lambda e: e.tensor_reduce(out=s16[:], in_=sq[:].rearrange("p (h d) -> p h d", d=64), axis=AX.X, op=ALU.add),
                      ["sq"], ["s16"])
                kb.ts(s16[:], s16[:], 1.0 / 64, ALU.mult, ["s16"], ["s16"], s2=EPS, op1=ALU.add)
                kb.act(s16[:], s16[:], AF.Sqrt, ["s16"], ["s16"])
                kb.op("dve", lambda e: e.reciprocal(out=s16[:], in_=s16[:]), ["s16"], ["s16"])
                kb.tt(m1[:].rearrange("p (h d) -> p h d", d=64), ot[b][:].rearrange("p (h d) -> p h d", d=64),
                      s16[:].unsqueeze(2).to_broadcast([128, 16, 64]), ALU.mult, ["ot%d" % b, "s16"], ["m1"])
                kb.act(sil[:], rct[b][:], AF.Silu, ["rct%d" % b], ["sil"])
                kb.tt(m1[:, 768:1024], m1[:, 768:1024], sil[:], ALU.mult, ["m1", "sil"], ["m1"])
                kb.tt(mix[:], m1[:], gm[:], ALU.mult, ["m1", "gm"], ["mix"])
                for k in range(8):
                    kb.tr(pT[:, k * 128:(k + 1) * 128], mix[:, k * 128:(k + 1) * 128], idb[:], ["mix", "ident_b"], ["pT"], inc=(k == 7))
                kb.cp(mixT[:], pT, ["pT"], ["mixT"], eng="act")
                for dh in range(2):
                    for k in range(8):
                        kb.mm(ps[1 + dh][:], mixT[:, k * 128:(k + 1) * 128], Wo[:, k, dh * 512:(dh + 1) * 512], k == 0, k == 7,
                              ["mixT", "Wo"], ["pd%d" % dh], inc=(k == 7))
                    kb.tt(h1[:, dh * 512:(dh + 1) * 512], ht[b][:, dh * 512:(dh + 1) * 512], ps[1 + dh][:], ALU.add,
                          ["ht%d" % b, "pd%d" % dh], ["h1"])
                kb.dma("pool", h1_d[rs, :], h1[:], reads=["h1"], writes=["h1_d"])
                _rstd(kb, h1[:], junk[:], ss[:], D, ["h1"], "b")
                kb.ts(xn[:], h1[:], ss[:, 0:1], ALU.mult, ["h1", "bss"], ["xn"])
                for k in range(8):
                    kb.tr(pT[:, k * 128:(k + 1) * 128], xn[:, k * 128:(k + 1) * 128], idb[:], ["xn", "ident_b"], ["pT"], inc=(k == 7))
                kb.cp(xnT[:], pT, ["pT"], ["xnT"], eng="act")
                kb.dma("pool", xT_d[rs, :], xnT[:], reads=["xnT"], writes=["xT_d"])
                for cg in range(4):
                    pq = ps[3 + cg % 2]
                    for cc in range(4):
                        cidx = cg * 4 + cc
                        for k in range(8):
                            kb.mm(pq[:, cc * 128:(cc + 1) * 128], Wq[:, k, cidx * 128:(cidx + 1) * 128], xnT[:, k * 128:(k + 1) * 128],
                                  k == 0, k == 7, ["Wq", "xnT"], ["pq%d" % (cg % 2)], inc=(k == 7 and cc == 3))
                    kb.cp(qT[:, cg * 4:(cg + 1) * 4, :], pq[:].rearrange("p (c t) -> p c t", t=128), ["pq%d" % (cg % 2)], ["qT"],
                          eng=("act" if cg % 2 else "dve"))
                for cidx in range(16):
                    pscb = ps[5 + cidx // 8]
                    kb.mm(pscb[:, (cidx % 8) * 64:(cidx % 8 + 1) * 64], qT[:, cidx, :], Ks[:, cidx, :], True, True, ["qT", "Ks"],
                          ["psc%d" % (cidx // 8)], inc=(cidx % 8 == 7))
                kb.cp(sct[:, 0:512], ps[5][:], ["psc0"], ["sct"], eng="act")
                kb.cp(sct[:, 512:1024], ps[6][:], ["psc1"], ["sct"], eng="dve")
                kb.dma("pool", sc_d[rs, :], sct[:], reads=["sct"], writes=["sc_d"])
                for hd in range(8):
                    for side, tt_ in ((0, t1), (1, t2)):
                        sv = sct[:, hd * 128 + side * 64:hd * 128 + side * 64 + 64]
                        kb.op("dve", lambda e, o_=tt_[:, hd, 0:8], i_=sv: e.max(out=o_, in_=i_), ["sct"], ["tt"])
                        kb.op("dve", lambda e, o_=wk[:, 0:64], r_=tt_[:, hd, 0:8], i_=sv: e.match_replace(out=o_, in_to_replace=r_, in_values=i_, imm_value=-1e30),
                              ["sct", "tt"], ["wk"])
                        kb.op("dve", lambda e, o_=tt_[:, hd, 8:16], i_=wk[:, 0:64]: e.max(out=o_, in_=i_), ["wk"], ["tt"])
                kb.tt(cand[:].rearrange("p h (a b) -> p h a b", b=16), t1[:].unsqueeze(3).to_broadcast([128, 8, 16, 16]),
                      t2[:].unsqueeze(2).to_broadcast([128, 8, 16, 16]), ALU.add, ["tt"], ["cand"])
                for hd in range(8):
                    kb.op("dve", lambda e, o_=c8a[:, hd, :], i_=cand[:, hd, :]: e.max(out=o_, in_=i_), ["cand"], ["c8"])
                    kb.op("dve", lambda e, o_=wk[:], r_=c8a[:, hd, :], i_=cand[:, hd, :]: e.match_replace(out=o_, in_to_replace=r_, in_values=i_, imm_value=-1e30),
                          ["cand", "c8"], ["wk"])
                    kb.op("dve", lambda e, o_=c8b[:, hd, :], i_=wk[:]: e.max(out=o_, in_=i_), ["wk"], ["c8"])
                kb.tt(csh[:], cand[:], c8a[:, :, 0:1].to_broadcast([128, 8, 256]), ALU.subtract, ["cand", "c8"], ["csh"])
                kb.act(ec[:], csh[:], AF.Exp, ["csh"], ["ec"])
                kb.tt(mk8[:], cand[:], c8b[:, :, 7:8].to_broadcast([128, 8, 256]), ALU.is_ge, ["cand", "c8"], ["mk8"])
                kb.tt(ec[:], ec[:], mk8[:], ALU.mult, ["ec", "mk8"], ["ec"])
                kb.op("dve", lambda e: e.tensor_reduce(out=Z[:], in_=ec[:], axis=AX.X, op=ALU.add), ["ec"], ["Z"])
                kb.act(Z[:], Z[:], AF.Ln, ["Z"], ["Z"])
                kb.cp(tb[:, 0:8], c8b[:, :, 7], ["c8"], ["tb"])
                kb.stt(tb[:, 8:16], c8a[:, :, 0], -1.0, Z[:], ALU.mult, ALU.subtract, ["c8", "Z"], ["tb"])
                kb.dma("pool", tb_d[rs, :], tb[:], reads=["tb"], writes=["tb_d"])
            kb.barrier()
            kb.emit()
        with ExitStack() as sb_:
            T = lambda name, shape, dt=F32: sb_.enter_context(nc.sbuf_tensor(name, shape, dt))
            Ub = T("Ub", [128, 8, 4096], BF16); Vv = T("Vv", [128, 32, D], BF16)
            load_w(Ub, uT, 8, 4096, "Ub", scale_col=True)
            load_w(Vv, v, 32, D, "Vv")
            gf = T("gf", [128, D])
            if final:
                kb.dma("sp", gf[:], gfin, writes=["gf"])
            h1t = [T("h1t%d" % i, [128, D]) for i in range(2)]
            sct = [T("sctb%d" % i, [128, D]) for i in range(2)]
            xT = [T("xTb%d" % i, [128, D], BF16) for i in range(2)]
            tbt = [T("tbt%d" % i, [128, 16]) for i in range(2)]
            Sg = [T("Sg%d" % i, [128, 16, 64]) for i in range(2)]
            Eg = [T("Eg%d" % i, [128, 1024], BF16) for i in range(2)]
            Gh = T("Gh", [128, 1024], BF16); G = T("G", [128, 1024])
            gl = [T("gl%d" % i, [128, 512]) for i in range(2)]
            Wb = T("Wb", [128, 1024], BF16); WT = T("WT", [128, 1024], BF16)
            ho = T("ho", [128, D]); junk = T("junkb", [128, D], BF16); ss = T("ssb", [128, 1])
            pH = [ps[0], ps[1]]; ptr = ps[2][:].bitcast(BF16); po = [ps[3], ps[4]]

            def loadB(i):
                b = i % 2
                rs = slice(i * 128, (i + 1) * 128)
                kb.dma("sp", h1t[b][:], h1_d[rs, :], reads=["h1_d"], writes=["h1t%d" % b])
                kb.dma("sp", sct[b][:], sc_d[rs, :], reads=["sc_d"], writes=["sctb%d" % b])
                kb.dma("sp", xT[b][:], xT_d[rs, :], reads=["xT_d"], writes=["xTb%d" % b])
                kb.dma("sp", tbt[b][:], tb_d[rs, :], reads=["tb_d"], writes=["tbt%d" % b])

            loadB(0)
            cnt = 0
            for i in range(NT):
                b = i % 2
                rs = slice(i * 128, (i + 1) * 128)
                if i + 1 < NT:
                    loadB(i + 1)
                for eq in range(4):
                    for hd in range(8):
                        j = cnt % 2
                        cnt += 1
                        s1 = sct[b][:, hd * 128 + 16 * eq:hd * 128 + 16 * eq + 16]
                        s2 = sct[b][:, hd * 128 + 64:hd * 128 + 128]
                        kb.tt(Sg[j][:], s1.unsqueeze(2).to_broadcast([128, 16, 64]), s2.unsqueeze(1).to_broadcast([128, 16, 64]), ALU.add,
                              ["sctb%d" % b], ["Sg%d" % j], eng="pool")
                        Sf = Sg[j][:].rearrange("p a b -> p (a b)")
                        kb.act(Eg[j][:], Sf, AF.Exp, ["Sg%d" % j, "tbt%d" % b], ["Eg%d" % j], bias=tbt[b][:, 8 + hd:9 + hd], scale=1.0)
                        if hd == 0:
                            kb.stt(G[:], Sf, tbt[b][:, hd:hd + 1], Eg[j][:], ALU.is_ge, ALU.mult, ["Sg%d" % j, "Eg%d" % j, "tbt%d" % b], ["G"])
                        else:
                            kb.stt(Gh[:], Sf, tbt[b][:, hd:hd + 1], Eg[j][:], ALU.is_ge, ALU.mult, ["Sg%d" % j, "Eg%d" % j, "tbt%d" % b], ["Gh"])
                            kb.tt(G[:], G[:], Gh[:], ALU.add, ["G", "Gh"], ["G"])
                    for g2 in range(2):
                        e0 = eq * 1024 + g2 * 512
                        for k in range(8):
                            kb.mm(pH[g2][:], xT[b][:, k * 128:(k + 1) * 128], Ub[:, k, e0:e0 + 512], k == 0, k == 7,
                                  ["xTb%d" % b, "Ub"], ["pH%d" % g2], inc=(k == 7))
                        kb.act(gl[g2][:], pH[g2][:], AF.Gelu, ["pH%d" % g2], ["gl%d" % g2])
                        kb.tt(Wb[:, g2 * 512:(g2 + 1) * 512], gl[g2][:], G[:, g2 * 512:(g2 + 1) * 512], ALU.mult, ["gl%d" % g2, "G"], ["Wb"])
                    for cc in range(8):
                        kb.tr(ptr[:, cc * 128:(cc + 1) * 128], Wb[:, cc * 128:(cc + 1) * 128], idb[:], ["Wb", "ident_b"], ["ptr"], inc=(cc == 7))
                    kb.cp(WT[:], ptr, ["ptr"], ["WT"], eng="act")
                    for dh in range(2):
                        for cc in range(8):
                            kb.mm(po[dh][:], WT[:, cc * 128:(cc + 1) * 128], Vv[:, eq * 8 + cc, dh * 512:(dh + 1) * 512],
                                  (eq == 0 and cc == 0), (eq == 3 and cc == 7), ["WT", "Vv"], ["po%d" % dh], inc=(cc == 7))
                for dh in range(2):
                    kb.tt(ho[:, dh * 512:(dh + 1) * 512], h1t[b][:, dh * 512:(dh + 1) * 512], po[dh][:], ALU.add,
                          ["h1t%d" % b, "po%d" % dh], ["ho"])
                if final:
                    _rstd(kb, ho[:], junk[:], ss[:], D, ["ho"], "f")
                    kb.stt(ho[:], ho[:], ss[:, 0:1], gf[:], ALU.mult, ALU.mult, ["ho", "fss", "gf"], ["ho"])
                kb.dma("sp", hout[rs, :], ho[:], reads=["ho"], writes=["hout"])
            kb.wait_all("sp", ["hout"])
            kb.emit()
    return nc


_CACHE = {}


def _prog(name, *args):
    key = (name,) + args
    if key not in _CACHE:
        _CACHE[key] = {"p1": build_p1, "p2": build_p2, "p3": build_p3}[name](*args)
    return _CACHE[key]


def _run(nc, in_maps):
    res = run_bass_kernel_spmd(nc, in_maps, core_ids=list(range(NCORES)))
    return res.results


def _c(a):
    return np.ascontiguousarray(a)


def _gk(g):
    return _c(np.asarray(g, np.float32).reshape(8, 128).T)


def forward(x, meta_tokens, attn_norm, w_in, attn_sinks, gla_gate_w2, gla_gate_b, swa_out_norm,
            sb_out_norm, gla_out_norm, w_out, ffn_norm, peer_w_q, peer_sub_keys, peer_u, peer_v, final_norm):
    x = np.asarray(x, np.float32)
    B, SEQ, _ = x.shape
    depth = attn_norm.shape[0]
    L = SEQ + 128
    Lp = ((L + 511) // 512) * 512
    T = B * L
    NT = (T // 128 + NCORES - 1) // NCORES
    Tp = NT * 128 * NCORES
    hfull = np.zeros((Tp, D), np.float32)
    for b in range(B):
        hfull[b * L + 112:b * L + 128] = meta_tokens
        hfull[b * L + 128:(b + 1) * L] = x[b]
    p1 = _prog("p1", NT)
    p2 = _prog("p2", Lp)
    tsl = [slice(c * NT * 128, (c + 1) * NT * 128) for c in range(NCORES)]
    bf = ml_dtypes.bfloat16
    for i in range(depth):
        w_i = _c(w_in[i]); g_i = _gk(attn_norm[i])
        r = _run(p1, [{"h": hfull[tsl[c]], "w": w_i, "g": g_i} for c in range(NCORES)])
        proj = np.concatenate([np.asarray(r[c]["proj"]).view(bf) if np.asarray(r[c]["proj"]).dtype != bf else np.asarray(r[c]["proj"])
                               for c in range(NCORES)], axis=0)
        maps = []
        for c in range(NCORES):
            b, j = c // 4, c % 4
            pb = np.zeros((Lp, IN_COLS), bf)
            pb[:L] = proj[b * L:(b + 1) * L]
            kv = j // 2
            ka = pb[:, 512 + 64 * kv:512 + 64 * kv + 64].T
            maps.append({
                "qaT": _c(pb[:, 128 * j:128 * j + 128].T), "kaT": _c(np.concatenate([ka, ka], 0)),
                "va": _c(pb[:, 640 + 64 * kv:640 + 64 * kv + 64]),
                "qbT": _c(pb[:, 768 + 64 * j:768 + 64 * j + 64].T), "kbT": _c(pb[:, 1024 + 64 * j:1024 + 64 * j + 64].T),
                "vb": _c(pb[:, 1280 + 64 * j:1280 + 64 * j + 64]),
                "qcT": _c(pb[:, 1536 + 32 * j:1536 + 32 * j + 32].T), "kcT": _c(pb[:, 1664 + 32 * j:1664 + 32 * j + 32].T),
                "vc": _c(pb[:, 1792 + 64 * j:1792 + 64 * j + 64]), "glrT": _c(pb[:, 2048:2064].T),
                "w2": _c(np.asarray(gla_gate_w2[i], np.float32)[:, 32 * j:32 * j + 32]),
                "gb": _c(np.asarray(gla_gate_b[i], np.float32)[32 * j:32 * j + 32].reshape(32, 1)),
                "sinks": _c(np.broadcast_to(np.asarray(attn_sinks[i], np.float32)[2 * j:2 * j + 2][None, :], (128, 2))),
            })
        r = _run(p2, maps)
        ofull = np.zeros((Tp, D), np.float32)
        for c in range(NCORES):
            b, j = c // 4, c % 4
            ofull[b * L:(b + 1) * L, 128 * j:128 * j + 128] = np.asarray(r[c]["oa"])[:L]
            ofull[b * L:(b + 1) * L, 512 + 64 * j:512 + 64 * j + 64] = np.asarray(r[c]["obT"]).T[:L]
            ofull[b * L:(b + 1) * L, 768 + 64 * j:768 + 64 * j + 64] = np.asarray(r[c]["oc"])[:L]
        rcfull = np.zeros((Tp, 256), bf)
        rcfull[:T] = proj[:T, 2064:2320]
        fin = (i == depth - 1)
        p3 = _prog("p3", NT, fin)
        gmix = np.concatenate([swa_out_norm[i], sb_out_norm[i], gla_out_norm[i]]).astype(np.float32)
        shared = {
            "gmix": _c(np.broadcast_to(gmix[None, :], (128, D))), "wout": _c(w_out[i]), "gffn": _gk(ffn_norm[i]),
            "wq": _c(peer_w_q[i]), "ksub": _c(np.transpose(np.asarray(peer_sub_keys[i], np.float32).reshape(16, 64, 128), (2, 0, 1))),
            "uT": _c(np.asarray(peer_u[i], np.float32).T), "v": _c(peer_v[i]),
            "gfin": _c(np.broadcast_to(np.asarray(final_norm, np.float32)[None, :], (128, D))),
        }
        r = _run(p3, [dict(shared, h=hfull[tsl[c]], o=ofull[tsl[c]], rc=rcfull[tsl[c]]) for c in range(NCORES)])
        hfull = np.concatenate([np.asarray(r[c]["hout"]) for c in range(NCORES)], axis=0)
    out = np.stack([hfull[b * L + 128:(b + 1) * L] for b in range(B)], 0)
    return np.ascontiguousarray(out.astype(np.float32))


def kernel(**inputs):
    return forward_fused(**{k: np.asarray(v) for k, v in inputs.items()})


import os as _os
_STOP = _os.environ.get("FUSED_STOP", "")
_POOL_HEADS = tuple(int(c_) for c_ in _os.environ.get("POOL_HEADS", "01234"))
CJ = 720
RG = [[0, 1, 2, 3], [4, 5, 6, 7]]


def build_fused(Lp, depth):
    TQ = Lp // 4
    NT = TQ // 128
    NCH = Lp // 512
    NB = Lp // 128
    nc = bass.Bass("TRN2", target_bir_lowering=False)
    IN = lambda n, s, dt=F32: nc.dram_tensor(n, s, dt, kind="ExternalInput").ap()
    h0 = IN("h0", [TQ, D])
    w_in = IN("w_in", [depth, D, CJ]); g_attn = IN("g_attn", [depth, 128, 8])
    w2_i = IN("w2", [depth, 16, 32]); gb_i = IN("gb", [depth, 32, 1]); sinks_i = IN("sinks", [depth, 128, 2])
    gmix_i = IN("gmix", [depth, 128, 256]); wout_i = IN("wout", [depth, 128, 2, D])
    gffn_i = IN("gffn", [depth, 128, 8]); wq_i = IN("wq", [depth, D, 2048]); ksub_i = IN("ksub", [depth, 128, 16, 64])
    uT_i = IN("uT", [depth, D, 4096]); v_i = IN("v", [depth, 4096, D]); gfin = IN("gfin", [128, D])
    out = nc.dram_tensor("out", [TQ, D], F32, kind="ExternalOutput").ap()
    DT = lambda n, s, dt=F32: nc.dram_tensor(n, s, dt, kind="Internal").ap()
    GC = 128 * max(d for d in range(1, 5) if NT % d == 0)
    NG = TQ // GC
    xT_loc = [DT("xT_loc%d" % g, [D, GC], BF16) for g in range(NG)]
    xT_all = [DT("xT_all%d" % g, [4 * D, GC], BF16) for g in range(NG)]
    part_loc = DT("part_loc", [Lp, D]); delta_loc = DT("delta_loc", [TQ, D])
    hl = [DT("hl0", [TQ, D]), DT("hl1", [TQ, D])]
    h1_d = DT("h1_d", [TQ, D]); sc_d = DT("sc_d", [TQ, D]); tb_d = DT("tb_d", [TQ, 16]); xT_d = DT("xT_d", [TQ, D], BF16)

    with ExitStack() as st:
        kb = KB(nc, st)
        kb.excl.update(["pT", "pX", "pA", "pB", "pO", "pz0", "pz1", "pc0", "pc1", "pq0", "pq1", "psc0", "psc1",
                        "pH0", "pH1", "ptr", "po0", "po1"])
        ps = [st.enter_context(nc.psum_tensor("ps%d" % i, [128, 512], F32)) for i in range(8)]
        T0 = lambda name, shape, dt=F32: st.enter_context(nc.sbuf_tensor(name, shape, dt))
        idf, idb = _ident(kb, T0)
        stage = [T0("stage%d" % i, [128, 1024]) for i in range(2)]
        scnt = [0]

        def load_w(dst, src, rows_k, cols, key, gt=None):
            for k in range(rows_k):
                for c0 in range(0, cols, 1024):
                    cw = min(1024, cols - c0)
                    si = scnt[0] % 2
                    scnt[0] += 1
                    sk = "stage%d" % si
                    kb.dma("sp", stage[si][:, 0:cw], src[k * 128:(k + 1) * 128, c0:c0 + cw], writes=[sk])
                    if gt is not None:
                        kb.ts(dst[:, k, c0:c0 + cw], stage[si][:, 0:cw], gt[:, k:k + 1], ALU.mult, [sk, "gt"], [key])
                    else:
                        kb.cp(dst[:, k, c0:c0 + cw], stage[si][:, 0:cw], [sk], [key], eng="pool")

        def phase_end():
            kb.barrier()
            kb.emit()

        for li in range(depth):
            hsrc = h0 if li == 0 else hl[(li - 1) % 2]
            hdst = out if li == depth - 1 else hl[li % 2]
            final = (li == depth - 1)
            with ExitStack() as sa:
                T = lambda name, shape, dt=F32, _p="L%dA_" % li: sa.enter_context(nc.sbuf_tensor(_p + name, shape, dt))
                ht = [T("ht%d" % i, [128, D]) for i in range(2)]
                junk = T("junk", [128, D], BF16); ss = T("ss", [128, 1])
                xn = T("xn", [128, D], BF16); xnT = [T("xnT%d" % i, [128, D], BF16) for i in range(2)]
                pT = ps[0][:].bitcast(BF16)
                xv = [x_.rearrange("(k p) t -> p k t", p=128) for x_ in xT_loc]
                for i in range(NT):
                    b = i % 2
                    kb.dma("sp", ht[b][:], hsrc[i * 128:(i + 1) * 128, :], writes=["ht%d" % b])
                    _rstd(kb, ht[b][:], junk[:], ss[:], D, ["ht%d" % b], "a")
                    kb.ts(xn[:], ht[b][:], ss[:, 0:1], ALU.mult, ["ht%d" % b, "ass"], ["xn"])
                    for k in range(8):
                        kb.tr(pT[:, k * 128:(k + 1) * 128], xn[:, k * 128:(k + 1) * 128], idb[:], ["xn", "ident_b"], ["pT"], inc=(k == 7))
                    kb.cp(xnT[b][:], pT, ["pT"], ["xnT%d" % b], eng="act")
                    g_, o_ = (i * 128) // GC, (i * 128) % GC
                    kb.dma("pool", xv[g_][:, :, o_:o_ + 128], xnT[b][:].rearrange("p (k t) -> p k t", t=128),
                           reads=["xnT%d" % b], writes=["xT_loc"])
                kb.barrier()
                for g_ in range(NG):
                    kb.coll("AllGather", ALU.bypass, RG, xT_loc[g_], xT_all[g_], ["xT_loc"], ["xT_all"])
                phase_end()
            if _STOP == "A":
                break
            with ExitStack() as sb_:
                T = lambda name, shape, dt=F32, _p="L%dB_" % li: sb_.enter_context(nc.sbuf_tensor(_p + name, shape, dt))
                gt = T("gt", [128, 8])
                kb.dma("sp", gt[:], g_attn[li], writes=["gt"])
                Wj = T("Wj", [128, 8, 768], BF16)
                load_w(Wj, w_in[li], 8, CJ, "Wj", gt=gt)
                Wo = T("Wo", [128, 2, D], BF16)
                for kc in range(2):
                    si = scnt[0] % 2; scnt[0] += 1
                    kb.dma("sp", stage[si][:], wout_i[li][:, kc, :], writes=["stage%d" % si])
                    kb.cp(Wo[:, kc, :], stage[si][:], ["stage%d" % si], ["Wo"], eng="pool")
                gm = T("gm", [128, 256]); kb.dma("sp", gm[:], gmix_i[li], writes=["gm"])
                m_gen = T("m_gen", [128, 256]); m_n0 = T("m_n0", [128, 256]); m_n1 = T("m_n1", [128, 256])
                for m, nm, extra in ((m_gen, "m_gen", None), (m_n0, "m_n0", -240), (m_n1, "m_n1", -112)):
                    kb.memset(m[:], 0.0, [nm])
                    kb.asel(m[:], ALU.is_ge, NEG, -1, -1, [[1, 256]], nm)
                    kb.asel(m[:], ALU.is_ge, NEG, 128, 1, [[-1, 256]], nm)
                    if extra is not None:
                        kb.asel(m[:], ALU.is_ge, NEG, extra, 0, [[1, 256]], nm)
                sbm_f = T("sbm_f", [128, 512])
                sbm = [T("sbm%d" % d, [128, 512], BF16) for d in range(4)]
                for d in range(4):
                    kb.memset(sbm_f[:], 1.0, ["sbm_f"])
                    kb.asel(sbm_f[:], ALU.is_ge, 0.0, -1 - 128 * d, -1, [[1, 512]], "sbm_f")
                    kb.cp(sbm[d][:], sbm_f[:], ["sbm_f"], ["sbm%d" % d], eng="pool")
                ntri_f = T("ntri_f", [128, 128]); ntri = T("ntri", [128, 128], BF16); nones = T("nones", [128, 128], BF16)
                kb.memset(ntri_f[:], -1.0, ["ntri_f"])
                kb.asel(ntri_f[:], ALU.is_ge, 0.0, 0, 1, [[-1, 128]], "ntri_f")
                kb.cp(ntri[:], ntri_f[:], ["ntri_f"], ["ntri"], eng="pool")
                kb.memset(nones[:], -1.0, ["nones"])
                mle = T("mle", [128, 128])
                kb.memset(mle[:], 1.0, ["mle"])
                kb.asel(mle[:], ALU.is_ge, 0.0, 0, -1, [[1, 128]], "mle")
                rmask = T("rmask", [32, 512])
                kb.memset(rmask[:], 1.0, ["rmask"])
                for b4 in range(4):
                    kb.memset(rmask[:, b4 * 128:b4 * 128 + 1], 0.0, ["rmask"])
                w2f = T("w2f", [16, 32]); w2b = T("w2b", [16, 32], BF16); gbt = T("gbt", [32, 1]); sk_t = T("sk_t", [128, 2])
                kb.dma("sp", w2f[:], w2_i[li], writes=["w2f"]); kb.dma("sp", gbt[:], gb_i[li], writes=["gbt"])
                kb.dma("sp", sk_t[:], sinks_i[li], writes=["sk"])
                kb.cp(w2b[:], w2f[:], ["w2f"], ["w2b"])
                kb.ts(gbt[:], gbt[:], -1.0, ALU.mult, ["gbt"], ["gbt"])
                KbT = T("KbT", [64, Lp], BF16); Vb = T("Vb", [128, NB, 64], BF16)
                S = T("S", [32, 64]); Sb = T("Sb", [32, 64], BF16)
                kb.memset(S[:], 0.0, ["S"]); kb.memset(Sb[:], 0.0, ["Sb"])
                xc = [T("xc%d" % i, [128, 8, 512], BF16) for i in range(2)]
                qa_c = [T("qa_c%d" % i, [128, 512], BF16) for i in range(2)]
                ka_c = [T("ka_c%d" % i, [128, 640], BF16) for i in range(2)]
                va_c = [T("va_c%d" % i, [128, 5, 64], BF16) for i in range(2)]
                qs_c = [T("qs_c%d" % i, [64, 512], BF16) for i in range(2)]
                qc_c = [T("qc_c%d" % i, [32, 512], BF16) for i in range(2)]
                kc_c = [T("kc_c%d" % i, [32, 512], BF16) for i in range(2)]
                vc_c = [T("vc_c%d" % i, [128, 4, 64], BF16) for i in range(2)]
                rc_c = [T("rc_c%d" % i, [128, 4, 64]) for i in range(2)]
                gl_c = [T("gl_c%d" % i, [16, 512], BF16) for i in range(2)]
                mo = [T("mo%d" % i, [128, 4, 256]) for i in range(2)]
                sm = T("sm", [128, 256]); pexp = T("pexp", [128, 256], BF16); pTs = T("pTs", [128, 256], BF16)
                st8 = T("st8", [128, 8])
                ge = T("ge", [32, 512]); gsp = T("gsp", [32, 512]); gcs = T("gcs", [32, 512])
                geq = T("geq", [32, 512]); gek = T("gek", [32, 512])
                qt = T("qt", [32, 512], BF16); kt = T("kt", [32, 512], BF16)
                scb = T("scb", [128, 128], BF16); ktm = T("ktm", [128, 32], BF16)
                stmp = T("stmp", [32, 64])
                e_t = [T("e_t%d" % i, [128, 512]) for i in range(2)]
                sp_t = [[T("sp_t%d_%d" % (pp_, i), [128, 512], BF16) for i in range(2)] for pp_ in range(2)]
                a_t = [T("a_t%d" % i, [128, 512], BF16) for i in range(2)]
                R = T("R", [128, 512], BF16)
                ob_t = T("ob_t", [64, 512])
                sq4 = T("sq4", [128, 1024]); s16 = T("s16", [128, 16]); m14 = T("m14", [128, 1024]); sil4 = T("sil4", [128, 4, 64])
                mixb4 = T("mixb4", [128, 1024], BF16); mixT4 = T("mixT4", [128, 1024], BF16)
                pt = [T("pt%d" % i, [128, D]) for i in range(2)]
                pz = [ps[0], ps[1]]; pc = [ps[2], ps[3]]; pO = ps[4]
                pA = ps[5]; pX = ps[6]; pB = ps[7]
                pT_bf = pB[:].bitcast(BF16)

                def pieces(c0, n):
                    t = c0
                    while t < c0 + n:
                        q = t // TQ; tl = t % TQ; g_ = tl // GC; o_ = tl % GC; m = min(c0 + n - t, GC - o_)
                        yield q, g_, o_, t - c0, m
                        t += m

                def load_xc(c):
                    i = c % 2
                    for q, g_, o_, off, m in pieces(c * 512, 512):
                        kb.dma("sp", xc[i][:, :, off:off + m],
                               xT_all[g_][q * D:(q + 1) * D, o_:o_ + m].rearrange("(k p) t -> p k t", p=128), writes=["xc%d" % i])

                def inproj(c):
                    i = c % 2
                    xk = "xc%d" % i
                    if c == 0:
                        kb.memset(ka_c[i][:, 0:128], 0.0, ["ka_c%d" % i])
                        kb.memset(va_c[i][:, 0, :], 0.0, ["va_c%d" % i])
                    else:
                        kb.cp(ka_c[i][:, 0:128], ka_c[1 - i][:, 512:640], ["ka_c%d" % (1 - i)], ["ka_c%d" % i], eng="pool")
                        kb.cp(va_c[i][:, 0, :], va_c[1 - i][:, 4, :], ["va_c%d" % (1 - i)], ["va_c%d" % i], eng="pool")
                    groups = [(0, 128, qa_c[i][:], "qa_c%d" % i, None), (128, 128, ka_c[i][:, 128:640], "ka_c%d" % i, None),
                              (256, 64, qs_c[i][:], "qs_c%d" % i, 0.125), (320, 64, KbT[:, c * 512:(c + 1) * 512], "KbT", None),
                              (384, 32, qc_c[i][:], "qc_c%d" % i, None), (416, 32, kc_c[i][:], "kc_c%d" % i, None),
                              (448, 16, gl_c[i][:], "gl_c%d" % i, None)]
                    for gi, (c0, rows, dst, dk, scale) in enumerate(groups):
                        mr = max(rows, 32)
                        for k in range(8):
                            kb.mm(pX[0:mr, :], Wj[:, k, c0:c0 + mr], xc[i][:, k, :], k == 0, k == 7, ["Wj", xk], ["pX"], inc=(k == 7))
                        if scale is not None:
                            kb.ts(dst, pX[0:rows, :], scale, ALU.mult, ["pX"], [dk])
                        elif gi % 2:
                            kb.cp(dst, pX[0:rows, :], ["pX"], [dk], eng="act")
                        else:
                            kb.cp(dst, pX[0:rows, :], ["pX"], [dk])
                    for blk in (range(4) if _STOP != "B1f" else []):
                        n = 4 * c + blk
                        for k in range(8):
                            kb.mm(pX[:, 0:256], xc[i][:, k, blk * 128:(blk + 1) * 128], Wj[:, k, 464:720], k == 0, k == 7, ["Wj", xk], ["pX"], inc=(k == 7))
                        kb.cp(va_c[i][:, blk + 1, :], pX[:, 0:64], ["pX"], ["va_c%d" % i], eng="act")
                        kb.cp(Vb[:, n, :], pX[:, 64:128], ["pX"], ["Vb"])
                        kb.cp(vc_c[i][:, blk, :], pX[:, 128:192], ["pX"], ["vc_c%d" % i], eng="act")
                        kb.cp(rc_c[i][:, blk, :], pX[:, 192:256], ["pX"], ["rc_c%d" % i])

                def swa(c):
                    i = c % 2
                    mk_ = "mo%d" % i
                    for blk in range(4):
                        n = 4 * c + blk
                        msk = m_n0 if n == 0 else (m_n1 if n == 1 else m_gen)
                        mk = "m_n0" if n == 0 else ("m_n1" if n == 1 else "m_gen")
                        for hh in range(2):
                            hs = slice(hh * 64, (hh + 1) * 64)
                            kb.mm(pA[:, 0:256], qa_c[i][hs, blk * 128:(blk + 1) * 128], ka_c[i][hs, blk * 128:blk * 128 + 256],
                                  True, True, ["qa_c%d" % i, "ka_c%d" % i], ["pA"])
                            kb.stt(sm[:], pA[:, 0:256], 0.125, msk[:], ALU.mult, ALU.add, ["pA", mk], ["sm"])
                            kb.op("dve", lambda e: e.tensor_reduce(out=st8[:, 0:1], in_=sm[:], axis=AX.X, op=ALU.max), ["sm"], ["st8"])
                            kb.tt(st8[:, 0:1], st8[:, 0:1], sk_t[:, hh:hh + 1], ALU.max, ["st8", "sk"], ["st8"])
                            kb.ts(st8[:, 1:2], st8[:, 0:1], -1.0, ALU.mult, ["st8"], ["st8"])
                            kb.act(pexp[:], sm[:], AF.Exp, ["sm", "st8"], ["pexp", "st8"], bias=st8[:, 1:2], scale=1.0, accum_out=st8[:, 2:3])
                            kb.act(st8[:, 3:4], sk_t[:, hh:hh + 1], AF.Exp, ["sk", "st8"], ["st8"], bias=st8[:, 1:2], scale=1.0)
                            kb.tt(st8[:, 4:5], st8[:, 2:3], st8[:, 3:4], ALU.add, ["st8"], ["st8"])
                            kb.op("dve", lambda e: e.reciprocal(out=st8[:, 5:6], in_=st8[:, 4:5]), ["st8"], ["st8"])
                            kb.tr(pT_bf[:, 0:128], pexp[:, 0:128], idb[:], ["pexp", "ident_b"], ["pB"], inc=False)
                            kb.tr(pT_bf[:, 128:256], pexp[:, 128:256], idb[:], ["pexp", "ident_b"], ["pB"])
                            kb.cp(pTs[:], pT_bf[:, 0:256], ["pB"], ["pTs"], eng="act")
                            kb.mm(pA[:, 256:320], pTs[:, 0:128], va_c[i][:, blk, :], True, False, ["pTs", "va_c%d" % i], ["pA"], inc=False)
                            kb.mm(pA[:, 256:320], pTs[:, 128:256], va_c[i][:, blk + 1, :], False, True, ["pTs", "va_c%d" % i], ["pA"])
                            kb.ts(mo[i][:, blk, hh * 64:(hh + 1) * 64], pA[:, 256:320], st8[:, 5:6], ALU.mult, ["pA", "st8"], [mk_])

                def gla(c):
                    i = c % 2
                    kb.mm(pX[0:32, :], w2b[:], gl_c[i][:], True, True, ["w2b", "gl_c%d" % i], ["pX"])
                    kb.act(ge[:], pX[0:32, :], AF.Exp, ["pX", "gbt"], ["ge"], bias=gbt[:, 0:1], scale=-1.0)
                    kb.act(gsp[:], ge[:], AF.Ln, ["ge"], ["gsp"], bias=1.0, scale=1.0)
                    kb.op("dve", lambda e: e.tensor_tensor_scan(out=gcs[:], data0=rmask[:], data1=gsp[:], initial=0.0, op0=ALU.mult, op1=ALU.add),
                          ["rmask", "gsp"], ["gcs"])
                    kb.act(geq[:], gcs[:], AF.Exp, ["gcs"], ["geq"], scale=-1.0 / 16.0)
                    kb.act(gek[:], gcs[:], AF.Exp, ["gcs"], ["gek"], scale=1.0 / 16.0)
                    kb.stt(qt[:], qc_c[i][:], 32.0 ** -0.5, geq[:], ALU.mult, ALU.mult, ["qc_c%d" % i, "geq"], ["qt"])
                    kb.tt(kt[:], kc_c[i][:], gek[:], ALU.mult, ["kc_c%d" % i, "gek"], ["kt"])
                    for blk in range(4):
                        bs = slice(blk * 128, (blk + 1) * 128)
                        kb.mm(pA[:, 384:512], kt[:, bs], qt[:, bs], True, True, ["kt", "qt"], ["pA"])
                        kb.tt(scb[:], pA[:, 384:512], mle[:], ALU.mult, ["pA", "mle"], ["scb"])
                        kb.tr(pT_bf[:, 512:544], kt[:, bs], idb[0:32, 0:32], ["kt", "ident_b"], ["pB"])
                        kb.cp(ktm[:], pT_bf[:, 512:544], ["pB"], ["ktm"], eng="act")
                        kb.mm(pA[:, 320:384], scb[:], vc_c[i][:, blk, :], True, False, ["scb", "vc_c%d" % i], ["pA"], inc=False)
                        kb.mm(pA[:, 320:384], qt[:, bs], Sb[:], False, True, ["qt", "Sb"], ["pA"])
                        kb.cp(mo[i][:, blk, 192:256], pA[:, 320:384], ["pA"], ["mo%d" % i], eng="act")
                        kb.mm(pB[0:32, 384:448], ktm[:], vc_c[i][:, blk, :], True, True, ["ktm", "vc_c%d" % i], ["pB"])
                        kb.tt(stmp[:], pB[0:32, 384:448], S[:], ALU.add, ["pB", "S"], ["stmp"])
                        kb.ts(S[:], stmp[:], geq[:, blk * 128 + 127:blk * 128 + 128], ALU.mult, ["stmp", "geq"], ["S"])
                        kb.cp(Sb[:], S[:], ["S"], ["Sb"])

                def sbk(c):
                    i = c % 2
                    nkb = 4 * c + 4
                    npairs = nkb // 2
                    qk = "qs_c%d" % i

                    def kof(p, j):
                        return nkb - 1 - (2 * p + j)

                    def zmm(p):
                        for j in range(2):
                            kblk = kof(p, j)
                            kb.mm(pz[j][:], KbT[:, kblk * 128:(kblk + 1) * 128], qs_c[i][:], True, True, ["KbT", qk], ["pz%d" % j])

                    zmm(0)
                    for p in range(npairs + 1):
                        pp = p % 2
                        if p < npairs:
                            for j in range(2):
                                kb.act(e_t[j][:], pz[j][:], AF.Exp, ["pz%d" % j], ["e_t%d" % j])
                            for j in range(2):
                                kb.act(sp_t[pp][j][:], e_t[j][:], AF.Ln, ["e_t%d" % j], ["sp_t%d_%d" % (pp, j)], bias=1.0, scale=1.0)
                            for j in range(2):
                                dg = kof(p, j) - 4 * c
                                if dg >= 0:
                                    kb.tt(sp_t[pp][j][:], sp_t[pp][j][:], sbm[dg][:], ALU.mult, ["sp_t%d_%d" % (pp, j), "sbm%d" % dg], ["sp_t%d_%d" % (pp, j)])
                        if p >= 1:
                            q = p - 1
                            qq = q % 2
                            first, lastp = (q == 0), (q == npairs - 1)
                            for j in range(2):
                                kblk = kof(q, j)
                                sk_ = "sp_t%d_%d" % (qq, j)
                                kb.mm(pc[j][:], ntri[:], sp_t[qq][j][:], True, False, ["ntri", sk_], ["pc%d" % j], inc=False)
                                if j == 1:
                                    kb.mm(pc[j][:], nones[:], sp_t[qq][0][:], False, False, ["nones", "sp_t%d_0" % qq], ["pc%d" % j], inc=False)
                                if not first:
                                    kb.mm(pc[j][:], nones[:], R[:], False, False, ["nones", "R"], ["pc%d" % j], inc=False)
                                kb.mm(pc[j][:], KbT[:, kblk * 128:(kblk + 1) * 128], qs_c[i][:], False, True, ["KbT", qk], ["pc%d" % j])
                        if p + 1 < npairs:
                            zmm(p + 1)
                        if p >= 1:
                            if not lastp:
                                if first:
                                    kb.tt(R[:], sp_t[qq][0][:], sp_t[qq][1][:], ALU.add, ["sp_t%d_0" % qq, "sp_t%d_1" % qq], ["R"])
                                else:
                                    kb.tt(R[:], R[:], sp_t[qq][0][:], ALU.add, ["R", "sp_t%d_0" % qq], ["R"])
                                    kb.tt(R[:], R[:], sp_t[qq][1][:], ALU.add, ["R", "sp_t%d_1" % qq], ["R"])
                            for j in range(2):
                                kb.act(a_t[j][:], pc[j][:], AF.Exp, ["pc%d" % j], ["a_t%d" % j])
                            for j in range(2):
                                dg = kof(q, j) - 4 * c
                                if dg >= 0:
                                    kb.tt(a_t[j][:], a_t[j][:], sbm[dg][:], ALU.mult, ["a_t%d" % j, "sbm%d" % dg], ["a_t%d" % j])
                            for j in range(2):
                                kblk = kof(q, j)
                                kb.mm(pO[0:64, :], Vb[:, kblk, :], a_t[j][:], (q == 0 and j == 0), (kblk == 0), ["Vb", "a_t%d" % j], ["pO"])
                    kb.cp(ob_t[:], pO[0:64, :], ["pO"], ["ob_t"])

                def post(c):
                    i = c % 2
                    mk_ = "mo%d" % i
                    for blk in range(4):
                        kb.tr(pA[:, blk * 64:(blk + 1) * 64], ob_t[:, blk * 128:(blk + 1) * 128], idf[0:64, 0:64], ["ob_t", "ident_f"], ["pA"], inc=(blk == 3))
                    kb.cp(mo[i][:, :, 128:192], pA[:, 0:256].rearrange("p (b d) -> p b d", d=64), ["pA"], [mk_], eng="act")
                    mof = mo[i][:].rearrange("p b c -> p (b c)")
                    kb.act(sq4[:], mof, AF.Square, [mk_], ["sq4"])
                    kb.op("dve", lambda e: e.tensor_reduce(out=s16[:], in_=sq4[:].rearrange("p (h d) -> p h d", d=64), axis=AX.X, op=ALU.add),
                          ["sq4"], ["s16"])
                    kb.ts(s16[:], s16[:], 1.0 / 64, ALU.mult, ["s16"], ["s16"], s2=EPS, op1=ALU.add)
                    kb.act(s16[:], s16[:], AF.Sqrt, ["s16"], ["s16"])
                    kb.op("dve", lambda e: e.reciprocal(out=s16[:], in_=s16[:]), ["s16"], ["s16"])
                    kb.tt(m14[:].rearrange("p (h d) -> p h d", d=64), mof.rearrange("p (h d) -> p h d", d=64),
                          s16[:].unsqueeze(2).to_broadcast([128, 16, 64]), ALU.mult, [mk_, "s16"], ["m14"])
                    kb.act(sil4[:], rc_c[i][:], AF.Silu, ["rc_c%d" % i], ["sil4"])
                    m14v = m14[:].rearrange("p (b c) -> p b c", c=256)
                    kb.tt(m14v[:, :, 192:256], m14v[:, :, 192:256], sil4[:], ALU.mult, ["m14", "sil4"], ["m14"])
                    kb.tt(mixb4[:].rearrange("p (b c) -> p b c", c=256), m14v, gm[:].unsqueeze(1).to_broadcast([128, 4, 256]), ALU.mult,
                          ["m14", "gm"], ["mixb4"])
                    for t8 in range(8):
                        kb.tr(pT_bf[:, t8 * 128:(t8 + 1) * 128], mixb4[:, t8 * 128:(t8 + 1) * 128], idb[:], ["mixb4", "ident_b"], ["pB"], inc=(t8 == 7))
                    kb.cp(mixT4[:], pT_bf, ["pB"], ["mixT4"], eng="act")
                    for blk in range(4):
                        n = 4 * c + blk
                        pk = "pt%d" % (n % 2)
                        for dh in range(2):
                            pbank, pkey = (pA, "pA") if dh == 0 else (pX, "pX")
                            for kc in range(2):
                                kb.mm(pbank[:], mixT4[:, (2 * blk + kc) * 128:(2 * blk + kc + 1) * 128], Wo[:, kc, dh * 512:(dh + 1) * 512], kc == 0, kc == 1,
                                      ["mixT4", "Wo"], [pkey], inc=(kc == 1))
                            kb.cp(pt[n % 2][:, dh * 512:(dh + 1) * 512], pbank[:], [pkey], [pk], eng=("act" if dh else "dve"))
                        kb.dma("pool", part_loc[n * 128:(n + 1) * 128, :], pt[n % 2][:], reads=[pk], writes=["part_loc"])

                load_xc(0)
                for c in range(NCH):
                    if _STOP == "B0":
                        continue
                    if c + 1 < NCH:
                        load_xc(c + 1)
                    if _STOP == "B0x":
                        continue
                    inproj(c)
                    if _STOP in ("B1", "B1f"):
                        continue
                    swa(c)
                    gla(c)
                    sbk(c)
                    if _STOP == "B2":
                        continue
                    post(c)
                kb.barrier()
                if _STOP not in ("B0", "B0x", "B1", "B1f", "B2", "B3"):
                    kb.coll("ReduceScatter", ALU.add, RG, part_loc, delta_loc, ["part_loc"], ["delta_loc"])
                phase_end()
            if _STOP in ("B", "B0", "B0x", "B1", "B1f", "B2", "B3"):
                break
            with ExitStack() as sc_:
                T = lambda name, shape, dt=F32, _p="L%dC_" % li: sc_.enter_context(nc.sbuf_tensor(_p + name, shape, dt))
                gft = T("gft", [128, 8])
                kb.dma("sp", gft[:], gffn_i[li], writes=["gt"])
                Wq = T("Wq", [128, 8, 2048], BF16); Ks = T("Ks", [128, 16, 64], BF16); ksf = T("ksf", [128, 16, 64])
                kb.dma("sp", ksf[:], ksub_i[li], writes=["ksf"])
                kb.cp(Ks[:], ksf[:], ["ksf"], ["Ks"])
                load_w(Wq, wq_i[li], 8, 2048, "Wq", gt=gft)
                ht = [T("ht%d" % i, [128, D]) for i in range(2)]
                dt_ = [T("dt%d" % i, [128, D]) for i in range(2)]
                h1 = T("h1", [128, D]); junk = T("junk", [128, D], BF16); ss = T("ss", [128, 1])
                xn = T("xn", [128, D], BF16); xnT = T("xnT", [128, D], BF16)
                qT = T("qT", [128, 16, 128], BF16); sct = T("sct", [128, D])
                t1 = T("t1", [128, 8, 16]); t2 = T("t2", [128, 8, 16]); wk1 = T("wk1", [128, 16, 64]); wk2 = T("wk2", [128, 8, 256])
                cand = T("cand", [128, 8, 256]); c8a = T("c8a", [128, 8, 8]); c8b = T("c8b", [128, 8, 8])
                csh = T("csh", [128, 8, 256]); ec = T("ec", [128, 8, 256]); mk8 = T("mk8", [128, 8, 256])
                Z = T("Z", [128, 8]); tb = T("tb", [128, 16])
                pT = ps[0][:].bitcast(BF16)
                for i in range(NT):
                    b = i % 2
                    rs = slice(i * 128, (i + 1) * 128)
                    kb.dma("sp", ht[b][:], hsrc[rs, :], writes=["ht%d" % b])
                    kb.dma("sp", dt_[b][:], delta_loc[rs, :], writes=["dt%d" % b])
                    kb.tt(h1[:], ht[b][:], dt_[b][:], ALU.add, ["ht%d" % b, "dt%d" % b], ["h1"])
                    kb.dma("pool", h1_d[rs, :], h1[:], reads=["h1"], writes=["h1_d"])
                    _rstd(kb, h1[:], junk[:], ss[:], D, ["h1"], "b")
                    kb.ts(xn[:], h1[:], ss[:, 0:1], ALU.mult, ["h1", "bss"], ["xn"])
                    for k in range(8):
                        kb.tr(pT[:, k * 128:(k + 1) * 128], xn[:, k * 128:(k + 1) * 128], idb[:], ["xn", "ident_b"], ["pT"], inc=(k == 7))
                    kb.cp(xnT[:], pT, ["pT"], ["xnT"], eng="act")
                    kb.dma("pool", xT_d[rs, :], xnT[:], reads=["xnT"], writes=["xT_d"])
                    for cg in range(4):
                        pq = ps[3 + cg % 2]
                        for cc in range(4):
                            cidx = cg * 4 + cc
                            for k in range(8):
                                kb.mm(pq[:, cc * 128:(cc + 1) * 128], Wq[:, k, cidx * 128:(cidx + 1) * 128], xnT[:, k * 128:(k + 1) * 128],
                                      k == 0, k == 7, ["Wq", "xnT"], ["pq%d" % (cg % 2)], inc=(k == 7 and cc == 3))
                        kb.cp(qT[:, cg * 4:(cg + 1) * 4, :], pq[:].rearrange("p (c t) -> p c t", t=128), ["pq%d" % (cg % 2)], ["qT"],
                              eng=("act" if cg % 2 else "dve"))
                    for cidx in range(16):
                        pscb = ps[5 + cidx // 8]
                        kb.mm(pscb[:, (cidx % 8) * 64:(cidx % 8 + 1) * 64], qT[:, cidx, :], Ks[:, cidx, :], True, True, ["qT", "Ks"],
                              ["psc%d" % (cidx // 8)], inc=(cidx % 8 == 7))
                    kb.cp(sct[:, 0:512], ps[5][:], ["psc0"], ["sct"], eng="act")
                    kb.cp(sct[:, 512:1024], ps[6][:], ["psc1"], ["sct"], eng="dve")
                    kb.dma("pool", sc_d[rs, :], sct[:], reads=["sct"], writes=["sc_d"])
                    chains = [(hd, side, (t1, t2)[side]) for hd in range(8) for side in range(2)]
                    tkeys = ["tt%d_%d" % (side, hd) for hd, side, _ in chains]
                    for ci_, (hd, side, tt_) in enumerate(chains):
                        sv = sct[:, hd * 128 + side * 64:hd * 128 + side * 64 + 64]
                        kb.op("dve", lambda e, o_=tt_[:, hd, 0:8], i_=sv: e.max(out=o_, in_=i_), ["sct"], [tkeys[ci_]])
                    for ci_, (hd, side, tt_) in enumerate(chains):
                        sv = sct[:, hd * 128 + side * 64:hd * 128 + side * 64 + 64]
                        kb.op("dve", lambda e, o_=wk1[:, ci_, :], r_=tt_[:, hd, 0:8], i_=sv: e.match_replace(out=o_, in_to_replace=r_, in_values=i_, imm_value=-1e30),
                              ["sct", tkeys[ci_]], ["wk1_%d" % ci_])
                    for ci_, (hd, side, tt_) in enumerate(chains):
                        kb.op("dve", lambda e, o_=tt_[:, hd, 8:16], i_=wk1[:, ci_, :]: e.max(out=o_, in_=i_), ["wk1_%d" % ci_], [tkeys[ci_]])
                    kb.tt(cand[:].rearrange("p h (a b) -> p h a b", b=16), t1[:].unsqueeze(3).to_broadcast([128, 8, 16, 16]),
                          t2[:].unsqueeze(2).to_broadcast([128, 8, 16, 16]), ALU.add, tkeys, ["cand"])
                    ckeys = ["c8_%d" % hd for hd in range(8)]
                    for hd in range(8):
                        kb.op("dve", lambda e, o_=c8a[:, hd, :], i_=cand[:, hd, :]: e.max(out=o_, in_=i_), ["cand"], [ckeys[hd]])
                    for hd in range(8):
                        kb.op("dve", lambda e, o_=wk2[:, hd, :], r_=c8a[:, hd, :], i_=cand[:, hd, :]: e.match_replace(out=o_, in_to_replace=r_, in_values=i_, imm_value=-1e30),
                              ["cand", ckeys[hd]], ["wk2_%d" % hd])
                    for hd in range(8):
                        kb.op("dve", lambda e, o_=c8b[:, hd, :], i_=wk2[:, hd, :]: e.max(out=o_, in_=i_), ["wk2_%d" % hd], [ckeys[hd]])
                    kb.tt(csh[:], cand[:], c8a[:, :, 0:1].to_broadcast([128, 8, 256]), ALU.subtract, ["cand"] + ckeys, ["csh"])
                    kb.act(ec[:], csh[:], AF.Exp, ["csh"], ["ec"])
                    kb.tt(mk8[:], cand[:], c8b[:, :, 7:8].to_broadcast([128, 8, 256]), ALU.is_ge, ["cand"] + ckeys, ["mk8"])
                    kb.tt(ec[:], ec[:], mk8[:], ALU.mult, ["ec", "mk8"], ["ec"])
                    kb.op("dve", lambda e: e.tensor_reduce(out=Z[:], in_=ec[:], axis=AX.X, op=ALU.add), ["ec"], ["Z"])
                    kb.act(Z[:], Z[:], AF.Ln, ["Z"], ["Z"])
                    kb.cp(tb[:, 0:8], c8b[:, :, 7], ckeys, ["tb"])
                    kb.stt(tb[:, 8:16], c8a[:, :, 0], -1.0, Z[:], ALU.mult, ALU.subtract, ckeys + ["Z"], ["tb"])
                    kb.dma("pool", tb_d[rs, :], tb[:], reads=["tb"], writes=["tb_d"])
                phase_end()
            if _STOP == "C":
                break
            with ExitStack() as sd_:
                T = lambda name, shape, dt=F32, _p="L%dD_" % li: sd_.enter_context(nc.sbuf_tensor(_p + name, shape, dt))
                gft = T("gft", [128, 8])
                kb.dma("sp", gft[:], gffn_i[li], writes=["gt"])
                Ub = T("Ub", [128, 8, 4096], BF16); Vv = T("Vv", [128, 32, D], BF16)
                load_w(Ub, uT_i[li], 8, 4096, "Ub", gt=gft)
                load_w(Vv, v_i[li], 32, D, "Vv")
                gf = T("gf", [128, D])
                if final:
                    kb.dma("sp", gf[:], gfin, writes=["gf"])
                h1t = [T("h1t%d" % i, [128, D]) for i in range(2)]
                sct = [T("sctb%d" % i, [128, D]) for i in range(2)]
                xT = [T("xTb%d" % i, [128, D], BF16) for i in range(2)]
                tbt = [T("tbt%d" % i, [128, 16]) for i in range(2)]
                NBG = 4
                Sg = [T("Sg%d" % i, [128, 16, 64]) for i in range(NBG)]
                Eg = [T("Eg%d" % i, [128, 1024], BF16) for i in range(NBG)]
                Gh = [T("Gh%d" % i, [128, 1024], BF16) for i in range(2)]; G = T("G", [128, 1024], BF16)
                gl = [T("gl%d" % i, [128, 512]) for i in range(2)]
                Wb = T("Wb", [128, 1024], BF16); WT = T("WT", [128, 1024], BF16)
                ho = T("ho", [128, D]); junk = T("junkb", [128, D], BF16); ss = T("ssb", [128, 1])
                pH = [ps[0], ps[1]]; ptr = ps[2][:].bitcast(BF16); po = [ps[3], ps[4]]

                def loadB(i):
                    b = i % 2
                    rs = slice(i * 128, (i + 1) * 128)
                    kb.dma("sp", h1t[b][:], h1_d[rs, :], writes=["h1t%d" % b])
                    kb.dma("sp", sct[b][:], sc_d[rs, :], writes=["sctb%d" % b])
                    kb.dma("sp", xT[b][:], xT_d[rs, :], writes=["xTb%d" % b])
                    kb.dma("sp", tbt[b][:], tb_d[rs, :], writes=["tbt%d" % b])

                loadB(0)
                cnt = 0
                for i in range(NT):
                    b = i % 2
                    rs = slice(i * 128, (i + 1) * 128)
                    if i + 1 < NT:
                        loadB(i + 1)
                    for eq in range(4):
                        deferred = None
                        for hd in range(8):
                            j = cnt % NBG
                            gj = cnt % 2
                            cnt += 1
                            s1 = sct[b][:, hd * 128 + 16 * eq:hd * 128 + 16 * eq + 16]
                            s2 = sct[b][:, hd * 128 + 64:hd * 128 + 128]
                            kb.tt(Sg[j][:], s1.unsqueeze(2).to_broadcast([128, 16, 64]), s2.unsqueeze(1).to_broadcast([128, 16, 64]), ALU.add,
                                  ["sctb%d" % b], ["Sg%d" % j], eng=("pool" if hd in _POOL_HEADS else "dve"))
                            Sf = Sg[j][:].rearrange("p a b -> p (a b)")
                            kb.act(Eg[j][:], Sf, AF.Exp, ["Sg%d" % j, "tbt%d" % b], ["Eg%d" % j], bias=tbt[b][:, 8 + hd:9 + hd], scale=1.0)
                            if hd == 0:
                                kb.stt(G[:], Sf, tbt[b][:, hd:hd + 1], Eg[j][:], ALU.is_ge, ALU.mult, ["Sg%d" % j, "Eg%d" % j, "tbt%d" % b], ["G"])
                            else:
                                kb.stt(Gh[gj][:], Sf, tbt[b][:, hd:hd + 1], Eg[j][:], ALU.is_ge, ALU.mult, ["Sg%d" % j, "Eg%d" % j, "tbt%d" % b], ["Gh%d" % gj])
                                if deferred is not None:
                                    deferred()
                                deferred = (lambda gj=gj: kb.tt(G[:], G[:], Gh[gj][:], ALU.add, ["G", "Gh%d" % gj], ["G"]))
                        if deferred is not None:
                            deferred()
                        for g2 in range(2):
                            e0 = eq * 1024 + g2 * 512
                            for k in range(8):
                                kb.mm(pH[g2][:], xT[b][:, k * 128:(k + 1) * 128], Ub[:, k, e0:e0 + 512], k == 0, k == 7,
                                      ["xTb%d" % b, "Ub"], ["pH%d" % g2], inc=(k == 7))
                            kb.act(gl[g2][:], pH[g2][:], AF.Gelu, ["pH%d" % g2], ["gl%d" % g2])
                            kb.tt(Wb[:, g2 * 512:(g2 + 1) * 512], gl[g2][:], G[:, g2 * 512:(g2 + 1) * 512], ALU.mult, ["gl%d" % g2, "G"], ["Wb"])
                        for cc in range(8):
                            kb.tr(ptr[:, cc * 128:(cc + 1) * 128], Wb[:, cc * 128:(cc + 1) * 128], idb[:], ["Wb", "ident_b"], ["ptr"], inc=(cc == 7))
                        kb.cp(WT[:], ptr, ["ptr"], ["WT"], eng="act")
                        for dh in range(2):
                            for cc in range(8):
                                kb.mm(po[dh][:], WT[:, cc * 128:(cc + 1) * 128], Vv[:, eq * 8 + cc, dh * 512:(dh + 1) * 512],
                                      (eq == 0 and cc == 0), (eq == 3 and cc == 7), ["WT", "Vv"], ["po%d" % dh], inc=(cc == 7))
                    for dh in range(2):
                        kb.tt(ho[:, dh * 512:(dh + 1) * 512], h1t[b][:, dh * 512:(dh + 1) * 512], po[dh][:], ALU.add,
                              ["h1t%d" % b, "po%d" % dh], ["ho"])
                    if final:
                        _rstd(kb, ho[:], junk[:], ss[:], D, ["ho"], "f")
                        kb.stt(ho[:], ho[:], ss[:, 0:1], gf[:], ALU.mult, ALU.mult, ["ho", "fss", "gf"], ["ho"])
                    kb.dma("sp", hdst[rs, :], ho[:], reads=["ho"], writes=["hdst"])
                phase_end()
    return nc


def forward_fused(x, meta_tokens, attn_norm, w_in, attn_sinks, gla_gate_w2, gla_gate_b, swa_out_norm,
                  sb_out_norm, gla_out_norm, w_out, ffn_norm, peer_w_q, peer_sub_keys, peer_u, peer_v, final_norm, runner=None):
    f32 = lambda a: np.asarray(a, np.float32)
    x = f32(x)
    B, SEQ, _ = x.shape
    depth = attn_norm.shape[0]
    L = SEQ + 128
    Lp = ((L + 511) // 512) * 512
    TQ = Lp // 4
    assert B * 4 == NCORES
    nc = _prog_fused(Lp, depth)
    w_in, w_out = f32(w_in), f32(w_out)
    shared = {
        "g_attn": _c(np.stack([_gk(attn_norm[i]) for i in range(depth)])),
        "gffn": _c(np.stack([_gk(ffn_norm[i]) for i in range(depth)])),
        "wq": _c(f32(peer_w_q)),
        "ksub": _c(np.stack([np.transpose(f32(peer_sub_keys[i]).reshape(16, 64, 128), (2, 0, 1)) for i in range(depth)])),
        "uT": _c(np.transpose(f32(peer_u), (0, 2, 1))), "v": _c(f32(peer_v)),
        "gfin": _c(np.broadcast_to(f32(final_norm)[None, :], (128, D))),
    }
    per_j = []
    for j in range(4):
        kv = j // 2
        cols = np.concatenate([np.arange(128 * j, 128 * j + 128), np.arange(512 + 64 * kv, 512 + 64 * kv + 64),
                               np.arange(512 + 64 * kv, 512 + 64 * kv + 64), np.arange(768 + 64 * j, 768 + 64 * j + 64),
                               np.arange(1024 + 64 * j, 1024 + 64 * j + 64), np.arange(1536 + 32 * j, 1536 + 32 * j + 32),
                               np.arange(1664 + 32 * j, 1664 + 32 * j + 32), np.arange(2048, 2064),
                               np.arange(640 + 64 * kv, 640 + 64 * kv + 64), np.arange(1280 + 64 * j, 1280 + 64 * j + 64),
                               np.arange(1792 + 64 * j, 1792 + 64 * j + 64), np.arange(2064 + 64 * j, 2064 + 64 * j + 64)])
        assert len(cols) == CJ
        rows = np.concatenate([np.arange(128 * j, 128 * j + 128), np.arange(512 + 64 * j, 512 + 64 * j + 64),
                               np.arange(768 + 64 * j, 768 + 64 * j + 64)])
        gm = np.stack([np.concatenate([f32(swa_out_norm[i])[128 * j:128 * j + 128], f32(sb_out_norm[i])[64 * j:64 * j + 64],
                                       f32(gla_out_norm[i])[64 * j:64 * j + 64]]) for i in range(depth)])
        per_j.append({
            "w_in": _c(w_in[:, :, cols]),
            "w2": _c(f32(gla_gate_w2)[:, :, 32 * j:32 * j + 32]),
            "gb": _c(f32(gla_gate_b)[:, 32 * j:32 * j + 32].reshape(depth, 32, 1)),
            "sinks": _c(np.broadcast_to(f32(attn_sinks)[:, None, 2 * j:2 * j + 2], (depth, 128, 2))),
            "gmix": _c(np.broadcast_to(gm[:, None, :], (depth, 128, 256))),
            "wout": _c(np.transpose(w_out[:, rows, :].reshape(depth, 2, 128, D), (0, 2, 1, 3))),
        })
    maps = []
    for c in range(NCORES):
        b, r = c // 4, c % 4
        hp = np.zeros((Lp, D), np.float32)
        hp[112:128] = f32(meta_tokens)
        hp[128:L] = x[b]
        maps.append(dict(shared, **per_j[r], h0=_c(hp[r * TQ:(r + 1) * TQ])))
    res = (runner or _run)(nc, maps)
    out = np.zeros((B, SEQ, D), np.float32)
    for b in range(B):
        full = np.concatenate([np.asarray(res[b * 4 + r]["out"]) for r in range(4)], 0)
        out[b] = full[128:L]
    return out


def _prog_fused(Lp, depth):
    key = ("fused", Lp, depth)
    if key not in _CACHE:
        _CACHE[key] = build_fused(Lp, depth)
    return _CACHE[key]
```

```python
import numpy as np
from contextlib import ExitStack
import ml_dtypes
import concourse.bass as bass
import concourse.mybir as mybir
from concourse.bass_utils import run_bass_kernel_spmd

F32 = mybir.dt.float32
BF16 = mybir.dt.bfloat16
AF = mybir.ActivationFunctionType
ALU = mybir.AluOpType
AX = mybir.AxisListType

D = 1024
IN_COLS = 2320
EPS = 1e-6
NEG = -30000.0
NCORES = 8


class KB:
    ENGS = ("pe", "act", "dve", "pool", "sp")
    NDMA = 8

    def __init__(self, nc, stack):
        self.nc = nc
        self._stack = stack
        self.q = {e: [] for e in self.ENGS}
        self.cnt = {e: 0 for e in self.ENGS}
        self.sem = {e: stack.enter_context(nc.semaphore("s_" + e)) for e in self.ENGS}
        self.dsem = {e: [stack.enter_context(nc.semaphore("d_%s%d" % (e, i))) for i in range(self.NDMA)]
                     for e in ("sp", "pool")}
        self.dcnt = {e: [0] * self.NDMA for e in self.dsem}
        self.drot = {e: 0 for e in self.dsem}
        self.seen = {e: {} for e in self.ENGS}
        self.lastw = {}
        self.readers = {}
        self.pending_noinc = {e: False for e in self.ENGS}
        self.excl = set()

    def _deps(self, eng, reads, writes):
        toks = []
        for k in list(reads) + list(writes):
            t = self.lastw.get(k)
            if t is not None:
                toks.append(t)
        for k in writes:
            toks.extend(self.readers.get(k, {}).values())
        waits = {}
        for (sem, val, src) in toks:
            if src == "pe" and eng == "pe":
                continue
            sid = id(sem)
            if self.seen[eng].get(sid, 0) >= val:
                continue
            if sid not in waits or waits[sid][1] < val:
                waits[sid] = (sem, val)
        for sid, (sem, val) in waits.items():
            self.seen[eng][sid] = val
        return list(waits.values())

    def _record(self, rkey, tok, reads, writes):
        for k in writes:
            self.lastw[k] = tok
            self.readers[k] = {}
        for k in reads:
            self.readers.setdefault(k, {})[rkey] = tok

    def op(self, eng, fn, reads=(), writes=(), inc=True):
        ex = [k for k in reads if k in self.excl and k not in writes]
        if ex:
            writes = list(writes) + ex
        waits = self._deps(eng, reads, writes)
        sem = self.sem[eng]
        if inc:
            self.cnt[eng] += 1
            tok = (sem, self.cnt[eng], eng)
            self.pending_noinc[eng] = False
        else:
            tok = (sem, self.cnt[eng] + 1, eng)
            self.pending_noinc[eng] = True
        self.q[eng].append((waits, fn, (sem, 1) if inc else None))
        self._record(eng, tok, reads, writes)

    def dma(self, eng, out, in_, reads=(), writes=()):
        waits = self._deps(eng, reads, writes)
        r = self.drot[eng]
        self.drot[eng] = (r + 1) % self.NDMA
        sem = self.dsem[eng][r]
        prev = self.dcnt[eng][r]
        if prev > 0 and self.seen[eng].get(id(sem), 0) < prev:
            waits.append((sem, prev))
            self.seen[eng][id(sem)] = prev
        self.dcnt[eng][r] += 16
        tok = (sem, self.dcnt[eng][r], "dma")
        self.q[eng].append((waits, lambda e, o=out, i=in_: e.dma_start(out=o, in_=i), (sem, 16)))
        self._record(("dma", id(sem)), tok, reads, writes)

    def coll(self, kind, op, groups, in_, out, reads, writes):
        waits = self._deps("pool", reads, writes)
        if not hasattr(self, "csem"):
            self.csem = self._stack.enter_context(self.nc.semaphore("s_cc"))
            self.ccnt = 0
        if self.ccnt > 0 and self.seen["pool"].get(id(self.csem), 0) < self.ccnt:
            waits.append((self.csem, self.ccnt))
            self.seen["pool"][id(self.csem)] = self.ccnt
        self.ccnt += 1
        tok = (self.csem, self.ccnt, "dma")
        self.q["pool"].append((waits, lambda e, k=kind, o=op, g=groups, i=in_, u=out:
                               e.collective_compute(k, o, replica_groups=g, ins=[i.opt()], outs=[u.opt()]), (self.csem, 1)))
        self._record(("dma", id(self.csem)), tok, reads, writes)

    def wait_all(self, eng, keys):
        waits = self._deps(eng, keys, ())
        self.q[eng].append((waits, None, None))

    def barrier(self):
        for e in self.ENGS:
            waits = []
            for f in self.ENGS:
                if f != e and self.cnt[f] > 0 and self.seen[e].get(id(self.sem[f]), 0) < self.cnt[f]:
                    waits.append((self.sem[f], self.cnt[f]))
                    self.seen[e][id(self.sem[f])] = self.cnt[f]
            for q in self.dsem:
                for r in range(self.NDMA):
                    s, v = self.dsem[q][r], self.dcnt[q][r]
                    if v > 0 and self.seen[e].get(id(s), 0) < v:
                        waits.append((s, v))
                        self.seen[e][id(s)] = v
            if hasattr(self, "csem") and self.ccnt > 0 and self.seen[e].get(id(self.csem), 0) < self.ccnt:
                waits.append((self.csem, self.ccnt))
                self.seen[e][id(self.csem)] = self.ccnt
            self.q[e].append((waits, None, None))

    def emit(self):
        nc = self.nc
        for e in self.ENGS:
            assert not self.pending_noinc[e], "trailing non-inc op on " + e
        qs = self.q
        self.q = {e: [] for e in self.ENGS}
        with nc.Block() as block:
            def run(engname):
                def body(e):
                    for waits, fn, inc in qs[engname]:
                        for sem, val in waits:
                            e.wait_ge(sem, val)
                        if fn is None:
                            continue
                        ins = fn(e)
                        if inc is not None:
                            ins.then_inc(inc[0], inc[1])
                return body
            block.tensor(run("pe"))
            block.scalar(run("act"))
            block.vector(run("dve"))
            block.gpsimd(run("pool"))
            block.sync(run("sp"))

    def act(self, out, in_, func, r, w, eng="act", **kw):
        self.op(eng, lambda e, o=out, i=in_, f=func, k=kw: e.activation(out=o, in_=i, func=f, **k), r, w)

    def tt(self, out, in0, in1, op, r, w, eng="dve"):
        self.op(eng, lambda e, o=out, a=in0, b=in1, p=op: e.tensor_tensor(out=o, in0=a, in1=b, op=p), r, w)

    def ts(self, out, in0, s1, op0, r, w, s2=None, op1=None, eng="dve"):
        if op1 is None:
            self.op(eng, lambda e, o=out, a=in0, x=s1, p=op0: e.tensor_scalar(out=o, in0=a, scalar1=x, scalar2=None, op0=p), r, w)
        else:
            self.op(eng, lambda e, o=out, a=in0, x=s1, y=s2, p=op0, q=op1: e.tensor_scalar(out=o, in0=a, scalar1=x, scalar2=y, op0=p, op1=q), r, w)

    def stt(self, out, in0, scalar, in1, op0, op1, r, w, **kw):
        self.op("dve", lambda e, o=out, a=in0, s=scalar, b=in1, p=op0, q=op1, k=kw:
                e.scalar_tensor_tensor(out=o, in0=a, scalar=s, in1=b, op0=p, op1=q, **k), r, w)

    def cp(self, out, in_, r, w, eng="dve"):
        if eng == "act":
            self.op("act", lambda e, o=out, i=in_: e.activation(out=o, in_=i, func=AF.Copy), r, w)
        else:
            self.op(eng, lambda e, o=out, i=in_: e.tensor_copy(out=o, in_=i), r, w)

    def mm(self, out, lhsT, rhs, start, stop, r, w, inc=True):
        self.op("pe", lambda e, o=out, l=lhsT, x=rhs, s=start, t=stop: e.matmul(o, lhsT=l, rhs=x, start=s, stop=t), r, w, inc=inc)

    def tr(self, out, in_, ident, r, w, inc=True):
        self.op("pe", lambda e, o=out, i=in_, d=ident: e.transpose(o, i, d), r, w, inc=inc)

    def memset(self, ap, val, w, eng="pool"):
        self.op(eng, lambda e, a=ap, v=val: e.memset(a, v), (), w)

    def asel(self, ap, cmp, fill, base, cm, pattern, key):
        self.op("pool", lambda e, a=ap, c=cmp, f=fill, b=base, m=cm, p=pattern:
                e.affine_select(out=a, in_=a, compare_op=c, fill=f, base=b, pattern=p, channel_multiplier=m), [key], [key])


def _ident(kb, T, name="ident"):
    idf = T(name + "_f", [128, 128], F32)
    idb = T(name + "_b", [128, 128], BF16)
    kb.memset(idf[:], 0.0, [name + "_f"])
    kb.asel(idf[:], ALU.not_equal, 1.0, 0, 1, [[-1, 128]], name + "_f")
    kb.cp(idb[:], idf[:], [name + "_f"], [name + "_b"], eng="pool")
    return idf, idb


def _rstd(kb, src, junk, ss, n, rkeys, pfx):
    kb.act(junk, src, AF.Square, rkeys, [pfx + "junk", pfx + "ss"], accum_out=ss)
    kb.ts(ss, ss, 1.0 / n, ALU.mult, [pfx + "ss"], [pfx + "ss"], s2=EPS, op1=ALU.add)
    kb.act(ss, ss, AF.Sqrt, [pfx + "ss"], [pfx + "ss"])
    kb.op("dve", lambda e, a=ss: e.reciprocal(out=a, in_=a), [pfx + "ss"], [pfx + "ss"])


def build_p1(NT):
    nc = bass.Bass("TRN2", target_bir_lowering=False)
    h = nc.dram_tensor("h", [NT * 128, D], F32, kind="ExternalInput").ap()
    w = nc.dram_tensor("w", [D, IN_COLS], F32, kind="ExternalInput").ap()
    g = nc.dram_tensor("g", [128, 8], F32, kind="ExternalInput").ap()
    proj = nc.dram_tensor("proj", [NT * 128, IN_COLS], BF16, kind="ExternalOutput").ap()
    with ExitStack() as st:
        kb = KB(nc, st)
        T = lambda name, shape, dt=F32: st.enter_context(nc.sbuf_tensor(name, shape, dt))
        ps = [st.enter_context(nc.psum_tensor("ps%d" % i, [128, 512], F32)) for i in range(8)]
        idf, idb = _ident(kb, T)
        gt = T("gt", [128, 8])
        kb.dma("sp", gt[:], g, writes=["gt"])
        Wg = T("Wg", [128, 8, IN_COLS], BF16)
        stage = [T("stage%d" % i, [128, IN_COLS]) for i in range(2)]
        for k in range(8):
            sk = "stage%d" % (k % 2)
            kb.dma("sp", stage[k % 2][:], w[k * 128:(k + 1) * 128, :], writes=[sk])
            kb.ts(Wg[:, k, :], stage[k % 2][:], gt[:, k:k + 1], ALU.mult, [sk, "gt"], ["Wg%d" % k])
        ht = [T("ht%d" % i, [128, D]) for i in range(2)]
        junk = T("junk", [128, D], BF16)
        ss = T("ss", [128, 1])
        xn = T("xn", [128, D], BF16)
        xnT = T("xnT", [128, D], BF16)
        pr = [T("pr%d" % i, [128, IN_COLS], BF16) for i in range(2)]
        pT = ps[0][:].bitcast(BF16)
        cgs = [(c0, min(512, IN_COLS - c0)) for c0 in range(0, IN_COLS, 512)]
        wkeys = ["Wg%d" % k for k in range(8)]
        for i in range(NT):
            hk = "ht%d" % (i % 2)
            hb = ht[i % 2]
            kb.dma("sp", hb[:], h[i * 128:(i + 1) * 128, :], writes=[hk])
            _rstd(kb, hb[:], junk[:], ss[:], D, [hk], "a")
            kb.ts(xn[:], hb[:], ss[:, 0:1], ALU.mult, [hk, "ass"], ["xn"])
            for k in range(8):
                kb.tr(pT[:, k * 128:(k + 1) * 128], xn[:, k * 128:(k + 1) * 128], idb[:], ["xn", "ident_b"], ["pT"], inc=(k == 7))
            kb.cp(xnT[:], pT, ["pT"], ["xnT"], eng="act")
            prk = "pr%d" % (i % 2)
            for ci, (c0, cw) in enumerate(cgs):
                pk = "pp%d" % (ci % 2)
                pp = ps[1 + ci % 2]
                for k in range(8):
                    kb.mm(pp[:, 0:cw], xnT[:, k * 128:(k + 1) * 128], Wg[:, k, c0:c0 + cw], k == 0, k == 7,
                          ["xnT", wkeys[k]], [pk], inc=(k == 7))
                kb.cp(pr[i % 2][:, c0:c0 + cw], pp[:, 0:cw], [pk], [prk], eng=("act" if ci % 2 else "dve"))
            kb.dma("pool", proj[i * 128:(i + 1) * 128, :], pr[i % 2][:], reads=[prk], writes=["out"])
        kb.wait_all("pool", ["out"])
        kb.emit()
    return nc


def build_p2(Lp, parts=(1, 1, 1)):
    NCH = Lp // 512
    NB = Lp // 128
    nc = bass.Bass("TRN2", target_bir_lowering=False)
    IN = lambda n, s, dt=BF16: nc.dram_tensor(n, s, dt, kind="ExternalInput").ap()
    qaT = IN("qaT", [128, Lp]); kaT = IN("kaT", [128, Lp]); va = IN("va", [Lp, 64])
    qbT = IN("qbT", [64, Lp]); kbT = IN("kbT", [64, Lp]); vb = IN("vb", [Lp, 64])
    qcT = IN("qcT", [32, Lp]); kcT = IN("kcT", [32, Lp]); vc = IN("vc", [Lp, 64])
    glrT = IN("glrT", [16, Lp])
    w2 = IN("w2", [16, 32], F32); gb = IN("gb", [32, 1], F32); sinks = IN("sinks", [128, 2], F32)
    oa = nc.dram_tensor("oa", [Lp, 128], F32, kind="ExternalOutput").ap()
    obT = nc.dram_tensor("obT", [64, Lp], F32, kind="ExternalOutput").ap()
    oc = nc.dram_tensor("oc", [Lp, 64], F32, kind="ExternalOutput").ap()
    with ExitStack() as st:
        kb = KB(nc, st)
        T = lambda name, shape, dt=F32: st.enter_context(nc.sbuf_tensor(name, shape, dt))
        ps = [st.enter_context(nc.psum_tensor("ps%d" % i, [128, 512], F32)) for i in range(8)]
        idf, idb = _ident(kb, T)
        m_gen = T("m_gen", [128, 256]); m_n0 = T("m_n0", [128, 256]); m_n1 = T("m_n1", [128, 256])
        for m, nm, extra in ((m_gen, "m_gen", None), (m_n0, "m_n0", -240), (m_n1, "m_n1", -112)):
            kb.memset(m[:], 0.0, [nm])
            kb.asel(m[:], ALU.is_ge, NEG, -1, -1, [[1, 256]], nm)
            kb.asel(m[:], ALU.is_ge, NEG, 128, 1, [[-1, 256]], nm)
            if extra is not None:
                kb.asel(m[:], ALU.is_ge, NEG, extra, 0, [[1, 256]], nm)
        sbm_f = T("sbm_f", [128, 512])
        sbm = [T("sbm%d" % d, [128, 512], BF16) for d in range(4)]
        for d in range(4):
            kb.memset(sbm_f[:], 1.0, ["sbm_f"])
            kb.asel(sbm_f[:], ALU.is_ge, 0.0, -1 - 128 * d, -1, [[1, 512]], "sbm_f")
            kb.cp(sbm[d][:], sbm_f[:], ["sbm_f"], ["sbm%d" % d], eng="pool")
        ntri_f = T("ntri_f", [128, 128]); ntri = T("ntri", [128, 128], BF16); nones = T("nones", [128, 128], BF16)
        kb.memset(ntri_f[:], -1.0, ["ntri_f"])
        kb.asel(ntri_f[:], ALU.is_ge, 0.0, 0, 1, [[-1, 128]], "ntri_f")
        kb.cp(ntri[:], ntri_f[:], ["ntri_f"], ["ntri"], eng="pool")
        kb.memset(nones[:], -1.0, ["nones"])
        mle = T("mle", [128, 128])
        kb.memset(mle[:], 1.0, ["mle"])
        kb.asel(mle[:], ALU.is_ge, 0.0, 0, -1, [[1, 128]], "mle")
        rmask = T("rmask", [32, 512])
        kb.memset(rmask[:], 1.0, ["rmask"])
        for b in range(4):
            kb.memset(rmask[:, b * 128:b * 128 + 1], 0.0, ["rmask"])
        w2f = T("w2f", [16, 32]); w2b = T("w2b", [16, 32], BF16); gbt = T("gbt", [32, 1]); sk_t = T("sk_t", [128, 2])
        kb.dma("sp", w2f[:], w2, writes=["w2f"]); kb.dma("sp", gbt[:], gb, writes=["gbt"]); kb.dma("sp", sk_t[:], sinks, writes=["sk"])
        kb.cp(w2b[:], w2f[:], ["w2f"], ["w2b"])
        kb.ts(gbt[:], gbt[:], -1.0, ALU.mult, ["gbt"], ["gbt"])
        KbT = T("KbT", [64, Lp], BF16); Vb = T("Vb", [128, NB, 64], BF16)
        kb.dma("sp", KbT[:], kbT, writes=["KbT"])
        for n0 in range(0, NB, 16):
            n1 = min(NB, n0 + 16)
            kb.dma("sp", Vb[:, n0:n1, :], vb[n0 * 128:n1 * 128, :].rearrange("(n p) d -> p n d", p=128), writes=["Vb"])
        S = T("S", [32, 64]); Sb = T("Sb", [32, 64], BF16)
        kb.memset(S[:], 0.0, ["S"]); kb.memset(Sb[:], 0.0, ["Sb"])
        qa_c = [T("qa_c%d" % i, [128, 512], BF16) for i in range(2)]
        ka_c = [T("ka_c%d" % i, [128, 640], BF16) for i in range(2)]
        va_c = [T("va_c%d" % i, [128, 5, 64], BF16) for i in range(2)]
        qb_c = [T("qb_c%d" % i, [64, 512], BF16) for i in range(2)]
        qs_c = [T("qs_c%d" % i, [64, 512], BF16) for i in range(2)]
        qc_c = [T("qc_c%d" % i, [32, 512], BF16) for i in range(2)]
        kc_c = [T("kc_c%d" % i, [32, 512], BF16) for i in range(2)]
        vc_c = [T("vc_c%d" % i, [128, 4, 64], BF16) for i in range(2)]
        gl_c = [T("gl_c%d" % i, [16, 512], BF16) for i in range(2)]
        sm = T("sm", [128, 256]); pexp = T("pexp", [128, 256], BF16); pTs = T("pTs", [128, 256], BF16)
        st8 = T("st8", [128, 8]); oa_t = [T("oa_t%d" % i, [128, 128]) for i in range(2)]
        ge = T("ge", [32, 512]); gsp = T("gsp", [32, 512]); gcs = T("gcs", [32, 512])
        geq = T("geq", [32, 512]); gek = T("gek", [32, 512])
        qt = T("qt", [32, 512], BF16); kt = T("kt", [32, 512], BF16)
        scb = T("scb", [128, 128], BF16); ktm = T("ktm", [128, 32], BF16); oc_t = [T("oc_t%d" % i, [128, 64]) for i in range(2)]
        stmp = T("stmp", [32, 64])
        e_t = [T("e_t%d" % i, [128, 512]) for i in range(2)]
        sp_t = [T("sp_t%d" % i, [128, 512], BF16) for i in range(2)]
        a_t = [T("a_t%d" % i, [128, 512], BF16) for i in range(2)]
        R = T("R", [128, 512], BF16)
        ob_t = [T("ob_t%d" % i, [64, 512]) for i in range(2)]
        pz = [ps[0], ps[1]]; pc = [ps[2], ps[3]]; pO = ps[4]
        pA = ps[5]; pX = ps[6]; pB = ps[7]
        pT_bf = pB[:].bitcast(BF16)

        def load_chunk(c):
            i = c % 2
            c0 = c * 512
            kb.dma("sp", qa_c[i][:], qaT[:, c0:c0 + 512], writes=["qa_c%d" % i])
            if c == 0:
                kb.memset(ka_c[i][:, 0:128], 0.0, ["ka_c%d" % i])
                kb.memset(va_c[i][:, 0, :], 0.0, ["va_c%d" % i])
                kb.dma("sp", ka_c[i][:, 128:640], kaT[:, 0:512], writes=["ka_c%d" % i])
                kb.dma("sp", va_c[i][:, 1:5, :], va[0:512, :].rearrange("(n p) d -> p n d", p=128), writes=["va_c%d" % i])
            else:
                kb.dma("sp", ka_c[i][:], kaT[:, c0 - 128:c0 + 512], writes=["ka_c%d" % i])
                kb.dma("sp", va_c[i][:], va[c0 - 128:c0 + 512, :].rearrange("(n p) d -> p n d", p=128), writes=["va_c%d" % i])
            kb.dma("sp", qb_c[i][:], qbT[:, c0:c0 + 512], writes=["qb_c%d" % i])
            kb.dma("sp", qc_c[i][:], qcT[:, c0:c0 + 512], writes=["qc_c%d" % i])
            kb.dma("sp", kc_c[i][:], kcT[:, c0:c0 + 512], writes=["kc_c%d" % i])
            kb.dma("sp", vc_c[i][:], vc[c0:c0 + 512, :].rearrange("(n p) d -> p n d", p=128), writes=["vc_c%d" % i])
            kb.dma("sp", gl_c[i][:], glrT[:, c0:c0 + 512], writes=["gl_c%d" % i])

        def _gla(c, i):
            kb.mm(pX[0:32, :], w2b[:], gl_c[i][:], True, True, ["w2b", "gl_c%d" % i], ["pX"])
            kb.act(ge[:], pX[0:32, :], AF.Exp, ["pX", "gbt"], ["ge"], bias=gbt[:, 0:1], scale=-1.0)
            kb.act(gsp[:], ge[:], AF.Ln, ["ge"], ["gsp"], bias=1.0, scale=1.0)
            kb.op("dve", lambda e: e.tensor_tensor_scan(out=gcs[:], data0=rmask[:], data1=gsp[:], initial=0.0, op0=ALU.mult, op1=ALU.add),
                  ["rmask", "gsp"], ["gcs"])
            kb.act(geq[:], gcs[:], AF.Exp, ["gcs"], ["geq"], scale=-1.0 / 16.0)
            kb.act(gek[:], gcs[:], AF.Exp, ["gcs"], ["gek"], scale=1.0 / 16.0)
            kb.stt(qt[:], qc_c[i][:], 32.0 ** -0.5, geq[:], ALU.mult, ALU.mult, ["qc_c%d" % i, "geq"], ["qt"])
            kb.tt(kt[:], kc_c[i][:], gek[:], ALU.mult, ["kc_c%d" % i, "gek"], ["kt"])
            for blk in range(4):
                n = 4 * c + blk
                bs = slice(blk * 128, (blk + 1) * 128)
                kb.mm(pA[:, 384:512], kt[:, bs], qt[:, bs], True, True, ["kt", "qt"], ["pA"])
                kb.tt(scb[:], pA[:, 384:512], mle[:], ALU.mult, ["pA", "mle"], ["scb"])
                kb.tr(pT_bf[:, 512:544], kt[:, bs], idb[0:32, 0:32], ["kt", "ident_b"], ["pB"])
                kb.cp(ktm[:], pT_bf[:, 512:544], ["pB"], ["ktm"], eng="act")
                kb.mm(pA[:, 320:384], scb[:], vc_c[i][:, blk, :], True, False, ["scb", "vc_c%d" % i], ["pA"], inc=False)
                kb.mm(pA[:, 320:384], qt[:, bs], Sb[:], False, True, ["qt", "Sb"], ["pA"])
                ock = "oc_t%d" % (n % 2)
                kb.cp(oc_t[n % 2][:], pA[:, 320:384], ["pA"], [ock], eng="act")
                kb.dma("pool", oc[n * 128:(n + 1) * 128, :], oc_t[n % 2][:], reads=[ock], writes=["oc"])
                kb.mm(pB[0:32, 384:448], ktm[:], vc_c[i][:, blk, :], True, True, ["ktm", "vc_c%d" % i], ["pB"])
                kb.tt(stmp[:], pB[0:32, 384:448], S[:], ALU.add, ["pB", "S"], ["stmp"])
                kb.ts(S[:], stmp[:], geq[:, blk * 128 + 127:blk * 128 + 128], ALU.mult, ["stmp", "geq"], ["S"])
                kb.cp(Sb[:], S[:], ["S"], ["Sb"])

        def _sb(c, i):
            kb.ts(qs_c[i][:], qb_c[i][:], 0.125, ALU.mult, ["qb_c%d" % i], ["qs_c%d" % i])
            nkb = 4 * c + 4
            for it in range(nkb):
                kblk = nkb - 1 - it
                j = it % 2
                dg = kblk - 4 * c
                first, last = (it == 0), (kblk == 0)
                ksl = KbT[:, kblk * 128:(kblk + 1) * 128]
                kb.mm(pz[j][:], ksl, qs_c[i][:], True, True, ["KbT", "qs_c%d" % i], ["pz%d" % j])
                kb.act(e_t[j][:], pz[j][:], AF.Exp, ["pz%d" % j], ["e_t%d" % j])
                kb.act(sp_t[j][:], e_t[j][:], AF.Ln, ["e_t%d" % j], ["sp_t%d" % j], bias=1.0, scale=1.0)
                if dg >= 0:
                    kb.tt(sp_t[j][:], sp_t[j][:], sbm[dg][:], ALU.mult, ["sp_t%d" % j, "sbm%d" % dg], ["sp_t%d" % j])
                kb.mm(pc[j][:], ntri[:], sp_t[j][:], True, False, ["ntri", "sp_t%d" % j], ["pc%d" % j], inc=False)
                if not first:
                    kb.mm(pc[j][:], nones[:], R[:], False, False, ["nones", "R"], ["pc%d" % j], inc=False)
                kb.mm(pc[j][:], ksl, qs_c[i][:], False, True, ["KbT", "qs_c%d" % i], ["pc%d" % j])
                if not last:
                    if first:
                        kb.cp(R[:], sp_t[j][:], ["sp_t%d" % j], ["R"], eng="pool")
                    else:
                        kb.tt(R[:], R[:], sp_t[j][:], ALU.add, ["R", "sp_t%d" % j], ["R"], eng="pool")
                kb.act(a_t[j][:], pc[j][:], AF.Exp, ["pc%d" % j], ["a_t%d" % j])
                if dg >= 0:
                    kb.tt(a_t[j][:], a_t[j][:], sbm[dg][:], ALU.mult, ["a_t%d" % j, "sbm%d" % dg], ["a_t%d" % j])
                kb.mm(pO[0:64, :], Vb[:, kblk, :], a_t[j][:], first, last, ["Vb", "a_t%d" % j], ["pO"], inc=True)
            kb.cp(ob_t[i][:], pO[0:64, :], ["pO"], ["ob_t%d" % i])
            kb.dma("pool", obT[:, c * 512:(c + 1) * 512], ob_t[i][:], reads=["ob_t%d" % i], writes=["ob"])

        load_chunk(0)
        for c in range(NCH):
            i = c % 2
            if c + 1 < NCH:
                load_chunk(c + 1)
            for blk in (range(4) if parts[0] else []):
                n = 4 * c + blk
                msk = m_n0 if n == 0 else (m_n1 if n == 1 else m_gen)
                mk = "m_n0" if n == 0 else ("m_n1" if n == 1 else "m_gen")
                ok = "oa_t%d" % (n % 2)
                for hh in range(2):
                    hs = slice(hh * 64, (hh + 1) * 64)
                    kb.mm(pA[:, 0:256], qa_c[i][hs, blk * 128:(blk + 1) * 128], ka_c[i][hs, blk * 128:blk * 128 + 256],
                          True, True, ["qa_c%d" % i, "ka_c%d" % i], ["pA"])
                    kb.stt(sm[:], pA[:, 0:256], 0.125, msk[:], ALU.mult, ALU.add, ["pA", mk], ["sm"])
                    kb.op("dve", lambda e: e.tensor_reduce(out=st8[:, 0:1], in_=sm[:], axis=AX.X, op=ALU.max), ["sm"], ["st8"])
                    kb.tt(st8[:, 0:1], st8[:, 0:1], sk_t[:, hh:hh + 1], ALU.max, ["st8", "sk"], ["st8"])
                    kb.ts(st8[:, 1:2], st8[:, 0:1], -1.0, ALU.mult, ["st8"], ["st8"])
                    kb.act(pexp[:], sm[:], AF.Exp, ["sm", "st8"], ["pexp", "st8"], bias=st8[:, 1:2], scale=1.0, accum_out=st8[:, 2:3])
                    kb.act(st8[:, 3:4], sk_t[:, hh:hh + 1], AF.Exp, ["sk", "st8"], ["st8"], bias=st8[:, 1:2], scale=1.0)
                    kb.tt(st8[:, 4:5], st8[:, 2:3], st8[:, 3:4], ALU.add, ["st8"], ["st8"])
                    kb.op("dve", lambda e: e.reciprocal(out=st8[:, 5:6], in_=st8[:, 4:5]), ["st8"], ["st8"])
                    kb.tr(pT_bf[:, 0:128], pexp[:, 0:128], idb[:], ["pexp", "ident_b"], ["pB"], inc=False)
                    kb.tr(pT_bf[:, 128:256], pexp[:, 128:256], idb[:], ["pexp", "ident_b"], ["pB"])
                    kb.cp(pTs[:], pT_bf[:, 0:256], ["pB"], ["pTs"], eng="act")
                    kb.mm(pA[:, 256:320], pTs[:, 0:128], va_c[i][:, blk, :], True, False, ["pTs", "va_c%d" % i], ["pA"], inc=False)
                    kb.mm(pA[:, 256:320], pTs[:, 128:256], va_c[i][:, blk + 1, :], False, True, ["pTs", "va_c%d" % i], ["pA"])
                    kb.ts(oa_t[n % 2][:, hs], pA[:, 256:320], st8[:, 5:6], ALU.mult, ["pA", "st8"], [ok])
                kb.dma("pool", oa[n * 128:(n + 1) * 128, :], oa_t[n % 2][:], reads=[ok], writes=["oa"])
            if parts[1]:
              _gla(c, i)
            if parts[2]:
              _sb(c, i)
        kb.wait_all("pool", ["oa", "oc", "ob"])
        kb.emit()
    return nc


def build_p3(NT, final):
    nc = bass.Bass("TRN2", target_bir_lowering=False)
    IN = lambda n, s, dt=F32: nc.dram_tensor(n, s, dt, kind="ExternalInput").ap()
    h = IN("h", [NT * 128, D]); o = IN("o", [NT * 128, D]); rc = IN("rc", [NT * 128, 256], BF16)
    gmix = IN("gmix", [128, D]); wout = IN("wout", [D, D]); gffn = IN("gffn", [128, 8])
    wq = IN("wq", [D, 2048]); ksub = IN("ksub", [128, 16, 64]); uT = IN("uT", [D, 4096]); v = IN("v", [4096, D])
    gfin = IN("gfin", [128, D])
    hout = nc.dram_tensor("hout", [NT * 128, D], F32, kind="ExternalOutput").ap()
    h1_d = nc.dram_tensor("h1_d", [NT * 128, D], F32, kind="Internal").ap()
    sc_d = nc.dram_tensor("sc_d", [NT * 128, D], F32, kind="Internal").ap()
    tb_d = nc.dram_tensor("tb_d", [NT * 128, 16], F32, kind="Internal").ap()
    xT_d = nc.dram_tensor("xT_d", [NT * 128, D], BF16, kind="Internal").ap()
    with ExitStack() as st:
        kb = KB(nc, st)
        ps = [st.enter_context(nc.psum_tensor("ps%d" % i, [128, 512], F32)) for i in range(8)]
        T0 = lambda name, shape, dt=F32: st.enter_context(nc.sbuf_tensor(name, shape, dt))
        idf, idb = _ident(kb, T0)
        gft = T0("gft", [128, 8])
        kb.dma("sp", gft[:], gffn, writes=["gft"])
        stage = [T0("stage%d" % i, [128, 1024]) for i in range(2)]
        scnt = [0]

        def load_w(dst, src, rows_k, cols, key, scale_col=None):
            for k in range(rows_k):
                for c0 in range(0, cols, 1024):
                    cw = min(1024, cols - c0)
                    si = scnt[0] % 2
                    scnt[0] += 1
                    sk = "stage%d" % si
                    kb.dma("sp", stage[si][:, 0:cw], src[k * 128:(k + 1) * 128, c0:c0 + cw], writes=[sk])
                    if scale_col:
                        kb.ts(dst[:, k, c0:c0 + cw], stage[si][:, 0:cw], gft[:, k:k + 1], ALU.mult, [sk, "gft"], [key])
                    else:
                        kb.cp(dst[:, k, c0:c0 + cw], stage[si][:, 0:cw], [sk], [key], eng="pool")

        with ExitStack() as sa:
            T = lambda name, shape, dt=F32: sa.enter_context(nc.sbuf_tensor(name, shape, dt))
            Wo = T("Wo", [128, 8, D], BF16); Wq = T("Wq", [128, 8, 2048], BF16); Ks = T("Ks", [128, 16, 64], BF16)
            gm = T("gm", [128, D]); ksf = T("ksf", [128, 16, 64])
            kb.dma("sp", gm[:], gmix, writes=["gm"])
            kb.dma("sp", ksf[:], ksub, writes=["ksf"])
            kb.cp(Ks[:], ksf[:], ["ksf"], ["Ks"])
            load_w(Wo, wout, 8, D, "Wo")
            load_w(Wq, wq, 8, 2048, "Wq", scale_col=True)
            ht = [T("ht%d" % i, [128, D]) for i in range(2)]
            ot = [T("ot%d" % i, [128, D]) for i in range(2)]
            rct = [T("rct%d" % i, [128, 256], BF16) for i in range(2)]
            sq = T("sq", [128, D]); m1 = T("m1", [128, D]); sil = T("sil", [128, 256])
            s16 = T("s16", [128, 16]); mix = T("mix", [128, D], BF16); mixT = T("mixT", [128, D], BF16)
            h1 = T("h1", [128, D]); junk = T("junk", [128, D], BF16); ss = T("ss", [128, 1])
            xn = T("xn", [128, D], BF16); xnT = T("xnT", [128, D], BF16)
            qT = T("qT", [128, 16, 128], BF16); sct = T("sct", [128, D])
            t1 = T("t1", [128, 8, 16]); t2 = T("t2", [128, 8, 16]); wk = T("wk", [128, 256])
            cand = T("cand", [128, 8, 256]); c8a = T("c8a", [128, 8, 8]); c8b = T("c8b", [128, 8, 8])
            csh = T("csh", [128, 8, 256]); ec = T("ec", [128, 8, 256]); mk8 = T("mk8", [128, 8, 256])
            Z = T("Z", [128, 8]); tb = T("tb", [128, 16])
            pT = ps[0][:].bitcast(BF16)
            for i in range(NT):
                b = i % 2
                rs = slice(i * 128, (i + 1) * 128)
                kb.dma("sp", ht[b][:], h[rs, :], writes=["ht%d" % b])
                kb.dma("sp", ot[b][:], o[rs, :], writes=["ot%d" % b])
                kb.dma("sp", rct[b][:], rc[rs, :], writes=["rct%d" % b])
                kb.act(sq[:], ot[b][:], AF.Square, ["ot%d" % b], ["sq"])
                kb.op("dve", lambda e: e.tensor_reduce(out=s16[:], in_=sq[:].rearrange("p (h d) -> p h d", d=64), axis=AX.X, op=ALU.add),
                      ["sq"], ["s16"])
                kb.ts(s16[:], s16[:], 1.0 / 64, ALU.mult, ["s16"], ["s16"], s2=EPS, op1=ALU.add)
                kb.act(s16[:], s16[:], AF.Sqrt, ["s16"], ["s16"])
                kb.op("dve", lambda e: e.reciprocal(out=s16[:], in_=s16[:]), ["s16"], ["s16"])
                kb.tt(m1[:].rearrange("p (h d) -> p h d", d=64), ot[b][:].rearrange("p (h d) -> p h d", d=64),
                      s16[:].unsqueeze(2).to_broadcast([128, 16, 64]), ALU.mult, ["ot%d" % b, "s16"], ["m1"])
                kb.act(sil[:], rct[b][:], AF.Silu, ["rct%d" % b], ["sil"])
                kb.tt(m1[:, 768:1024], m1[:, 768:1024], sil[:], ALU.mult, ["m1", "sil"], ["m1"])
                kb.tt(mix[:], m1[:], gm[:], ALU.mult, ["m1", "gm"], ["mix"])
                for k in range(8):
                    kb.tr(pT[:, k * 128:(k + 1) * 128], mix[:, k * 128:(k + 1) * 128], idb[:], ["mix", "ident_b"], ["pT"], inc=(k == 7))
                kb.cp(mixT[:], pT, ["pT"], ["mixT"], eng="act")
                for dh in range(2):
                    for k in range(8):
                        kb.mm(ps[1 + dh][:], mixT[:, k * 128:(k + 1) * 128], Wo[:, k, dh * 512:(dh + 1) * 512], k == 0, k == 7,
                              ["mixT", "Wo"], ["pd%d" % dh], inc=(k == 7))
                    kb.tt(h1[:, dh * 512:(dh + 1) * 512], ht[b][:, dh * 512:(dh + 1) * 512], ps[1 + dh][:], ALU.add,
                          ["ht%d" % b, "pd%d" % dh], ["h1"])
                kb.dma("pool", h1_d[rs, :], h1[:], reads=["h1"], writes=["h1_d"])
                _rstd(kb, h1[:], junk[:], ss[:], D, ["h1"], "b")
                kb.ts(xn[:], h1[:], ss[:, 0:1], ALU.mult, ["h1", "bss"], ["xn"])
                for k in range(8):
                    kb.tr(pT[:, k * 128:(k + 1) * 128], xn[:, k * 128:(k + 1) * 128], idb[:], ["xn", "ident_b"], ["pT"], inc=(k == 7))
                kb.cp(xnT[:], pT, ["pT"], ["xnT"], eng="act")
                kb.dma("pool", xT_d[rs, :], xnT[:], reads=["xnT"], writes=["xT_d"])
                for cg in range(4):
                    pq = ps[3 + cg % 2]
                    for cc in range(4):
                        cidx = cg * 4 + cc
                        for k in range(8):
                            kb.mm(pq[:, cc * 128:(cc + 1) * 128], Wq[:, k, cidx * 128:(cidx + 1) * 128], xnT[:, k * 128:(k + 1) * 128],
                                  k == 0, k == 7, ["Wq", "xnT"], ["pq%d" % (cg % 2)], inc=(k == 7 and cc == 3))
                    kb.cp(qT[:, cg * 4:(cg + 1) * 4, :], pq[:].rearrange("p (c t) -> p c t", t=128), ["pq%d" % (cg % 2)], ["qT"],
                          eng=("act" if cg % 2 else "dve"))
                for cidx in range(16):
                    pscb = ps[5 + cidx // 8]
                    kb.mm(pscb[:, (cidx % 8) * 64:(cidx % 8 + 1) * 64], qT[:, cidx, :], Ks[:, cidx, :], True, True, ["qT", "Ks"],
                          ["psc%d" % (cidx // 8)], inc=(cidx % 8 == 7))
                kb.cp(sct[:, 0:512], ps[5][:], ["psc0"], ["sct"], eng="act")
                kb.cp(sct[:, 512:1024], ps[6][:], ["psc1"], ["sct"], eng="dve")
                kb.dma("pool", sc_d[rs, :], sct[:], reads=["sct"], writes=["sc_d"])
                for hd in range(8):
                    for side, tt_ in ((0, t1), (1, t2)):
                        sv = sct[:, hd * 128 + side * 64:hd * 128 + side * 64 + 64]
                        kb.op("dve", lambda e, o_=tt_[:, hd, 0:8], i_=sv: e.max(out=o_, in_=i_), ["sct"], ["tt"])
                        kb.op("dve", lambda e, o_=wk[:, 0:64], r_=tt_[:, hd, 0:8], i_=sv: e.match_replace(out=o_, in_to_replace=r_, in_values=i_, imm_value=-1e30),
                              ["sct", "tt"], ["wk"])
                        kb.op("dve", lambda e, o_=tt_[:, hd, 8:16], i_=wk[:, 0:64]: e.max(out=o_, in_=i_), ["wk"], ["tt"])
                kb.tt(cand[:].rearrange("p h (a b) -> p h a b", b=16), t1[:].unsqueeze(3).to_broadcast([128, 8, 16, 16]),
                      t2[:].unsqueeze(2).to_broadcast([128, 8, 16, 16]), ALU.add, ["tt"], ["cand"])
                for hd in range(8):
                    kb.op("dve", lambda e, o_=c8a[:, hd, :], i_=cand[:, hd, :]: e.max(out=o_, in_=i_), ["cand"], ["c8"])
                    kb.op("dve", lambda e, o_=wk[:], r_=c8a[:, hd, :], i_=cand[:, hd, :]: e.match_replace(out=o_, in_to_replace=r_, in_values=i_, imm_value=-1e30),
                          ["cand", "c8"], ["wk"])
                    kb.op("dve", lambda e, o_=c8b[:, hd, :], i_=wk[:]: e.max(out=o_, in_=i_), ["wk"], ["c8"])
                kb.tt(csh[:], cand[:], c8a[:, :, 0:1].to_broadcast([128, 8, 256]), ALU.subtract, ["cand", "c8"], ["csh"])
                kb.act(ec[:], csh[:], AF.Exp, ["csh"], ["ec"])
                kb.tt(mk8[:], cand[:], c8b[:, :, 7:8].to_broadcast([128, 8, 256]), ALU.is_ge, ["cand", "c8"], ["mk8"])
                kb.tt(ec[:], ec[:], mk8[:], ALU.mult, ["ec", "mk8"], ["ec"])
                kb.op("dve", lambda e: e.tensor_reduce(out=Z[:], in_=ec[:], axis=AX.X, op=ALU.add), ["ec"], ["Z"])
                kb.act(Z[:], Z[:], AF.Ln, ["Z"], ["Z"])
                kb.cp(tb[:, 0:8], c8b[:, :, 7], ["c8"], ["tb"])
                kb.stt(tb[:, 8:16], c8a[:, :, 0], -1.0, Z[:], ALU.mult, ALU.subtract, ["c8", "Z"], ["tb"])
                kb.dma("pool", tb_d[rs, :], tb[:], reads=["tb"], writes=["tb_d"])
            kb.barrier()
            kb.emit()
        with ExitStack() as sb_:
            T = lambda name, shape, dt=F32: sb_.enter_context(nc.sbuf_tensor(name, shape, dt))
            Ub = T("Ub", [128, 8, 4096], BF16); Vv = T("Vv", [128, 32, D], BF16)
            load_w(Ub, uT, 8, 4096, "Ub", scale_col=True)
            load_w(Vv, v, 32, D, "Vv")
            gf = T("gf", [128, D])
            if final:
                kb.dma("sp", gf[:], gfin, writes=["gf"])
            h1t = [T("h1t%d" % i, [128, D]) for i in range(2)]
            sct = [T("sctb%d" % i, [128, D]) for i in range(2)]
            xT = [T("xTb%d" % i, [128, D], BF16) for i in range(2)]
            tbt = [T("tbt%d" % i, [128, 16]) for i in range(2)]
            Sg = [T("Sg%d" % i, [128, 16, 64]) for i in range(2)]
            Eg = [T("Eg%d" % i, [128, 1024], BF16) for i in range(2)]
            Gh = T("Gh", [128, 1024], BF16); G = T("G", [128, 1024])
            gl = [T("gl%d" % i, [128, 512]) for i in range(2)]
            Wb = T("Wb", [128, 1024], BF16); WT = T("WT", [128, 1024], BF16)
            ho = T("ho", [128, D]); junk = T("junkb", [128, D], BF16); ss = T("ssb", [128, 1])
            pH = [ps[0], ps[1]]; ptr = ps[2][:].bitcast(BF16); po = [ps[3], ps[4]]

            def loadB(i):
                b = i % 2
                rs = slice(i * 128, (i + 1) * 128)
                kb.dma("sp", h1t[b][:], h1_d[rs, :], reads=["h1_d"], writes=["h1t%d" % b])
                kb.dma("sp", sct[b][:], sc_d[rs, :], reads=["sc_d"], writes=["sctb%d" % b])
                kb.dma("sp", xT[b][:], xT_d[rs, :], reads=["xT_d"], writes=["xTb%d" % b])
                kb.dma("sp", tbt[b][:], tb_d[rs, :], reads=["tb_d"], writes=["tbt%d" % b])

            loadB(0)
            cnt = 0
            for i in range(NT):
                b = i % 2
                rs = slice(i * 128, (i + 1) * 128)
                if i + 1 < NT:
                    loadB(i + 1)
                for eq in range(4):
                    for hd in range(8):
                        j = cnt % 2
                        cnt += 1
                        s1 = sct[b][:, hd * 128 + 16 * eq:hd * 128 + 16 * eq + 16]
                        s2 = sct[b][:, hd * 128 + 64:hd * 128 + 128]
                        kb.tt(Sg[j][:], s1.unsqueeze(2).to_broadcast([128, 16, 64]), s2.unsqueeze(1).to_broadcast([128, 16, 64]), ALU.add,
                              ["sctb%d" % b], ["Sg%d" % j], eng="pool")
                        Sf = Sg[j][:].rearrange("p a b -> p (a b)")
                        kb.act(Eg[j][:], Sf, AF.Exp, ["Sg%d" % j, "tbt%d" % b], ["Eg%d" % j], bias=tbt[b][:, 8 + hd:9 + hd], scale=1.0)
                        if hd == 0:
                            kb.stt(G[:], Sf, tbt[b][:, hd:hd + 1], Eg[j][:], ALU.is_ge, ALU.mult, ["Sg%d" % j, "Eg%d" % j, "tbt%d" % b], ["G"])
                        else:
                            kb.stt(Gh[:], Sf, tbt[b][:, hd:hd + 1], Eg[j][:], ALU.is_ge, ALU.mult, ["Sg%d" % j, "Eg%d" % j, "tbt%d" % b], ["Gh"])
                            kb.tt(G[:], G[:], Gh[:], ALU.add, ["G", "Gh"], ["G"])
                    for g2 in range(2):
                        e0 = eq * 1024 + g2 * 512
                        for k in range(8):
                            kb.mm(pH[g2][:], xT[b][:, k * 128:(k + 1) * 128], Ub[:, k, e0:e0 + 512], k == 0, k == 7,
                                  ["xTb%d" % b, "Ub"], ["pH%d" % g2], inc=(k == 7))
                        kb.act(gl[g2][:], pH[g2][:], AF.Gelu, ["pH%d" % g2], ["gl%d" % g2])
                        kb.tt(Wb[:, g2 * 512:(g2 + 1) * 512], gl[g2][:], G[:, g2 * 512:(g2 + 1) * 512], ALU.mult, ["gl%d" % g2, "G"], ["Wb"])
                    for cc in range(8):
                        kb.tr(ptr[:, cc * 128:(cc + 1) * 128], Wb[:, cc * 128:(cc + 1) * 128], idb[:], ["Wb", "ident_b"], ["ptr"], inc=(cc == 7))
                    kb.cp(WT[:], ptr, ["ptr"], ["WT"], eng="act")
                    for dh in range(2):
                        for cc in range(8):
                            kb.mm(po[dh][:], WT[:, cc * 128:(cc + 1) * 128], Vv[:, eq * 8 + cc, dh * 512:(dh + 1) * 512],
                                  (eq == 0 and cc == 0), (eq == 3 and cc == 7), ["WT", "Vv"], ["po%d" % dh], inc=(cc == 7))
                for dh in range(2):
                    kb.tt(ho[:, dh * 512:(dh + 1) * 512], h1t[b][:, dh * 512:(dh + 1) * 512], po[dh][:], ALU.add,
                          ["h1t%d" % b, "po%d" % dh], ["ho"])
                if final:
                    _rstd(kb, ho[:], junk[:], ss[:], D, ["ho"], "f")
                    kb.stt(ho[:], ho[:], ss[:, 0:1], gf[:], ALU.mult, ALU.mult, ["ho", "fss", "gf"], ["ho"])
                kb.dma("sp", hout[rs, :], ho[:], reads=["ho"], writes=["hout"])
            kb.wait_all("sp", ["hout"])
            kb.emit()
    return nc


_CACHE = {}


def _prog(name, *args):
    key = (name,) + args
    if key not in _CACHE:
        _CACHE[key] = {"p1": build_p1, "p2": build_p2, "p3": build_p3}[name](*args)
    return _CACHE[key]


def _run(nc, in_maps):
    res = run_bass_kernel_spmd(nc, in_maps, core_ids=list(range(NCORES)))
    return res.results


def _c(a):
    return np.ascontiguousarray(a)


def _gk(g):
    return _c(np.asarray(g, np.float32).reshape(8, 128).T)


def forward(x, meta_tokens, attn_norm, w_in, attn_sinks, gla_gate_w2, gla_gate_b, swa_out_norm,
            sb_out_norm, gla_out_norm, w_out, ffn_norm, peer_w_q, peer_sub_keys, peer_u, peer_v, final_norm):
    x = np.asarray(x, np.float32)
    B, SEQ, _ = x.shape
    depth = attn_norm.shape[0]
    L = SEQ + 128
    Lp = ((L + 511) // 512) * 512
    T = B * L
    NT = (T // 128 + NCORES - 1) // NCORES
    Tp = NT * 128 * NCORES
    hfull = np.zeros((Tp, D), np.float32)
    for b in range(B):
        hfull[b * L + 112:b * L + 128] = meta_tokens
        hfull[b * L + 128:(b + 1) * L] = x[b]
    p1 = _prog("p1", NT)
    p2 = _prog("p2", Lp)
    tsl = [slice(c * NT * 128, (c + 1) * NT * 128) for c in range(NCORES)]
    bf = ml_dtypes.bfloat16
    for i in range(depth):
        w_i = _c(w_in[i]); g_i = _gk(attn_norm[i])
        r = _run(p1, [{"h": hfull[tsl[c]], "w": w_i, "g": g_i} for c in range(NCORES)])
        proj = np.concatenate([np.asarray(r[c]["proj"]).view(bf) if np.asarray(r[c]["proj"]).dtype != bf else np.asarray(r[c]["proj"])
                               for c in range(NCORES)], axis=0)
        maps = []
        for c in range(NCORES):
            b, j = c // 4, c % 4
            pb = np.zeros((Lp, IN_COLS), bf)
            pb[:L] = proj[b * L:(b + 1) * L]
            kv = j // 2
            ka = pb[:, 512 + 64 * kv:512 + 64 * kv + 64].T
            maps.append({
                "qaT": _c(pb[:, 128 * j:128 * j + 128].T), "kaT": _c(np.concatenate([ka, ka], 0)),
                "va": _c(pb[:, 640 + 64 * kv:640 + 64 * kv + 64]),
                "qbT": _c(pb[:, 768 + 64 * j:768 + 64 * j + 64].T), "kbT": _c(pb[:, 1024 + 64 * j:1024 + 64 * j + 64].T),
                "vb": _c(pb[:, 1280 + 64 * j:1280 + 64 * j + 64]),
                "qcT": _c(pb[:, 1536 + 32 * j:1536 + 32 * j + 32].T), "kcT": _c(pb[:, 1664 + 32 * j:1664 + 32 * j + 32].T),
                "vc": _c(pb[:, 1792 + 64 * j:1792 + 64 * j + 64]), "glrT": _c(pb[:, 2048:2064].T),
                "w2": _c(np.asarray(gla_gate_w2[i], np.float32)[:, 32 * j:32 * j + 32]),
                "gb": _c(np.asarray(gla_gate_b[i], np.float32)[32 * j:32 * j + 32].reshape(32, 1)),
                "sinks": _c(np.broadcast_to(np.asarray(attn_sinks[i], np.float32)[2 * j:2 * j + 2][None, :], (128, 2))),
            })
        r = _run(p2, maps)
        ofull = np.zeros((Tp, D), np.float32)
        for c in range(NCORES):
            b, j = c // 4, c % 4
            ofull[b * L:(b + 1) * L, 128 * j:128 * j + 128] = np.asarray(r[c]["oa"])[:L]
            ofull[b * L:(b + 1) * L, 512 + 64 * j:512 + 64 * j + 64] = np.asarray(r[c]["obT"]).T[:L]
            ofull[b * L:(b + 1) * L, 768 + 64 * j:768 + 64 * j + 64] = np.asarray(r[c]["oc"])[:L]
        rcfull = np.zeros((Tp, 256), bf)
        rcfull[:T] = proj[:T, 2064:2320]
        fin = (i == depth - 1)
        p3 = _prog("p3", NT, fin)
        gmix = np.concatenate([swa_out_norm[i], sb_out_norm[i], gla_out_norm[i]]).astype(np.float32)
        shared = {
            "gmix": _c(np.broadcast_to(gmix[None, :], (128, D))), "wout": _c(w_out[i]), "gffn": _gk(ffn_norm[i]),
            "wq": _c(peer_w_q[i]), "ksub": _c(np.transpose(np.asarray(peer_sub_keys[i], np.float32).reshape(16, 64, 128), (2, 0, 1))),
            "uT": _c(np.asarray(peer_u[i], np.float32).T), "v": _c(peer_v[i]),
            "gfin": _c(np.broadcast_to(np.asarray(final_norm, np.float32)[None, :], (128, D))),
        }
        r = _run(p3, [dict(shared, h=hfull[tsl[c]], o=ofull[tsl[c]], rc=rcfull[tsl[c]]) for c in range(NCORES)])
        hfull = np.concatenate([np.asarray(r[c]["hout"]) for c in range(NCORES)], axis=0)
    out = np.stack([hfull[b * L + 128:(b + 1) * L] for b in range(B)], 0)
    return np.ascontiguousarray(out.astype(np.float32))


def kernel(**inputs):
    return forward_fused(**{k: np.asarray(v) for k, v in inputs.items()})


import os as _os
_STOP = _os.environ.get("FUSED_STOP", "")
_POOL_HEADS = tuple(int(c_) for c_ in _os.environ.get("POOL_HEADS", "01234567"))
CJ = 720
RG = [[0, 1, 2, 3], [4, 5, 6, 7]]


def build_fused(Lp, depth):
    TQ = Lp // 4
    NT = TQ // 128
    NCH = Lp // 512
    NB = Lp // 128
    nc = bass.Bass("TRN2", target_bir_lowering=False)
    IN = lambda n, s, dt=F32: nc.dram_tensor(n, s, dt, kind="ExternalInput").ap()
    h0 = IN("h0", [TQ, D])
    w_in = IN("w_in", [depth, D, CJ]); g_attn = IN("g_attn", [depth, 128, 8])
    w2_i = IN("w2", [depth, 16, 32]); gb_i = IN("gb", [depth, 32, 1]); sinks_i = IN("sinks", [depth, 128, 2])
    gmix_i = IN("gmix", [depth, 128, 256]); wout_i = IN("wout", [depth, 128, 2, D])
    gffn_i = IN("gffn", [depth, 128, 8]); wq_i = IN("wq", [depth, D, 2048]); ksub_i = IN("ksub", [depth, 128, 16, 64])
    uT_i = IN("uT", [depth, D, 4096]); v_i = IN("v", [depth, 4096, D]); gfin = IN("gfin", [128, D])
    out = nc.dram_tensor("out", [TQ, D], F32, kind="ExternalOutput").ap()
    DT = lambda n, s, dt=F32: nc.dram_tensor(n, s, dt, kind="Internal").ap()
    GC = 128 * max(d for d in range(1, 5) if NT % d == 0)
    NG = TQ // GC
    xT_loc = [DT("xT_loc%d" % g, [D, GC], BF16) for g in range(NG)]
    xT_all = [DT("xT_all%d" % g, [4 * D, GC], BF16) for g in range(NG)]
    part_loc = DT("part_loc", [Lp, D]); delta_loc = DT("delta_loc", [TQ, D])
    hl = [DT("hl0", [TQ, D]), DT("hl1", [TQ, D])]
    h1_d = DT("h1_d", [TQ, D]); sc_d = DT("sc_d", [TQ, D]); tb_d = DT("tb_d", [TQ, 16]); xT_d = DT("xT_d", [TQ, D], BF16)

    with ExitStack() as st:
        kb = KB(nc, st)
        kb.excl.update(["pT", "pX", "pA", "pB", "pO", "pz0", "pz1", "pc0", "pc1", "pq0", "pq1", "psc0", "psc1",
                        "pH0", "pH1", "ptr", "po0", "po1"])
        ps = [st.enter_context(nc.psum_tensor("ps%d" % i, [128, 512], F32)) for i in range(8)]
        T0 = lambda name, shape, dt=F32: st.enter_context(nc.sbuf_tensor(name, shape, dt))
        idf, idb = _ident(kb, T0)
        stage = [T0("stage%d" % i, [128, 1024]) for i in range(2)]
        scnt = [0]

        def load_w(dst, src, rows_k, cols, key, gt=None):
            for k in range(rows_k):
                for c0 in range(0, cols, 1024):
                    cw = min(1024, cols - c0)
                    si = scnt[0] % 2
                    scnt[0] += 1
                    sk = "stage%d" % si
                    kb.dma("sp", stage[si][:, 0:cw], src[k * 128:(k + 1) * 128, c0:c0 + cw], writes=[sk])
                    if gt is not None:
                        kb.ts(dst[:, k, c0:c0 + cw], stage[si][:, 0:cw], gt[:, k:k + 1], ALU.mult, [sk, "gt"], [key])
                    else:
                        kb.cp(dst[:, k, c0:c0 + cw], stage[si][:, 0:cw], [sk], [key], eng="pool")

        def phase_end():
            kb.barrier()
            kb.emit()

        for li in range(depth):
            hsrc = h0 if li == 0 else hl[(li - 1) % 2]
            hdst = out if li == depth - 1 else hl[li % 2]
            final = (li == depth - 1)
            with ExitStack() as sa:
                T = lambda name, shape, dt=F32, _p="L%dA_" % li: sa.enter_context(nc.sbuf_tensor(_p + name, shape, dt))
                ht = [T("ht%d" % i, [128, D]) for i in range(2)]
                junk = T("junk", [128, D], BF16); ss = T("ss", [128, 1])
                xn = T("xn", [128, D], BF16); xnT = [T("xnT%d" % i, [128, D], BF16) for i in range(2)]
                pT = ps[0][:].bitcast(BF16)
                xv = [x_.rearrange("(k p) t -> p k t", p=128) for x_ in xT_loc]
                for i in range(NT):
                    b = i % 2
                    kb.dma("sp", ht[b][:], hsrc[i * 128:(i + 1) * 128, :], writes=["ht%d" % b])
                    _rstd(kb, ht[b][:], junk[:], ss[:], D, ["ht%d" % b], "a")
                    kb.ts(xn[:], ht[b][:], ss[:, 0:1], ALU.mult, ["ht%d" % b, "ass"], ["xn"])
                    for k in range(8):
                        kb.tr(pT[:, k * 128:(k + 1) * 128], xn[:, k * 128:(k + 1) * 128], idb[:], ["xn", "ident_b"], ["pT"], inc=(k == 7))
                    kb.cp(xnT[b][:], pT, ["pT"], ["xnT%d" % b], eng="act")
                    g_, o_ = (i * 128) // GC, (i * 128) % GC
                    kb.dma("pool", xv[g_][:, :, o_:o_ + 128], xnT[b][:].rearrange("p (k t) -> p k t", t=128),
                           reads=["xnT%d" % b], writes=["xT_loc"])
                kb.barrier()
                for g_ in range(NG):
                    kb.coll("AllGather", ALU.bypass, RG, xT_loc[g_], xT_all[g_], ["xT_loc"], ["xT_all"])
                phase_end()
            if _STOP == "A":
                break
            with ExitStack() as sb_:
                T = lambda name, shape, dt=F32, _p="L%dB_" % li: sb_.enter_context(nc.sbuf_tensor(_p + name, shape, dt))
                gt = T("gt", [128, 8])
                kb.dma("sp", gt[:], g_attn[li], writes=["gt"])
                Wj = T("Wj", [128, 8, 768], BF16)
                load_w(Wj, w_in[li], 8, CJ, "Wj", gt=gt)
                Wo = T("Wo", [128, 2, D], BF16)
                for kc in range(2):
                    si = scnt[0] % 2; scnt[0] += 1
                    kb.dma("sp", stage[si][:], wout_i[li][:, kc, :], writes=["stage%d" % si])
                    kb.cp(Wo[:, kc, :], stage[si][:], ["stage%d" % si], ["Wo"], eng="pool")
                gm = T("gm", [128, 256]); kb.dma("sp", gm[:], gmix_i[li], writes=["gm"])
                m_gen = T("m_gen", [128, 256]); m_n0 = T("m_n0", [128, 256]); m_n1 = T("m_n1", [128, 256])
                for m, nm, extra in ((m_gen, "m_gen", None), (m_n0, "m_n0", -240), (m_n1, "m_n1", -112)):
                    kb.memset(m[:], 0.0, [nm])
                    kb.asel(m[:], ALU.is_ge, NEG, -1, -1, [[1, 256]], nm)
                    kb.asel(m[:], ALU.is_ge, NEG, 128, 1, [[-1, 256]], nm)
                    if extra is not None:
                        kb.asel(m[:], ALU.is_ge, NEG, extra, 0, [[1, 256]], nm)
                sbm_f = T("sbm_f", [128, 512])
                sbm = [T("sbm%d" % d, [128, 512], BF16) for d in range(4)]
                for d in range(4):
                    kb.memset(sbm_f[:], 1.0, ["sbm_f"])
                    kb.asel(sbm_f[:], ALU.is_ge, 0.0, -1 - 128 * d, -1, [[1, 512]], "sbm_f")
                    kb.cp(sbm[d][:], sbm_f[:], ["sbm_f"], ["sbm%d" % d], eng="pool")
                ntri_f = T("ntri_f", [128, 128]); ntri = T("ntri", [128, 128], BF16); nones = T("nones", [128, 128], BF16)
                kb.memset(ntri_f[:], -1.0, ["ntri_f"])
                kb.asel(ntri_f[:], ALU.is_ge, 0.0, 0, 1, [[-1, 128]], "ntri_f")
                kb.cp(ntri[:], ntri_f[:], ["ntri_f"], ["ntri"], eng="pool")
                kb.memset(nones[:], -1.0, ["nones"])
                mle = T("mle", [128, 128])
                kb.memset(mle[:], 1.0, ["mle"])
                kb.asel(mle[:], ALU.is_ge, 0.0, 0, -1, [[1, 128]], "mle")
                rmask = T("rmask", [32, 512])
                kb.memset(rmask[:], 1.0, ["rmask"])
                for b4 in range(4):
                    kb.memset(rmask[:, b4 * 128:b4 * 128 + 1], 0.0, ["rmask"])
                w2f = T("w2f", [16, 32]); w2b = T("w2b", [16, 32], BF16); gbt = T("gbt", [32, 1]); sk_t = T("sk_t", [128, 2])
                kb.dma("sp", w2f[:], w2_i[li], writes=["w2f"]); kb.dma("sp", gbt[:], gb_i[li], writes=["gbt"])
                kb.dma("sp", sk_t[:], sinks_i[li], writes=["sk"])
                kb.cp(w2b[:], w2f[:], ["w2f"], ["w2b"])
                kb.ts(gbt[:], gbt[:], -1.0, ALU.mult, ["gbt"], ["gbt"])
                KbT = T("KbT", [64, Lp], BF16); Vb = T("Vb", [128, NB, 64], BF16)
                S = T("S", [32, 64]); Sb = T("Sb", [32, 64], BF16)
                kb.memset(S[:], 0.0, ["S"]); kb.memset(Sb[:], 0.0, ["Sb"])
                xc = [T("xc%d" % i, [128, 8, 512], BF16) for i in range(2)]
                qa_c = [T("qa_c%d" % i, [128, 512], BF16) for i in range(2)]
                ka_c = [T("ka_c%d" % i, [128, 640], BF16) for i in range(2)]
                va_c = [T("va_c%d" % i, [128, 5, 64], BF16) for i in range(2)]
                qs_c = [T("qs_c%d" % i, [64, 512], BF16) for i in range(2)]
                qc_c = [T("qc_c%d" % i, [32, 512], BF16) for i in range(2)]
                kc_c = [T("kc_c%d" % i, [32, 512], BF16) for i in range(2)]
                vc_c = [T("vc_c%d" % i, [128, 4, 64], BF16) for i in range(2)]
                rc_c = [T("rc_c%d" % i, [128, 4, 64]) for i in range(2)]
                gl_c = [T("gl_c%d" % i, [16, 512], BF16) for i in range(2)]
                mo = [T("mo%d" % i, [128, 4, 256]) for i in range(2)]
                sm = T("sm", [128, 256]); pexp = T("pexp", [128, 256], BF16); pTs = T("pTs", [128, 256], BF16)
                st8 = T("st8", [128, 8])
                ge = T("ge", [32, 512]); gsp = T("gsp", [32, 512]); gcs = T("gcs", [32, 512])
                geq = T("geq", [32, 512]); gek = T("gek", [32, 512])
                qt = T("qt", [32, 512], BF16); kt = T("kt", [32, 512], BF16)
                scb = T("scb", [128, 128], BF16); ktm = T("ktm", [128, 32], BF16)
                stmp = T("stmp", [32, 64])
                e_t = [T("e_t%d" % i, [128, 512]) for i in range(2)]
                sp_t = [[T("sp_t%d_%d" % (pp_, i), [128, 512], BF16) for i in range(2)] for pp_ in range(2)]
                a_t = [T("a_t%d" % i, [128, 512], BF16) for i in range(2)]
                R = T("R", [128, 512], BF16)
                ob_t = T("ob_t", [64, 512])
                sq4 = T("sq4", [128, 1024]); s16 = T("s16", [128, 16]); m14 = T("m14", [128, 1024]); sil4 = T("sil4", [128, 4, 64])
                mixb4 = T("mixb4", [128, 1024], BF16); mixT4 = T("mixT4", [128, 1024], BF16)
                pt = [T("pt%d" % i, [128, D]) for i in range(2)]
                pz = [ps[0], ps[1]]; pc = [ps[2], ps[3]]; pO = ps[4]
                pA = ps[5]; pX = ps[6]; pB = ps[7]
                pT_bf = pB[:].bitcast(BF16)

                def pieces(c0, n):
                    t = c0
                    while t < c0 + n:
                        q = t // TQ; tl = t % TQ; g_ = tl // GC; o_ = tl % GC; m = min(c0 + n - t, GC - o_)
                        yield q, g_, o_, t - c0, m
                        t += m

                def load_xc(c):
                    i = c % 2
                    for q, g_, o_, off, m in pieces(c * 512, 512):
                        kb.dma("sp", xc[i][:, :, off:off + m],
                               xT_all[g_][q * D:(q + 1) * D, o_:o_ + m].rearrange("(k p) t -> p k t", p=128), writes=["xc%d" % i])

                def inproj(c):
                    i = c % 2
                    xk = "xc%d" % i
                    if c == 0:
                        kb.memset(ka_c[i][:, 0:128], 0.0, ["ka_c%d" % i])
                        kb.memset(va_c[i][:, 0, :], 0.0, ["va_c%d" % i])
                    else:
                        kb.cp(ka_c[i][:, 0:128], ka_c[1 - i][:, 512:640], ["ka_c%d" % (1 - i)], ["ka_c%d" % i], eng="pool")
                        kb.cp(va_c[i][:, 0, :], va_c[1 - i][:, 4, :], ["va_c%d" % (1 - i)], ["va_c%d" % i], eng="pool")
                    groups = [(0, 128, qa_c[i][:], "qa_c%d" % i, None), (128, 128, ka_c[i][:, 128:640], "ka_c%d" % i, None),
                              (256, 64, qs_c[i][:], "qs_c%d" % i, 0.125), (320, 64, KbT[:, c * 512:(c + 1) * 512], "KbT", None),
                              (384, 32, qc_c[i][:], "qc_c%d" % i, None), (416, 32, kc_c[i][:], "kc_c%d" % i, None),
                              (448, 16, gl_c[i][:], "gl_c%d" % i, None)]
                    for gi, (c0, rows, dst, dk, scale) in enumerate(groups):
                        mr = max(rows, 32)
                        for k in range(8):
                            kb.mm(pX[0:mr, :], Wj[:, k, c0:c0 + mr], xc[i][:, k, :], k == 0, k == 7, ["Wj", xk], ["pX"], inc=(k == 7))
                        if scale is not None:
                            kb.ts(dst, pX[0:rows, :], scale, ALU.mult, ["pX"], [dk])
                        elif gi % 2:
                            kb.cp(dst, pX[0:rows, :], ["pX"], [dk], eng="act")
                        else:
                            kb.cp(dst, pX[0:rows, :], ["pX"], [dk])
                    for blk in (range(4) if _STOP != "B1f" else []):
                        n = 4 * c + blk
                        for k in range(8):
                            kb.mm(pX[:, 0:256], xc[i][:, k, blk * 128:(blk + 1) * 128], Wj[:, k, 464:720], k == 0, k == 7, ["Wj", xk], ["pX"], inc=(k == 7))
                        kb.cp(va_c[i][:, blk + 1, :], pX[:, 0:64], ["pX"], ["va_c%d" % i], eng="act")
                        kb.cp(Vb[:, n, :], pX[:, 64:128], ["pX"], ["Vb"])
                        kb.cp(vc_c[i][:, blk, :], pX[:, 128:192], ["pX"], ["vc_c%d" % i], eng="act")
                        kb.cp(rc_c[i][:, blk, :], pX[:, 192:256], ["pX"], ["rc_c%d" % i])

                def swa(c):
                    i = c % 2
                    mk_ = "mo%d" % i
                    for blk in range(4):
                        n = 4 * c + blk
                        msk = m_n0 if n == 0 else (m_n1 if n == 1 else m_gen)
                        mk = "m_n0" if n == 0 else ("m_n1" if n == 1 else "m_gen")
                        for hh in range(2):
                            hs = slice(hh * 64, (hh + 1) * 64)
                            kb.mm(pA[:, 0:256], qa_c[i][hs, blk * 128:(blk + 1) * 128], ka_c[i][hs, blk * 128:blk * 128 + 256],
                                  True, True, ["qa_c%d" % i, "ka_c%d" % i], ["pA"])
                            kb.stt(sm[:], pA[:, 0:256], 0.125, msk[:], ALU.mult, ALU.add, ["pA", mk], ["sm"])
                            kb.op("dve", lambda e: e.tensor_reduce(out=st8[:, 0:1], in_=sm[:], axis=AX.X, op=ALU.max), ["sm"], ["st8"])
                            kb.tt(st8[:, 0:1], st8[:, 0:1], sk_t[:, hh:hh + 1], ALU.max, ["st8", "sk"], ["st8"])
                            kb.ts(st8[:, 1:2], st8[:, 0:1], -1.0, ALU.mult, ["st8"], ["st8"])
                            kb.act(pexp[:], sm[:], AF.Exp, ["sm", "st8"], ["pexp", "st8"], bias=st8[:, 1:2], scale=1.0, accum_out=st8[:, 2:3])
                            kb.act(st8[:, 3:4], sk_t[:, hh:hh + 1], AF.Exp, ["sk", "st8"], ["st8"], bias=st8[:, 1:2], scale=1.0)
                            kb.tt(st8[:, 4:5], st8[:, 2:3], st8[:, 3:4], ALU.add, ["st8"], ["st8"])
                            kb.op("dve", lambda e: e.reciprocal(out=st8[:, 5:6], in_=st8[:, 4:5]), ["st8"], ["st8"])
                            kb.tr(pT_bf[:, 0:128], pexp[:, 0:128], idb[:], ["pexp", "ident_b"], ["pB"], inc=False)
                            kb.tr(pT_bf[:, 128:256], pexp[:, 128:256], idb[:], ["pexp", "ident_b"], ["pB"])
                            kb.cp(pTs[:], pT_bf[:, 0:256], ["pB"], ["pTs"], eng="act")
                            kb.mm(pA[:, 256:320], pTs[:, 0:128], va_c[i][:, blk, :], True, False, ["pTs", "va_c%d" % i], ["pA"], inc=False)
                            kb.mm(pA[:, 256:320], pTs[:, 128:256], va_c[i][:, blk + 1, :], False, True, ["pTs", "va_c%d" % i], ["pA"])
                            kb.ts(mo[i][:, blk, hh * 64:(hh + 1) * 64], pA[:, 256:320], st8[:, 5:6], ALU.mult, ["pA", "st8"], [mk_])

                def gla(c):
                    i = c % 2
                    kb.mm(pX[0:32, :], w2b[:], gl_c[i][:], True, True, ["w2b", "gl_c%d" % i], ["pX"])
                    kb.act(ge[:], pX[0:32, :], AF.Exp, ["pX", "gbt"], ["ge"], bias=gbt[:, 0:1], scale=-1.0)
                    kb.act(gsp[:], ge[:], AF.Ln, ["ge"], ["gsp"], bias=1.0, scale=1.0)
                    kb.op("dve", lambda e: e.tensor_tensor_scan(out=gcs[:], data0=rmask[:], data1=gsp[:], initial=0.0, op0=ALU.mult, op1=ALU.add),
                          ["rmask", "gsp"], ["gcs"])
                    kb.act(geq[:], gcs[:], AF.Exp, ["gcs"], ["geq"], scale=-1.0 / 16.0)
                    kb.act(gek[:], gcs[:], AF.Exp, ["gcs"], ["gek"], scale=1.0 / 16.0)
                    kb.stt(qt[:], qc_c[i][:], 32.0 ** -0.5, geq[:], ALU.mult, ALU.mult, ["qc_c%d" % i, "geq"], ["qt"])
                    kb.tt(kt[:], kc_c[i][:], gek[:], ALU.mult, ["kc_c%d" % i, "gek"], ["kt"])
                    for blk in range(4):
                        bs = slice(blk * 128, (blk + 1) * 128)
                        kb.mm(pA[:, 384:512], kt[:, bs], qt[:, bs], True, True, ["kt", "qt"], ["pA"])
                        kb.tt(scb[:], pA[:, 384:512], mle[:], ALU.mult, ["pA", "mle"], ["scb"])
                        kb.tr(pT_bf[:, 512:544], kt[:, bs], idb[0:32, 0:32], ["kt", "ident_b"], ["pB"])
                        kb.cp(ktm[:], pT_bf[:, 512:544], ["pB"], ["ktm"], eng="act")
                        kb.mm(pA[:, 320:384], scb[:], vc_c[i][:, blk, :], True, False, ["scb", "vc_c%d" % i], ["pA"], inc=False)
                        kb.mm(pA[:, 320:384], qt[:, bs], Sb[:], False, True, ["qt", "Sb"], ["pA"])
                        kb.cp(mo[i][:, blk, 192:256], pA[:, 320:384], ["pA"], ["mo%d" % i], eng="act")
                        kb.mm(pB[0:32, 384:448], ktm[:], vc_c[i][:, blk, :], True, True, ["ktm", "vc_c%d" % i], ["pB"])
                        kb.tt(stmp[:], pB[0:32, 384:448], S[:], ALU.add, ["pB", "S"], ["stmp"])
                        kb.ts(S[:], stmp[:], geq[:, blk * 128 + 127:blk * 128 + 128], ALU.mult, ["stmp", "geq"], ["S"])
                        kb.cp(Sb[:], S[:], ["S"], ["Sb"])

                def sbk(c):
                    i = c % 2
                    nkb = 4 * c + 4
                    npairs = nkb // 2
                    qk = "qs_c%d" % i

                    def kof(p, j):
                        return nkb - 1 - (2 * p + j)

                    def zmm(p):
                        for j in range(2):
                            kblk = kof(p, j)
                            kb.mm(pz[j][:], KbT[:, kblk * 128:(kblk + 1) * 128], qs_c[i][:], True, True, ["KbT", qk], ["pz%d" % j])

                    zmm(0)
                    for p in range(npairs + 1):
                        pp = p % 2
                        if p < npairs:
                            for j in range(2):
                                kb.act(e_t[j][:], pz[j][:], AF.Exp, ["pz%d" % j], ["e_t%d" % j])
                            for j in range(2):
                                kb.act(sp_t[pp][j][:], e_t[j][:], AF.Ln, ["e_t%d" % j], ["sp_t%d_%d" % (pp, j)], bias=1.0, scale=1.0)
                            for j in range(2):
                                dg = kof(p, j) - 4 * c
                                if dg >= 0:
                                    kb.tt(sp_t[pp][j][:], sp_t[pp][j][:], sbm[dg][:], ALU.mult, ["sp_t%d_%d" % (pp, j), "sbm%d" % dg], ["sp_t%d_%d" % (pp, j)])
                        if p >= 1:
                            q = p - 1
                            qq = q % 2
                            first, lastp = (q == 0), (q == npairs - 1)
                            for j in range(2):
                                kblk = kof(q, j)
                                sk_ = "sp_t%d_%d" % (qq, j)
                                kb.mm(pc[j][:], ntri[:], sp_t[qq][j][:], True, False, ["ntri", sk_], ["pc%d" % j], inc=False)
                                if j == 1:
                                    kb.mm(pc[j][:], nones[:], sp_t[qq][0][:], False, False, ["nones", "sp_t%d_0" % qq], ["pc%d" % j], inc=False)
                                if not first:
                                    kb.mm(pc[j][:], nones[:], R[:], False, False, ["nones", "R"], ["pc%d" % j], inc=False)
                                kb.mm(pc[j][:], KbT[:, kblk * 128:(kblk + 1) * 128], qs_c[i][:], False, True, ["KbT", qk], ["pc%d" % j])
                        if p + 1 < npairs:
                            zmm(p + 1)
                        if p >= 1:
                            if not lastp:
                                if first:
                                    kb.tt(R[:], sp_t[qq][0][:], sp_t[qq][1][:], ALU.add, ["sp_t%d_0" % qq, "sp_t%d_1" % qq], ["R"])
                                else:
                                    kb.tt(R[:], R[:], sp_t[qq][0][:], ALU.add, ["R", "sp_t%d_0" % qq], ["R"])
                                    kb.tt(R[:], R[:], sp_t[qq][1][:], ALU.add, ["R", "sp_t%d_1" % qq], ["R"])
                            for j in range(2):
                                kb.act(a_t[j][:], pc[j][:], AF.Exp, ["pc%d" % j], ["a_t%d" % j])
                            for j in range(2):
                                dg = kof(q, j) - 4 * c
                                if dg >= 0:
                                    kb.tt(a_t[j][:], a_t[j][:], sbm[dg][:], ALU.mult, ["a_t%d" % j, "sbm%d" % dg], ["a_t%d" % j])
                            for j in range(2):
                                kblk = kof(q, j)
                                kb.mm(pO[0:64, :], Vb[:, kblk, :], a_t[j][:], (q == 0 and j == 0), (kblk == 0), ["Vb", "a_t%d" % j], ["pO"])
                    kb.cp(ob_t[:], pO[0:64, :], ["pO"], ["ob_t"])

                def post(c):
                    i = c % 2
                    mk_ = "mo%d" % i
                    for blk in range(4):
                        kb.tr(pA[:, blk * 64:(blk + 1) * 64], ob_t[:, blk * 128:(blk + 1) * 128], idf[0:64, 0:64], ["ob_t", "ident_f"], ["pA"], inc=(blk == 3))
                    kb.cp(mo[i][:, :, 128:192], pA[:, 0:256].rearrange("p (b d) -> p b d", d=64), ["pA"], [mk_], eng="act")
                    mof = mo[i][:].rearrange("p b c -> p (b c)")
                    kb.act(sq4[:], mof, AF.Square, [mk_], ["sq4"])
                    kb.op("dve", lambda e: e.tensor_reduce(out=s16[:], in_=sq4[:].rearrange("p (h d) -> p h d", d=64), axis=AX.X, op=ALU.add),
                          ["sq4"], ["s16"])
                    kb.ts(s16[:], s16[:], 1.0 / 64, ALU.mult, ["s16"], ["s16"], s2=EPS, op1=ALU.add)
                    kb.act(s16[:], s16[:], AF.Sqrt, ["s16"], ["s16"])
                    kb.op("dve", lambda e: e.reciprocal(out=s16[:], in_=s16[:]), ["s16"], ["s16"])
                    kb.tt(m14[:].rearrange("p (h d) -> p h d", d=64), mof.rearrange("p (h d) -> p h d", d=64),
                          s16[:].unsqueeze(2).to_broadcast([128, 16, 64]), ALU.mult, [mk_, "s16"], ["m14"])
                    kb.act(sil4[:], rc_c[i][:], AF.Silu, ["rc_c%d" % i], ["sil4"])
                    m14v = m14[:].rearrange("p (b c) -> p b c", c=256)
                    kb.tt(m14v[:, :, 192:256], m14v[:, :, 192:256], sil4[:], ALU.mult, ["m14", "sil4"], ["m14"])
                    kb.tt(mixb4[:].rearrange("p (b c) -> p b c", c=256), m14v, gm[:].unsqueeze(1).to_broadcast([128, 4, 256]), ALU.mult,
                          ["m14", "gm"], ["mixb4"])
                    for t8 in range(8):
                        kb.tr(pT_bf[:, t8 * 128:(t8 + 1) * 128], mixb4[:, t8 * 128:(t8 + 1) * 128], idb[:], ["mixb4", "ident_b"], ["pB"], inc=(t8 == 7))
                    kb.cp(mixT4[:], pT_bf, ["pB"], ["mixT4"], eng="act")
                    for blk in range(4):
                        n = 4 * c + blk
                        pk = "pt%d" % (n % 2)
                        for dh in range(2):
                            pbank, pkey = (pA, "pA") if dh == 0 else (pX, "pX")
                            for kc in range(2):
                                kb.mm(pbank[:], mixT4[:, (2 * blk + kc) * 128:(2 * blk + kc + 1) * 128], Wo[:, kc, dh * 512:(dh + 1) * 512], kc == 0, kc == 1,
                                      ["mixT4", "Wo"], [pkey], inc=(kc == 1))
                            kb.cp(pt[n % 2][:, dh * 512:(dh + 1) * 512], pbank[:], [pkey], [pk], eng=("act" if dh else "dve"))
                        kb.dma("pool", part_loc[n * 128:(n + 1) * 128, :], pt[n % 2][:], reads=[pk], writes=["part_loc"])

                load_xc(0)
                for c in range(NCH):
                    if _STOP == "B0":
                        continue
                    if c + 1 < NCH:
                        load_xc(c + 1)
                    if _STOP == "B0x":
                        continue
                    inproj(c)
                    if _STOP in ("B1", "B1f"):
                        continue
                    swa(c)
                    gla(c)
                    sbk(c)
                    if _STOP == "B2":
                        continue
                    post(c)
                kb.barrier()
                if _STOP not in ("B0", "B0x", "B1", "B1f", "B2", "B3"):
                    kb.coll("ReduceScatter", ALU.add, RG, part_loc, delta_loc, ["part_loc"], ["delta_loc"])
                phase_end()
            if _STOP in ("B", "B0", "B0x", "B1", "B1f", "B2", "B3"):
                break
            with ExitStack() as sc_:
                T = lambda name, shape, dt=F32, _p="L%dC_" % li: sc_.enter_context(nc.sbuf_tensor(_p + name, shape, dt))
                gft = T("gft", [128, 8])
                kb.dma("sp", gft[:], gffn_i[li], writes=["gt"])
                Wq = T("Wq", [128, 8, 2048], BF16); Ks = T("Ks", [128, 16, 64], BF16); ksf = T("ksf", [128, 16, 64])
                kb.dma("sp", ksf[:], ksub_i[li], writes=["ksf"])
                kb.cp(Ks[:], ksf[:], ["ksf"], ["Ks"])
                load_w(Wq, wq_i[li], 8, 2048, "Wq", gt=gft)
                ht = [T("ht%d" % i, [128, D]) for i in range(2)]
                dt_ = [T("dt%d" % i, [128, D]) for i in range(2)]
                h1 = T("h1", [128, D]); junk = T("junk", [128, D], BF16); ss = T("ss", [128, 1])
                xn = T("xn", [128, D], BF16); xnT = T("xnT", [128, D], BF16)
                qT = T("qT", [128, 16, 128], BF16); sct = T("sct", [128, D])
                t1 = T("t1", [128, 8, 16]); t2 = T("t2", [128, 8, 16]); wk1 = T("wk1", [128, 16, 64]); wk2 = T("wk2", [128, 8, 256])
                cand = T("cand", [128, 8, 256]); c8a = T("c8a", [128, 8, 8]); c8b = T("c8b", [128, 8, 8])
                csh = T("csh", [128, 8, 256]); ec = T("ec", [128, 8, 256]); mk8 = T("mk8", [128, 8, 256])
                Z = T("Z", [128, 8]); tb = T("tb", [128, 16])
                pT = ps[0][:].bitcast(BF16)
                for i in range(NT):
                    b = i % 2
                    rs = slice(i * 128, (i + 1) * 128)
                    kb.dma("sp", ht[b][:], hsrc[rs, :], writes=["ht%d" % b])
                    kb.dma("sp", dt_[b][:], delta_loc[rs, :], writes=["dt%d" % b])
                    kb.tt(h1[:], ht[b][:], dt_[b][:], ALU.add, ["ht%d" % b, "dt%d" % b], ["h1"])
                    kb.dma("pool", h1_d[rs, :], h1[:], reads=["h1"], writes=["h1_d"])
                    _rstd(kb, h1[:], junk[:], ss[:], D, ["h1"], "b")
                    kb.ts(xn[:], h1[:], ss[:, 0:1], ALU.mult, ["h1", "bss"], ["xn"])
                    for k in range(8):
                        kb.tr(pT[:, k * 128:(k + 1) * 128], xn[:, k * 128:(k + 1) * 128], idb[:], ["xn", "ident_b"], ["pT"], inc=(k == 7))
                    kb.cp(xnT[:], pT, ["pT"], ["xnT"], eng="act")
                    kb.dma("pool", xT_d[rs, :], xnT[:], reads=["xnT"], writes=["xT_d"])
                    for cg in range(4):
                        pq = ps[3 + cg % 2]
                        for cc in range(4):
                            cidx = cg * 4 + cc
                            for k in range(8):
                                kb.mm(pq[:, cc * 128:(cc + 1) * 128], Wq[:, k, cidx * 128:(cidx + 1) * 128], xnT[:, k * 128:(k + 1) * 128],
                                      k == 0, k == 7, ["Wq", "xnT"], ["pq%d" % (cg % 2)], inc=(k == 7 and cc == 3))
                        kb.cp(qT[:, cg * 4:(cg + 1) * 4, :], pq[:].rearrange("p (c t) -> p c t", t=128), ["pq%d" % (cg % 2)], ["qT"],
                              eng=("act" if cg % 2 else "dve"))
                    for cidx in range(16):
                        pscb = ps[5 + cidx // 8]
                        kb.mm(pscb[:, (cidx % 8) * 64:(cidx % 8 + 1) * 64], qT[:, cidx, :], Ks[:, cidx, :], True, True, ["qT", "Ks"],
                              ["psc%d" % (cidx // 8)], inc=(cidx % 8 == 7))
                    kb.cp(sct[:, 0:512], ps[5][:], ["psc0"], ["sct"], eng="act")
                    kb.cp(sct[:, 512:1024], ps[6][:], ["psc1"], ["sct"], eng="dve")
                    kb.dma("pool", sc_d[rs, :], sct[:], reads=["sct"], writes=["sc_d"])
                    chains = [(hd, side, (t1, t2)[side]) for hd in range(8) for side in range(2)]
                    tkeys = ["tt%d_%d" % (side, hd) for hd, side, _ in chains]
                    for ci_, (hd, side, tt_) in enumerate(chains):
                        sv = sct[:, hd * 128 + side * 64:hd * 128 + side * 64 + 64]
                        kb.op("dve", lambda e, o_=tt_[:, hd, 0:8], i_=sv: e.max(out=o_, in_=i_), ["sct"], [tkeys[ci_]])
                    for ci_, (hd, side, tt_) in enumerate(chains):
                        sv = sct[:, hd * 128 + side * 64:hd * 128 + side * 64 + 64]
                        kb.op("dve", lambda e, o_=wk1[:, ci_, :], r_=tt_[:, hd, 0:8], i_=sv: e.match_replace(out=o_, in_to_replace=r_, in_values=i_, imm_value=-1e30),
                              ["sct", tkeys[ci_]], ["wk1_%d" % ci_])
                    for ci_, (hd, side, tt_) in enumerate(chains):
                        kb.op("dve", lambda e, o_=tt_[:, hd, 8:16], i_=wk1[:, ci_, :]: e.max(out=o_, in_=i_), ["wk1_%d" % ci_], [tkeys[ci_]])
                    kb.tt(cand[:].rearrange("p h (a b) -> p h a b", b=16), t1[:].unsqueeze(3).to_broadcast([128, 8, 16, 16]),
                          t2[:].unsqueeze(2).to_broadcast([128, 8, 16, 16]), ALU.add, tkeys, ["cand"])
                    ckeys = ["c8_%d" % hd for hd in range(8)]
                    for hd in range(8):
                        kb.op("dve", lambda e, o_=c8a[:, hd, :], i_=cand[:, hd, :]: e.max(out=o_, in_=i_), ["cand"], [ckeys[hd]])
                    for hd in range(8):
                        kb.op("dve", lambda e, o_=wk2[:, hd, :], r_=c8a[:, hd, :], i_=cand[:, hd, :]: e.match_replace(out=o_, in_to_replace=r_, in_values=i_, imm_value=-1e30),
                              ["cand", ckeys[hd]], ["wk2_%d" % hd])
                    for hd in range(8):
                        kb.op("dve", lambda e, o_=c8b[:, hd, :], i_=wk2[:, hd, :]: e.max(out=o_, in_=i_), ["wk2_%d" % hd], [ckeys[hd]])
                    kb.tt(csh[:], cand[:], c8a[:, :, 0:1].to_broadcast([128, 8, 256]), ALU.subtract, ["cand"] + ckeys, ["csh"])
                    kb.act(ec[:], csh[:], AF.Exp, ["csh"], ["ec"])
                    kb.tt(mk8[:], cand[:], c8b[:, :, 7:8].to_broadcast([128, 8, 256]), ALU.is_ge, ["cand"] + ckeys, ["mk8"])
                    kb.tt(ec[:], ec[:], mk8[:], ALU.mult, ["ec", "mk8"], ["ec"])
                    kb.op("dve", lambda e: e.tensor_reduce(out=Z[:], in_=ec[:], axis=AX.X, op=ALU.add), ["ec"], ["Z"])
                    kb.act(Z[:], Z[:], AF.Ln, ["Z"], ["Z"])
                    kb.cp(tb[:, 0:8], c8b[:, :, 7], ckeys, ["tb"])
                    kb.stt(tb[:, 8:16], c8a[:, :, 0], -1.0, Z[:], ALU.mult, ALU.subtract, ckeys + ["Z"], ["tb"])
                    kb.dma("pool", tb_d[rs, :], tb[:], reads=["tb"], writes=["tb_d"])
                phase_end()
            if _STOP == "C":
                break
            with ExitStack() as sd_:
                T = lambda name, shape, dt=F32, _p="L%dD_" % li: sd_.enter_context(nc.sbuf_tensor(_p + name, shape, dt))
                gft = T("gft", [128, 8])
                kb.dma("sp", gft[:], gffn_i[li], writes=["gt"])
                Ub = T("Ub", [128, 8, 4096], BF16); Vv = T("Vv", [128, 32, D], BF16)
                load_w(Ub, uT_i[li], 8, 4096, "Ub", gt=gft)
                load_w(Vv, v_i[li], 32, D, "Vv")
                gf = T("gf", [128, D])
                if final:
                    kb.dma("sp", gf[:], gfin, writes=["gf"])
                h1t = [T("h1t%d" % i, [128, D]) for i in range(2)]
                sct = [T("sctb%d" % i, [128, D]) for i in range(2)]
                xT = [T("xTb%d" % i, [128, D], BF16) for i in range(2)]
                tbt = [T("tbt%d" % i, [128, 16]) for i in range(2)]
                NBG = 4
                Sg = [T("Sg%d" % i, [128, 16, 64]) for i in range(NBG)]
                Eg = [T("Eg%d" % i, [128, 1024], BF16) for i in range(NBG)]
                Gh = [T("Gh%d" % i, [128, 1024], BF16) for i in range(2)]; G = T("G", [128, 1024], BF16)
                gl = [T("gl%d" % i, [128, 512]) for i in range(2)]
                Wb = T("Wb", [128, 1024], BF16); WT = T("WT", [128, 1024], BF16)
                ho = T("ho", [128, D]); junk = T("junkb", [128, D], BF16); ss = T("ssb", [128, 1])
                pH = [ps[0], ps[1]]; ptr = ps[2][:].bitcast(BF16); po = [ps[3], ps[4]]

                def loadB(i):
                    b = i % 2
                    rs = slice(i * 128, (i + 1) * 128)
                    kb.dma("sp", h1t[b][:], h1_d[rs, :], writes=["h1t%d" % b])
                    kb.dma("sp", sct[b][:], sc_d[rs, :], writes=["sctb%d" % b])
                    kb.dma("sp", xT[b][:], xT_d[rs, :], writes=["xTb%d" % b])
                    kb.dma("sp", tbt[b][:], tb_d[rs, :], writes=["tbt%d" % b])

                loadB(0)
                cnt = 0
                for i in range(NT):
                    b = i % 2
                    rs = slice(i * 128, (i + 1) * 128)
                    if i + 1 < NT:
                        loadB(i + 1)
                    for eq in range(4):
                        deferred = None
                        for hd in range(8):
                            j = cnt % NBG
                            gj = cnt % 2
                            cnt += 1
                            s1 = sct[b][:, hd * 128 + 16 * eq:hd * 128 + 16 * eq + 16]
                            s2 = sct[b][:, hd * 128 + 64:hd * 128 + 128]
                            kb.tt(Sg[j][:], s1.unsqueeze(2).to_broadcast([128, 16, 64]), s2.unsqueeze(1).to_broadcast([128, 16, 64]), ALU.add,
                                  ["sctb%d" % b], ["Sg%d" % j], eng=("pool" if hd in _POOL_HEADS else "dve"))
                            Sf = Sg[j][:].rearrange("p a b -> p (a b)")
                            kb.act(Eg[j][:], Sf, AF.Exp, ["Sg%d" % j, "tbt%d" % b], ["Eg%d" % j], bias=tbt[b][:, 8 + hd:9 + hd], scale=1.0)
                            if hd == 0:
                                kb.stt(G[:], Sf, tbt[b][:, hd:hd + 1], Eg[j][:], ALU.is_ge, ALU.mult, ["Sg%d" % j, "Eg%d" % j, "tbt%d" % b], ["G"])
                            else:
                                kb.stt(Gh[gj][:], Sf, tbt[b][:, hd:hd + 1], Eg[j][:], ALU.is_ge, ALU.mult, ["Sg%d" % j, "Eg%d" % j, "tbt%d" % b], ["Gh%d" % gj])
                                if deferred is not None:
                                    deferred()
                                deferred = (lambda gj=gj: kb.tt(G[:], G[:], Gh[gj][:], ALU.add, ["G", "Gh%d" % gj], ["G"]))
                        if deferred is not None:
                            deferred()
                        for g2 in range(2):
                            e0 = eq * 1024 + g2 * 512
                            for k in range(8):
                                kb.mm(pH[g2][:], xT[b][:, k * 128:(k + 1) * 128], Ub[:, k, e0:e0 + 512], k == 0, k == 7,
                                      ["xTb%d" % b, "Ub"], ["pH%d" % g2], inc=(k == 7))
                            kb.act(gl[g2][:], pH[g2][:], AF.Gelu, ["pH%d" % g2], ["gl%d" % g2])
                            kb.tt(Wb[:, g2 * 512:(g2 + 1) * 512], gl[g2][:], G[:, g2 * 512:(g2 + 1) * 512], ALU.mult, ["gl%d" % g2, "G"], ["Wb"])
                        for cc in range(8):
                            kb.tr(ptr[:, cc * 128:(cc + 1) * 128], Wb[:, cc * 128:(cc + 1) * 128], idb[:], ["Wb", "ident_b"], ["ptr"], inc=(cc == 7))
                        kb.cp(WT[:], ptr, ["ptr"], ["WT"], eng="act")
                        for dh in range(2):
                            for cc in range(8):
                                kb.mm(po[dh][:], WT[:, cc * 128:(cc + 1) * 128], Vv[:, eq * 8 + cc, dh * 512:(dh + 1) * 512],
                                      (eq == 0 and cc == 0), (eq == 3 and cc == 7), ["WT", "Vv"], ["po%d" % dh], inc=(cc == 7))
                    for dh in range(2):
                        kb.tt(ho[:, dh * 512:(dh + 1) * 512], h1t[b][:, dh * 512:(dh + 1) * 512], po[dh][:], ALU.add,
                              ["h1t%d" % b, "po%d" % dh], ["ho"])
                    if final:
                        _rstd(kb, ho[:], junk[:], ss[:], D, ["ho"], "f")
                        kb.stt(ho[:], ho[:], ss[:, 0:1], gf[:], ALU.mult, ALU.mult, ["ho", "fss", "gf"], ["ho"])
                    kb.dma("sp", hdst[rs, :], ho[:], reads=["ho"], writes=["hdst"])
                phase_end()
    return nc


def forward_fused(x, meta_tokens, attn_norm, w_in, attn_sinks, gla_gate_w2, gla_gate_b, swa_out_norm,
                  sb_out_norm, gla_out_norm, w_out, ffn_norm, peer_w_q, peer_sub_keys, peer_u, peer_v, final_norm, runner=None):
    f32 = lambda a: np.asarray(a, np.float32)
    x = f32(x)
    B, SEQ, _ = x.shape
    depth = attn_norm.shape[0]
    L = SEQ + 128
    Lp = ((L + 511) // 512) * 512
    TQ = Lp // 4
    assert B * 4 == NCORES
    nc = _prog_fused(Lp, depth)
    w_in, w_out = f32(w_in), f32(w_out)
    shared = {
        "g_attn": _c(np.stack([_gk(attn_norm[i]) for i in range(depth)])),
        "gffn": _c(np.stack([_gk(ffn_norm[i]) for i in range(depth)])),
        "wq": _c(f32(peer_w_q)),
        "ksub": _c(np.stack([np.transpose(f32(peer_sub_keys[i]).reshape(16, 64, 128), (2, 0, 1)) for i in range(depth)])),
        "uT": _c(np.transpose(f32(peer_u), (0, 2, 1))), "v": _c(f32(peer_v)),
        "gfin": _c(np.broadcast_to(f32(final_norm)[None, :], (128, D))),
    }
    per_j = []
    for j in range(4):
        kv = j // 2
        cols = np.concatenate([np.arange(128 * j, 128 * j + 128), np.arange(512 + 64 * kv, 512 + 64 * kv + 64),
                               np.arange(512 + 64 * kv, 512 + 64 * kv + 64), np.arange(768 + 64 * j, 768 + 64 * j + 64),
                               np.arange(1024 + 64 * j, 1024 + 64 * j + 64), np.arange(1536 + 32 * j, 1536 + 32 * j + 32),
                               np.arange(1664 + 32 * j, 1664 + 32 * j + 32), np.arange(2048, 2064),
                               np.arange(640 + 64 * kv, 640 + 64 * kv + 64), np.arange(1280 + 64 * j, 1280 + 64 * j + 64),
                               np.arange(1792 + 64 * j, 1792 + 64 * j + 64), np.arange(2064 + 64 * j, 2064 + 64 * j + 64)])
        assert len(cols) == CJ
        rows = np.concatenate([np.arange(128 * j, 128 * j + 128), np.arange(512 + 64 * j, 512 + 64 * j + 64),
                               np.arange(768 + 64 * j, 768 + 64 * j + 64)])
        gm = np.stack([np.concatenate([f32(swa_out_norm[i])[128 * j:128 * j + 128], f32(sb_out_norm[i])[64 * j:64 * j + 64],
                                       f32(gla_out_norm[i])[64 * j:64 * j + 64]]) for i in range(depth)])
        per_j.append({
            "w_in": _c(w_in[:, :, cols]),
            "w2": _c(f32(gla_gate_w2)[:, :, 32 * j:32 * j + 32]),
            "gb": _c(f32(gla_gate_b)[:, 32 * j:32 * j + 32].reshape(depth, 32, 1)),
            "sinks": _c(np.broadcast_to(f32(attn_sinks)[:, None, 2 * j:2 * j + 2], (depth, 128, 2))),
            "gmix": _c(np.broadcast_to(gm[:, None, :], (depth, 128, 256))),
            "wout": _c(np.transpose(w_out[:, rows, :].reshape(depth, 2, 128, D), (0, 2, 1, 3))),
        })
    maps = []
    for c in range(NCORES):
        b, r = c // 4, c % 4
        hp = np.zeros((Lp, D), np.float32)
        hp[112:128] = f32(meta_tokens)
        hp[128:L] = x[b]
        maps.append(dict(shared, **per_j[r], h0=_c(hp[r * TQ:(r + 1) * TQ])))
    res = (runner or _run)(nc, maps)
    out = np.zeros((B, SEQ, D), np.float32)
    for b in range(B):
        full = np.concatenate([np.asarray(res[b * 4 + r]["out"]) for r in range(4)], 0)
        out[b] = full[128:L]
    return out


def _prog_fused(Lp, depth):
    key = ("fused", Lp, depth)
    if key not in _CACHE:
        _CACHE[key] = build_fused(Lp, depth)
    return _CACHE[key]
```

```python
import numpy as np
from contextlib import ExitStack
import ml_dtypes
import concourse.bass as bass
import concourse.mybir as mybir
from concourse.bass_utils import run_bass_kernel_spmd

F32 = mybir.dt.float32
BF16 = mybir.dt.bfloat16
AF = mybir.ActivationFunctionType
ALU = mybir.AluOpType
AX = mybir.AxisListType

D = 1024
IN_COLS = 2320
EPS = 1e-6
NEG = -30000.0
NCORES = 8


class KB:
    ENGS = ("pe", "act", "dve", "pool", "sp")
    NDMA = 8

    def __init__(self, nc, stack):
        self.nc = nc
        self._stack = stack
        self.q = {e: [] for e in self.ENGS}
        self.cnt = {e: 0 for e in self.ENGS}
        self.sem = {e: stack.enter_context(nc.semaphore("s_" + e)) for e in self.ENGS}
        self.dsem = {e: [stack.enter_context(nc.semaphore("d_%s%d" % (e, i))) for i in range(self.NDMA)]
                     for e in ("sp", "pool")}
        self.dcnt = {e: [0] * self.NDMA for e in self.dsem}
        self.drot = {e: 0 for e in self.dsem}
        self.seen = {e: {} for e in self.ENGS}
        self.lastw = {}
        self.readers = {}
        self.pending_noinc = {e: False for e in self.ENGS}
        self.excl = set()

    def _deps(self, eng, reads, writes):
        toks = []
        for k in list(reads) + list(writes):
            t = self.lastw.get(k)
            if t is not None:
                toks.append(t)
        for k in writes:
            toks.extend(self.readers.get(k, {}).values())
        waits = {}
        for (sem, val, src) in toks:
            if src == "pe" and eng == "pe":
                continue
            sid = id(sem)
            if self.seen[eng].get(sid, 0) >= val:
                continue
            if sid not in waits or waits[sid][1] < val:
                waits[sid] = (sem, val)
        for sid, (sem, val) in waits.items():
            self.seen[eng][sid] = val
        return list(waits.values())

    def _record(self, rkey, tok, reads, writes):
        for k in writes:
            self.lastw[k] = tok
            self.readers[k] = {}
        for k in reads:
            self.readers.setdefault(k, {})[rkey] = tok

    def op(self, eng, fn, reads=(), writes=(), inc=True):
        ex = [k for k in reads if k in self.excl and k not in writes]
        if ex:
            writes = list(writes) + ex
        waits = self._deps(eng, reads, writes)
        sem = self.sem[eng]
        if inc:
            self.cnt[eng] += 1
            tok = (sem, self.cnt[eng], eng)
            self.pending_noinc[eng] = False
        else:
            tok = (sem, self.cnt[eng] + 1, eng)
            self.pending_noinc[eng] = True
        self.q[eng].append((waits, fn, (sem, 1) if inc else None))
        self._record(eng, tok, reads, writes)

    def dma(self, eng, out, in_, reads=(), writes=()):
        waits = self._deps(eng, reads, writes)
        r = self.drot[eng]
        self.drot[eng] = (r + 1) % self.NDMA
        sem = self.dsem[eng][r]
        prev = self.dcnt[eng][r]
        if prev > 0 and self.seen[eng].get(id(sem), 0) < prev:
            waits.append((sem, prev))
            self.seen[eng][id(sem)] = prev
        self.dcnt[eng][r] += 16
        tok = (sem, self.dcnt[eng][r], "dma")
        self.q[eng].append((waits, lambda e, o=out, i=in_: e.dma_start(out=o, in_=i), (sem, 16)))
        self._record(("dma", id(sem)), tok, reads, writes)

    def coll(self, kind, op, groups, in_, out, reads, writes):
        waits = self._deps("pool", reads, writes)
        if not hasattr(self, "csem"):
            self.csem = self._stack.enter_context(self.nc.semaphore("s_cc"))
            self.ccnt = 0
        if self.ccnt > 0 and self.seen["pool"].get(id(self.csem), 0) < self.ccnt:
            waits.append((self.csem, self.ccnt))
            self.seen["pool"][id(self.csem)] = self.ccnt
        self.ccnt += 1
        tok = (self.csem, self.ccnt, "dma")
        self.q["pool"].append((waits, lambda e, k=kind, o=op, g=groups, i=in_, u=out:
                               e.collective_compute(k, o, replica_groups=g, ins=[i.opt()], outs=[u.opt()]), (self.csem, 1)))
        self._record(("dma", id(self.csem)), tok, reads, writes)

    def wait_all(self, eng, keys):
        waits = self._deps(eng, keys, ())
        self.q[eng].append((waits, None, None))

    def barrier(self):
        for e in self.ENGS:
            waits = []
            for f in self.ENGS:
                if f != e and self.cnt[f] > 0 and self.seen[e].get(id(self.sem[f]), 0) < self.cnt[f]:
                    waits.append((self.sem[f], self.cnt[f]))
                    self.seen[e][id(self.sem[f])] = self.cnt[f]
            for q in self.dsem:
                for r in range(self.NDMA):
                    s, v = self.dsem[q][r], self.dcnt[q][r]
                    if v > 0 and self.seen[e].get(id(s), 0) < v:
                        waits.append((s, v))
                        self.seen[e][id(s)] = v
            if hasattr(self, "csem") and self.ccnt > 0 and self.seen[e].get(id(self.csem), 0) < self.ccnt:
                waits.append((self.csem, self.ccnt))
                self.seen[e][id(self.csem)] = self.ccnt
            self.q[e].append((waits, None, None))

    def emit(self):
        nc = self.nc
        for e in self.ENGS:
            assert not self.pending_noinc[e], "trailing non-inc op on " + e
        qs = self.q
        self.q = {e: [] for e in self.ENGS}
        with nc.Block() as block:
            def run(engname):
                def body(e):
                    for waits, fn, inc in qs[engname]:
                        for sem, val in waits:
                            e.wait_ge(sem, val)
                        if fn is None:
                            continue
                        ins = fn(e)
                        if inc is not None:
                            ins.then_inc(inc[0], inc[1])
                return body
            block.tensor(run("pe"))
            block.scalar(run("act"))
            block.vector(run("dve"))
            block.gpsimd(run("pool"))
            block.sync(run("sp"))

    def act(self, out, in_, func, r, w, eng="act", **kw):
        self.op(eng, lambda e, o=out, i=in_, f=func, k=kw: e.activation(out=o, in_=i, func=f, **k), r, w)

    def tt(self, out, in0, in1, op, r, w, eng="dve"):
        self.op(eng, lambda e, o=out, a=in0, b=in1, p=op: e.tensor_tensor(out=o, in0=a, in1=b, op=p), r, w)

    def ts(self, out, in0, s1, op0, r, w, s2=None, op1=None, eng="dve"):
        if op1 is None:
            self.op(eng, lambda e, o=out, a=in0, x=s1, p=op0: e.tensor_scalar(out=o, in0=a, scalar1=x, scalar2=None, op0=p), r, w)
        else:
            self.op(eng, lambda e, o=out, a=in0, x=s1, y=s2, p=op0, q=op1: e.tensor_scalar(out=o, in0=a, scalar1=x, scalar2=y, op0=p, op1=q), r, w)

    def stt(self, out, in0, scalar, in1, op0, op1, r, w, **kw):
        self.op("dve", lambda e, o=out, a=in0, s=scalar, b=in1, p=op0, q=op1, k=kw:
                e.scalar_tensor_tensor(out=o, in0=a, scalar=s, in1=b, op0=p, op1=q, **k), r, w)

    def cp(self, out, in_, r, w, eng="dve"):
        if eng == "act":
            self.op("act", lambda e, o=out, i=in_: e.activation(out=o, in_=i, func=AF.Copy), r, w)
        else:
            self.op(eng, lambda e, o=out, i=in_: e.tensor_copy(out=o, in_=i), r, w)

    def mm(self, out, lhsT, rhs, start, stop, r, w, inc=True):
        self.op("pe", lambda e, o=out, l=lhsT, x=rhs, s=start, t=stop: e.matmul(o, lhsT=l, rhs=x, start=s, stop=t), r, w, inc=inc)

    def tr(self, out, in_, ident, r, w, inc=True):
        self.op("pe", lambda e, o=out, i=in_, d=ident: e.transpose(o, i, d), r, w, inc=inc)

    def memset(self, ap, val, w, eng="pool"):
        self.op(eng, lambda e, a=ap, v=val: e.memset(a, v), (), w)

    def asel(self, ap, cmp, fill, base, cm, pattern, key):
        self.op("pool", lambda e, a=ap, c=cmp, f=fill, b=base, m=cm, p=pattern:
                e.affine_select(out=a, in_=a, compare_op=c, fill=f, base=b, pattern=p, channel_multiplier=m), [key], [key])


def _ident(kb, T, name="ident"):
    idf = T(name + "_f", [128, 128], F32)
    idb = T(name + "_b", [128, 128], BF16)
    kb.memset(idf[:], 0.0, [name + "_f"])
    kb.asel(idf[:], ALU.not_equal, 1.0, 0, 1, [[-1, 128]], name + "_f")
    kb.cp(idb[:], idf[:], [name + "_f"], [name + "_b"], eng="pool")
    return idf, idb


def _rstd(kb, src, junk, ss, n, rkeys, pfx):
    kb.act(junk, src, AF.Square, rkeys, [pfx + "junk", pfx + "ss"], accum_out=ss)
    kb.ts(ss, ss, 1.0 / n, ALU.mult, [pfx + "ss"], [pfx + "ss"], s2=EPS, op1=ALU.add)
    kb.act(ss, ss, AF.Sqrt, [pfx + "ss"], [pfx + "ss"])
    kb.op("dve", lambda e, a=ss: e.reciprocal(out=a, in_=a), [pfx + "ss"], [pfx + "ss"])


def build_p1(NT):
    nc = bass.Bass("TRN2", target_bir_lowering=False)
    h = nc.dram_tensor("h", [NT * 128, D], F32, kind="ExternalInput").ap()
    w = nc.dram_tensor("w", [D, IN_COLS], F32, kind="ExternalInput").ap()
    g = nc.dram_tensor("g", [128, 8], F32, kind="ExternalInput").ap()
    proj = nc.dram_tensor("proj", [NT * 128, IN_COLS], BF16, kind="ExternalOutput").ap()
    with ExitStack() as st:
        kb = KB(nc, st)
        T = lambda name, shape, dt=F32: st.enter_context(nc.sbuf_tensor(name, shape, dt))
        ps = [st.enter_context(nc.psum_tensor("ps%d" % i, [128, 512], F32)) for i in range(8)]
        idf, idb = _ident(kb, T)
        gt = T("gt", [128, 8])
        kb.dma("sp", gt[:], g, writes=["gt"])
        Wg = T("Wg", [128, 8, IN_COLS], BF16)
        stage = [T("stage%d" % i, [128, IN_COLS]) for i in range(2)]
        for k in range(8):
            sk = "stage%d" % (k % 2)
            kb.dma("sp", stage[k % 2][:], w[k * 128:(k + 1) * 128, :], writes=[sk])
            kb.ts(Wg[:, k, :], stage[k % 2][:], gt[:, k:k + 1], ALU.mult, [sk, "gt"], ["Wg%d" % k])
        ht = [T("ht%d" % i, [128, D]) for i in range(2)]
        junk = T("junk", [128, D], BF16)
        ss = T("ss", [128, 1])
        xn = T("xn", [128, D], BF16)
        xnT = T("xnT", [128, D], BF16)
        pr = [T("pr%d" % i, [128, IN_COLS], BF16) for i in range(2)]
        pT = ps[0][:].bitcast(BF16)
        cgs = [(c0, min(512, IN_COLS - c0)) for c0 in range(0, IN_COLS, 512)]
        wkeys = ["Wg%d" % k for k in range(8)]
        for i in range(NT):
            hk = "ht%d" % (i % 2)
            hb = ht[i % 2]
            kb.dma("sp", hb[:], h[i * 128:(i + 1) * 128, :], writes=[hk])
            _rstd(kb, hb[:], junk[:], ss[:], D, [hk], "a")
            kb.ts(xn[:], hb[:], ss[:, 0:1], ALU.mult, [hk, "ass"], ["xn"])
            for k in range(8):
                kb.tr(pT[:, k * 128:(k + 1) * 128], xn[:, k * 128:(k + 1) * 128], idb[:], ["xn", "ident_b"], ["pT"], inc=(k == 7))
            kb.cp(xnT[:], pT, ["pT"], ["xnT"], eng="act")
            prk = "pr%d" % (i % 2)
            for ci, (c0, cw) in enumerate(cgs):
                pk = "pp%d" % (ci % 2)
                pp = ps[1 + ci % 2]
                for k in range(8):
                    kb.mm(pp[:, 0:cw], xnT[:, k * 128:(k + 1) * 128], Wg[:, k, c0:c0 + cw], k == 0, k == 7,
                          ["xnT", wkeys[k]], [pk], inc=(k == 7))
                kb.cp(pr[i % 2][:, c0:c0 + cw], pp[:, 0:cw], [pk], [prk], eng=("act" if ci % 2 else "dve"))
            kb.dma("pool", proj[i * 128:(i + 1) * 128, :], pr[i % 2][:], reads=[prk], writes=["out"])
        kb.wait_all("pool", ["out"])
        kb.emit()
    return nc


def build_p2(Lp, parts=(1, 1, 1)):
    NCH = Lp // 512
    NB = Lp // 128
    nc = bass.Bass("TRN2", target_bir_lowering=False)
    IN = lambda n, s, dt=BF16: nc.dram_tensor(n, s, dt, kind="ExternalInput").ap()
    qaT = IN("qaT", [128, Lp]); kaT = IN("kaT", [128, Lp]); va = IN("va", [Lp, 64])
    qbT = IN("qbT", [64, Lp]); kbT = IN("kbT", [64, Lp]); vb = IN("vb", [Lp, 64])
    qcT = IN("qcT", [32, Lp]); kcT = IN("kcT", [32, Lp]); vc = IN("vc", [Lp, 64])
    glrT = IN("glrT", [16, Lp])
    w2 = IN("w2", [16, 32], F32); gb = IN("gb", [32, 1], F32); sinks = IN("sinks", [128, 2], F32)
    oa = nc.dram_tensor("oa", [Lp, 128], F32, kind="ExternalOutput").ap()
    obT = nc.dram_tensor("obT", [64, Lp], F32, kind="ExternalOutput").ap()
    oc = nc.dram_tensor("oc", [Lp, 64], F32, kind="ExternalOutput").ap()
    with ExitStack() as st:
        kb = KB(nc, st)
        T = lambda name, shape, dt=F32: st.enter_context(nc.sbuf_tensor(name, shape, dt))
        ps = [st.enter_context(nc.psum_tensor("ps%d" % i, [128, 512], F32)) for i in range(8)]
        idf, idb = _ident(kb, T)
        m_gen = T("m_gen", [128, 256]); m_n0 = T("m_n0", [128, 256]); m_n1 = T("m_n1", [128, 256])
        for m, nm, extra in ((m_gen, "m_gen", None), (m_n0, "m_n0", -240), (m_n1, "m_n1", -112)):
            kb.memset(m[:], 0.0, [nm])
            kb.asel(m[:], ALU.is_ge, NEG, -1, -1, [[1, 256]], nm)
            kb.asel(m[:], ALU.is_ge, NEG, 128, 1, [[-1, 256]], nm)
            if extra is not None:
                kb.asel(m[:], ALU.is_ge, NEG, extra, 0, [[1, 256]], nm)
        sbm_f = T("sbm_f", [128, 512])
        sbm = [T("sbm%d" % d, [128, 512], BF16) for d in range(4)]
        for d in range(4):
            kb.memset(sbm_f[:], 1.0, ["sbm_f"])
            kb.asel(sbm_f[:], ALU.is_ge, 0.0, -1 - 128 * d, -1, [[1, 512]], "sbm_f")
            kb.cp(sbm[d][:], sbm_f[:], ["sbm_f"], ["sbm%d" % d], eng="pool")
        ntri_f = T("ntri_f", [128, 128]); ntri = T("ntri", [128, 128], BF16); nones = T("nones", [128, 128], BF16)
        kb.memset(ntri_f[:], -1.0, ["ntri_f"])
        kb.asel(ntri_f[:], ALU.is_ge, 0.0, 0, 1, [[-1, 128]], "ntri_f")
        kb.cp(ntri[:], ntri_f[:], ["ntri_f"], ["ntri"], eng="pool")
        kb.memset(nones[:], -1.0, ["nones"])
        mle = T("mle", [128, 128])
        kb.memset(mle[:], 1.0, ["mle"])
        kb.asel(mle[:], ALU.is_ge, 0.0, 0, -1, [[1, 128]], "mle")
        rmask = T("rmask", [32, 512])
        kb.memset(rmask[:], 1.0, ["rmask"])
        for b in range(4):
            kb.memset(rmask[:, b * 128:b * 128 + 1], 0.0, ["rmask"])
        w2f = T("w2f", [16, 32]); w2b = T("w2b", [16, 32], BF16); gbt = T("gbt", [32, 1]); sk_t = T("sk_t", [128, 2])
        kb.dma("sp", w2f[:], w2, writes=["w2f"]); kb.dma("sp", gbt[:], gb, writes=["gbt"]); kb.dma("sp", sk_t[:], sinks, writes=["sk"])
        kb.cp(w2b[:], w2f[:], ["w2f"], ["w2b"])
        kb.ts(gbt[:], gbt[:], -1.0, ALU.mult, ["gbt"], ["gbt"])
        KbT = T("KbT", [64, Lp], BF16); Vb = T("Vb", [128, NB, 64], BF16)
        kb.dma("sp", KbT[:], kbT, writes=["KbT"])
        for n0 in range(0, NB, 16):
            n1 = min(NB, n0 + 16)
            kb.dma("sp", Vb[:, n0:n1, :], vb[n0 * 128:n1 * 128, :].rearrange("(n p) d -> p n d", p=128), writes=["Vb"])
        S = T("S", [32, 64]); Sb = T("Sb", [32, 64], BF16)
        kb.memset(S[:], 0.0, ["S"]); kb.memset(Sb[:], 0.0, ["Sb"])
        qa_c = [T("qa_c%d" % i, [128, 512], BF16) for i in range(2)]
        ka_c = [T("ka_c%d" % i, [128, 640], BF16) for i in range(2)]
        va_c = [T("va_c%d" % i, [128, 5, 64], BF16) for i in range(2)]
        qb_c = [T("qb_c%d" % i, [64, 512], BF16) for i in range(2)]
        qs_c = [T("qs_c%d" % i, [64, 512], BF16) for i in range(2)]
        qc_c = [T("qc_c%d" % i, [32, 512], BF16) for i in range(2)]
        kc_c = [T("kc_c%d" % i, [32, 512], BF16) for i in range(2)]
        vc_c = [T("vc_c%d" % i, [128, 4, 64], BF16) for i in range(2)]
        gl_c = [T("gl_c%d" % i, [16, 512], BF16) for i in range(2)]
        sm = T("sm", [128, 256]); pexp = T("pexp", [128, 256], BF16); pTs = T("pTs", [128, 256], BF16)
        st8 = T("st8", [128, 8]); oa_t = [T("oa_t%d" % i, [128, 128]) for i in range(2)]
        ge = T("ge", [32, 512]); gsp = T("gsp", [32, 512]); gcs = T("gcs", [32, 512])
        geq = T("geq", [32, 512]); gek = T("gek", [32, 512])
        qt = T("qt", [32, 512], BF16); kt = T("kt", [32, 512], BF16)
        scb = T("scb", [128, 128], BF16); ktm = T("ktm", [128, 32], BF16); oc_t = [T("oc_t%d" % i, [128, 64]) for i in range(2)]
        stmp = T("stmp", [32, 64])
        e_t = [T("e_t%d" % i, [128, 512]) for i in range(2)]
        sp_t = [T("sp_t%d" % i, [128, 512], BF16) for i in range(2)]
        a_t = [T("a_t%d" % i, [128, 512], BF16) for i in range(2)]
        R = T("R", [128, 512], BF16)
        ob_t = [T("ob_t%d" % i, [64, 512]) for i in range(2)]
        pz = [ps[0], ps[1]]; pc = [ps[2], ps[3]]; pO = ps[4]
        pA = ps[5]; pX = ps[6]; pB = ps[7]
        pT_bf = pB[:].bitcast(BF16)

        def load_chunk(c):
            i = c % 2
            c0 = c * 512
            kb.dma("sp", qa_c[i][:], qaT[:, c0:c0 + 512], writes=["qa_c%d" % i])
            if c == 0:
                kb.memset(ka_c[i][:, 0:128], 0.0, ["ka_c%d" % i])
                kb.memset(va_c[i][:, 0, :], 0.0, ["va_c%d" % i])
                kb.dma("sp", ka_c[i][:, 128:640], kaT[:, 0:512], writes=["ka_c%d" % i])
                kb.dma("sp", va_c[i][:, 1:5, :], va[0:512, :].rearrange("(n p) d -> p n d", p=128), writes=["va_c%d" % i])
            else:
                kb.dma("sp", ka_c[i][:], kaT[:, c0 - 128:c0 + 512], writes=["ka_c%d" % i])
                kb.dma("sp", va_c[i][:], va[c0 - 128:c0 + 512, :].rearrange("(n p) d -> p n d", p=128), writes=["va_c%d" % i])
            kb.dma("sp", qb_c[i][:], qbT[:, c0:c0 + 512], writes=["qb_c%d" % i])
            kb.dma("sp", qc_c[i][:], qcT[:, c0:c0 + 512], writes=["qc_c%d" % i])
            kb.dma("sp", kc_c[i][:], kcT[:, c0:c0 + 512], writes=["kc_c%d" % i])
            kb.dma("sp", vc_c[i][:], vc[c0:c0 + 512, :].rearrange("(n p) d -> p n d", p=128), writes=["vc_c%d" % i])
            kb.dma("sp", gl_c[i][:], glrT[:, c0:c0 + 512], writes=["gl_c%d" % i])

        def _gla(c, i):
            kb.mm(pX[0:32, :], w2b[:], gl_c[i][:], True, True, ["w2b", "gl_c%d" % i], ["pX"])
            kb.act(ge[:], pX[0:32, :], AF.Exp, ["pX", "gbt"], ["ge"], bias=gbt[:, 0:1], scale=-1.0)
            kb.act(gsp[:], ge[:], AF.Ln, ["ge"], ["gsp"], bias=1.0, scale=1.0)
            kb.op("dve", lambda e: e.tensor_tensor_scan(out=gcs[:], data0=rmask[:], data1=gsp[:], initial=0.0, op0=ALU.mult, op1=ALU.add),
                  ["rmask", "gsp"], ["gcs"])
            kb.act(geq[:], gcs[:], AF.Exp, ["gcs"], ["geq"], scale=-1.0 / 16.0)
            kb.act(gek[:], gcs[:], AF.Exp, ["gcs"], ["gek"], scale=1.0 / 16.0)
            kb.stt(qt[:], qc_c[i][:], 32.0 ** -0.5, geq[:], ALU.mult, ALU.mult, ["qc_c%d" % i, "geq"], ["qt"])
            kb.tt(kt[:], kc_c[i][:], gek[:], ALU.mult, ["kc_c%d" % i, "gek"], ["kt"])
            for blk in range(4):
                n = 4 * c + blk
                bs = slice(blk * 128, (blk + 1) * 128)
                kb.mm(pA[:, 384:512], kt[:, bs], qt[:, bs], True, True, ["kt", "qt"], ["pA"])
                kb.tt(scb[:], pA[:, 384:512], mle[:], ALU.mult, ["pA", "mle"], ["scb"])
                kb.tr(pT_bf[:, 512:544], kt[:, bs], idb[0:32, 0:32], ["kt", "ident_b"], ["pB"])
                kb.cp(ktm[:], pT_bf[:, 512:544], ["pB"], ["ktm"], eng="act")
                kb.mm(pA[:, 320:384], scb[:], vc_c[i][:, blk, :], True, False, ["scb", "vc_c%d" % i], ["pA"], inc=False)
                kb.mm(pA[:, 320:384], qt[:, bs], Sb[:], False, True, ["qt", "Sb"], ["pA"])
                ock = "oc_t%d" % (n % 2)
                kb.cp(oc_t[n % 2][:], pA[:, 320:384], ["pA"], [ock], eng="act")
                kb.dma("pool", oc[n * 128:(n + 1) * 128, :], oc_t[n % 2][:], reads=[ock], writes=["oc"])
                kb.mm(pB[0:32, 384:448], ktm[:], vc_c[i][:, blk, :], True, True, ["ktm", "vc_c%d" % i], ["pB"])
                kb.tt(stmp[:], pB[0:32, 384:448], S[:], ALU.add, ["pB", "S"], ["stmp"])
                kb.ts(S[:], stmp[:], geq[:, blk * 128 + 127:blk * 128 + 128], ALU.mult, ["stmp", "geq"], ["S"])
                kb.cp(Sb[:], S[:], ["S"], ["Sb"])

        def _sb(c, i):
            kb.ts(qs_c[i][:], qb_c[i][:], 0.125, ALU.mult, ["qb_c%d" % i], ["qs_c%d" % i])
            nkb = 4 * c + 4
            for it in range(nkb):
                kblk = nkb - 1 - it
                j = it % 2
                dg = kblk - 4 * c
                first, last = (it == 0), (kblk == 0)
                ksl = KbT[:, kblk * 128:(kblk + 1) * 128]
                kb.mm(pz[j][:], ksl, qs_c[i][:], True, True, ["KbT", "qs_c%d" % i], ["pz%d" % j])
                kb.act(e_t[j][:], pz[j][:], AF.Exp, ["pz%d" % j], ["e_t%d" % j])
                kb.act(sp_t[j][:], e_t[j][:], AF.Ln, ["e_t%d" % j], ["sp_t%d" % j], bias=1.0, scale=1.0)
                if dg >= 0:
                    kb.tt(sp_t[j][:], sp_t[j][:], sbm[dg][:], ALU.mult, ["sp_t%d" % j, "sbm%d" % dg], ["sp_t%d" % j])
                kb.mm(pc[j][:], ntri[:], sp_t[j][:], True, False, ["ntri", "sp_t%d" % j], ["pc%d" % j], inc=False)
                if not first:
                    kb.mm(pc[j][:], nones[:], R[:], False, False, ["nones", "R"], ["pc%d" % j], inc=False)
                kb.mm(pc[j][:], ksl, qs_c[i][:], False, True, ["KbT", "qs_c%d" % i], ["pc%d" % j])
                if not last:
                    if first:
                        kb.cp(R[:], sp_t[j][:], ["sp_t%d" % j], ["R"], eng="pool")
                    else:
                        kb.tt(R[:], R[:], sp_t[j][:], ALU.add, ["R", "sp_t%d" % j], ["R"], eng="pool")
                kb.act(a_t[j][:], pc[j][:], AF.Exp, ["pc%d" % j], ["a_t%d" % j])
                if dg >= 0:
                    kb.tt(a_t[j][:], a_t[j][:], sbm[dg][:], ALU.mult, ["a_t%d" % j, "sbm%d" % dg], ["a_t%d" % j])
                kb.mm(pO[0:64, :], Vb[:, kblk, :], a_t[j][:], first, last, ["Vb", "a_t%d" % j], ["pO"], inc=True)
            kb.cp(ob_t[i][:], pO[0:64, :], ["pO"], ["ob_t%d" % i])
            kb.dma("pool", obT[:, c * 512:(c + 1) * 512], ob_t[i][:], reads=["ob_t%d" % i], writes=["ob"])

        load_chunk(0)
        for c in range(NCH):
            i = c % 2
            if c + 1 < NCH:
                load_chunk(c + 1)
            for blk in (range(4) if parts[0] else []):
                n = 4 * c + blk
                msk = m_n0 if n == 0 else (m_n1 if n == 1 else m_gen)
                mk = "m_n0" if n == 0 else ("m_n1" if n == 1 else "m_gen")
                ok = "oa_t%d" % (n % 2)
                for hh in range(2):
                    hs = slice(hh * 64, (hh + 1) * 64)
                    kb.mm(pA[:, 0:256], qa_c[i][hs, blk * 128:(blk + 1) * 128], ka_c[i][hs, blk * 128:blk * 128 + 256],
                          True, True, ["qa_c%d" % i, "ka_c%d" % i], ["pA"])
                    kb.stt(sm[:], pA[:, 0:256], 0.125, msk[:], ALU.mult, ALU.add, ["pA", mk], ["sm"])
                    kb.op("dve", lambda e: e.tensor_reduce(out=st8[:, 0:1], in_=sm[:], axis=AX.X, op=ALU.max), ["sm"], ["st8"])
                    kb.tt(st8[:, 0:1], st8[:, 0:1], sk_t[:, hh:hh + 1], ALU.max, ["st8", "sk"], ["st8"])
                    kb.ts(st8[:, 1:2], st8[:, 0:1], -1.0, ALU.mult, ["st8"], ["st8"])
                    kb.act(pexp[:], sm[:], AF.Exp, ["sm", "st8"], ["pexp", "st8"], bias=st8[:, 1:2], scale=1.0, accum_out=st8[:, 2:3])
                    kb.act(st8[:, 3:4], sk_t[:, hh:hh + 1], AF.Exp, ["sk", "st8"], ["st8"], bias=st8[:, 1:2], scale=1.0)
                    kb.tt(st8[:, 4:5], st8[:, 2:3], st8[:, 3:4], ALU.add, ["st8"], ["st8"])
                    kb.op("dve", lambda e: e.reciprocal(out=st8[:, 5:6], in_=st8[:, 4:5]), ["st8"], ["st8"])
                    kb.tr(pT_bf[:, 0:128], pexp[:, 0:128], idb[:], ["pexp", "ident_b"], ["pB"], inc=False)
                    kb.tr(pT_bf[:, 128:256], pexp[:, 128:256], idb[:], ["pexp", "ident_b"], ["pB"])
                    kb.cp(pTs[:], pT_bf[:, 0:256], ["pB"], ["pTs"], eng="act")
                    kb.mm(pA[:, 256:320], pTs[:, 0:128], va_c[i][:, blk, :], True, False, ["pTs", "va_c%d" % i], ["pA"], inc=False)
                    kb.mm(pA[:, 256:320], pTs[:, 128:256], va_c[i][:, blk + 1, :], False, True, ["pTs", "va_c%d" % i], ["pA"])
                    kb.ts(oa_t[n % 2][:, hs], pA[:, 256:320], st8[:, 5:6], ALU.mult, ["pA", "st8"], [ok])
                kb.dma("pool", oa[n * 128:(n + 1) * 128, :], oa_t[n % 2][:], reads=[ok], writes=["oa"])
            if parts[1]:
              _gla(c, i)
            if parts[2]:
              _sb(c, i)
        kb.wait_all("pool", ["oa", "oc", "ob"])
        kb.emit()
    return nc


def build_p3(NT, final):
    nc = bass.Bass("TRN2", target_bir_lowering=False)
    IN = lambda n, s, dt=F32: nc.dram_tensor(n, s, dt, kind="ExternalInput").ap()
    h = IN("h", [NT * 128, D]); o = IN("o", [NT * 128, D]); rc = IN("rc", [NT * 128, 256], BF16)
    gmix = IN("gmix", [128, D]); wout = IN("wout", [D, D]); gffn = IN("gffn", [128, 8])
    wq = IN("wq", [D, 2048]); ksub = IN("ksub", [128, 16, 64]); uT = IN("uT", [D, 4096]); v = IN("v", [4096, D])
    gfin = IN("gfin", [128, D])
    hout = nc.dram_tensor("hout", [NT * 128, D], F32, kind="ExternalOutput").ap()
    h1_d = nc.dram_tensor("h1_d", [NT * 128, D], F32, kind="Internal").ap()
    sc_d = nc.dram_tensor("sc_d", [NT * 128, D], F32, kind="Internal").ap()
    tb_d = nc.dram_tensor("tb_d", [NT * 128, 16], F32, kind="Internal").ap()
    xT_d = nc.dram_tensor("xT_d", [NT * 128, D], BF16, kind="Internal").ap()
    with ExitStack() as st:
        kb = KB(nc, st)
        ps = [st.enter_context(nc.psum_tensor("ps%d" % i, [128, 512], F32)) for i in range(8)]
        T0 = lambda name, shape, dt=F32: st.enter_context(nc.sbuf_tensor(name, shape, dt))
        idf, idb = _ident(kb, T0)
        gft = T0("gft", [128, 8])
        kb.dma("sp", gft[:], gffn, writes=["gft"])
        stage = [T0("stage%d" % i, [128, 1024]) for i in range(2)]
        scnt = [0]

        def load_w(dst, src, rows_k, cols, key, scale_col=None):
            for k in range(rows_k):
                for c0 in range(0, cols, 1024):
                    cw = min(1024, cols - c0)
                    si = scnt[0] % 2
                    scnt[0] += 1
                    sk = "stage%d" % si
                    kb.dma("sp", stage[si][:, 0:cw], src[k * 128:(k + 1) * 128, c0:c0 + cw], writes=[sk])
                    if scale_col:
                        kb.ts(dst[:, k, c0:c0 + cw], stage[si][:, 0:cw], gft[:, k:k + 1], ALU.mult, [sk, "gft"], [key])
                    else:
                        kb.cp(dst[:, k, c0:c0 + cw], stage[si][:, 0:cw], [sk], [key], eng="pool")

        with ExitStack() as sa:
            T = lambda name, shape, dt=F32: sa.enter_context(nc.sbuf_tensor(name, shape, dt))
            Wo = T("Wo", [128, 8, D], BF16); Wq = T("Wq", [128, 8, 2048], BF16); Ks = T("Ks", [128, 16, 64], BF16)
            gm = T("gm", [128, D]); ksf = T("ksf", [128, 16, 64])
            kb.dma("sp", gm[:], gmix, writes=["gm"])
            kb.dma("sp", ksf[:], ksub, writes=["ksf"])
            kb.cp(Ks[:], ksf[:], ["ksf"], ["Ks"])
            load_w(Wo, wout, 8, D, "Wo")
            load_w(Wq, wq, 8, 2048, "Wq", scale_col=True)
            ht = [T("ht%d" % i, [128, D]) for i in range(2)]
            ot = [T("ot%d" % i, [128, D]) for i in range(2)]
            rct = [T("rct%d" % i, [128, 256], BF16) for i in range(2)]
            sq = T("sq", [128, D]); m1 = T("m1", [128, D]); sil = T("sil", [128, 256])
            s16 = T("s16", [128, 16]); mix = T("mix", [128, D], BF16); mixT = T("mixT", [128, D], BF16)
            h1 = T("h1", [128, D]); junk = T("junk", [128, D], BF16); ss = T("ss", [128, 1])
            xn = T("xn", [128, D], BF16); xnT = T("xnT", [128, D], BF16)
            qT = T("qT", [128, 16, 128], BF16); sct = T("sct", [128, D])
            t1 = T("t1", [128, 8, 16]); t2 = T("t2", [128, 8, 16]); wk = T("wk", [128, 256])
            cand = T("cand", [128, 8, 256]); c8a = T("c8a", [128, 8, 8]); c8b = T("c8b", [128, 8, 8])
            csh = T("csh", [128, 8, 256]); ec = T("ec", [128, 8, 256]); mk8 = T("mk8", [128, 8, 256])
            Z = T("Z", [128, 8]); tb = T("tb", [128, 16])
            pT = ps[0][:].bitcast(BF16)
            for i in range(NT):
                b = i % 2
                rs = slice(i * 128, (i + 1) * 128)
                kb.dma("sp", ht[b][:], h[rs, :], writes=["ht%d" % b])
                kb.dma("sp", ot[b][:], o[rs, :], writes=["ot%d" % b])
                kb.dma("sp", rct[b][:], rc[rs, :], writes=["rct%d" % b])
                kb.act(sq[:], ot[b][:], AF.Square, ["ot%d" % b], ["sq"])
                kb.op("dve", lambda e: e.tensor_reduce(out=s16[:], in_=sq[:].rearrange("p (h d) -> p h d", d=64), axis=AX.X, op=ALU.add),
                      ["sq"], ["s16"])
                kb.ts(s16[:], s16[:], 1.0 / 64, ALU.mult, ["s16"], ["s16"], s2=EPS, op1=ALU.add)
                kb.act(s16[:], s16[:], AF.Sqrt, ["s16"], ["s16"])
                kb.op("dve", lambda e: e.reciprocal(out=s16[:], in_=s16[:]), ["s16"], ["s16"])
                kb.tt(m1[:].rearrange("p (h d) -> p h d", d=64), ot[b][:].rearrange("p (h d) -> p h d", d=64),
                      s16[:].unsqueeze(2).to_broadcast([128, 16, 64]), ALU.mult, ["ot%d" % b, "s16"], ["m1"])
                kb.act(sil[:], rct[b][:], AF.Silu, ["rct%d" % b], ["sil"])
                kb.tt(m1[:, 768:1024], m1[:, 768:1024], sil[:], ALU.mult, ["m1", "sil"], ["m1"])
                kb.tt(mix[:], m1[:], gm[:], ALU.mult, ["m1", "gm"], ["mix"])
                for k in range(8):
                    kb.tr(pT[:, k * 128:(k + 1) * 128], mix[:, k * 128:(k + 1) * 128], idb[:], ["mix", "ident_b"], ["pT"], inc=(k == 7))
                kb.cp(mixT[:], pT, ["pT"], ["mixT"], eng="act")
                for dh in range(2):
                    for k in range(8):
                        kb.mm(ps[1 + dh][:], mixT[:, k * 128:(k + 1) * 128], Wo[:, k, dh * 512:(dh + 1) * 512], k == 0, k == 7,
                              ["mixT", "Wo"], ["pd%d" % dh], inc=(k == 7))
                    kb.tt(h1[:, dh * 512:(dh + 1) * 512], ht[b][:, dh * 512:(dh + 1) * 512], ps[1 + dh][:], ALU.add,
                          ["ht%d" % b, "pd%d" % dh], ["h1"])
                kb.dma("pool", h1_d[rs, :], h1[:], reads=["h1"], writes=["h1_d"])
                _rstd(kb, h1[:], junk[:], ss[:], D, ["h1"], "b")
                kb.ts(xn[:], h1[:], ss[:, 0:1], ALU.mult, ["h1", "bss"], ["xn"])
                for k in range(8):
                    kb.tr(pT[:, k * 128:(k + 1) * 128], xn[:, k * 128:(k + 1) * 128], idb[:], ["xn", "ident_b"], ["pT"], inc=(k == 7))
                kb.cp(xnT[:], pT, ["pT"], ["xnT"], eng="act")
                kb.dma("pool", xT_d[rs, :], xnT[:], reads=["xnT"], writes=["xT_d"])
                for cg in range(4):
                    pq = ps[3 + cg % 2]
                    for cc in range(4):
                        cidx = cg * 4 + cc
                        for k in range(8):
                            kb.mm(pq[:, cc * 128:(cc + 1) * 128], Wq[:, k, cidx * 128:(cidx + 1) * 128], xnT[:, k * 128:(k + 1) * 128],
                                  k == 0, k == 7, ["Wq", "xnT"], ["pq%d" % (cg % 2)], inc=(k == 7 and cc == 3))
                    kb.cp(qT[:, cg * 4:(cg + 1) * 4, :], pq[:].rearrange("p (c t) -> p c t", t=128), ["pq%d" % (cg % 2)], ["qT"],
                          eng=("act" if cg % 2 else "dve"))
                for cidx in range(16):
                    pscb = ps[5 + cidx // 8]
                    kb.mm(pscb[:, (cidx % 8) * 64:(cidx % 8 + 1) * 64], qT[:, cidx, :], Ks[:, cidx, :], True, True, ["qT", "Ks"],
                          ["psc%d" % (cidx // 8)], inc=(cidx % 8 == 7))
                kb.cp(sct[:, 0:512], ps[5][:], ["psc0"], ["sct"], eng="act")
                kb.cp(sct[:, 512:1024], ps[6][:], ["psc1"], ["sct"], eng="dve")
                kb.dma("pool", sc_d[rs, :], sct[:], reads=["sct"], writes=["sc_d"])
                for hd in range(8):
                    for side, tt_ in ((0, t1), (1, t2)):
                        sv = sct[:, hd * 128 + side * 64:hd * 128 + side * 64 + 64]
                        kb.op("dve", lambda e, o_=tt_[:, hd, 0:8], i_=sv: e.max(out=o_, in_=i_), ["sct"], ["tt"])
                        kb.op("dve", lambda e, o_=wk[:, 0:64], r_=tt_[:, hd, 0:8], i_=sv: e.match_replace(out=o_, in_to_replace=r_, in_values=i_, imm_value=-1e30),
                              ["sct", "tt"], ["wk"])
                        kb.op("dve", lambda e, o_=tt_[:, hd, 8:16], i_=wk[:, 0:64]: e.max(out=o_, in_=i_), ["wk"], ["tt"])
                kb.tt(cand[:].rearrange("p h (a b) -> p h a b", b=16), t1[:].unsqueeze(3).to_broadcast([128, 8, 16, 16]),
                      t2[:].unsqueeze(2).to_broadcast([128, 8, 16, 16]), ALU.add, ["tt"], ["cand"])
                for hd in range(8):
                    kb.op("dve", lambda e, o_=c8a[:, hd, :], i_=cand[:, hd, :]: e.max(out=o_, in_=i_), ["cand"], ["c8"])
                    kb.op("dve", lambda e, o_=wk[:], r_=c8a[:, hd, :], i_=cand[:, hd, :]: e.match_replace(out=o_, in_to_replace=r_, in_values=i_, imm_value=-1e30),
                          ["cand", "c8"], ["wk"])
                    kb.op("dve", lambda e, o_=c8b[:, hd, :], i_=wk[:]: e.max(out=o_, in_=i_), ["wk"], ["c8"])
                kb.tt(csh[:], cand[:], c8a[:, :, 0:1].to_broadcast([128, 8, 256]), ALU.subtract, ["cand", "c8"], ["csh"])
                kb.act(ec[:], csh[:], AF.Exp, ["csh"], ["ec"])
                kb.tt(mk8[:], cand[:], c8b[:, :, 7:8].to_broadcast([128, 8, 256]), ALU.is_ge, ["cand", "c8"], ["mk8"])
                kb.tt(ec[:], ec[:], mk8[:], ALU.mult, ["ec", "mk8"], ["ec"])
                kb.op("dve", lambda e: e.tensor_reduce(out=Z[:], in_=ec[:], axis=AX.X, op=ALU.add), ["ec"], ["Z"])
                kb.act(Z[:], Z[:], AF.Ln, ["Z"], ["Z"])
                kb.cp(tb[:, 0:8], c8b[:, :, 7], ["c8"], ["tb"])
                kb.stt(tb[:, 8:16], c8a[:, :, 0], -1.0, Z[:], ALU.mult, ALU.subtract, ["c8", "Z"], ["tb"])
                kb.dma("pool", tb_d[rs, :], tb[:], reads=["tb"], writes=["tb_d"])
            kb.barrier()
            kb.emit()
        with ExitStack() as sb_:
            T = lambda name, shape, dt=F32: sb_.enter_context(nc.sbuf_tensor(name, shape, dt))
            Ub = T("Ub", [128, 8, 4096], BF16); Vv = T("Vv", [128, 32, D], BF16)
            load_w(Ub, uT, 8, 4096, "Ub", scale_col=True)
            load_w(Vv, v, 32, D, "Vv")
            gf = T("gf", [128, D])
            if final:
                kb.dma("sp", gf[:], gfin, writes=["gf"])
            h1t = [T("h1t%d" % i, [128, D]) for i in range(2)]
            sct = [T("sctb%d" % i, [128, D]) for i in range(2)]
            xT = [T("xTb%d" % i, [128, D], BF16) for i in range(2)]
            tbt = [T("tbt%d" % i, [128, 16]) for i in range(2)]
            Sg = [T("Sg%d" % i, [128, 16, 64]) for i in range(2)]
            Eg = [T("Eg%d" % i, [128, 1024], BF16) for i in range(2)]
            Gh = T("Gh", [128, 1024], BF16); G = T("G", [128, 1024])
            gl = [T("gl%d" % i, [128, 512]) for i in range(2)]
            Wb = T("Wb", [128, 1024], BF16); WT = T("WT", [128, 1024], BF16)
            ho = T("ho", [128, D]); junk = T("junkb", [128, D], BF16); ss = T("ssb", [128, 1])
            pH = [ps[0], ps[1]]; ptr = ps[2][:].bitcast(BF16); po = [ps[3], ps[4]]

            def loadB(i):
                b = i % 2
                rs = slice(i * 128, (i + 1) * 128)
                kb.dma("sp", h1t[b][:], h1_d[rs, :], reads=["h1_d"], writes=["h1t%d" % b])
                kb.dma("sp", sct[b][:], sc_d[rs, :], reads=["sc_d"], writes=["sctb%d" % b])
                kb.dma("sp", xT[b][:], xT_d[rs, :], reads=["xT_d"], writes=["xTb%d" % b])
                kb.dma("sp", tbt[b][:], tb_d[rs, :], reads=["tb_d"], writes=["tbt%d" % b])

            loadB(0)
            cnt = 0
            for i in range(NT):
                b = i % 2
                rs = slice(i * 128, (i + 1) * 128)
                if i + 1 < NT:
                    loadB(i + 1)
                for eq in range(4):
                    for hd in range(8):
                        j = cnt % 2
                        cnt += 1
                        s1 = sct[b][:, hd * 128 + 16 * eq:hd * 128 + 16 * eq + 16]
                        s2 = sct[b][:, hd * 128 + 64:hd * 128 + 128]
                        kb.tt(Sg[j][:], s1.unsqueeze(2).to_broadcast([128, 16, 64]), s2.unsqueeze(1).to_broadcast([128, 16, 64]), ALU.add,
                              ["sctb%d" % b], ["Sg%d" % j], eng="pool")
                        Sf = Sg[j][:].rearrange("p a b -> p (a b)")
                        kb.act(Eg[j][:], Sf, AF.Exp, ["Sg%d" % j, "tbt%d" % b], ["Eg%d" % j], bias=tbt[b][:, 8 + hd:9 + hd], scale=1.0)
                        if hd == 0:
                            kb.stt(G[:], Sf, tbt[b][:, hd:hd + 1], Eg[j][:], ALU.is_ge, ALU.mult, ["Sg%d" % j, "Eg%d" % j, "tbt%d" % b], ["G"])
                        else:
                            kb.stt(Gh[:], Sf, tbt[b][:, hd:hd + 1], Eg[j][:], ALU.is_ge, ALU.mult, ["Sg%d" % j, "Eg%d" % j, "tbt%d" % b], ["Gh"])
                            kb.tt(G[:], G[:], Gh[:], ALU.add, ["G", "Gh"], ["G"])
                    for g2 in range(2):
                        e0 = eq * 1024 + g2 * 512
                        for k in range(8):
                            kb.mm(pH[g2][:], xT[b][:, k * 128:(k + 1) * 128], Ub[:, k, e0:e0 + 512], k == 0, k == 7,
                                  ["xTb%d" % b, "Ub"], ["pH%d" % g2], inc=(k == 7))
                        kb.act(gl[g2][:], pH[g2][:], AF.Gelu, ["pH%d" % g2], ["gl%d" % g2])
                        kb.tt(Wb[:, g2 * 512:(g2 + 1) * 512], gl[g2][:], G[:, g2 * 512:(g2 + 1) * 512], ALU.mult, ["gl%d" % g2, "G"], ["Wb"])
                    for cc in range(8):
                        kb.tr(ptr[:, cc * 128:(cc + 1) * 128], Wb[:, cc * 128:(cc + 1) * 128], idb[:], ["Wb", "ident_b"], ["ptr"], inc=(cc == 7))
                    kb.cp(WT[:], ptr, ["ptr"], ["WT"], eng="act")
                    for dh in range(2):
                        for cc in range(8):
                            kb.mm(po[dh][:], WT[:, cc * 128:(cc + 1) * 128], Vv[:, eq * 8 + cc, dh * 512:(dh + 1) * 512],
                                  (eq == 0 and cc == 0), (eq == 3 and cc == 7), ["WT", "Vv"], ["po%d" % dh], inc=(cc == 7))
                for dh in range(2):
                    kb.tt(ho[:, dh * 512:(dh + 1) * 512], h1t[b][:, dh * 512:(dh + 1) * 512], po[dh][:], ALU.add,
                          ["h1t%d" % b, "po%d" % dh], ["ho"])
                if final:
                    _rstd(kb, ho[:], junk[:], ss[:], D, ["ho"], "f")
                    kb.stt(ho[:], ho[:], ss[:, 0:1], gf[:], ALU.mult, ALU.mult, ["ho", "fss", "gf"], ["ho"])
                kb.dma("sp", hout[rs, :], ho[:], reads=["ho"], writes=["hout"])
            kb.wait_all("sp", ["hout"])
            kb.emit()
    return nc


_CACHE = {}


def _prog(name, *args):
    key = (name,) + args
    if key not in _CACHE:
        _CACHE[key] = {"p1": build_p1, "p2": build_p2, "p3": build_p3}[name](*args)
    return _CACHE[key]


def _run(nc, in_maps):
    res = run_bass_kernel_spmd(nc, in_maps, core_ids=list(range(NCORES)))
    return res.results


def _c(a):
    return np.ascontiguousarray(a)


def _gk(g):
    return _c(np.asarray(g, np.float32).reshape(8, 128).T)


def forward(x, meta_tokens, attn_norm, w_in, attn_sinks, gla_gate_w2, gla_gate_b, swa_out_norm,
            sb_out_norm, gla_out_norm, w_out, ffn_norm, peer_w_q, peer_sub_keys, peer_u, peer_v, final_norm):
    x = np.asarray(x, np.float32)
    B, SEQ, _ = x.shape
    depth = attn_norm.shape[0]
    L = SEQ + 128
    Lp = ((L + 511) // 512) * 512
    T = B * L
    NT = (T // 128 + NCORES - 1) // NCORES
    Tp = NT * 128 * NCORES
    hfull = np.zeros((Tp, D), np.float32)
    for b in range(B):
        hfull[b * L + 112:b * L + 128] = meta_tokens
        hfull[b * L + 128:(b + 1) * L] = x[b]
    p1 = _prog("p1", NT)
    p2 = _prog("p2", Lp)
    tsl = [slice(c * NT * 128, (c + 1) * NT * 128) for c in range(NCORES)]
    bf = ml_dtypes.bfloat16
    for i in range(depth):
        w_i = _c(w_in[i]); g_i = _gk(attn_norm[i])
        r = _run(p1, [{"h": hfull[tsl[c]], "w": w_i, "g": g_i} for c in range(NCORES)])
        proj = np.concatenate([np.asarray(r[c]["proj"]).view(bf) if np.asarray(r[c]["proj"]).dtype != bf else np.asarray(r[c]["proj"])
                               for c in range(NCORES)], axis=0)
        maps = []
        for c in range(NCORES):
            b, j = c // 4, c % 4
            pb = np.zeros((Lp, IN_COLS), bf)
            pb[:L] = proj[b * L:(b + 1) * L]
            kv = j // 2
            ka = pb[:, 512 + 64 * kv:512 + 64 * kv + 64].T
            maps.append({
                "qaT": _c(pb[:, 128 * j:128 * j + 128].T), "kaT": _c(np.concatenate([ka, ka], 0)),
                "va": _c(pb[:, 640 + 64 * kv:640 + 64 * kv + 64]),
                "qbT": _c(pb[:, 768 + 64 * j:768 + 64 * j + 64].T), "kbT": _c(pb[:, 1024 + 64 * j:1024 + 64 * j + 64].T),
                "vb": _c(pb[:, 1280 + 64 * j:1280 + 64 * j + 64]),
                "qcT": _c(pb[:, 1536 + 32 * j:1536 + 32 * j + 32].T), "kcT": _c(pb[:, 1664 + 32 * j:1664 + 32 * j + 32].T),
                "vc": _c(pb[:, 1792 + 64 * j:1792 + 64 * j + 64]), "glrT": _c(pb[:, 2048:2064].T),
                "w2": _c(np.asarray(gla_gate_w2[i], np.float32)[:, 32 * j:32 * j + 32]),
                "gb": _c(np.asarray(gla_gate_b[i], np.float32)[32 * j:32 * j + 32].reshape(32, 1)),
                "sinks": _c(np.broadcast_to(np.asarray(attn_sinks[i], np.float32)[2 * j:2 * j + 2][None, :], (128, 2))),
            })
        r = _run(p2, maps)
        ofull = np.zeros((Tp, D), np.float32)
        for c in range(NCORES):
            b, j = c // 4, c % 4
            ofull[b * L:(b + 1) * L, 128 * j:128 * j + 128] = np.asarray(r[c]["oa"])[:L]
            ofull[b * L:(b + 1) * L, 512 + 64 * j:512 + 64 * j + 64] = np.asarray(r[c]["obT"]).T[:L]
            ofull[b * L:(b + 1) * L, 768 + 64 * j:768 + 64 * j + 64] = np.asarray(r[c]["oc"])[:L]
        rcfull = np.zeros((Tp, 256), bf)
        rcfull[:T] = proj[:T, 2064:2320]
        fin = (i == depth - 1)
        p3 = _prog("p3", NT, fin)
        gmix = np.concatenate([swa_out_norm[i], sb_out_norm[i], gla_out_norm[i]]).astype(np.float32)
        shared = {
            "gmix": _c(np.broadcast_to(gmix[None, :], (128, D))), "wout": _c(w_out[i]), "gffn": _gk(ffn_norm[i]),
            "wq": _c(peer_w_q[i]), "ksub": _c(np.transpose(np.asarray(peer_sub_keys[i], np.float32).reshape(16, 64, 128), (2, 0, 1))),
            "uT": _c(np.asarray(peer_u[i], np.float32).T), "v": _c(peer_v[i]),
            "gfin": _c(np.broadcast_to(np.asarray(final_norm, np.float32)[None, :], (128, D))),
        }
        r = _run(p3, [dict(shared, h=hfull[tsl[c]], o=ofull[tsl[c]], rc=rcfull[tsl[c]]) for c in range(NCORES)])
        hfull = np.concatenate([np.asarray(r[c]["hout"]) for c in range(NCORES)], axis=0)
    out = np.stack([hfull[b * L + 128:(b + 1) * L] for b in range(B)], 0)
    return np.ascontiguousarray(out.astype(np.float32))


def kernel(**inputs):
    return forward_fused(**{k: np.asarray(v) for k, v in inputs.items()})


import os as _os
_STOP = _os.environ.get("FUSED_STOP", "")
_POOL_HEADS = tuple(int(c_) for c_ in _os.environ.get("POOL_HEADS", "01234567"))
CJ = 720
RG = [[0, 1, 2, 3], [4, 5, 6, 7]]


def build_fused(Lp, depth):
    TQ = Lp // 4
    NT = TQ // 128
    NCH = Lp // 512
    NB = Lp // 128
    nc = bass.Bass("TRN2", target_bir_lowering=False)
    IN = lambda n, s, dt=F32: nc.dram_tensor(n, s, dt, kind="ExternalInput").ap()
    h0 = IN("h0", [TQ, D])
    w_in = IN("w_in", [depth, D, CJ]); g_attn = IN("g_attn", [depth, 128, 8])
    w2_i = IN("w2", [depth, 16, 32]); gb_i = IN("gb", [depth, 32, 1]); sinks_i = IN("sinks", [depth, 128, 2])
    gmix_i = IN("gmix", [depth, 128, 256]); wout_i = IN("wout", [depth, 128, 2, D])
    gffn_i = IN("gffn", [depth, 128, 8]); wq_i = IN("wq", [depth, D, 2048]); ksub_i = IN("ksub", [depth, 128, 16, 64])
    uT_i = IN("uT", [depth, D, 4096]); v_i = IN("v", [depth, 4096, D]); gfin = IN("gfin", [128, D])
    out = nc.dram_tensor("out", [TQ, D], F32, kind="ExternalOutput").ap()
    DT = lambda n, s, dt=F32: nc.dram_tensor(n, s, dt, kind="Internal").ap()
    GC = 128 * max(d for d in range(1, 5) if NT % d == 0)
    NG = TQ // GC
    xT_loc = [DT("xT_loc%d" % g, [D, GC], BF16) for g in range(NG)]
    xT_all = [DT("xT_all%d" % g, [4 * D, GC], BF16) for g in range(NG)]
    part_loc = DT("part_loc", [Lp, D]); delta_loc = DT("delta_loc", [TQ, D])
    hl = [DT("hl0", [TQ, D]), DT("hl1", [TQ, D])]
    h1_d = DT("h1_d", [TQ, D]); sc_d = DT("sc_d", [TQ, D]); tb_d = DT("tb_d", [TQ, 16]); xT_d = DT("xT_d", [TQ, D], BF16)

    with ExitStack() as st:
        kb = KB(nc, st)
        kb.excl.update(["pT", "pX", "pA", "pB", "pO", "pz0", "pz1", "pc0", "pc1", "pq0", "pq1", "psc0", "psc1",
                        "pH0", "pH1", "ptr", "po0", "po1"])
        ps = [st.enter_context(nc.psum_tensor("ps%d" % i, [128, 512], F32)) for i in range(8)]
        T0 = lambda name, shape, dt=F32: st.enter_context(nc.sbuf_tensor(name, shape, dt))
        idf, idb = _ident(kb, T0)
        stage = [T0("stage%d" % i, [128, 1024]) for i in range(2)]
        scnt = [0]

        def load_w(dst, src, rows_k, cols, key, gt=None):
            for k in range(rows_k):
                for c0 in range(0, cols, 1024):
                    cw = min(1024, cols - c0)
                    si = scnt[0] % 2
                    scnt[0] += 1
                    sk = "stage%d" % si
                    kb.dma("sp", stage[si][:, 0:cw], src[k * 128:(k + 1) * 128, c0:c0 + cw], writes=[sk])
                    if gt is not None:
                        kb.ts(dst[:, k, c0:c0 + cw], stage[si][:, 0:cw], gt[:, k:k + 1], ALU.mult, [sk, "gt"], [key])
                    else:
                        kb.cp(dst[:, k, c0:c0 + cw], stage[si][:, 0:cw], [sk], [key], eng="pool")

        def phase_end():
            kb.barrier()
            kb.emit()

        for li in range(depth):
            hsrc = h0 if li == 0 else hl[(li - 1) % 2]
            hdst = out if li == depth - 1 else hl[li % 2]
            final = (li == depth - 1)
            with ExitStack() as sa:
                T = lambda name, shape, dt=F32, _p="L%dA_" % li: sa.enter_context(nc.sbuf_tensor(_p + name, shape, dt))
                ht = [T("ht%d" % i, [128, D]) for i in range(2)]
                junk = T("junk", [128, D], BF16); ss = T("ss", [128, 1])
                xn = T("xn", [128, D], BF16); xnT = [T("xnT%d" % i, [128, D], BF16) for i in range(2)]
                pT = ps[0][:].bitcast(BF16)
                xv = [x_.rearrange("(k p) t -> p k t", p=128) for x_ in xT_loc]
                for i in range(NT):
                    b = i % 2
                    kb.dma("sp", ht[b][:], hsrc[i * 128:(i + 1) * 128, :], writes=["ht%d" % b])
                    _rstd(kb, ht[b][:], junk[:], ss[:], D, ["ht%d" % b], "a")
                    kb.ts(xn[:], ht[b][:], ss[:, 0:1], ALU.mult, ["ht%d" % b, "ass"], ["xn"])
                    for k in range(8):
                        kb.tr(pT[:, k * 128:(k + 1) * 128], xn[:, k * 128:(k + 1) * 128], idb[:], ["xn", "ident_b"], ["pT"], inc=(k == 7))
                    kb.cp(xnT[b][:], pT, ["pT"], ["xnT%d" % b], eng="act")
                    g_, o_ = (i * 128) // GC, (i * 128) % GC
                    kb.dma("pool", xv[g_][:, :, o_:o_ + 128], xnT[b][:].rearrange("p (k t) -> p k t", t=128),
                           reads=["xnT%d" % b], writes=["xT_loc"])
                kb.barrier()
                for g_ in range(NG):
                    kb.coll("AllGather", ALU.bypass, RG, xT_loc[g_], xT_all[g_], ["xT_loc"], ["xT_all"])
                phase_end()
            if _STOP == "A":
                break
            with ExitStack() as sb_:
                T = lambda name, shape, dt=F32, _p="L%dB_" % li: sb_.enter_context(nc.sbuf_tensor(_p + name, shape, dt))
                gt = T("gt", [128, 8])
                kb.dma("sp", gt[:], g_attn[li], writes=["gt"])
                Wj = T("Wj", [128, 8, 768], BF16)
                load_w(Wj, w_in[li], 8, CJ, "Wj", gt=gt)
                Wo = T("Wo", [128, 2, D], BF16)
                for kc in range(2):
                    si = scnt[0] % 2; scnt[0] += 1
                    kb.dma("sp", stage[si][:], wout_i[li][:, kc, :], writes=["stage%d" % si])
                    kb.cp(Wo[:, kc, :], stage[si][:], ["stage%d" % si], ["Wo"], eng="pool")
                gm = T("gm", [128, 256]); kb.dma("sp", gm[:], gmix_i[li], writes=["gm"])
                m_gen = T("m_gen", [128, 256]); m_n0 = T("m_n0", [128, 256]); m_n1 = T("m_n1", [128, 256])
                for m, nm, extra in ((m_gen, "m_gen", None), (m_n0, "m_n0", -240), (m_n1, "m_n1", -112)):
                    kb.memset(m[:], 0.0, [nm])
                    kb.asel(m[:], ALU.is_ge, NEG, -1, -1, [[1, 256]], nm)
                    kb.asel(m[:], ALU.is_ge, NEG, 128, 1, [[-1, 256]], nm)
                    if extra is not None:
                        kb.asel(m[:], ALU.is_ge, NEG, extra, 0, [[1, 256]], nm)
                sbm_f = T("sbm_f", [128, 512])
                sbm = [T("sbm%d" % d, [128, 512], BF16) for d in range(4)]
                for d in range(4):
                    kb.memset(sbm_f[:], 1.0, ["sbm_f"])
                    kb.asel(sbm_f[:], ALU.is_ge, 0.0, -1 - 128 * d, -1, [[1, 512]], "sbm_f")
                    kb.cp(sbm[d][:], sbm_f[:], ["sbm_f"], ["sbm%d" % d], eng="pool")
                ntri_f = T("ntri_f", [128, 128]); ntri = T("ntri", [128, 128], BF16); nones = T("nones", [128, 128], BF16)
                kb.memset(ntri_f[:], -1.0, ["ntri_f"])
                kb.asel(ntri_f[:], ALU.is_ge, 0.0, 0, 1, [[-1, 128]], "ntri_f")
                kb.cp(ntri[:], ntri_f[:], ["ntri_f"], ["ntri"], eng="pool")
                kb.memset(nones[:], -1.0, ["nones"])
                mle = T("mle", [128, 128])
                kb.memset(mle[:], 1.0, ["mle"])
                kb.asel(mle[:], ALU.is_ge, 0.0, 0, -1, [[1, 128]], "mle")
                rmask = T("rmask", [32, 512])
                kb.memset(rmask[:], 1.0, ["rmask"])
                for b4 in range(4):
                    kb.memset(rmask[:, b4 * 128:b4 * 128 + 1], 0.0, ["rmask"])
                w2f = T("w2f", [16, 32]); w2b = T("w2b", [16, 32], BF16); gbt = T("gbt", [32, 1]); sk_t = T("sk_t", [128, 2])
                kb.dma("sp", w2f[:], w2_i[li], writes=["w2f"]); kb.dma("sp", gbt[:], gb_i[li], writes=["gbt"])
                kb.dma("sp", sk_t[:], sinks_i[li], writes=["sk"])
                kb.cp(w2b[:], w2f[:], ["w2f"], ["w2b"])
                kb.ts(gbt[:], gbt[:], -1.0, ALU.mult, ["gbt"], ["gbt"])
                KbT = T("KbT", [64, Lp], BF16); Vb = T("Vb", [128, NB, 64], BF16)
                S = T("S", [32, 64]); Sb = T("Sb", [32, 64], BF16)
                kb.memset(S[:], 0.0, ["S"]); kb.memset(Sb[:], 0.0, ["Sb"])
                xc = [T("xc%d" % i, [128, 8, 512], BF16) for i in range(2)]
                qa_c = [T("qa_c%d" % i, [128, 512], BF16) for i in range(2)]
                ka_c = [T("ka_c%d" % i, [128, 640], BF16) for i in range(2)]
                va_c = [T("va_c%d" % i, [128, 5, 64], BF16) for i in range(2)]
                qs_c = [T("qs_c%d" % i, [64, 512], BF16) for i in range(2)]
                qc_c = [T("qc_c%d" % i, [32, 512], BF16) for i in range(2)]
                kc_c = [T("kc_c%d" % i, [32, 512], BF16) for i in range(2)]
                vc_c = [T("vc_c%d" % i, [128, 4, 64], BF16) for i in range(2)]
                rc_c = [T("rc_c%d" % i, [128, 4, 64]) for i in range(2)]
                gl_c = [T("gl_c%d" % i, [16, 512], BF16) for i in range(2)]
                mo = [T("mo%d" % i, [128, 4, 256]) for i in range(2)]
                sm = T("sm", [128, 256]); pexp = T("pexp", [128, 256], BF16); pTs = T("pTs", [128, 256], BF16)
                st8 = T("st8", [128, 8])
                ge = T("ge", [32, 512]); gsp = T("gsp", [32, 512]); gcs = T("gcs", [32, 512])
                geq = T("geq", [32, 512]); gek = T("gek", [32, 512])
                qt = T("qt", [32, 512], BF16); kt = T("kt", [32, 512], BF16)
                scb = T("scb", [128, 128], BF16); ktm = T("ktm", [128, 32], BF16)
                stmp = T("stmp", [32, 64])
                e_t = [T("e_t%d" % i, [128, 512]) for i in range(2)]
                sp_t = [[T("sp_t%d_%d" % (pp_, i), [128, 512], BF16) for i in range(2)] for pp_ in range(2)]
                a_t = [T("a_t%d" % i, [128, 512], BF16) for i in range(2)]
                R = T("R", [128, 512], BF16)
                ob_t = T("ob_t", [64, 512])
                sq4 = T("sq4", [128, 1024]); s16 = T("s16", [128, 16]); m14 = T("m14", [128, 1024]); sil4 = T("sil4", [128, 4, 64])
                mixb4 = T("mixb4", [128, 1024], BF16); mixT4 = T("mixT4", [128, 1024], BF16)
                pt = [T("pt%d" % i, [128, D]) for i in range(2)]
                pz = [ps[0], ps[1]]; pc = [ps[2], ps[3]]; pO = ps[4]
                pA = ps[5]; pX = ps[6]; pB = ps[7]
                pT_bf = pB[:].bitcast(BF16)

                def pieces(c0, n):
                    t = c0
                    while t < c0 + n:
                        q = t // TQ; tl = t % TQ; g_ = tl // GC; o_ = tl % GC; m = min(c0 + n - t, GC - o_)
                        yield q, g_, o_, t - c0, m
                        t += m

                def load_xc(c):
                    i = c % 2
                    for q, g_, o_, off, m in pieces(c * 512, 512):
                        kb.dma("sp", xc[i][:, :, off:off + m],
                               xT_all[g_][q * D:(q + 1) * D, o_:o_ + m].rearrange("(k p) t -> p k t", p=128), writes=["xc%d" % i])

                def inproj(c):
                    i = c % 2
                    xk = "xc%d" % i
                    if c == 0:
                        kb.memset(ka_c[i][:, 0:128], 0.0, ["ka_c%d" % i])
                        kb.memset(va_c[i][:, 0, :], 0.0, ["va_c%d" % i])
                    else:
                        kb.cp(ka_c[i][:, 0:128], ka_c[1 - i][:, 512:640], ["ka_c%d" % (1 - i)], ["ka_c%d" % i], eng="pool")
                        kb.cp(va_c[i][:, 0, :], va_c[1 - i][:, 4, :], ["va_c%d" % (1 - i)], ["va_c%d" % i], eng="pool")
                    groups = [(0, 128, qa_c[i][:], "qa_c%d" % i, None), (128, 128, ka_c[i][:, 128:640], "ka_c%d" % i, None),
                              (256, 64, qs_c[i][:], "qs_c%d" % i, 0.125), (320, 64, KbT[:, c * 512:(c + 1) * 512], "KbT", None),
                              (384, 32, qc_c[i][:], "qc_c%d" % i, None), (416, 32, kc_c[i][:], "kc_c%d" % i, None),
                              (448, 16, gl_c[i][:], "gl_c%d" % i, None)]
                    for gi, (c0, rows, dst, dk, scale) in enumerate(groups):
                        mr = max(rows, 32)
                        pI, pIk = ((pX, "pX"), (pA, "pA"))[gi % 2]
                        for k in range(8):
                            kb.mm(pI[0:mr, :], Wj[:, k, c0:c0 + mr], xc[i][:, k, :], k == 0, k == 7, ["Wj", xk], [pIk], inc=(k == 7))
                        if scale is not None:
                            kb.ts(dst, pI[0:rows, :], scale, ALU.mult, [pIk], [dk])
                        elif gi % 2:
                            kb.cp(dst, pI[0:rows, :], [pIk], [dk], eng="act")
                        else:
                            kb.cp(dst, pI[0:rows, :], [pIk], [dk])
                    for blk in (range(4) if _STOP != "B1f" else []):
                        n = 4 * c + blk
                        pI, pIk = ((pA, "pA"), (pX, "pX"))[blk % 2]
                        for k in range(8):
                            kb.mm(pI[:, 0:256], xc[i][:, k, blk * 128:(blk + 1) * 128], Wj[:, k, 464:720], k == 0, k == 7, ["Wj", xk], [pIk], inc=(k == 7))
                        ev = "act" if blk % 2 else "dve"
                        kb.cp(va_c[i][:, blk + 1, :], pI[:, 0:64], [pIk], ["va_c%d" % i], eng=ev)
                        kb.cp(Vb[:, n, :], pI[:, 64:128], [pIk], ["Vb"], eng=ev)
                        kb.cp(vc_c[i][:, blk, :], pI[:, 128:192], [pIk], ["vc_c%d" % i], eng=ev)
                        kb.cp(rc_c[i][:, blk, :], pI[:, 192:256], [pIk], ["rc_c%d" % i], eng=ev)

                def swa(c):
                    i = c % 2
                    mk_ = "mo%d" % i
                    for blk in range(4):
                        n = 4 * c + blk
                        msk = m_n0 if n == 0 else (m_n1 if n == 1 else m_gen)
                        mk = "m_n0" if n == 0 else ("m_n1" if n == 1 else "m_gen")
                        for hh in range(2):
                            hs = slice(hh * 64, (hh + 1) * 64)
                            kb.mm(pA[:, 0:256], qa_c[i][hs, blk * 128:(blk + 1) * 128], ka_c[i][hs, blk * 128:blk * 128 + 256],
                                  True, True, ["qa_c%d" % i, "ka_c%d" % i], ["pA"])
                            kb.stt(sm[:], pA[:, 0:256], 0.125, msk[:], ALU.mult, ALU.add, ["pA", mk], ["sm"])
                            kb.op("dve", lambda e: e.tensor_reduce(out=st8[:, 0:1], in_=sm[:], axis=AX.X, op=ALU.max), ["sm"], ["st8"])
                            kb.tt(st8[:, 0:1], st8[:, 0:1], sk_t[:, hh:hh + 1], ALU.max, ["st8", "sk"], ["st8"])
                            kb.ts(st8[:, 1:2], st8[:, 0:1], -1.0, ALU.mult, ["st8"], ["st8"])
                            kb.act(pexp[:], sm[:], AF.Exp, ["sm", "st8"], ["pexp", "st8"], bias=st8[:, 1:2], scale=1.0, accum_out=st8[:, 2:3])
                            kb.act(st8[:, 3:4], sk_t[:, hh:hh + 1], AF.Exp, ["sk", "st8"], ["st8"], bias=st8[:, 1:2], scale=1.0)
                            kb.tt(st8[:, 4:5], st8[:, 2:3], st8[:, 3:4], ALU.add, ["st8"], ["st8"])
                            kb.op("dve", lambda e: e.reciprocal(out=st8[:, 5:6], in_=st8[:, 4:5]), ["st8"], ["st8"])
                            kb.tr(pT_bf[:, 0:128], pexp[:, 0:128], idb[:], ["pexp", "ident_b"], ["pB"], inc=False)
                            kb.tr(pT_bf[:, 128:256], pexp[:, 128:256], idb[:], ["pexp", "ident_b"], ["pB"])
                            kb.cp(pTs[:], pT_bf[:, 0:256], ["pB"], ["pTs"], eng="act")
                            kb.mm(pA[:, 256:320], pTs[:, 0:128], va_c[i][:, blk, :], True, False, ["pTs", "va_c%d" % i], ["pA"], inc=False)
                            kb.mm(pA[:, 256:320], pTs[:, 128:256], va_c[i][:, blk + 1, :], False, True, ["pTs", "va_c%d" % i], ["pA"])
                            kb.ts(mo[i][:, blk, hh * 64:(hh + 1) * 64], pA[:, 256:320], st8[:, 5:6], ALU.mult, ["pA", "st8"], [mk_])

                def gla(c):
                    i = c % 2
                    kb.mm(pX[0:32, :], w2b[:], gl_c[i][:], True, True, ["w2b", "gl_c%d" % i], ["pX"])
                    kb.act(ge[:], pX[0:32, :], AF.Exp, ["pX", "gbt"], ["ge"], bias=gbt[:, 0:1], scale=-1.0)
                    kb.act(gsp[:], ge[:], AF.Ln, ["ge"], ["gsp"], bias=1.0, scale=1.0)
                    kb.op("dve", lambda e: e.tensor_tensor_scan(out=gcs[:], data0=rmask[:], data1=gsp[:], initial=0.0, op0=ALU.mult, op1=ALU.add),
                          ["rmask", "gsp"], ["gcs"])
                    kb.act(geq[:], gcs[:], AF.Exp, ["gcs"], ["geq"], scale=-1.0 / 16.0)
                    kb.act(gek[:], gcs[:], AF.Exp, ["gcs"], ["gek"], scale=1.0 / 16.0)
                    kb.stt(qt[:], qc_c[i][:], 32.0 ** -0.5, geq[:], ALU.mult, ALU.mult, ["qc_c%d" % i, "geq"], ["qt"])
                    kb.tt(kt[:], kc_c[i][:], gek[:], ALU.mult, ["kc_c%d" % i, "gek"], ["kt"])
                    for blk in range(4):
                        bs = slice(blk * 128, (blk + 1) * 128)
                        kb.mm(pA[:, 384:512], kt[:, bs], qt[:, bs], True, True, ["kt", "qt"], ["pA"])
                        kb.tt(scb[:], pA[:, 384:512], mle[:], ALU.mult, ["pA", "mle"], ["scb"])
                        kb.tr(pT_bf[:, 512:544], kt[:, bs], idb[0:32, 0:32], ["kt", "ident_b"], ["pB"])
                        kb.cp(ktm[:], pT_bf[:, 512:544], ["pB"], ["ktm"], eng="act")
                        kb.mm(pA[:, 320:384], scb[:], vc_c[i][:, blk, :], True, False, ["scb", "vc_c%d" % i], ["pA"], inc=False)
                        kb.mm(pA[:, 320:384], qt[:, bs], Sb[:], False, True, ["qt", "Sb"], ["pA"])
                        kb.cp(mo[i][:, blk, 192:256], pA[:, 320:384], ["pA"], ["mo%d" % i], eng="act")
                        kb.mm(pB[0:32, 384:448], ktm[:], vc_c[i][:, blk, :], True, True, ["ktm", "vc_c%d" % i], ["pB"])
                        kb.tt(stmp[:], pB[0:32, 384:448], S[:], ALU.add, ["pB", "S"], ["stmp"])
                        kb.ts(S[:], stmp[:], geq[:, blk * 128 + 127:blk * 128 + 128], ALU.mult, ["stmp", "geq"], ["S"])
                        kb.cp(Sb[:], S[:], ["S"], ["Sb"])

                def sbk(c):
                    i = c % 2
                    nkb = 4 * c + 4
                    npairs = nkb // 2
                    qk = "qs_c%d" % i

                    def kof(p, j):
                        return nkb - 1 - (2 * p + j)

                    def zmm(p):
                        for j in range(2):
                            kblk = kof(p, j)
                            kb.mm(pz[j][:], KbT[:, kblk * 128:(kblk + 1) * 128], qs_c[i][:], True, True, ["KbT", qk], ["pz%d" % j])

                    zmm(0)
                    for p in range(npairs + 1):
                        pp = p % 2
                        if p < npairs:
                            for j in range(2):
                                kb.act(e_t[j][:], pz[j][:], AF.Exp, ["pz%d" % j], ["e_t%d" % j])
                            for j in range(2):
                                kb.act(sp_t[pp][j][:], e_t[j][:], AF.Ln, ["e_t%d" % j], ["sp_t%d_%d" % (pp, j)], bias=1.0, scale=1.0)
                            for j in range(2):
                                dg = kof(p, j) - 4 * c
                                if dg >= 0:
                                    kb.tt(sp_t[pp][j][:], sp_t[pp][j][:], sbm[dg][:], ALU.mult, ["sp_t%d_%d" % (pp, j), "sbm%d" % dg], ["sp_t%d_%d" % (pp, j)])
                        if p >= 1:
                            q = p - 1
                            qq = q % 2
                            first, lastp = (q == 0), (q == npairs - 1)
                            for j in range(2):
                                kblk = kof(q, j)
                                sk_ = "sp_t%d_%d" % (qq, j)
                                kb.mm(pc[j][:], ntri[:], sp_t[qq][j][:], True, False, ["ntri", sk_], ["pc%d" % j], inc=False)
                                if j == 1:
                                    kb.mm(pc[j][:], nones[:], sp_t[qq][0][:], False, False, ["nones", "sp_t%d_0" % qq], ["pc%d" % j], inc=False)
                                if not first:
                                    kb.mm(pc[j][:], nones[:], R[:], False, False, ["nones", "R"], ["pc%d" % j], inc=False)
                                kb.mm(pc[j][:], KbT[:, kblk * 128:(kblk + 1) * 128], qs_c[i][:], False, True, ["KbT", qk], ["pc%d" % j])
                        if p + 1 < npairs:
                            zmm(p + 1)
                        if p >= 1:
                            if not lastp:
                                if first:
                                    kb.tt(R[:], sp_t[qq][0][:], sp_t[qq][1][:], ALU.add, ["sp_t%d_0" % qq, "sp_t%d_1" % qq], ["R"])
                                else:
                                    kb.tt(R[:], R[:], sp_t[qq][0][:], ALU.add, ["R", "sp_t%d_0" % qq], ["R"])
                                    kb.tt(R[:], R[:], sp_t[qq][1][:], ALU.add, ["R", "sp_t%d_1" % qq], ["R"])
                            for j in range(2):
                                kb.act(a_t[j][:], pc[j][:], AF.Exp, ["pc%d" % j], ["a_t%d" % j])
                            for j in range(2):
                                dg = kof(q, j) - 4 * c
                                if dg >= 0:
                                    kb.tt(a_t[j][:], a_t[j][:], sbm[dg][:], ALU.mult, ["a_t%d" % j, "sbm%d" % dg], ["a_t%d" % j])
                            for j in range(2):
                                kblk = kof(q, j)
                                kb.mm(pO[0:64, :], Vb[:, kblk, :], a_t[j][:], (q == 0 and j == 0), (kblk == 0), ["Vb", "a_t%d" % j], ["pO"])
                    kb.cp(ob_t[:], pO[0:64, :], ["pO"], ["ob_t"])

                def post(c):
                    i = c % 2
                    mk_ = "mo%d" % i
                    for blk in range(4):
                        kb.tr(pA[:, blk * 64:(blk + 1) * 64], ob_t[:, blk * 128:(blk + 1) * 128], idf[0:64, 0:64], ["ob_t", "ident_f"], ["pA"], inc=(blk == 3))
                    kb.cp(mo[i][:, :, 128:192], pA[:, 0:256].rearrange("p (b d) -> p b d", d=64), ["pA"], [mk_], eng="act")
                    mof = mo[i][:].rearrange("p b c -> p (b c)")
                    kb.act(sq4[:], mof, AF.Square, [mk_], ["sq4"])
                    kb.op("dve", lambda e: e.tensor_reduce(out=s16[:], in_=sq4[:].rearrange("p (h d) -> p h d", d=64), axis=AX.X, op=ALU.add),
                          ["sq4"], ["s16"])
                    kb.ts(s16[:], s16[:], 1.0 / 64, ALU.mult, ["s16"], ["s16"], s2=EPS, op1=ALU.add)
                    kb.act(s16[:], s16[:], AF.Sqrt, ["s16"], ["s16"])
                    kb.op("dve", lambda e: e.reciprocal(out=s16[:], in_=s16[:]), ["s16"], ["s16"])
                    kb.tt(m14[:].rearrange("p (h d) -> p h d", d=64), mof.rearrange("p (h d) -> p h d", d=64),
                          s16[:].unsqueeze(2).to_broadcast([128, 16, 64]), ALU.mult, [mk_, "s16"], ["m14"])
                    kb.act(sil4[:], rc_c[i][:], AF.Silu, ["rc_c%d" % i], ["sil4"])
                    m14v = m14[:].rearrange("p (b c) -> p b c", c=256)
                    kb.tt(m14v[:, :, 192:256], m14v[:, :, 192:256], sil4[:], ALU.mult, ["m14", "sil4"], ["m14"])
                    kb.tt(mixb4[:].rearrange("p (b c) -> p b c", c=256), m14v, gm[:].unsqueeze(1).to_broadcast([128, 4, 256]), ALU.mult,
                          ["m14", "gm"], ["mixb4"])
                    for t8 in range(8):
                        kb.tr(pT_bf[:, t8 * 128:(t8 + 1) * 128], mixb4[:, t8 * 128:(t8 + 1) * 128], idb[:], ["mixb4", "ident_b"], ["pB"], inc=(t8 == 7))
                    kb.cp(mixT4[:], pT_bf, ["pB"], ["mixT4"], eng="act")
                    for blk in range(4):
                        n = 4 * c + blk
                        pk = "pt%d" % (n % 2)
                        for dh in range(2):
                            pbank, pkey = (pA, "pA") if dh == 0 else (pX, "pX")
                            for kc in range(2):
                                kb.mm(pbank[:], mixT4[:, (2 * blk + kc) * 128:(2 * blk + kc + 1) * 128], Wo[:, kc, dh * 512:(dh + 1) * 512], kc == 0, kc == 1,
                                      ["mixT4", "Wo"], [pkey], inc=(kc == 1))
                            kb.cp(pt[n % 2][:, dh * 512:(dh + 1) * 512], pbank[:], [pkey], [pk], eng=("act" if dh else "dve"))
                        kb.dma("pool", part_loc[n * 128:(n + 1) * 128, :], pt[n % 2][:], reads=[pk], writes=["part_loc"])

                load_xc(0)
                for c in range(NCH):
                    if _STOP == "B0":
                        continue
                    if c + 1 < NCH:
                        load_xc(c + 1)
                    if _STOP == "B0x":
                        continue
                    inproj(c)
                    if _STOP in ("B1", "B1f"):
                        continue
                    swa(c)
                    gla(c)
                    sbk(c)
                    if _STOP == "B2":
                        continue
                    post(c)
                kb.barrier()
                if _STOP not in ("B0", "B0x", "B1", "B1f", "B2", "B3"):
                    kb.coll("ReduceScatter", ALU.add, RG, part_loc, delta_loc, ["part_loc"], ["delta_loc"])
                phase_end()
            if _STOP in ("B", "B0", "B0x", "B1", "B1f", "B2", "B3"):
                break
            with ExitStack() as sc_:
                T = lambda name, shape, dt=F32, _p="L%dC_" % li: sc_.enter_context(nc.sbuf_tensor(_p + name, shape, dt))
                gft = T("gft", [128, 8])
                kb.dma("sp", gft[:], gffn_i[li], writes=["gt"])
                Wq = T("Wq", [128, 8, 2048], BF16); Ks = T("Ks", [128, 16, 64], BF16); ksf = T("ksf", [128, 16, 64])
                kb.dma("sp", ksf[:], ksub_i[li], writes=["ksf"])
                kb.cp(Ks[:], ksf[:], ["ksf"], ["Ks"])
                load_w(Wq, wq_i[li], 8, 2048, "Wq", gt=gft)
                ht = [T("ht%d" % i, [128, D]) for i in range(2)]
                dt_ = [T("dt%d" % i, [128, D]) for i in range(2)]
                h1 = T("h1", [128, D]); junk = T("junk", [128, D], BF16); ss = T("ss", [128, 1])
                xn = T("xn", [128, D], BF16); xnT = T("xnT", [128, D], BF16)
                qT = T("qT", [128, 16, 128], BF16); sct = T("sct", [128, D])
                t1 = T("t1", [128, 8, 16]); t2 = T("t2", [128, 8, 16]); wk1 = T("wk1", [128, 16, 64]); wk2 = T("wk2", [128, 8, 256])
                cand = T("cand", [128, 8, 256]); c8a = T("c8a", [128, 8, 8]); c8b = T("c8b", [128, 8, 8])
                csh = T("csh", [128, 8, 256]); ec = T("ec", [128, 8, 256]); mk8 = T("mk8", [128, 8, 256])
                Z = T("Z", [128, 8]); tb = T("tb", [128, 16])
                pT = ps[0][:].bitcast(BF16)
                for i in range(NT):
                    b = i % 2
                    rs = slice(i * 128, (i + 1) * 128)
                    kb.dma("sp", ht[b][:], hsrc[rs, :], writes=["ht%d" % b])
                    kb.dma("sp", dt_[b][:], delta_loc[rs, :], writes=["dt%d" % b])
                    kb.tt(h1[:], ht[b][:], dt_[b][:], ALU.add, ["ht%d" % b, "dt%d" % b], ["h1"])
                    kb.dma("pool", h1_d[rs, :], h1[:], reads=["h1"], writes=["h1_d"])
                    _rstd(kb, h1[:], junk[:], ss[:], D, ["h1"], "b")
                    kb.ts(xn[:], h1[:], ss[:, 0:1], ALU.mult, ["h1", "bss"], ["xn"])
                    for k in range(8):
                        kb.tr(pT[:, k * 128:(k + 1) * 128], xn[:, k * 128:(k + 1) * 128], idb[:], ["xn", "ident_b"], ["pT"], inc=(k == 7))
                    kb.cp(xnT[:], pT, ["pT"], ["xnT"], eng="act")
                    kb.dma("pool", xT_d[rs, :], xnT[:], reads=["xnT"], writes=["xT_d"])
                    for cg in range(4):
                        pq = ps[3 + cg % 2]
                        for cc in range(4):
                            cidx = cg * 4 + cc
                            for k in range(8):
                                kb.mm(pq[:, cc * 128:(cc + 1) * 128], Wq[:, k, cidx * 128:(cidx + 1) * 128], xnT[:, k * 128:(k + 1) * 128],
                                      k == 0, k == 7, ["Wq", "xnT"], ["pq%d" % (cg % 2)], inc=(k == 7 and cc == 3))
                        kb.cp(qT[:, cg * 4:(cg + 1) * 4, :], pq[:].rearrange("p (c t) -> p c t", t=128), ["pq%d" % (cg % 2)], ["qT"],
                              eng=("act" if cg % 2 else "dve"))
                    for cidx in range(16):
                        pscb = ps[5 + cidx // 8]
                        kb.mm(pscb[:, (cidx % 8) * 64:(cidx % 8 + 1) * 64], qT[:, cidx, :], Ks[:, cidx, :], True, True, ["qT", "Ks"],
                              ["psc%d" % (cidx // 8)], inc=(cidx % 8 == 7))
                    kb.cp(sct[:, 0:512], ps[5][:], ["psc0"], ["sct"], eng="act")
                    kb.cp(sct[:, 512:1024], ps[6][:], ["psc1"], ["sct"], eng="dve")
                    kb.dma("pool", sc_d[rs, :], sct[:], reads=["sct"], writes=["sc_d"])
                    chains = [(hd, side, (t1, t2)[side]) for hd in range(8) for side in range(2)]
                    tkeys = ["tt%d_%d" % (side, hd) for hd, side, _ in chains]
                    for ci_, (hd, side, tt_) in enumerate(chains):
                        sv = sct[:, hd * 128 + side * 64:hd * 128 + side * 64 + 64]
                        kb.op("dve", lambda e, o_=tt_[:, hd, 0:8], i_=sv: e.max(out=o_, in_=i_), ["sct"], [tkeys[ci_]])
                    for ci_, (hd, side, tt_) in enumerate(chains):
                        sv = sct[:, hd * 128 + side * 64:hd * 128 + side * 64 + 64]
                        kb.op("dve", lambda e, o_=wk1[:, ci_, :], r_=tt_[:, hd, 0:8], i_=sv: e.match_replace(out=o_, in_to_replace=r_, in_values=i_, imm_value=-1e30),
                              ["sct", tkeys[ci_]], ["wk1_%d" % ci_])
                    for ci_, (hd, side, tt_) in enumerate(chains):
                        kb.op("dve", lambda e, o_=tt_[:, hd, 8:16], i_=wk1[:, ci_, :]: e.max(out=o_, in_=i_), ["wk1_%d" % ci_], [tkeys[ci_]])
                    kb.tt(cand[:].rearrange("p h (a b) -> p h a b", b=16), t1[:].unsqueeze(3).to_broadcast([128, 8, 16, 16]),
                          t2[:].unsqueeze(2).to_broadcast([128, 8, 16, 16]), ALU.add, tkeys, ["cand"])
                    ckeys = ["c8_%d" % hd for hd in range(8)]
                    for hd in range(8):
                        kb.op("dve", lambda e, o_=c8a[:, hd, :], i_=cand[:, hd, :]: e.max(out=o_, in_=i_), ["cand"], [ckeys[hd]])
                    for hd in range(8):
                        kb.op("dve", lambda e, o_=wk2[:, hd, :], r_=c8a[:, hd, :], i_=cand[:, hd, :]: e.match_replace(out=o_, in_to_replace=r_, in_values=i_, imm_value=-1e30),
                              ["cand", ckeys[hd]], ["wk2_%d" % hd])
                    for hd in range(8):
                        kb.op("dve", lambda e, o_=c8b[:, hd, :], i_=wk2[:, hd, :]: e.max(out=o_, in_=i_), ["wk2_%d" % hd], [ckeys[hd]])
                    kb.tt(csh[:], cand[:], c8a[:, :, 0:1].to_broadcast([128, 8, 256]), ALU.subtract, ["cand"] + ckeys, ["csh"])
                    kb.act(ec[:], csh[:], AF.Exp, ["csh"], ["ec"])
                    kb.tt(mk8[:], cand[:], c8b[:, :, 7:8].to_broadcast([128, 8, 256]), ALU.is_ge, ["cand"] + ckeys, ["mk8"])
                    kb.tt(ec[:], ec[:], mk8[:], ALU.mult, ["ec", "mk8"], ["ec"])
                    kb.op("dve", lambda e: e.tensor_reduce(out=Z[:], in_=ec[:], axis=AX.X, op=ALU.add), ["ec"], ["Z"])
                    kb.act(Z[:], Z[:], AF.Ln, ["Z"], ["Z"])
                    kb.cp(tb[:, 0:8], c8b[:, :, 7], ckeys, ["tb"])
                    kb.stt(tb[:, 8:16], c8a[:, :, 0], -1.0, Z[:], ALU.mult, ALU.subtract, ckeys + ["Z"], ["tb"])
                    kb.dma("pool", tb_d[rs, :], tb[:], reads=["tb"], writes=["tb_d"])
                phase_end()
            if _STOP == "C":
                break
            with ExitStack() as sd_:
                T = lambda name, shape, dt=F32, _p="L%dD_" % li: sd_.enter_context(nc.sbuf_tensor(_p + name, shape, dt))
                gft = T("gft", [128, 8])
                kb.dma("sp", gft[:], gffn_i[li], writes=["gt"])
                Ub = T("Ub", [128, 8, 4096], BF16); Vv = T("Vv", [128, 32, D], BF16)
                load_w(Ub, uT_i[li], 8, 4096, "Ub", gt=gft)
                load_w(Vv, v_i[li], 32, D, "Vv")
                gf = T("gf", [128, D])
                if final:
                    kb.dma("sp", gf[:], gfin, writes=["gf"])
                h1t = [T("h1t%d" % i, [128, D]) for i in range(2)]
                sct = [T("sctb%d" % i, [128, D]) for i in range(2)]
                xT = [T("xTb%d" % i, [128, D], BF16) for i in range(2)]
                tbt = [T("tbt%d" % i, [128, 16]) for i in range(2)]
                NBG = 4
                Sg = [T("Sg%d" % i, [128, 16, 64]) for i in range(NBG)]
                Eg = [T("Eg%d" % i, [128, 1024], BF16) for i in range(NBG)]
                Gh = [T("Gh%d" % i, [128, 1024], BF16) for i in range(2)]; G = T("G", [128, 1024], BF16)
                gl = [T("gl%d" % i, [128, 512]) for i in range(2)]
                Wb = T("Wb", [128, 1024], BF16); WT = T("WT", [128, 1024], BF16)
                ho = T("ho", [128, D]); junk = T("junkb", [128, D], BF16); ss = T("ssb", [128, 1])
                pH = [ps[0], ps[1]]; ptr = ps[2][:].bitcast(BF16); po = [ps[3], ps[4]]

                def loadB(i):
                    b = i % 2
                    rs = slice(i * 128, (i + 1) * 128)
                    kb.dma("sp", h1t[b][:], h1_d[rs, :], writes=["h1t%d" % b])
                    kb.dma("sp", sct[b][:], sc_d[rs, :], writes=["sctb%d" % b])
                    kb.dma("sp", xT[b][:], xT_d[rs, :], writes=["xTb%d" % b])
                    kb.dma("sp", tbt[b][:], tb_d[rs, :], writes=["tbt%d" % b])

                loadB(0)
                cnt = 0
                for i in range(NT):
                    b = i % 2
                    rs = slice(i * 128, (i + 1) * 128)
                    if i + 1 < NT:
                        loadB(i + 1)
                    for eq in range(4):
                        deferred = None
                        for hd in range(8):
                            j = cnt % NBG
                            gj = cnt % 2
                            cnt += 1
                            s1 = sct[b][:, hd * 128 + 16 * eq:hd * 128 + 16 * eq + 16]
                            s2 = sct[b][:, hd * 128 + 64:hd * 128 + 128]
                            kb.tt(Sg[j][:], s1.unsqueeze(2).to_broadcast([128, 16, 64]), s2.unsqueeze(1).to_broadcast([128, 16, 64]), ALU.add,
                                  ["sctb%d" % b], ["Sg%d" % j], eng=("pool" if hd in _POOL_HEADS else "dve"))
                            Sf = Sg[j][:].rearrange("p a b -> p (a b)")
                            kb.act(Eg[j][:], Sf, AF.Exp, ["Sg%d" % j, "tbt%d" % b], ["Eg%d" % j], bias=tbt[b][:, 8 + hd:9 + hd], scale=1.0)
                            if hd == 0:
                                kb.stt(G[:], Sf, tbt[b][:, hd:hd + 1], Eg[j][:], ALU.is_ge, ALU.mult, ["Sg%d" % j, "Eg%d" % j, "tbt%d" % b], ["G"])
                            else:
                                kb.stt(Gh[gj][:], Sf, tbt[b][:, hd:hd + 1], Eg[j][:], ALU.is_ge, ALU.mult, ["Sg%d" % j, "Eg%d" % j, "tbt%d" % b], ["Gh%d" % gj])
                                if deferred is not None:
                                    deferred()
                                deferred = (lambda gj=gj: kb.tt(G[:], G[:], Gh[gj][:], ALU.add, ["G", "Gh%d" % gj], ["G"]))
                        if deferred is not None:
                            deferred()
                        for g2 in range(2):
                            e0 = eq * 1024 + g2 * 512
                            for k in range(8):
                                kb.mm(pH[g2][:], xT[b][:, k * 128:(k + 1) * 128], Ub[:, k, e0:e0 + 512], k == 0, k == 7,
                                      ["xTb%d" % b, "Ub"], ["pH%d" % g2], inc=(k == 7))
                            kb.act(gl[g2][:], pH[g2][:], AF.Gelu, ["pH%d" % g2], ["gl%d" % g2])
                            kb.tt(Wb[:, g2 * 512:(g2 + 1) * 512], gl[g2][:], G[:, g2 * 512:(g2 + 1) * 512], ALU.mult, ["gl%d" % g2, "G"], ["Wb"])
                        for cc in range(8):
                            kb.tr(ptr[:, cc * 128:(cc + 1) * 128], Wb[:, cc * 128:(cc + 1) * 128], idb[:], ["Wb", "ident_b"], ["ptr"], inc=(cc == 7))
                        kb.cp(WT[:], ptr, ["ptr"], ["WT"], eng="act")
                        for dh in range(2):
                            for cc in range(8):
                                kb.mm(po[dh][:], WT[:, cc * 128:(cc + 1) * 128], Vv[:, eq * 8 + cc, dh * 512:(dh + 1) * 512],
                                      (eq == 0 and cc == 0), (eq == 3 and cc == 7), ["WT", "Vv"], ["po%d" % dh], inc=(cc == 7))
                    for dh in range(2):
                        kb.tt(ho[:, dh * 512:(dh + 1) * 512], h1t[b][:, dh * 512:(dh + 1) * 512], po[dh][:], ALU.add,
                              ["h1t%d" % b, "po%d" % dh], ["ho"])
                    if final:
                        _rstd(kb, ho[:], junk[:], ss[:], D, ["ho"], "f")
                        kb.stt(ho[:], ho[:], ss[:, 0:1], gf[:], ALU.mult, ALU.mult, ["ho", "fss", "gf"], ["ho"])
                    kb.dma("sp", hdst[rs, :], ho[:], reads=["ho"], writes=["hdst"])
                phase_end()
    return nc


def forward_fused(x, meta_tokens, attn_norm, w_in, attn_sinks, gla_gate_w2, gla_gate_b, swa_out_norm,
                  sb_out_norm, gla_out_norm, w_out, ffn_norm, peer_w_q, peer_sub_keys, peer_u, peer_v, final_norm, runner=None):
    f32 = lambda a: np.asarray(a, np.float32)
    x = f32(x)
    B, SEQ, _ = x.shape
    depth = attn_norm.shape[0]
    L = SEQ + 128
    Lp = ((L + 511) // 512) * 512
    TQ = Lp // 4
    assert B * 4 == NCORES
    nc = _prog_fused(Lp, depth)
    w_in, w_out = f32(w_in), f32(w_out)
    shared = {
        "g_attn": _c(np.stack([_gk(attn_norm[i]) for i in range(depth)])),
        "gffn": _c(np.stack([_gk(ffn_norm[i]) for i in range(depth)])),
        "wq": _c(f32(peer_w_q)),
        "ksub": _c(np.stack([np.transpose(f32(peer_sub_keys[i]).reshape(16, 64, 128), (2, 0, 1)) for i in range(depth)])),
        "uT": _c(np.transpose(f32(peer_u), (0, 2, 1))), "v": _c(f32(peer_v)),
        "gfin": _c(np.broadcast_to(f32(final_norm)[None, :], (128, D))),
    }
    per_j = []
    for j in range(4):
        kv = j // 2
        cols = np.concatenate([np.arange(128 * j, 128 * j + 128), np.arange(512 + 64 * kv, 512 + 64 * kv + 64),
                               np.arange(512 + 64 * kv, 512 + 64 * kv + 64), np.arange(768 + 64 * j, 768 + 64 * j + 64),
                               np.arange(1024 + 64 * j, 1024 + 64 * j + 64), np.arange(1536 + 32 * j, 1536 + 32 * j + 32),
                               np.arange(1664 + 32 * j, 1664 + 32 * j + 32), np.arange(2048, 2064),
                               np.arange(640 + 64 * kv, 640 + 64 * kv + 64), np.arange(1280 + 64 * j, 1280 + 64 * j + 64),
                               np.arange(1792 + 64 * j, 1792 + 64 * j + 64), np.arange(2064 + 64 * j, 2064 + 64 * j + 64)])
        assert len(cols) == CJ
        rows = np.concatenate([np.arange(128 * j, 128 * j + 128), np.arange(512 + 64 * j, 512 + 64 * j + 64),
                               np.arange(768 + 64 * j, 768 + 64 * j + 64)])
        gm = np.stack([np.concatenate([f32(swa_out_norm[i])[128 * j:128 * j + 128], f32(sb_out_norm[i])[64 * j:64 * j + 64],
                                       f32(gla_out_norm[i])[64 * j:64 * j + 64]]) for i in range(depth)])
        per_j.append({
            "w_in": _c(w_in[:, :, cols]),
            "w2": _c(f32(gla_gate_w2)[:, :, 32 * j:32 * j + 32]),
            "gb": _c(f32(gla_gate_b)[:, 32 * j:32 * j + 32].reshape(depth, 32, 1)),
            "sinks": _c(np.broadcast_to(f32(attn_sinks)[:, None, 2 * j:2 * j + 2], (depth, 128, 2))),
            "gmix": _c(np.broadcast_to(gm[:, None, :], (depth, 128, 256))),
            "wout": _c(np.transpose(w_out[:, rows, :].reshape(depth, 2, 128, D), (0, 2, 1, 3))),
        })
    maps = []
    for c in range(NCORES):
        b, r = c // 4, c % 4
        hp = np.zeros((Lp, D), np.float32)
        hp[112:128] = f32(meta_tokens)
        hp[128:L] = x[b]
        maps.append(dict(shared, **per_j[r], h0=_c(hp[r * TQ:(r + 1) * TQ])))
    res = (runner or _run)(nc, maps)
    out = np.zeros((B, SEQ, D), np.float32)
    for b in range(B):
        full = np.concatenate([np.asarray(res[b * 4 + r]["out"]) for r in range(4)], 0)
        out[b] = full[128:L]
    return out


def _prog_fused(Lp, depth):
    key = ("fused", Lp, depth)
    if key not in _CACHE:
        _CACHE[key] = build_fused(Lp, depth)
    return _CACHE[key]
```

```python
import numpy as np
from contextlib import ExitStack
import ml_dtypes
import concourse.bass as bass
import concourse.mybir as mybir
from concourse.bass_utils import run_bass_kernel_spmd

F32 = mybir.dt.float32
BF16 = mybir.dt.bfloat16
AF = mybir.ActivationFunctionType
ALU = mybir.AluOpType
AX = mybir.AxisListType

D = 1024
IN_COLS = 2320
EPS = 1e-6
NEG = -30000.0
NCORES = 8


class KB:
    ENGS = ("pe", "act", "dve", "pool", "sp")
    NDMA = 8

    def __init__(self, nc, stack):
        self.nc = nc
        self._stack = stack
        self.q = {e: [] for e in self.ENGS}
        self.cnt = {e: 0 for e in self.ENGS}
        self.sem = {e: stack.enter_context(nc.semaphore("s_" + e)) for e in self.ENGS}
        self.dsem = {e: [stack.enter_context(nc.semaphore("d_%s%d" % (e, i))) for i in range(self.NDMA)]
                     for e in ("sp", "pool")}
        self.dcnt = {e: [0] * self.NDMA for e in self.dsem}
        self.drot = {e: 0 for e in self.dsem}
        self.seen = {e: {} for e in self.ENGS}
        self.lastw = {}
        self.readers = {}
        self.pending_noinc = {e: False for e in self.ENGS}
        self.excl = set()

    def _deps(self, eng, reads, writes):
        toks = []
        for k in list(reads) + list(writes):
            t = self.lastw.get(k)
            if t is not None:
                toks.append(t)
        for k in writes:
            toks.extend(self.readers.get(k, {}).values())
        waits = {}
        for (sem, val, src) in toks:
            if src == "pe" and eng == "pe":
                continue
            sid = id(sem)
            if self.seen[eng].get(sid, 0) >= val:
                continue
            if sid not in waits or waits[sid][1] < val:
                waits[sid] = (sem, val)
        for sid, (sem, val) in waits.items():
            self.seen[eng][sid] = val
        return list(waits.values())

    def _record(self, rkey, tok, reads, writes):
        for k in writes:
            self.lastw[k] = tok
            self.readers[k] = {}
        for k in reads:
            self.readers.setdefault(k, {})[rkey] = tok

    def op(self, eng, fn, reads=(), writes=(), inc=True):
        ex = [k for k in reads if k in self.excl and k not in writes]
        if ex:
            writes = list(writes) + ex
        waits = self._deps(eng, reads, writes)
        sem = self.sem[eng]
        if inc:
            self.cnt[eng] += 1
            tok = (sem, self.cnt[eng], eng)
            self.pending_noinc[eng] = False
        else:
            tok = (sem, self.cnt[eng] + 1, eng)
            self.pending_noinc[eng] = True
        self.q[eng].append((waits, fn, (sem, 1) if inc else None))
        self._record(eng, tok, reads, writes)

    def dma(self, eng, out, in_, reads=(), writes=()):
        waits = self._deps(eng, reads, writes)
        r = self.drot[eng]
        self.drot[eng] = (r + 1) % self.NDMA
        sem = self.dsem[eng][r]
        prev = self.dcnt[eng][r]
        if prev > 0 and self.seen[eng].get(id(sem), 0) < prev:
            waits.append((sem, prev))
            self.seen[eng][id(sem)] = prev
        self.dcnt[eng][r] += 16
        tok = (sem, self.dcnt[eng][r], "dma")
        self.q[eng].append((waits, lambda e, o=out, i=in_: e.dma_start(out=o, in_=i), (sem, 16)))
        self._record(("dma", id(sem)), tok, reads, writes)

    def coll(self, kind, op, groups, in_, out, reads, writes):
        waits = self._deps("pool", reads, writes)
        if not hasattr(self, "csem"):
            self.csem = self._stack.enter_context(self.nc.semaphore("s_cc"))
            self.ccnt = 0
        if self.ccnt > 0 and self.seen["pool"].get(id(self.csem), 0) < self.ccnt:
            waits.append((self.csem, self.ccnt))
            self.seen["pool"][id(self.csem)] = self.ccnt
        self.ccnt += 1
        tok = (self.csem, self.ccnt, "dma")
        self.q["pool"].append((waits, lambda e, k=kind, o=op, g=groups, i=in_, u=out:
                               e.collective_compute(k, o, replica_groups=g, ins=[i.opt()], outs=[u.opt()]), (self.csem, 1)))
        self._record(("dma", id(self.csem)), tok, reads, writes)

    def wait_all(self, eng, keys):
        waits = self._deps(eng, keys, ())
        self.q[eng].append((waits, None, None))

    def barrier(self):
        for e in self.ENGS:
            waits = []
            for f in self.ENGS:
                if f != e and self.cnt[f] > 0 and self.seen[e].get(id(self.sem[f]), 0) < self.cnt[f]:
                    waits.append((self.sem[f], self.cnt[f]))
                    self.seen[e][id(self.sem[f])] = self.cnt[f]
            for q in self.dsem:
                for r in range(self.NDMA):
                    s, v = self.dsem[q][r], self.dcnt[q][r]
                    if v > 0 and self.seen[e].get(id(s), 0) < v:
                        waits.append((s, v))
                        self.seen[e][id(s)] = v
            if hasattr(self, "csem") and self.ccnt > 0 and self.seen[e].get(id(self.csem), 0) < self.ccnt:
                waits.append((self.csem, self.ccnt))
                self.seen[e][id(self.csem)] = self.ccnt
            self.q[e].append((waits, None, None))

    def emit(self):
        nc = self.nc
        for e in self.ENGS:
            assert not self.pending_noinc[e], "trailing non-inc op on " + e
        qs = self.q
        self.q = {e: [] for e in self.ENGS}
        with nc.Block() as block:
            def run(engname):
                def body(e):
                    for waits, fn, inc in qs[engname]:
                        for sem, val in waits:
                            e.wait_ge(sem, val)
                        if fn is None:
                            continue
                        ins = fn(e)
                        if inc is not None:
                            ins.then_inc(inc[0], inc[1])
                return body
            block.tensor(run("pe"))
            block.scalar(run("act"))
            block.vector(run("dve"))
            block.gpsimd(run("pool"))
            block.sync(run("sp"))

    def act(self, out, in_, func, r, w, eng="act", **kw):
        self.op(eng, lambda e, o=out, i=in_, f=func, k=kw: e.activation(out=o, in_=i, func=f, **k), r, w)

    def tt(self, out, in0, in1, op, r, w, eng="dve"):
        self.op(eng, lambda e, o=out, a=in0, b=in1, p=op: e.tensor_tensor(out=o, in0=a, in1=b, op=p), r, w)

    def ts(self, out, in0, s1, op0, r, w, s2=None, op1=None, eng="dve"):
        if op1 is None:
            self.op(eng, lambda e, o=out, a=in0, x=s1, p=op0: e.tensor_scalar(out=o, in0=a, scalar1=x, scalar2=None, op0=p), r, w)
        else:
            self.op(eng, lambda e, o=out, a=in0, x=s1, y=s2, p=op0, q=op1: e.tensor_scalar(out=o, in0=a, scalar1=x, scalar2=y, op0=p, op1=q), r, w)

    def stt(self, out, in0, scalar, in1, op0, op1, r, w, **kw):
        self.op("dve", lambda e, o=out, a=in0, s=scalar, b=in1, p=op0, q=op1, k=kw:
                e.scalar_tensor_tensor(out=o, in0=a, scalar=s, in1=b, op0=p, op1=q, **k), r, w)

    def cp(self, out, in_, r, w, eng="dve"):
        if eng == "act":
            self.op("act", lambda e, o=out, i=in_: e.activation(out=o, in_=i, func=AF.Copy), r, w)
        else:
            self.op(eng, lambda e, o=out, i=in_: e.tensor_copy(out=o, in_=i), r, w)

    def mm(self, out, lhsT, rhs, start, stop, r, w, inc=True):
        self.op("pe", lambda e, o=out, l=lhsT, x=rhs, s=start, t=stop: e.matmul(o, lhsT=l, rhs=x, start=s, stop=t), r, w, inc=inc)

    def tr(self, out, in_, ident, r, w, inc=True):
        self.op("pe", lambda e, o=out, i=in_, d=ident: e.transpose(o, i, d), r, w, inc=inc)

    def memset(self, ap, val, w, eng="pool"):
        self.op(eng, lambda e, a=ap, v=val: e.memset(a, v), (), w)

    def asel(self, ap, cmp, fill, base, cm, pattern, key):
        self.op("pool", lambda e, a=ap, c=cmp, f=fill, b=base, m=cm, p=pattern:
                e.affine_select(out=a, in_=a, compare_op=c, fill=f, base=b, pattern=p, channel_multiplier=m), [key], [key])


def _ident(kb, T, name="ident"):
    idf = T(name + "_f", [128, 128], F32)
    idb = T(name + "_b", [128, 128], BF16)
    kb.memset(idf[:], 0.0, [name + "_f"])
    kb.asel(idf[:], ALU.not_equal, 1.0, 0, 1, [[-1, 128]], name + "_f")
    kb.cp(idb[:], idf[:], [name + "_f"], [name + "_b"], eng="pool")
    return idf, idb


def _rstd(kb, src, junk, ss, n, rkeys, pfx):
    kb.act(junk, src, AF.Square, rkeys, [pfx + "junk", pfx + "ss"], accum_out=ss)
    kb.ts(ss, ss, 1.0 / n, ALU.mult, [pfx + "ss"], [pfx + "ss"], s2=EPS, op1=ALU.add)
    kb.act(ss, ss, AF.Sqrt, [pfx + "ss"], [pfx + "ss"])
    kb.op("dve", lambda e, a=ss: e.reciprocal(out=a, in_=a), [pfx + "ss"], [pfx + "ss"])


def build_p1(NT):
    nc = bass.Bass("TRN2", target_bir_lowering=False)
    h = nc.dram_tensor("h", [NT * 128, D], F32, kind="ExternalInput").ap()
    w = nc.dram_tensor("w", [D, IN_COLS], F32, kind="ExternalInput").ap()
    g = nc.dram_tensor("g", [128, 8], F32, kind="ExternalInput").ap()
    proj = nc.dram_tensor("proj", [NT * 128, IN_COLS], BF16, kind="ExternalOutput").ap()
    with ExitStack() as st:
        kb = KB(nc, st)
        T = lambda name, shape, dt=F32: st.enter_context(nc.sbuf_tensor(name, shape, dt))
        ps = [st.enter_context(nc.psum_tensor("ps%d" % i, [128, 512], F32)) for i in range(8)]
        idf, idb = _ident(kb, T)
        gt = T("gt", [128, 8])
        kb.dma("sp", gt[:], g, writes=["gt"])
        Wg = T("Wg", [128, 8, IN_COLS], BF16)
        stage = [T("stage%d" % i, [128, IN_COLS]) for i in range(2)]
        for k in range(8):
            sk = "stage%d" % (k % 2)
            kb.dma("sp", stage[k % 2][:], w[k * 128:(k + 1) * 128, :], writes=[sk])
            kb.ts(Wg[:, k, :], stage[k % 2][:], gt[:, k:k + 1], ALU.mult, [sk, "gt"], ["Wg%d" % k])
        ht = [T("ht%d" % i, [128, D]) for i in range(2)]
        junk = T("junk", [128, D], BF16)
        ss = T("ss", [128, 1])
        xn = T("xn", [128, D], BF16)
        xnT = T("xnT", [128, D], BF16)
        pr = [T("pr%d" % i, [128, IN_COLS], BF16) for i in range(2)]
        pT = ps[0][:].bitcast(BF16)
        cgs = [(c0, min(512, IN_COLS - c0)) for c0 in range(0, IN_COLS, 512)]
        wkeys = ["Wg%d" % k for k in range(8)]
        for i in range(NT):
            hk = "ht%d" % (i % 2)
            hb = ht[i % 2]
            kb.dma("sp", hb[:], h[i * 128:(i + 1) * 128, :], writes=[hk])
            _rstd(kb, hb[:], junk[:], ss[:], D, [hk], "a")
            kb.ts(xn[:], hb[:], ss[:, 0:1], ALU.mult, [hk, "ass"], ["xn"])
            for k in range(8):
                kb.tr(pT[:, k * 128:(k + 1) * 128], xn[:, k * 128:(k + 1) * 128], idb[:], ["xn", "ident_b"], ["pT"], inc=(k == 7))
            kb.cp(xnT[:], pT, ["pT"], ["xnT"], eng="act")
            prk = "pr%d" % (i % 2)
            for ci, (c0, cw) in enumerate(cgs):
                pk = "pp%d" % (ci % 2)
                pp = ps[1 + ci % 2]
                for k in range(8):
                    kb.mm(pp[:, 0:cw], xnT[:, k * 128:(k + 1) * 128], Wg[:, k, c0:c0 + cw], k == 0, k == 7,
                          ["xnT", wkeys[k]], [pk], inc=(k == 7))
                kb.cp(pr[i % 2][:, c0:c0 + cw], pp[:, 0:cw], [pk], [prk], eng=("act" if ci % 2 else "dve"))
            kb.dma("pool", proj[i * 128:(i + 1) * 128, :], pr[i % 2][:], reads=[prk], writes=["out"])
        kb.wait_all("pool", ["out"])
        kb.emit()
    return nc


def build_p2(Lp, parts=(1, 1, 1)):
    NCH = Lp // 512
    NB = Lp // 128
    nc = bass.Bass("TRN2", target_bir_lowering=False)
    IN = lambda n, s, dt=BF16: nc.dram_tensor(n, s, dt, kind="ExternalInput").ap()
    qaT = IN("qaT", [128, Lp]); kaT = IN("kaT", [128, Lp]); va = IN("va", [Lp, 64])
    qbT = IN("qbT", [64, Lp]); kbT = IN("kbT", [64, Lp]); vb = IN("vb", [Lp, 64])
    qcT = IN("qcT", [32, Lp]); kcT = IN("kcT", [32, Lp]); vc = IN("vc", [Lp, 64])
    glrT = IN("glrT", [16, Lp])
    w2 = IN("w2", [16, 32], F32); gb = IN("gb", [32, 1], F32); sinks = IN("sinks", [128, 2], F32)
    oa = nc.dram_tensor("oa", [Lp, 128], F32, kind="ExternalOutput").ap()
    obT = nc.dram_tensor("obT", [64, Lp], F32, kind="ExternalOutput").ap()
    oc = nc.dram_tensor("oc", [Lp, 64], F32, kind="ExternalOutput").ap()
    with ExitStack() as st:
        kb = KB(nc, st)
        T = lambda name, shape, dt=F32: st.enter_context(nc.sbuf_tensor(name, shape, dt))
        ps = [st.enter_context(nc.psum_tensor("ps%d" % i, [128, 512], F32)) for i in range(8)]
        idf, idb = _ident(kb, T)
        m_gen = T("m_gen", [128, 256]); m_n0 = T("m_n0", [128, 256]); m_n1 = T("m_n1", [128, 256])
        for m, nm, extra in ((m_gen, "m_gen", None), (m_n0, "m_n0", -240), (m_n1, "m_n1", -112)):
            kb.memset(m[:], 0.0, [nm])
            kb.asel(m[:], ALU.is_ge, NEG, -1, -1, [[1, 256]], nm)
            kb.asel(m[:], ALU.is_ge, NEG, 128, 1, [[-1, 256]], nm)
            if extra is not None:
                kb.asel(m[:], ALU.is_ge, NEG, extra, 0, [[1, 256]], nm)
        sbm_f = T("sbm_f", [128, 512])
        sbm = [T("sbm%d" % d, [128, 512], BF16) for d in range(4)]
        for d in range(4):
            kb.memset(sbm_f[:], 1.0, ["sbm_f"])
            kb.asel(sbm_f[:], ALU.is_ge, 0.0, -1 - 128 * d, -1, [[1, 512]], "sbm_f")
            kb.cp(sbm[d][:], sbm_f[:], ["sbm_f"], ["sbm%d" % d], eng="pool")
        ntri_f = T("ntri_f", [128, 128]); ntri = T("ntri", [128, 128], BF16); nones = T("nones", [128, 128], BF16)
        kb.memset(ntri_f[:], -1.0, ["ntri_f"])
        kb.asel(ntri_f[:], ALU.is_ge, 0.0, 0, 1, [[-1, 128]], "ntri_f")
        kb.cp(ntri[:], ntri_f[:], ["ntri_f"], ["ntri"], eng="pool")
        kb.memset(nones[:], -1.0, ["nones"])
        mle = T("mle", [128, 128])
        kb.memset(mle[:], 1.0, ["mle"])
        kb.asel(mle[:], ALU.is_ge, 0.0, 0, -1, [[1, 128]], "mle")
        rmask = T("rmask", [32, 512])
        kb.memset(rmask[:], 1.0, ["rmask"])
        for b in range(4):
            kb.memset(rmask[:, b * 128:b * 128 + 1], 0.0, ["rmask"])
        w2f = T("w2f", [16, 32]); w2b = T("w2b", [16, 32], BF16); gbt = T("gbt", [32, 1]); sk_t = T("sk_t", [128, 2])
        kb.dma("sp", w2f[:], w2, writes=["w2f"]); kb.dma("sp", gbt[:], gb, writes=["gbt"]); kb.dma("sp", sk_t[:], sinks, writes=["sk"])
        kb.cp(w2b[:], w2f[:], ["w2f"], ["w2b"])
        kb.ts(gbt[:], gbt[:], -1.0, ALU.mult, ["gbt"], ["gbt"])
        KbT = T("KbT", [64, Lp], BF16); Vb = T("Vb", [128, NB, 64], BF16)
        kb.dma("sp", KbT[:], kbT, writes=["KbT"])
        for n0 in range(0, NB, 16):
            n1 = min(NB, n0 + 16)
            kb.dma("sp", Vb[:, n0:n1, :], vb[n0 * 128:n1 * 128, :].rearrange("(n p) d -> p n d", p=128), writes=["Vb"])
        S = T("S", [32, 64]); Sb = T("Sb", [32, 64], BF16)
        kb.memset(S[:], 0.0, ["S"]); kb.memset(Sb[:], 0.0, ["Sb"])
        qa_c = [T("qa_c%d" % i, [128, 512], BF16) for i in range(2)]
        ka_c = [T("ka_c%d" % i, [128, 640], BF16) for i in range(2)]
        va_c = [T("va_c%d" % i, [128, 5, 64], BF16) for i in range(2)]
        qb_c = [T("qb_c%d" % i, [64, 512], BF16) for i in range(2)]
        qs_c = [T("qs_c%d" % i, [64, 512], BF16) for i in range(2)]
        qc_c = [T("qc_c%d" % i, [32, 512], BF16) for i in range(2)]
        kc_c = [T("kc_c%d" % i, [32, 512], BF16) for i in range(2)]
        vc_c = [T("vc_c%d" % i, [128, 4, 64], BF16) for i in range(2)]
        gl_c = [T("gl_c%d" % i, [16, 512], BF16) for i in range(2)]
        sm = T("sm", [128, 256]); pexp = T("pexp", [128, 256], BF16); pTs = T("pTs", [128, 256], BF16)
        st8 = T("st8", [128, 8]); oa_t = [T("oa_t%d" % i, [128, 128]) for i in range(2)]
        ge = T("ge", [32, 512]); gsp = T("gsp", [32, 512]); gcs = T("gcs", [32, 512])
        geq = T("geq", [32, 512]); gek = T("gek", [32, 512])
        qt = T("qt", [32, 512], BF16); kt = T("kt", [32, 512], BF16)
        scb = T("scb", [128, 128], BF16); ktm = T("ktm", [128, 32], BF16); oc_t = [T("oc_t%d" % i, [128, 64]) for i in range(2)]
        stmp = T("stmp", [32, 64])
        e_t = [T("e_t%d" % i, [128, 512]) for i in range(2)]
        sp_t = [T("sp_t%d" % i, [128, 512], BF16) for i in range(2)]
        a_t = [T("a_t%d" % i, [128, 512], BF16) for i in range(2)]
        R = T("R", [128, 512], BF16)
        ob_t = [T("ob_t%d" % i, [64, 512]) for i in range(2)]
        pz = [ps[0], ps[1]]; pc = [ps[2], ps[3]]; pO = ps[4]
        pA = ps[5]; pX = ps[6]; pB = ps[7]
        pT_bf = pB[:].bitcast(BF16)

        def load_chunk(c):
            i = c % 2
            c0 = c * 512
            kb.dma("sp", qa_c[i][:], qaT[:, c0:c0 + 512], writes=["qa_c%d" % i])
            if c == 0:
                kb.memset(ka_c[i][:, 0:128], 0.0, ["ka_c%d" % i])
                kb.memset(va_c[i][:, 0, :], 0.0, ["va_c%d" % i])
                kb.dma("sp", ka_c[i][:, 128:640], kaT[:, 0:512], writes=["ka_c%d" % i])
                kb.dma("sp", va_c[i][:, 1:5, :], va[0:512, :].rearrange("(n p) d -> p n d", p=128), writes=["va_c%d" % i])
            else:
                kb.dma("sp", ka_c[i][:], kaT[:, c0 - 128:c0 + 512], writes=["ka_c%d" % i])
                kb.dma("sp", va_c[i][:], va[c0 - 128:c0 + 512, :].rearrange("(n p) d -> p n d", p=128), writes=["va_c%d" % i])
            kb.dma("sp", qb_c[i][:], qbT[:, c0:c0 + 512], writes=["qb_c%d" % i])
            kb.dma("sp", qc_c[i][:], qcT[:, c0:c0 + 512], writes=["qc_c%d" % i])
            kb.dma("sp", kc_c[i][:], kcT[:, c0:c0 + 512], writes=["kc_c%d" % i])
            kb.dma("sp", vc_c[i][:], vc[c0:c0 + 512, :].rearrange("(n p) d -> p n d", p=128), writes=["vc_c%d" % i])
            kb.dma("sp", gl_c[i][:], glrT[:, c0:c0 + 512], writes=["gl_c%d" % i])

        def _gla(c, i):
            kb.mm(pX[0:32, :], w2b[:], gl_c[i][:], True, True, ["w2b", "gl_c%d" % i], ["pX"])
            kb.act(ge[:], pX[0:32, :], AF.Exp, ["pX", "gbt"], ["ge"], bias=gbt[:, 0:1], scale=-1.0)
            kb.act(gsp[:], ge[:], AF.Ln, ["ge"], ["gsp"], bias=1.0, scale=1.0)
            kb.op("dve", lambda e: e.tensor_tensor_scan(out=gcs[:], data0=rmask[:], data1=gsp[:], initial=0.0, op0=ALU.mult, op1=ALU.add),
                  ["rmask", "gsp"], ["gcs"])
            kb.act(geq[:], gcs[:], AF.Exp, ["gcs"], ["geq"], scale=-1.0 / 16.0)
            kb.act(gek[:], gcs[:], AF.Exp, ["gcs"], ["gek"], scale=1.0 / 16.0)
            kb.stt(qt[:], qc_c[i][:], 32.0 ** -0.5, geq[:], ALU.mult, ALU.mult, ["qc_c%d" % i, "geq"], ["qt"])
            kb.tt(kt[:], kc_c[i][:], gek[:], ALU.mult, ["kc_c%d" % i, "gek"], ["kt"])
            for blk in range(4):
                n = 4 * c + blk
                bs = slice(blk * 128, (blk + 1) * 128)
                kb.mm(pA[:, 384:512], kt[:, bs], qt[:, bs], True, True, ["kt", "qt"], ["pA"])
                kb.tt(scb[:], pA[:, 384:512], mle[:], ALU.mult, ["pA", "mle"], ["scb"])
                kb.tr(pT_bf[:, 512:544], kt[:, bs], idb[0:32, 0:32], ["kt", "ident_b"], ["pB"])
                kb.cp(ktm[:], pT_bf[:, 512:544], ["pB"], ["ktm"], eng="act")
                kb.mm(pA[:, 320:384], scb[:], vc_c[i][:, blk, :], True, False, ["scb", "vc_c%d" % i], ["pA"], inc=False)
                kb.mm(pA[:, 320:384], qt[:, bs], Sb[:], False, True, ["qt", "Sb"], ["pA"])
                ock = "oc_t%d" % (n % 2)
                kb.cp(oc_t[n % 2][:], pA[:, 320:384], ["pA"], [ock], eng="act")
                kb.dma("pool", oc[n * 128:(n + 1) * 128, :], oc_t[n % 2][:], reads=[ock], writes=["oc"])
                kb.mm(pB[0:32, 384:448], ktm[:], vc_c[i][:, blk, :], True, True, ["ktm", "vc_c%d" % i], ["pB"])
                kb.tt(stmp[:], pB[0:32, 384:448], S[:], ALU.add, ["pB", "S"], ["stmp"])
                kb.ts(S[:], stmp[:], geq[:, blk * 128 + 127:blk * 128 + 128], ALU.mult, ["stmp", "geq"], ["S"])
                kb.cp(Sb[:], S[:], ["S"], ["Sb"])

        def _sb(c, i):
            kb.ts(qs_c[i][:], qb_c[i][:], 0.125, ALU.mult, ["qb_c%d" % i], ["qs_c%d" % i])
            nkb = 4 * c + 4
            for it in range(nkb):
                kblk = nkb - 1 - it
                j = it % 2
                dg = kblk - 4 * c
                first, last = (it == 0), (kblk == 0)
                ksl = KbT[:, kblk * 128:(kblk + 1) * 128]
                kb.mm(pz[j][:], ksl, qs_c[i][:], True, True, ["KbT", "qs_c%d" % i], ["pz%d" % j])
                kb.act(e_t[j][:], pz[j][:], AF.Exp, ["pz%d" % j], ["e_t%d" % j])
                kb.act(sp_t[j][:], e_t[j][:], AF.Ln, ["e_t%d" % j], ["sp_t%d" % j], bias=1.0, scale=1.0)
                if dg >= 0:
                    kb.tt(sp_t[j][:], sp_t[j][:], sbm[dg][:], ALU.mult, ["sp_t%d" % j, "sbm%d" % dg], ["sp_t%d" % j])
                kb.mm(pc[j][:], ntri[:], sp_t[j][:], True, False, ["ntri", "sp_t%d" % j], ["pc%d" % j], inc=False)
                if not first:
                    kb.mm(pc[j][:], nones[:], R[:], False, False, ["nones", "R"], ["pc%d" % j], inc=False)
                kb.mm(pc[j][:], ksl, qs_c[i][:], False, True, ["KbT", "qs_c%d" % i], ["pc%d" % j])
                if not last:
                    if first:
                        kb.cp(R[:], sp_t[j][:], ["sp_t%d" % j], ["R"], eng="pool")
                    else:
                        kb.tt(R[:], R[:], sp_t[j][:], ALU.add, ["R", "sp_t%d" % j], ["R"], eng="pool")
                kb.act(a_t[j][:], pc[j][:], AF.Exp, ["pc%d" % j], ["a_t%d" % j])
                if dg >= 0:
                    kb.tt(a_t[j][:], a_t[j][:], sbm[dg][:], ALU.mult, ["a_t%d" % j, "sbm%d" % dg], ["a_t%d" % j])
                kb.mm(pO[0:64, :], Vb[:, kblk, :], a_t[j][:], first, last, ["Vb", "a_t%d" % j], ["pO"], inc=True)
            kb.cp(ob_t[i][:], pO[0:64, :], ["pO"], ["ob_t%d" % i])
            kb.dma("pool", obT[:, c * 512:(c + 1) * 512], ob_t[i][:], reads=["ob_t%d" % i], writes=["ob"])

        load_chunk(0)
        for c in range(NCH):
            i = c % 2
            if c + 1 < NCH:
                load_chunk(c + 1)
            for blk in (range(4) if parts[0] else []):
                n = 4 * c + blk
                msk = m_n0 if n == 0 else (m_n1 if n == 1 else m_gen)
                mk = "m_n0" if n == 0 else ("m_n1" if n == 1 else "m_gen")
                ok = "oa_t%d" % (n % 2)
                for hh in range(2):
                    hs = slice(hh * 64, (hh + 1) * 64)
                    kb.mm(pA[:, 0:256], qa_c[i][hs, blk * 128:(blk + 1) * 128], ka_c[i][hs, blk * 128:blk * 128 + 256],
                          True, True, ["qa_c%d" % i, "ka_c%d" % i], ["pA"])
                    kb.stt(sm[:], pA[:, 0:256], 0.125, msk[:], ALU.mult, ALU.add, ["pA", mk], ["sm"])
                    kb.op("dve", lambda e: e.tensor_reduce(out=st8[:, 0:1], in_=sm[:], axis=AX.X, op=ALU.max), ["sm"], ["st8"])
                    kb.tt(st8[:, 0:1], st8[:, 0:1], sk_t[:, hh:hh + 1], ALU.max, ["st8", "sk"], ["st8"])
                    kb.ts(st8[:, 1:2], st8[:, 0:1], -1.0, ALU.mult, ["st8"], ["st8"])
                    kb.act(pexp[:], sm[:], AF.Exp, ["sm", "st8"], ["pexp", "st8"], bias=st8[:, 1:2], scale=1.0, accum_out=st8[:, 2:3])
                    kb.act(st8[:, 3:4], sk_t[:, hh:hh + 1], AF.Exp, ["sk", "st8"], ["st8"], bias=st8[:, 1:2], scale=1.0)
                    kb.tt(st8[:, 4:5], st8[:, 2:3], st8[:, 3:4], ALU.add, ["st8"], ["st8"])
                    kb.op("dve", lambda e: e.reciprocal(out=st8[:, 5:6], in_=st8[:, 4:5]), ["st8"], ["st8"])
                    kb.tr(pT_bf[:, 0:128], pexp[:, 0:128], idb[:], ["pexp", "ident_b"], ["pB"], inc=False)
                    kb.tr(pT_bf[:, 128:256], pexp[:, 128:256], idb[:], ["pexp", "ident_b"], ["pB"])
                    kb.cp(pTs[:], pT_bf[:, 0:256], ["pB"], ["pTs"], eng="act")
                    kb.mm(pA[:, 256:320], pTs[:, 0:128], va_c[i][:, blk, :], True, False, ["pTs", "va_c%d" % i], ["pA"], inc=False)
                    kb.mm(pA[:, 256:320], pTs[:, 128:256], va_c[i][:, blk + 1, :], False, True, ["pTs", "va_c%d" % i], ["pA"])
                    kb.ts(oa_t[n % 2][:, hs], pA[:, 256:320], st8[:, 5:6], ALU.mult, ["pA", "st8"], [ok])
                kb.dma("pool", oa[n * 128:(n + 1) * 128, :], oa_t[n % 2][:], reads=[ok], writes=["oa"])
            if parts[1]:
              _gla(c, i)
            if parts[2]:
              _sb(c, i)
        kb.wait_all("pool", ["oa", "oc", "ob"])
        kb.emit()
    return nc


def build_p3(NT, final):
    nc = bass.Bass("TRN2", target_bir_lowering=False)
    IN = lambda n, s, dt=F32: nc.dram_tensor(n, s, dt, kind="ExternalInput").ap()
    h = IN("h", [NT * 128, D]); o = IN("o", [NT * 128, D]); rc = IN("rc", [NT * 128, 256], BF16)
    gmix = IN("gmix", [128, D]); wout = IN("wout", [D, D]); gffn = IN("gffn", [128, 8])
    wq = IN("wq", [D, 2048]); ksub = IN("ksub", [128, 16, 64]); uT = IN("uT", [D, 4096]); v = IN("v", [4096, D])
    gfin = IN("gfin", [128, D])
    hout = nc.dram_tensor("hout", [NT * 128, D], F32, kind="ExternalOutput").ap()
    h1_d = nc.dram_tensor("h1_d", [NT * 128, D], F32, kind="Internal").ap()
    sc_d = nc.dram_tensor("sc_d", [NT * 128, D], F32, kind="Internal").ap()
    tb_d = nc.dram_tensor("tb_d", [NT * 128, 16], F32, kind="Internal").ap()
    xT_d = nc.dram_tensor("xT_d", [NT * 128, D], BF16, kind="Internal").ap()
    with ExitStack() as st:
        kb = KB(nc, st)
        ps = [st.enter_context(nc.psum_tensor("ps%d" % i, [128, 512], F32)) for i in range(8)]
        T0 = lambda name, shape, dt=F32: st.enter_context(nc.sbuf_tensor(name, shape, dt))
        idf, idb = _ident(kb, T0)
        gft = T0("gft", [128, 8])
        kb.dma("sp", gft[:], gffn, writes=["gft"])
        stage = [T0("stage%d" % i, [128, 1024]) for i in range(2)]
        scnt = [0]

        def load_w(dst, src, rows_k, cols, key, scale_col=None):
            for k in range(rows_k):
                for c0 in range(0, cols, 1024):
                    cw = min(1024, cols - c0)
                    si = scnt[0] % 2
                    scnt[0] += 1
                    sk = "stage%d" % si
                    kb.dma("sp", stage[si][:, 0:cw], src[k * 128:(k + 1) * 128, c0:c0 + cw], writes=[sk])
                    if scale_col:
                        kb.ts(dst[:, k, c0:c0 + cw], stage[si][:, 0:cw], gft[:, k:k + 1], ALU.mult, [sk, "gft"], [key])
                    else:
                        kb.cp(dst[:, k, c0:c0 + cw], stage[si][:, 0:cw], [sk], [key], eng="pool")

        with ExitStack() as sa:
            T = lambda name, shape, dt=F32: sa.enter_context(nc.sbuf_tensor(name, shape, dt))
            Wo = T("Wo", [128, 8, D], BF16); Wq = T("Wq", [128, 8, 2048], BF16); Ks = T("Ks", [128, 16, 64], BF16)
            gm = T("gm", [128, D]); ksf = T("ksf", [128, 16, 64])
            kb.dma("sp", gm[:], gmix, writes=["gm"])
            kb.dma("sp", ksf[:], ksub, writes=["ksf"])
            kb.cp(Ks[:], ksf[:], ["ksf"], ["Ks"])
            load_w(Wo, wout, 8, D, "Wo")
            load_w(Wq, wq, 8, 2048, "Wq", scale_col=True)
            ht = [T("ht%d" % i, [128, D]) for i in range(2)]
            ot = [T("ot%d" % i, [128, D]) for i in range(2)]
            rct = [T("rct%d" % i, [128, 256], BF16) for i in range(2)]
            sq = T("sq", [128, D]); m1 = T("m1", [128, D]); sil = T("sil", [128, 256])
            s16 = T("s16", [128, 16]); mix = T("mix", [128, D], BF16); mixT = T("mixT", [128, D], BF16)
            h1 = T("h1", [128, D]); junk = T("junk", [128, D], BF16); ss = T("ss", [128, 1])
            xn = T("xn", [128, D], BF16); xnT = T("xnT", [128, D], BF16)
            qT = T("qT", [128, 16, 128], BF16); sct = T("sct", [128, D])
            t1 = T("t1", [128, 8, 16]); t2 = T("t2", [128, 8, 16]); wk = T("wk", [128, 256])
            cand = T("cand", [128, 8, 256]); c8a = T("c8a", [128, 8, 8]); c8b = T("c8b", [128, 8, 8])
            csh = T("csh", [128, 8, 256]); ec = T("ec", [128, 8, 256]); mk8 = T("mk8", [128, 8, 256])
            Z = T("Z", [128, 8]); tb = T("tb", [128, 16])
            pT = ps[0][:].bitcast(BF16)
            for i in range(NT):
                b = i % 2
                rs = slice(i * 128, (i + 1) * 128)
                kb.dma("sp", ht[b][:], h[rs, :], writes=["ht%d" % b])
                kb.dma("sp", ot[b][:], o[rs, :], writes=["ot%d" % b])
                kb.dma("sp", rct[b][:], rc[rs, :], writes=["rct%d" % b])
                kb.act(sq[:], ot[b][:], AF.Square, ["ot%d" % b], ["sq"])
                kb.op("dve", lambda e: e.tensor_reduce(out=s16[:], in_=sq[:].rearrange("p (h d) -> p h d", d=64), axis=AX.X, op=ALU.add),
                      ["sq"], ["s16"])
                kb.ts(s16[:], s16[:], 1.0 / 64, ALU.mult, ["s16"], ["s16"], s2=EPS, op1=ALU.add)
                kb.act(s16[:], s16[:], AF.Sqrt, ["s16"], ["s16"])
                kb.op("dve", lambda e: e.reciprocal(out=s16[:], in_=s16[:]), ["s16"], ["s16"])
                kb.tt(m1[:].rearrange("p (h d) -> p h d", d=64), ot[b][:].rearrange("p (h d) -> p h d", d=64),
                      s16[:].unsqueeze(2).to_broadcast([128, 16, 64]), ALU.mult, ["ot%d" % b, "s16"], ["m1"])
                kb.act(sil[:], rct[b][:], AF.Silu, ["rct%d" % b], ["sil"])
                kb.tt(m1[:, 768:1024], m1[:, 768:1024], sil[:], ALU.mult, ["m1", "sil"], ["m1"])
                kb.tt(mix[:], m1[:], gm[:], ALU.mult, ["m1", "gm"], ["mix"])
                for k in range(8):
                    kb.tr(pT[:, k * 128:(k + 1) * 128], mix[:, k * 128:(k + 1) * 128], idb[:], ["mix", "ident_b"], ["pT"], inc=(k == 7))
                kb.cp(mixT[:], pT, ["pT"], ["mixT"], eng="act")
                for dh in range(2):
                    for k in range(8):
                        kb.mm(ps[1 + dh][:], mixT[:, k * 128:(k + 1) * 128], Wo[:, k, dh * 512:(dh + 1) * 512], k == 0, k == 7,
                              ["mixT", "Wo"], ["pd%d" % dh], inc=(k == 7))
                    kb.tt(h1[:, dh * 512:(dh + 1) * 512], ht[b][:, dh * 512:(dh + 1) * 512], ps[1 + dh][:], ALU.add,
                          ["ht%d" % b, "pd%d" % dh], ["h1"])
                kb.dma("pool", h1_d[rs, :], h1[:], reads=["h1"], writes=["h1_d"])
                _rstd(kb, h1[:], junk[:], ss[:], D, ["h1"], "b")
                kb.ts(xn[:], h1[:], ss[:, 0:1], ALU.mult, ["h1", "bss"], ["xn"])
                for k in range(8):
                    kb.tr(pT[:, k * 128:(k + 1) * 128], xn[:, k * 128:(k + 1) * 128], idb[:], ["xn", "ident_b"], ["pT"], inc=(k == 7))
                kb.cp(xnT[:], pT, ["pT"], ["xnT"], eng="act")
                kb.dma("pool", xT_d[rs, :], xnT[:], reads=["xnT"], writes=["xT_d"])
                for cg in range(4):
                    pq = ps[3 + cg % 2]
                    for cc in range(4):
                        cidx = cg * 4 + cc
                        for k in range(8):
                            kb.mm(pq[:, cc * 128:(cc + 1) * 128], Wq[:, k, cidx * 128:(cidx + 1) * 128], xnT[:, k * 128:(k + 1) * 128],
                                  k == 0, k == 7, ["Wq", "xnT"], ["pq%d" % (cg % 2)], inc=(k == 7 and cc == 3))
                    kb.cp(qT[:, cg * 4:(cg + 1) * 4, :], pq[:].rearrange("p (c t) -> p c t", t=128), ["pq%d" % (cg % 2)], ["qT"],
                          eng=("act" if cg % 2 else "dve"))
                for cidx in range(16):
                    pscb = ps[5 + cidx // 8]
                    kb.mm(pscb[:, (cidx % 8) * 64:(cidx % 8 + 1) * 64], qT[:, cidx, :], Ks[:, cidx, :], True, True, ["qT", "Ks"],
                          ["psc%d" % (cidx // 8)], inc=(cidx % 8 == 7))
                kb.cp(sct[:, 0:512], ps[5][:], ["psc0"], ["sct"], eng="act")
                kb.cp(sct[:, 512:1024], ps[6][:], ["psc1"], ["sct"], eng="dve")
                kb.dma("pool", sc_d[rs, :], sct[:], reads=["sct"], writes=["sc_d"])
                for hd in range(8):
                    for side, tt_ in ((0, t1), (1, t2)):
                        sv = sct[:, hd * 128 + side * 64:hd * 128 + side * 64 + 64]
                        kb.op("dve", lambda e, o_=tt_[:, hd, 0:8], i_=sv: e.max(out=o_, in_=i_), ["sct"], ["tt"])
                        kb.op("dve", lambda e, o_=wk[:, 0:64], r_=tt_[:, hd, 0:8], i_=sv: e.match_replace(out=o_, in_to_replace=r_, in_values=i_, imm_value=-1e30),
                              ["sct", "tt"], ["wk"])
                        kb.op("dve", lambda e, o_=tt_[:, hd, 8:16], i_=wk[:, 0:64]: e.max(out=o_, in_=i_), ["wk"], ["tt"])
                kb.tt(cand[:].rearrange("p h (a b) -> p h a b", b=16), t1[:].unsqueeze(3).to_broadcast([128, 8, 16, 16]),
                      t2[:].unsqueeze(2).to_broadcast([128, 8, 16, 16]), ALU.add, ["tt"], ["cand"])
                for hd in range(8):
                    kb.op("dve", lambda e, o_=c8a[:, hd, :], i_=cand[:, hd, :]: e.max(out=o_, in_=i_), ["cand"], ["c8"])
                    kb.op("dve", lambda e, o_=wk[:], r_=c8a[:, hd, :], i_=cand[:, hd, :]: e.match_replace(out=o_, in_to_replace=r_, in_values=i_, imm_value=-1e30),
                          ["cand", "c8"], ["wk"])
                    kb.op("dve", lambda e, o_=c8b[:, hd, :], i_=wk[:]: e.max(out=o_, in_=i_), ["wk"], ["c8"])
                kb.tt(csh[:], cand[:], c8a[:, :, 0:1].to_broadcast([128, 8, 256]), ALU.subtract, ["cand", "c8"], ["csh"])
                kb.act(ec[:], csh[:], AF.Exp, ["csh"], ["ec"])
                kb.tt(mk8[:], cand[:], c8b[:, :, 7:8].to_broadcast([128, 8, 256]), ALU.is_ge, ["cand", "c8"], ["mk8"])
                kb.tt(ec[:], ec[:], mk8[:], ALU.mult, ["ec", "mk8"], ["ec"])
                kb.op("dve", lambda e: e.tensor_reduce(out=Z[:], in_=ec[:], axis=AX.X, op=ALU.add), ["ec"], ["Z"])
                kb.act(Z[:], Z[:], AF.Ln, ["Z"], ["Z"])
                kb.cp(tb[:, 0:8], c8b[:, :, 7], ["c8"], ["tb"])
                kb.stt(tb[:, 8:16], c8a[:, :, 0], -1.0, Z[:], ALU.mult, ALU.subtract, ["c8", "Z"], ["tb"])
                kb.dma("pool", tb_d[rs, :], tb[:], reads=["tb"], writes=["tb_d"])
            kb.barrier()
            kb.emit()
        with ExitStack() as sb_:
            T = lambda name, shape, dt=F32: sb_.enter_context(nc.sbuf_tensor(name, shape, dt))
            Ub = T("Ub", [128, 8, 4096], BF16); Vv = T("Vv", [128, 32, D], BF16)
            load_w(Ub, uT, 8, 4096, "Ub", scale_col=True)
            load_w(Vv, v, 32, D, "Vv")
            gf = T("gf", [128, D])
            if final:
                kb.dma("sp", gf[:], gfin, writes=["gf"])
            h1t = [T("h1t%d" % i, [128, D]) for i in range(2)]
            sct = [T("sctb%d" % i, [128, D]) for i in range(2)]
            xT = [T("xTb%d" % i, [128, D], BF16) for i in range(2)]
            tbt = [T("tbt%d" % i, [128, 16]) for i in range(2)]
            Sg = [T("Sg%d" % i, [128, 16, 64]) for i in range(2)]
            Eg = [T("Eg%d" % i, [128, 1024], BF16) for i in range(2)]
            Gh = T("Gh", [128, 1024], BF16); G = T("G", [128, 1024])
            gl = [T("gl%d" % i, [128, 512]) for i in range(2)]
            Wb = T("Wb", [128, 1024], BF16); WT = T("WT", [128, 1024], BF16)
            ho = T("ho", [128, D]); junk = T("junkb", [128, D], BF16); ss = T("ssb", [128, 1])
            pH = [ps[0], ps[1]]; ptr = ps[2][:].bitcast(BF16); po = [ps[3], ps[4]]

            def loadB(i):
                b = i % 2
                rs = slice(i * 128, (i + 1) * 128)
                kb.dma("sp", h1t[b][:], h1_d[rs, :], reads=["h1_d"], writes=["h1t%d" % b])
                kb.dma("sp", sct[b][:], sc_d[rs, :], reads=["sc_d"], writes=["sctb%d" % b])
                kb.dma("sp", xT[b][:], xT_d[rs, :], reads=["xT_d"], writes=["xTb%d" % b])
                kb.dma("sp", tbt[b][:], tb_d[rs, :], reads=["tb_d"], writes=["tbt%d" % b])

            loadB(0)
            cnt = 0
            for i in range(NT):
                b = i % 2
                rs = slice(i * 128, (i + 1) * 128)
                if i + 1 < NT:
                    loadB(i + 1)
                for eq in range(4):
                    for hd in range(8):
                        j = cnt % 2
                        cnt += 1
                        s1 = sct[b][:, hd * 128 + 16 * eq:hd * 128 + 16 * eq + 16]
                        s2 = sct[b][:, hd * 128 + 64:hd * 128 + 128]
                        kb.tt(Sg[j][:], s1.unsqueeze(2).to_broadcast([128, 16, 64]), s2.unsqueeze(1).to_broadcast([128, 16, 64]), ALU.add,
                              ["sctb%d" % b], ["Sg%d" % j], eng="pool")
                        Sf = Sg[j][:].rearrange("p a b -> p (a b)")
                        kb.act(Eg[j][:], Sf, AF.Exp, ["Sg%d" % j, "tbt%d" % b], ["Eg%d" % j], bias=tbt[b][:, 8 + hd:9 + hd], scale=1.0)
                        if hd == 0:
                            kb.stt(G[:], Sf, tbt[b][:, hd:hd + 1], Eg[j][:], ALU.is_ge, ALU.mult, ["Sg%d" % j, "Eg%d" % j, "tbt%d" % b], ["G"])
                        else:
                            kb.stt(Gh[:], Sf, tbt[b][:, hd:hd + 1], Eg[j][:], ALU.is_ge, ALU.mult, ["Sg%d" % j, "Eg%d" % j, "tbt%d" % b], ["Gh"])
                            kb.tt(G[:], G[:], Gh[:], ALU.add, ["G", "Gh"], ["G"])
                    for g2 in range(2):
                        e0 = eq * 1024 + g2 * 512
                        for k in range(8):
                            kb.mm(pH[g2][:], xT[b][:, k * 128:(k + 1) * 128], Ub[:, k, e0:e0 + 512], k == 0, k == 7,
                                  ["xTb%d" % b, "Ub"], ["pH%d" % g2], inc=(k == 7))
                        kb.act(gl[g2][:], pH[g2][:], AF.Gelu, ["pH%d" % g2], ["gl%d" % g2])
                        kb.tt(Wb[:, g2 * 512:(g2 + 1) * 512], gl[g2][:], G[:, g2 * 512:(g2 + 1) * 512], ALU.mult, ["gl%d" % g2, "G"], ["Wb"])
                    for cc in range(8):
                        kb.tr(ptr[:, cc * 128:(cc + 1) * 128], Wb[:, cc * 128:(cc + 1) * 128], idb[:], ["Wb", "ident_b"], ["ptr"], inc=(cc == 7))
                    kb.cp(WT[:], ptr, ["ptr"], ["WT"], eng="act")
                    for dh in range(2):
                        for cc in range(8):
                            kb.mm(po[dh][:], WT[:, cc * 128:(cc + 1) * 128], Vv[:, eq * 8 + cc, dh * 512:(dh + 1) * 512],
                                  (eq == 0 and cc == 0), (eq == 3 and cc == 7), ["WT", "Vv"], ["po%d" % dh], inc=(cc == 7))
                for dh in range(2):
                    kb.tt(ho[:, dh * 512:(dh + 1) * 512], h1t[b][:, dh * 512:(dh + 1) * 512], po[dh][:], ALU.add,
                          ["h1t%d" % b, "po%d" % dh], ["ho"])
                if final:
                    _rstd(kb, ho[:], junk[:], ss[:], D, ["ho"], "f")
                    kb.stt(ho[:], ho[:], ss[:, 0:1], gf[:], ALU.mult, ALU.mult, ["ho", "fss", "gf"], ["ho"])
                kb.dma("sp", hout[rs, :], ho[:], reads=["ho"], writes=["hout"])
            kb.wait_all("sp", ["hout"])
            kb.emit()
    return nc


_CACHE = {}


def _prog(name, *args):
    key = (name,) + args
    if key not in _CACHE:
        _CACHE[key] = {"p1": build_p1, "p2": build_p2, "p3": build_p3}[name](*args)
    return _CACHE[key]


def _run(nc, in_maps):
    res = run_bass_kernel_spmd(nc, in_maps, core_ids=list(range(NCORES)))
    return res.results


def _c(a):
    return np.ascontiguousarray(a)


def _gk(g):
    return _c(np.asarray(g, np.float32).reshape(8, 128).T)


def forward(x, meta_tokens, attn_norm, w_in, attn_sinks, gla_gate_w2, gla_gate_b, swa_out_norm,
            sb_out_norm, gla_out_norm, w_out, ffn_norm, peer_w_q, peer_sub_keys, peer_u, peer_v, final_norm):
    x = np.asarray(x, np.float32)
    B, SEQ, _ = x.shape
    depth = attn_norm.shape[0]
    L = SEQ + 128
    Lp = ((L + 511) // 512) * 512
    T = B * L
    NT = (T // 128 + NCORES - 1) // NCORES
    Tp = NT * 128 * NCORES
    hfull = np.zeros((Tp, D), np.float32)
    for b in range(B):
        hfull[b * L + 112:b * L + 128] = meta_tokens
        hfull[b * L + 128:(b + 1) * L] = x[b]
    p1 = _prog("p1", NT)
    p2 = _prog("p2", Lp)
    tsl = [slice(c * NT * 128, (c + 1) * NT * 128) for c in range(NCORES)]
    bf = ml_dtypes.bfloat16
    for i in range(depth):
        w_i = _c(w_in[i]); g_i = _gk(attn_norm[i])
        r = _run(p1, [{"h": hfull[tsl[c]], "w": w_i, "g": g_i} for c in range(NCORES)])
        proj = np.concatenate([np.asarray(r[c]["proj"]).view(bf) if np.asarray(r[c]["proj"]).dtype != bf else np.asarray(r[c]["proj"])
                               for c in range(NCORES)], axis=0)
        maps = []
        for c in range(NCORES):
            b, j = c // 4, c % 4
            pb = np.zeros((Lp, IN_COLS), bf)
            pb[:L] = proj[b * L:(b + 1) * L]
            kv = j // 2
            ka = pb[:, 512 + 64 * kv:512 + 64 * kv + 64].T
            maps.append({
                "qaT": _c(pb[:, 128 * j:128 * j + 128].T), "kaT": _c(np.concatenate([ka, ka], 0)),
                "va": _c(pb[:, 640 + 64 * kv:640 + 64 * kv + 64]),
                "qbT": _c(pb[:, 768 + 64 * j:768 + 64 * j + 64].T), "kbT": _c(pb[:, 1024 + 64 * j:1024 + 64 * j + 64].T),
                "vb": _c(pb[:, 1280 + 64 * j:1280 + 64 * j + 64]),
                "qcT": _c(pb[:, 1536 + 32 * j:1536 + 32 * j + 32].T), "kcT": _c(pb[:, 1664 + 32 * j:1664 + 32 * j + 32].T),
                "vc": _c(pb[:, 1792 + 64 * j:1792 + 64 * j + 64]), "glrT": _c(pb[:, 2048:2064].T),
                "w2": _c(np.asarray(gla_gate_w2[i], np.float32)[:, 32 * j:32 * j + 32]),
                "gb": _c(np.asarray(gla_gate_b[i], np.float32)[32 * j:32 * j + 32].reshape(32, 1)),
                "sinks": _c(np.broadcast_to(np.asarray(attn_sinks[i], np.float32)[2 * j:2 * j + 2][None, :], (128, 2))),
            })
        r = _run(p2, maps)
        ofull = np.zeros((Tp, D), np.float32)
        for c in range(NCORES):
            b, j = c // 4, c % 4
            ofull[b * L:(b + 1) * L, 128 * j:128 * j + 128] = np.asarray(r[c]["oa"])[:L]
            ofull[b * L:(b + 1) * L, 512 + 64 * j:512 + 64 * j + 64] = np.asarray(r[c]["obT"]).T[:L]
            ofull[b * L:(b + 1) * L, 768 + 64 * j:768 + 64 * j + 64] = np.asarray(r[c]["oc"])[:L]
        rcfull = np.zeros((Tp, 256), bf)
        rcfull[:T] = proj[:T, 2064:2320]
        fin = (i == depth - 1)
        p3 = _prog("p3", NT, fin)
        gmix = np.concatenate([swa_out_norm[i], sb_out_norm[i], gla_out_norm[i]]).astype(np.float32)
        shared = {
            "gmix": _c(np.broadcast_to(gmix[None, :], (128, D))), "wout": _c(w_out[i]), "gffn": _gk(ffn_norm[i]),
            "wq": _c(peer_w_q[i]), "ksub": _c(np.transpose(np.asarray(peer_sub_keys[i], np.float32).reshape(16, 64, 128), (2, 0, 1))),
            "uT": _c(np.asarray(peer_u[i], np.float32).T), "v": _c(peer_v[i]),
            "gfin": _c(np.broadcast_to(np.asarray(final_norm, np.float32)[None, :], (128, D))),
        }
        r = _run(p3, [dict(shared, h=hfull[tsl[c]], o=ofull[tsl[c]], rc=rcfull[tsl[c]]) for c in range(NCORES)])
        hfull = np.concatenate([np.asarray(r[c]["hout"]) for c in range(NCORES)], axis=0)
    out = np.stack([hfull[b * L + 128:(b + 1) * L] for b in range(B)], 0)
    return np.ascontiguousarray(out.astype(np.float32))


def kernel(**inputs):
    return forward_fused(**{k: np.asarray(v) for k, v in inputs.items()})


import os as _os
_STOP = _os.environ.get("FUSED_STOP", "")
_POOL_HEADS = tuple(int(c_) for c_ in _os.environ.get("POOL_HEADS", "01234567"))
CJ = 720
RG = [[0, 1, 2, 3], [4, 5, 6, 7]]


def build_fused(Lp, depth):
    TQ = Lp // 4
    NT = TQ // 128
    NCH = Lp // 512
    NB = Lp // 128
    nc = bass.Bass("TRN2", target_bir_lowering=False)
    IN = lambda n, s, dt=F32: nc.dram_tensor(n, s, dt, kind="ExternalInput").ap()
    h0 = IN("h0", [TQ, D])
    w_in = IN("w_in", [depth, D, CJ]); g_attn = IN("g_attn", [depth, 128, 8])
    w2_i = IN("w2", [depth, 16, 32]); gb_i = IN("gb", [depth, 32, 1]); sinks_i = IN("sinks", [depth, 128, 2])
    gmix_i = IN("gmix", [depth, 128, 256]); wout_i = IN("wout", [depth, 128, 2, D])
    gffn_i = IN("gffn", [depth, 128, 8]); wq_i = IN("wq", [depth, D, 2048]); ksub_i = IN("ksub", [depth, 128, 16, 64])
    uT_i = IN("uT", [depth, D, 4096]); v_i = IN("v", [depth, 4096, D]); gfin = IN("gfin", [128, D])
    out = nc.dram_tensor("out", [TQ, D], F32, kind="ExternalOutput").ap()
    DT = lambda n, s, dt=F32: nc.dram_tensor(n, s, dt, kind="Internal").ap()
    GC = 128 * max(d for d in range(1, 5) if NT % d == 0)
    NG = TQ // GC
    xT_loc = [DT("xT_loc%d" % g, [D, GC], BF16) for g in range(NG)]
    xT_all = [DT("xT_all%d" % g, [4 * D, GC], BF16) for g in range(NG)]
    part_loc = DT("part_loc", [Lp, D]); delta_loc = DT("delta_loc", [TQ, D])
    hl = [DT("hl0", [TQ, D]), DT("hl1", [TQ, D])]
    h1_d = DT("h1_d", [TQ, D]); sc_d = DT("sc_d", [TQ, D]); tb_d = DT("tb_d", [TQ, 16]); xT_d = DT("xT_d", [TQ, D], BF16)

    with ExitStack() as st:
        kb = KB(nc, st)
        kb.excl.update(["pT", "pX", "pA", "pB", "pO", "pz0", "pz1", "pc0", "pc1", "pq0", "pq1", "psc0", "psc1",
                        "pH0", "pH1", "ptr", "po0", "po1"])
        ps = [st.enter_context(nc.psum_tensor("ps%d" % i, [128, 512], F32)) for i in range(8)]
        T0 = lambda name, shape, dt=F32: st.enter_context(nc.sbuf_tensor(name, shape, dt))
        idf, idb = _ident(kb, T0)
        stage = [T0("stage%d" % i, [128, 1024]) for i in range(2)]
        scnt = [0]

        def load_w(dst, src, rows_k, cols, key, gt=None):
            for k in range(rows_k):
                for c0 in range(0, cols, 1024):
                    cw = min(1024, cols - c0)
                    si = scnt[0] % 2
                    scnt[0] += 1
                    sk = "stage%d" % si
                    kb.dma("sp", stage[si][:, 0:cw], src[k * 128:(k + 1) * 128, c0:c0 + cw], writes=[sk])
                    if gt is not None:
                        kb.ts(dst[:, k, c0:c0 + cw], stage[si][:, 0:cw], gt[:, k:k + 1], ALU.mult, [sk, "gt"], [key])
                    else:
                        kb.cp(dst[:, k, c0:c0 + cw], stage[si][:, 0:cw], [sk], [key], eng="pool")

        def phase_end():
            kb.barrier()
            kb.emit()

        for li in range(depth):
            hsrc = h0 if li == 0 else hl[(li - 1) % 2]
            hdst = out if li == depth - 1 else hl[li % 2]
            final = (li == depth - 1)
            with ExitStack() as sa:
                T = lambda name, shape, dt=F32, _p="L%dA_" % li: sa.enter_context(nc.sbuf_tensor(_p + name, shape, dt))
                ht = [T("ht%d" % i, [128, D]) for i in range(2)]
                junk = T("junk", [128, D], BF16); ss = T("ss", [128, 1])
                xn = T("xn", [128, D], BF16); xnT = [T("xnT%d" % i, [128, D], BF16) for i in range(2)]
                pT = ps[0][:].bitcast(BF16)
                xv = [x_.rearrange("(k p) t -> p k t", p=128) for x_ in xT_loc]
                for i in range(NT):
                    b = i % 2
                    kb.dma("sp", ht[b][:], hsrc[i * 128:(i + 1) * 128, :], writes=["ht%d" % b])
                    _rstd(kb, ht[b][:], junk[:], ss[:], D, ["ht%d" % b], "a")
                    kb.ts(xn[:], ht[b][:], ss[:, 0:1], ALU.mult, ["ht%d" % b, "ass"], ["xn"])
                    for k in range(8):
                        kb.tr(pT[:, k * 128:(k + 1) * 128], xn[:, k * 128:(k + 1) * 128], idb[:], ["xn", "ident_b"], ["pT"], inc=(k == 7))
                    kb.cp(xnT[b][:], pT, ["pT"], ["xnT%d" % b], eng="act")
                    g_, o_ = (i * 128) // GC, (i * 128) % GC
                    kb.dma("pool", xv[g_][:, :, o_:o_ + 128], xnT[b][:].rearrange("p (k t) -> p k t", t=128),
                           reads=["xnT%d" % b], writes=["xT_loc"])
                kb.barrier()
                for g_ in range(NG):
                    kb.coll("AllGather", ALU.bypass, RG, xT_loc[g_], xT_all[g_], ["xT_loc"], ["xT_all"])
                phase_end()
            if _STOP == "A":
                break
            with ExitStack() as sb_:
                T = lambda name, shape, dt=F32, _p="L%dB_" % li: sb_.enter_context(nc.sbuf_tensor(_p + name, shape, dt))
                gt = T("gt", [128, 8])
                kb.dma("sp", gt[:], g_attn[li], writes=["gt"])
                Wj = T("Wj", [128, 8, 768], BF16)
                load_w(Wj, w_in[li], 8, CJ, "Wj", gt=gt)
                Wo = T("Wo", [128, 2, D], BF16)
                for kc in range(2):
                    si = scnt[0] % 2; scnt[0] += 1
                    kb.dma("sp", stage[si][:], wout_i[li][:, kc, :], writes=["stage%d" % si])
                    kb.cp(Wo[:, kc, :], stage[si][:], ["stage%d" % si], ["Wo"], eng="pool")
                gm = T("gm", [128, 256]); kb.dma("sp", gm[:], gmix_i[li], writes=["gm"])
                m_gen = T("m_gen", [128, 256]); m_n0 = T("m_n0", [128, 256]); m_n1 = T("m_n1", [128, 256])
                for m, nm, extra in ((m_gen, "m_gen", None), (m_n0, "m_n0", -240), (m_n1, "m_n1", -112)):
                    kb.memset(m[:], 0.0, [nm])
                    kb.asel(m[:], ALU.is_ge, NEG, -1, -1, [[1, 256]], nm)
                    kb.asel(m[:], ALU.is_ge, NEG, 128, 1, [[-1, 256]], nm)
                    if extra is not None:
                        kb.asel(m[:], ALU.is_ge, NEG, extra, 0, [[1, 256]], nm)
                sbm_f = T("sbm_f", [128, 512])
                sbm = [T("sbm%d" % d, [128, 512], BF16) for d in range(4)]
                for d in range(4):
                    kb.memset(sbm_f[:], 1.0, ["sbm_f"])
                    kb.asel(sbm_f[:], ALU.is_ge, 0.0, -1 - 128 * d, -1, [[1, 512]], "sbm_f")
                    kb.cp(sbm[d][:], sbm_f[:], ["sbm_f"], ["sbm%d" % d], eng="pool")
                ntri_f = T("ntri_f", [128, 128]); ntri = T("ntri", [128, 128], BF16); nones = T("nones", [128, 128], BF16)
                kb.memset(ntri_f[:], -1.0, ["ntri_f"])
                kb.asel(ntri_f[:], ALU.is_ge, 0.0, 0, 1, [[-1, 128]], "ntri_f")
                kb.cp(ntri[:], ntri_f[:], ["ntri_f"], ["ntri"], eng="pool")
                kb.memset(nones[:], -1.0, ["nones"])
                mle = T("mle", [128, 128])
                kb.memset(mle[:], 1.0, ["mle"])
                kb.asel(mle[:], ALU.is_ge, 0.0, 0, -1, [[1, 128]], "mle")
                rmask = T("rmask", [32, 512])
                kb.memset(rmask[:], 1.0, ["rmask"])
                for b4 in range(4):
                    kb.memset(rmask[:, b4 * 128:b4 * 128 + 1], 0.0, ["rmask"])
                w2f = T("w2f", [16, 32]); w2b = T("w2b", [16, 32], BF16); gbt = T("gbt", [32, 1]); sk_t = T("sk_t", [128, 2])
                kb.dma("sp", w2f[:], w2_i[li], writes=["w2f"]); kb.dma("sp", gbt[:], gb_i[li], writes=["gbt"])
                kb.dma("sp", sk_t[:], sinks_i[li], writes=["sk"])
                kb.cp(w2b[:], w2f[:], ["w2f"], ["w2b"])
                kb.ts(gbt[:], gbt[:], -1.0, ALU.mult, ["gbt"], ["gbt"])
                KbT = T("KbT", [64, Lp], BF16); Vb = T("Vb", [128, NB, 64], BF16)
                S = T("S", [32, 64]); Sb = T("Sb", [32, 64], BF16)
                kb.memset(S[:], 0.0, ["S"]); kb.memset(Sb[:], 0.0, ["Sb"])
                xc = [T("xc%d" % i, [128, 8, 512], BF16) for i in range(2)]
                qa_c = [T("qa_c%d" % i, [128, 512], BF16) for i in range(2)]
                ka_c = [T("ka_c%d" % i, [128, 640], BF16) for i in range(2)]
                va_c = [T("va_c%d" % i, [128, 5, 64], BF16) for i in range(2)]
                qs_c = [T("qs_c%d" % i, [64, 512], BF16) for i in range(2)]
                qc_c = [T("qc_c%d" % i, [32, 512], BF16) for i in range(2)]
                kc_c = [T("kc_c%d" % i, [32, 512], BF16) for i in range(2)]
                vc_c = [T("vc_c%d" % i, [128, 4, 64], BF16) for i in range(2)]
                rc_c = [T("rc_c%d" % i, [128, 4, 64]) for i in range(2)]
                gl_c = [T("gl_c%d" % i, [16, 512], BF16) for i in range(2)]
                mo = [T("mo%d" % i, [128, 4, 256]) for i in range(2)]
                sm = T("sm", [128, 256]); pexp = T("pexp", [128, 256], BF16); pTs = T("pTs", [128, 256], BF16)
                st8 = T("st8", [128, 8])
                ge = T("ge", [32, 512]); gsp = T("gsp", [32, 512]); gcs = T("gcs", [32, 512])
                geq = T("geq", [32, 512]); gek = T("gek", [32, 512])
                qt = T("qt", [32, 512], BF16); kt = T("kt", [32, 512], BF16)
                scb = T("scb", [128, 128], BF16); ktm = T("ktm", [128, 32], BF16)
                stmp = T("stmp", [32, 64])
                e_t = [T("e_t%d" % i, [128, 512]) for i in range(2)]
                sp_t = [[T("sp_t%d_%d" % (pp_, i), [128, 512], BF16) for i in range(2)] for pp_ in range(2)]
                a_t = [[T("a_t%d_%d" % (pp_, i), [128, 512], BF16) for i in range(2)] for pp_ in range(2)]
                R = T("R", [128, 512], BF16)
                ob_t = T("ob_t", [64, 512])
                sq4 = T("sq4", [128, 1024]); s16 = T("s16", [128, 16]); m14 = T("m14", [128, 1024]); sil4 = T("sil4", [128, 4, 64])
                mixb4 = T("mixb4", [128, 1024], BF16); mixT4 = T("mixT4", [128, 1024], BF16)
                pt = [T("pt%d" % i, [128, D]) for i in range(2)]
                pz = [ps[0], ps[1]]; pc = [ps[2], ps[3]]; pO = ps[4]
                pA = ps[5]; pX = ps[6]; pB = ps[7]
                pT_bf = pB[:].bitcast(BF16)

                def pieces(c0, n):
                    t = c0
                    while t < c0 + n:
                        q = t // TQ; tl = t % TQ; g_ = tl // GC; o_ = tl % GC; m = min(c0 + n - t, GC - o_)
                        yield q, g_, o_, t - c0, m
                        t += m

                def load_xc(c):
                    i = c % 2
                    for q, g_, o_, off, m in pieces(c * 512, 512):
                        kb.dma("sp", xc[i][:, :, off:off + m],
                               xT_all[g_][q * D:(q + 1) * D, o_:o_ + m].rearrange("(k p) t -> p k t", p=128), writes=["xc%d" % i])

                def inproj(c):
                    i = c % 2
                    xk = "xc%d" % i
                    if c == 0:
                        kb.memset(ka_c[i][:, 0:128], 0.0, ["ka_c%d" % i])
                        kb.memset(va_c[i][:, 0, :], 0.0, ["va_c%d" % i])
                    else:
                        kb.cp(ka_c[i][:, 0:128], ka_c[1 - i][:, 512:640], ["ka_c%d" % (1 - i)], ["ka_c%d" % i], eng="pool")
                        kb.cp(va_c[i][:, 0, :], va_c[1 - i][:, 4, :], ["va_c%d" % (1 - i)], ["va_c%d" % i], eng="pool")
                    groups = [(0, 128, qa_c[i][:], "qa_c%d" % i, None), (128, 128, ka_c[i][:, 128:640], "ka_c%d" % i, None),
                              (256, 64, qs_c[i][:], "qs_c%d" % i, 0.125), (320, 64, KbT[:, c * 512:(c + 1) * 512], "KbT", None),
                              (384, 32, qc_c[i][:], "qc_c%d" % i, None), (416, 32, kc_c[i][:], "kc_c%d" % i, None),
                              (448, 16, gl_c[i][:], "gl_c%d" % i, None)]
                    for gi, (c0, rows, dst, dk, scale) in enumerate(groups):
                        mr = max(rows, 32)
                        pI, pIk = ((pX, "pX"), (pA, "pA"))[gi % 2]
                        for k in range(8):
                            kb.mm(pI[0:mr, :], Wj[:, k, c0:c0 + mr], xc[i][:, k, :], k == 0, k == 7, ["Wj", xk], [pIk], inc=(k == 7))
                        if scale is not None:
                            kb.ts(dst, pI[0:rows, :], scale, ALU.mult, [pIk], [dk])
                        elif gi % 2:
                            kb.cp(dst, pI[0:rows, :], [pIk], [dk], eng="act")
                        else:
                            kb.cp(dst, pI[0:rows, :], [pIk], [dk])
                    for blk in (range(4) if _STOP != "B1f" else []):
                        n = 4 * c + blk
                        pI, pIk = ((pA, "pA"), (pX, "pX"))[blk % 2]
                        for k in range(8):
                            kb.mm(pI[:, 0:256], xc[i][:, k, blk * 128:(blk + 1) * 128], Wj[:, k, 464:720], k == 0, k == 7, ["Wj", xk], [pIk], inc=(k == 7))
                        ev = "act" if blk % 2 else "dve"
                        kb.cp(va_c[i][:, blk + 1, :], pI[:, 0:64], [pIk], ["va_c%d" % i], eng=ev)
                        kb.cp(Vb[:, n, :], pI[:, 64:128], [pIk], ["Vb"], eng=ev)
                        kb.cp(vc_c[i][:, blk, :], pI[:, 128:192], [pIk], ["vc_c%d" % i], eng=ev)
                        kb.cp(rc_c[i][:, blk, :], pI[:, 192:256], [pIk], ["rc_c%d" % i], eng=ev)

                def swa(c):
                    i = c % 2
                    mk_ = "mo%d" % i
                    for blk in range(4):
                        n = 4 * c + blk
                        msk = m_n0 if n == 0 else (m_n1 if n == 1 else m_gen)
                        mk = "m_n0" if n == 0 else ("m_n1" if n == 1 else "m_gen")
                        for hh in range(2):
                            hs = slice(hh * 64, (hh + 1) * 64)
                            kb.mm(pA[:, 0:256], qa_c[i][hs, blk * 128:(blk + 1) * 128], ka_c[i][hs, blk * 128:blk * 128 + 256],
                                  True, True, ["qa_c%d" % i, "ka_c%d" % i], ["pA"])
                            kb.stt(sm[:], pA[:, 0:256], 0.125, msk[:], ALU.mult, ALU.add, ["pA", mk], ["sm"])
                            kb.op("dve", lambda e: e.tensor_reduce(out=st8[:, 0:1], in_=sm[:], axis=AX.X, op=ALU.max), ["sm"], ["st8"])
                            kb.tt(st8[:, 0:1], st8[:, 0:1], sk_t[:, hh:hh + 1], ALU.max, ["st8", "sk"], ["st8"])
                            kb.ts(st8[:, 1:2], st8[:, 0:1], -1.0, ALU.mult, ["st8"], ["st8"])
                            kb.act(pexp[:], sm[:], AF.Exp, ["sm", "st8"], ["pexp", "st8"], bias=st8[:, 1:2], scale=1.0, accum_out=st8[:, 2:3])
                            kb.act(st8[:, 3:4], sk_t[:, hh:hh + 1], AF.Exp, ["sk", "st8"], ["st8"], bias=st8[:, 1:2], scale=1.0)
                            kb.tt(st8[:, 4:5], st8[:, 2:3], st8[:, 3:4], ALU.add, ["st8"], ["st8"])
                            kb.op("dve", lambda e: e.reciprocal(out=st8[:, 5:6], in_=st8[:, 4:5]), ["st8"], ["st8"])
                            kb.tr(pT_bf[:, 0:128], pexp[:, 0:128], idb[:], ["pexp", "ident_b"], ["pB"], inc=False)
                            kb.tr(pT_bf[:, 128:256], pexp[:, 128:256], idb[:], ["pexp", "ident_b"], ["pB"])
                            kb.cp(pTs[:], pT_bf[:, 0:256], ["pB"], ["pTs"], eng="act")
                            kb.mm(pA[:, 256:320], pTs[:, 0:128], va_c[i][:, blk, :], True, False, ["pTs", "va_c%d" % i], ["pA"], inc=False)
                            kb.mm(pA[:, 256:320], pTs[:, 128:256], va_c[i][:, blk + 1, :], False, True, ["pTs", "va_c%d" % i], ["pA"])
                            kb.ts(mo[i][:, blk, hh * 64:(hh + 1) * 64], pA[:, 256:320], st8[:, 5:6], ALU.mult, ["pA", "st8"], [mk_])

                def gla(c):
                    i = c % 2
                    kb.mm(pX[0:32, :], w2b[:], gl_c[i][:], True, True, ["w2b", "gl_c%d" % i], ["pX"])
                    kb.act(ge[:], pX[0:32, :], AF.Exp, ["pX", "gbt"], ["ge"], bias=gbt[:, 0:1], scale=-1.0)
                    kb.act(gsp[:], ge[:], AF.Ln, ["ge"], ["gsp"], bias=1.0, scale=1.0)
                    kb.op("dve", lambda e: e.tensor_tensor_scan(out=gcs[:], data0=rmask[:], data1=gsp[:], initial=0.0, op0=ALU.mult, op1=ALU.add),
                          ["rmask", "gsp"], ["gcs"])
                    kb.act(geq[:], gcs[:], AF.Exp, ["gcs"], ["geq"], scale=-1.0 / 16.0)
                    kb.act(gek[:], gcs[:], AF.Exp, ["gcs"], ["gek"], scale=1.0 / 16.0)
                    kb.stt(qt[:], qc_c[i][:], 32.0 ** -0.5, geq[:], ALU.mult, ALU.mult, ["qc_c%d" % i, "geq"], ["qt"])
                    kb.tt(kt[:], kc_c[i][:], gek[:], ALU.mult, ["kc_c%d" % i, "gek"], ["kt"])
                    for blk in range(4):
                        bs = slice(blk * 128, (blk + 1) * 128)
                        kb.mm(pA[:, 384:512], kt[:, bs], qt[:, bs], True, True, ["kt", "qt"], ["pA"])
                        kb.tt(scb[:], pA[:, 384:512], mle[:], ALU.mult, ["pA", "mle"], ["scb"])
                        kb.tr(pT_bf[:, 512:544], kt[:, bs], idb[0:32, 0:32], ["kt", "ident_b"], ["pB"])
                        kb.cp(ktm[:], pT_bf[:, 512:544], ["pB"], ["ktm"], eng="act")
                        kb.mm(pA[:, 320:384], scb[:], vc_c[i][:, blk, :], True, False, ["scb", "vc_c%d" % i], ["pA"], inc=False)
                        kb.mm(pA[:, 320:384], qt[:, bs], Sb[:], False, True, ["qt", "Sb"], ["pA"])
                        kb.cp(mo[i][:, blk, 192:256], pA[:, 320:384], ["pA"], ["mo%d" % i], eng="act")
                        kb.mm(pB[0:32, 384:448], ktm[:], vc_c[i][:, blk, :], True, True, ["ktm", "vc_c%d" % i], ["pB"])
                        kb.tt(stmp[:], pB[0:32, 384:448], S[:], ALU.add, ["pB", "S"], ["stmp"])
                        kb.ts(S[:], stmp[:], geq[:, blk * 128 + 127:blk * 128 + 128], ALU.mult, ["stmp", "geq"], ["S"])
                        kb.cp(Sb[:], S[:], ["S"], ["Sb"])

                def sbk(c):
                    i = c % 2
                    nkb = 4 * c + 4
                    npairs = nkb // 2
                    qk = "qs_c%d" % i

                    def kof(p, j):
                        return nkb - 1 - (2 * p + j)

                    def zmm(p):
                        for j in range(2):
                            kblk = kof(p, j)
                            kb.mm(pz[j][:], KbT[:, kblk * 128:(kblk + 1) * 128], qs_c[i][:], True, True, ["KbT", qk], ["pz%d" % j])

                    zmm(0)
                    for p in range(npairs + 2):
                        pp = p % 2
                        if p < npairs:
                            for j in range(2):
                                kb.act(e_t[j][:], pz[j][:], AF.Exp, ["pz%d" % j], ["e_t%d" % j])
                            for j in range(2):
                                kb.act(sp_t[pp][j][:], e_t[j][:], AF.Ln, ["e_t%d" % j], ["sp_t%d_%d" % (pp, j)], bias=1.0, scale=1.0)
                            for j in range(2):
                                dg = kof(p, j) - 4 * c
                                if dg >= 0:
                                    kb.tt(sp_t[pp][j][:], sp_t[pp][j][:], sbm[dg][:], ALU.mult, ["sp_t%d_%d" % (pp, j), "sbm%d" % dg], ["sp_t%d_%d" % (pp, j)])
                        back = 1 <= p <= npairs
                        if back:
                            q = p - 1
                            qq = q % 2
                            first, lastp = (q == 0), (q == npairs - 1)
                            for j in range(2):
                                kblk = kof(q, j)
                                sk_ = "sp_t%d_%d" % (qq, j)
                                kb.mm(pc[j][:], ntri[:], sp_t[qq][j][:], True, False, ["ntri", sk_], ["pc%d" % j], inc=False)
                                if j == 1:
                                    kb.mm(pc[j][:], nones[:], sp_t[qq][0][:], False, False, ["nones", "sp_t%d_0" % qq], ["pc%d" % j], inc=False)
                                if not first:
                                    kb.mm(pc[j][:], nones[:], R[:], False, False, ["nones", "R"], ["pc%d" % j], inc=False)
                                kb.mm(pc[j][:], KbT[:, kblk * 128:(kblk + 1) * 128], qs_c[i][:], False, True, ["KbT", qk], ["pc%d" % j])
                        if p + 1 < npairs:
                            zmm(p + 1)
                        if p >= 2:
                            r_ = p - 2
                            rr = r_ % 2
                            for j in range(2):
                                kblk = kof(r_, j)
                                kb.mm(pO[0:64, :], Vb[:, kblk, :], a_t[rr][j][:], (r_ == 0 and j == 0), (kblk == 0), ["Vb", "a_t%d_%d" % (rr, j)], ["pO"])
                        if back:
                            if not lastp:
                                if first:
                                    kb.tt(R[:], sp_t[qq][0][:], sp_t[qq][1][:], ALU.add, ["sp_t%d_0" % qq, "sp_t%d_1" % qq], ["R"])
                                else:
                                    kb.tt(R[:], R[:], sp_t[qq][0][:], ALU.add, ["R", "sp_t%d_0" % qq], ["R"])
                                    kb.tt(R[:], R[:], sp_t[qq][1][:], ALU.add, ["R", "sp_t%d_1" % qq], ["R"])
                            for j in range(2):
                                kb.act(a_t[qq][j][:], pc[j][:], AF.Exp, ["pc%d" % j], ["a_t%d_%d" % (qq, j)])
                            for j in range(2):
                                dg = kof(q, j) - 4 * c
                                if dg >= 0:
                                    kb.tt(a_t[qq][j][:], a_t[qq][j][:], sbm[dg][:], ALU.mult, ["a_t%d_%d" % (qq, j), "sbm%d" % dg], ["a_t%d_%d" % (qq, j)])
                    kb.cp(ob_t[:], pO[0:64, :], ["pO"], ["ob_t"])

                def post(c):
                    i = c % 2
                    mk_ = "mo%d" % i
                    for blk in range(4):
                        kb.tr(pA[:, blk * 64:(blk + 1) * 64], ob_t[:, blk * 128:(blk + 1) * 128], idf[0:64, 0:64], ["ob_t", "ident_f"], ["pA"], inc=(blk == 3))
                    kb.cp(mo[i][:, :, 128:192], pA[:, 0:256].rearrange("p (b d) -> p b d", d=64), ["pA"], [mk_], eng="act")
                    mof = mo[i][:].rearrange("p b c -> p (b c)")
                    kb.act(sq4[:], mof, AF.Square, [mk_], ["sq4"])
                    kb.op("dve", lambda e: e.tensor_reduce(out=s16[:], in_=sq4[:].rearrange("p (h d) -> p h d", d=64), axis=AX.X, op=ALU.add),
                          ["sq4"], ["s16"])
                    kb.ts(s16[:], s16[:], 1.0 / 64, ALU.mult, ["s16"], ["s16"], s2=EPS, op1=ALU.add)
                    kb.act(s16[:], s16[:], AF.Sqrt, ["s16"], ["s16"])
                    kb.op("dve", lambda e: e.reciprocal(out=s16[:], in_=s16[:]), ["s16"], ["s16"])
                    kb.tt(m14[:].rearrange("p (h d) -> p h d", d=64), mof.rearrange("p (h d) -> p h d", d=64),
                          s16[:].unsqueeze(2).to_broadcast([128, 16, 64]), ALU.mult, [mk_, "s16"], ["m14"])
                    kb.act(sil4[:], rc_c[i][:], AF.Silu, ["rc_c%d" % i], ["sil4"])
                    m14v = m14[:].rearrange("p (b c) -> p b c", c=256)
                    kb.tt(m14v[:, :, 192:256], m14v[:, :, 192:256], sil4[:], ALU.mult, ["m14", "sil4"], ["m14"])
                    kb.tt(mixb4[:].rearrange("p (b c) -> p b c", c=256), m14v, gm[:].unsqueeze(1).to_broadcast([128, 4, 256]), ALU.mult,
                          ["m14", "gm"], ["mixb4"])
                    for t8 in range(8):
                        kb.tr(pT_bf[:, t8 * 128:(t8 + 1) * 128], mixb4[:, t8 * 128:(t8 + 1) * 128], idb[:], ["mixb4", "ident_b"], ["pB"], inc=(t8 == 7))
                    kb.cp(mixT4[:], pT_bf, ["pB"], ["mixT4"], eng="act")
                    for blk in range(4):
                        n = 4 * c + blk
                        pk = "pt%d" % (n % 2)
                        for dh in range(2):
                            pbank, pkey = (pA, "pA") if dh == 0 else (pX, "pX")
                            for kc in range(2):
                                kb.mm(pbank[:], mixT4[:, (2 * blk + kc) * 128:(2 * blk + kc + 1) * 128], Wo[:, kc, dh * 512:(dh + 1) * 512], kc == 0, kc == 1,
                                      ["mixT4", "Wo"], [pkey], inc=(kc == 1))
                            kb.cp(pt[n % 2][:, dh * 512:(dh + 1) * 512], pbank[:], [pkey], [pk], eng=("act" if dh else "dve"))
                        kb.dma("pool", part_loc[n * 128:(n + 1) * 128, :], pt[n % 2][:], reads=[pk], writes=["part_loc"])

                load_xc(0)
                for c in range(NCH):
                    if _STOP == "B0":
                        continue
                    if c + 1 < NCH:
                        load_xc(c + 1)
                    if _STOP == "B0x":
                        continue
                    inproj(c)
                    if _STOP in ("B1", "B1f"):
                        continue
                    swa(c)
                    gla(c)
                    sbk(c)
                    if _STOP == "B2":
                        continue
                    post(c)
                kb.barrier()
                if _STOP not in ("B0", "B0x", "B1", "B1f", "B2", "B3"):
                    kb.coll("ReduceScatter", ALU.add, RG, part_loc, delta_loc, ["part_loc"], ["delta_loc"])
                phase_end()
            if _STOP in ("B", "B0", "B0x", "B1", "B1f", "B2", "B3"):
                break
            with ExitStack() as sc_:
                T = lambda name, shape, dt=F32, _p="L%dC_" % li: sc_.enter_context(nc.sbuf_tensor(_p + name, shape, dt))
                gft = T("gft", [128, 8])
                kb.dma("sp", gft[:], gffn_i[li], writes=["gt"])
                Wq = T("Wq", [128, 8, 2048], BF16); Ks = T("Ks", [128, 16, 64], BF16); ksf = T("ksf", [128, 16, 64])
                kb.dma("sp", ksf[:], ksub_i[li], writes=["ksf"])
                kb.cp(Ks[:], ksf[:], ["ksf"], ["Ks"])
                load_w(Wq, wq_i[li], 8, 2048, "Wq", gt=gft)
                ht = [T("ht%d" % i, [128, D]) for i in range(2)]
                dt_ = [T("dt%d" % i, [128, D]) for i in range(2)]
                h1 = T("h1", [128, D]); junk = T("junk", [128, D], BF16); ss = T("ss", [128, 1])
                xn = T("xn", [128, D], BF16); xnT = T("xnT", [128, D], BF16)
                qT = T("qT", [128, 16, 128], BF16); sct = T("sct", [128, D])
                t1 = T("t1", [128, 8, 16]); t2 = T("t2", [128, 8, 16]); wk1 = T("wk1", [128, 16, 64]); wk2 = T("wk2", [128, 8, 256])
                cand = T("cand", [128, 8, 256]); c8a = T("c8a", [128, 8, 8]); c8b = T("c8b", [128, 8, 8])
                csh = T("csh", [128, 8, 256]); ec = T("ec", [128, 8, 256]); mk8 = T("mk8", [128, 8, 256])
                Z = T("Z", [128, 8]); tb = T("tb", [128, 16])
                pT = ps[0][:].bitcast(BF16)
                for i in range(NT):
                    b = i % 2
                    rs = slice(i * 128, (i + 1) * 128)
                    kb.dma("sp", ht[b][:], hsrc[rs, :], writes=["ht%d" % b])
                    kb.dma("sp", dt_[b][:], delta_loc[rs, :], writes=["dt%d" % b])
                    kb.tt(h1[:], ht[b][:], dt_[b][:], ALU.add, ["ht%d" % b, "dt%d" % b], ["h1"])
                    kb.dma("pool", h1_d[rs, :], h1[:], reads=["h1"], writes=["h1_d"])
                    _rstd(kb, h1[:], junk[:], ss[:], D, ["h1"], "b")
                    kb.ts(xn[:], h1[:], ss[:, 0:1], ALU.mult, ["h1", "bss"], ["xn"])
                    for k in range(8):
                        kb.tr(pT[:, k * 128:(k + 1) * 128], xn[:, k * 128:(k + 1) * 128], idb[:], ["xn", "ident_b"], ["pT"], inc=(k == 7))
                    kb.cp(xnT[:], pT, ["pT"], ["xnT"], eng="act")
                    kb.dma("pool", xT_d[rs, :], xnT[:], reads=["xnT"], writes=["xT_d"])
                    for cg in range(4):
                        pq = ps[3 + cg % 2]
                        for cc in range(4):
                            cidx = cg * 4 + cc
                            for k in range(8):
                                kb.mm(pq[:, cc * 128:(cc + 1) * 128], Wq[:, k, cidx * 128:(cidx + 1) * 128], xnT[:, k * 128:(k + 1) * 128],
                                      k == 0, k == 7, ["Wq", "xnT"], ["pq%d" % (cg % 2)], inc=(k == 7 and cc == 3))
                        kb.cp(qT[:, cg * 4:(cg + 1) * 4, :], pq[:].rearrange("p (c t) -> p c t", t=128), ["pq%d" % (cg % 2)], ["qT"],
                              eng=("act" if cg % 2 else "dve"))
                    for cidx in range(16):
                        pscb = ps[5 + cidx // 8]
                        kb.mm(pscb[:, (cidx % 8) * 64:(cidx % 8 + 1) * 64], qT[:, cidx, :], Ks[:, cidx, :], True, True, ["qT", "Ks"],
                              ["psc%d" % (cidx // 8)], inc=(cidx % 8 == 7))
                    kb.cp(sct[:, 0:512], ps[5][:], ["psc0"], ["sct"], eng="act")
                    kb.cp(sct[:, 512:1024], ps[6][:], ["psc1"], ["sct"], eng="dve")
                    kb.dma("pool", sc_d[rs, :], sct[:], reads=["sct"], writes=["sc_d"])
                    chains = [(hd, side, (t1, t2)[side]) for hd in range(8) for side in range(2)]
                    tkeys = ["tt%d_%d" % (side, hd) for hd, side, _ in chains]
                    for ci_, (hd, side, tt_) in enumerate(chains):
                        sv = sct[:, hd * 128 + side * 64:hd * 128 + side * 64 + 64]
                        kb.op("dve", lambda e, o_=tt_[:, hd, 0:8], i_=sv: e.max(out=o_, in_=i_), ["sct"], [tkeys[ci_]])
                    for ci_, (hd, side, tt_) in enumerate(chains):
                        sv = sct[:, hd * 128 + side * 64:hd * 128 + side * 64 + 64]
                        kb.op("dve", lambda e, o_=wk1[:, ci_, :], r_=tt_[:, hd, 0:8], i_=sv: e.match_replace(out=o_, in_to_replace=r_, in_values=i_, imm_value=-1e30),
                              ["sct", tkeys[ci_]], ["wk1_%d" % ci_])
                    for ci_, (hd, side, tt_) in enumerate(chains):
                        kb.op("dve", lambda e, o_=tt_[:, hd, 8:16], i_=wk1[:, ci_, :]: e.max(out=o_, in_=i_), ["wk1_%d" % ci_], [tkeys[ci_]])
                    kb.tt(cand[:].rearrange("p h (a b) -> p h a b", b=16), t1[:].unsqueeze(3).to_broadcast([128, 8, 16, 16]),
                          t2[:].unsqueeze(2).to_broadcast([128, 8, 16, 16]), ALU.add, tkeys, ["cand"])
                    ckeys = ["c8_%d" % hd for hd in range(8)]
                    for hd in range(8):
                        kb.op("dve", lambda e, o_=c8a[:, hd, :], i_=cand[:, hd, :]: e.max(out=o_, in_=i_), ["cand"], [ckeys[hd]])
                    for hd in range(8):
                        kb.op("dve", lambda e, o_=wk2[:, hd, :], r_=c8a[:, hd, :], i_=cand[:, hd, :]: e.match_replace(out=o_, in_to_replace=r_, in_values=i_, imm_value=-1e30),
                              ["cand", ckeys[hd]], ["wk2_%d" % hd])
                    for hd in range(8):
                        kb.op("dve", lambda e, o_=c8b[:, hd, :], i_=wk2[:, hd, :]: e.max(out=o_, in_=i_), ["wk2_%d" % hd], [ckeys[hd]])
                    kb.tt(csh[:], cand[:], c8a[:, :, 0:1].to_broadcast([128, 8, 256]), ALU.subtract, ["cand"] + ckeys, ["csh"])
                    kb.act(ec[:], csh[:], AF.Exp, ["csh"], ["ec"])
                    kb.tt(mk8[:], cand[:], c8b[:, :, 7:8].to_broadcast([128, 8, 256]), ALU.is_ge, ["cand"] + ckeys, ["mk8"])
                    kb.tt(ec[:], ec[:], mk8[:], ALU.mult, ["ec", "mk8"], ["ec"])
                    kb.op("dve", lambda e: e.tensor_reduce(out=Z[:], in_=ec[:], axis=AX.X, op=ALU.add), ["ec"], ["Z"])
                    kb.act(Z[:], Z[:], AF.Ln, ["Z"], ["Z"])
                    kb.cp(tb[:, 0:8], c8b[:, :, 7], ckeys, ["tb"])
                    kb.stt(tb[:, 8:16], c8a[:, :, 0], -1.0, Z[:], ALU.mult, ALU.subtract, ckeys + ["Z"], ["tb"])
                    kb.dma("pool", tb_d[rs, :], tb[:], reads=["tb"], writes=["tb_d"])
                phase_end()
            if _STOP == "C":
                break
            with ExitStack() as sd_:
                T = lambda name, shape, dt=F32, _p="L%dD_" % li: sd_.enter_context(nc.sbuf_tensor(_p + name, shape, dt))
                gft = T("gft", [128, 8])
                kb.dma("sp", gft[:], gffn_i[li], writes=["gt"])
                Ub = T("Ub", [128, 8, 4096], BF16); Vv = T("Vv", [128, 32, D], BF16)
                load_w(Ub, uT_i[li], 8, 4096, "Ub", gt=gft)
                load_w(Vv, v_i[li], 32, D, "Vv")
                gf = T("gf", [128, D])
                if final:
                    kb.dma("sp", gf[:], gfin, writes=["gf"])
                h1t = [T("h1t%d" % i, [128, D]) for i in range(2)]
                sct = [T("sctb%d" % i, [128, D]) for i in range(2)]
                xT = [T("xTb%d" % i, [128, D], BF16) for i in range(2)]
                tbt = [T("tbt%d" % i, [128, 16]) for i in range(2)]
                NBG = 4
                Sg = [T("Sg%d" % i, [128, 16, 64]) for i in range(NBG)]
                Eg = [T("Eg%d" % i, [128, 1024], BF16) for i in range(NBG)]
                Gh = [T("Gh%d" % i, [128, 1024], BF16) for i in range(2)]; G = T("G", [128, 1024], BF16)
                gl = [T("gl%d" % i, [128, 512]) for i in range(2)]
                Wb = T("Wb", [128, 1024], BF16); WT = T("WT", [128, 1024], BF16)
                ho = T("ho", [128, D]); junk = T("junkb", [128, D], BF16); ss = T("ssb", [128, 1])
                pH = [ps[0], ps[1]]; ptr = ps[2][:].bitcast(BF16); po = [ps[3], ps[4]]

                def loadB(i):
                    b = i % 2
                    rs = slice(i * 128, (i + 1) * 128)
                    kb.dma("sp", h1t[b][:], h1_d[rs, :], writes=["h1t%d" % b])
                    kb.dma("sp", sct[b][:], sc_d[rs, :], writes=["sctb%d" % b])
                    kb.dma("sp", xT[b][:], xT_d[rs, :], writes=["xTb%d" % b])
                    kb.dma("sp", tbt[b][:], tb_d[rs, :], writes=["tbt%d" % b])

                loadB(0)
                cnt = 0
                for i in range(NT):
                    b = i % 2
                    rs = slice(i * 128, (i + 1) * 128)
                    if i + 1 < NT:
                        loadB(i + 1)
                    for eq in range(4):
                        deferred = None
                        for hd in range(8):
                            j = cnt % NBG
                            gj = cnt % 2
                            cnt += 1
                            s1 = sct[b][:, hd * 128 + 16 * eq:hd * 128 + 16 * eq + 16]
                            s2 = sct[b][:, hd * 128 + 64:hd * 128 + 128]
                            kb.tt(Sg[j][:], s1.unsqueeze(2).to_broadcast([128, 16, 64]), s2.unsqueeze(1).to_broadcast([128, 16, 64]), ALU.add,
                                  ["sctb%d" % b], ["Sg%d" % j], eng=("pool" if hd in _POOL_HEADS else "dve"))
                            Sf = Sg[j][:].rearrange("p a b -> p (a b)")
                            kb.act(Eg[j][:], Sf, AF.Exp, ["Sg%d" % j, "tbt%d" % b], ["Eg%d" % j], bias=tbt[b][:, 8 + hd:9 + hd], scale=1.0)
                            if hd == 0:
                                kb.stt(G[:], Sf, tbt[b][:, hd:hd + 1], Eg[j][:], ALU.is_ge, ALU.mult, ["Sg%d" % j, "Eg%d" % j, "tbt%d" % b], ["G"])
                            else:
                                kb.stt(Gh[gj][:], Sf, tbt[b][:, hd:hd + 1], Eg[j][:], ALU.is_ge, ALU.mult, ["Sg%d" % j, "Eg%d" % j, "tbt%d" % b], ["Gh%d" % gj])
                                if deferred is not None:
                                    deferred()
                                deferred = (lambda gj=gj: kb.tt(G[:], G[:], Gh[gj][:], ALU.add, ["G", "Gh%d" % gj], ["G"]))
                        if deferred is not None:
                            deferred()
                        for g2 in range(2):
                            e0 = eq * 1024 + g2 * 512
                            for k in range(8):
                                kb.mm(pH[g2][:], xT[b][:, k * 128:(k + 1) * 128], Ub[:, k, e0:e0 + 512], k == 0, k == 7,
                                      ["xTb%d" % b, "Ub"], ["pH%d" % g2], inc=(k == 7))
                            kb.act(gl[g2][:], pH[g2][:], AF.Gelu, ["pH%d" % g2], ["gl%d" % g2])
                            kb.tt(Wb[:, g2 * 512:(g2 + 1) * 512], gl[g2][:], G[:, g2 * 512:(g2 + 1) * 512], ALU.mult, ["gl%d" % g2, "G"], ["Wb"])
                        for cc in range(8):
                            kb.tr(ptr[:, cc * 128:(cc + 1) * 128], Wb[:, cc * 128:(cc + 1) * 128], idb[:], ["Wb", "ident_b"], ["ptr"], inc=(cc == 7))
                        kb.cp(WT[:], ptr, ["ptr"], ["WT"], eng="act")
                        for dh in range(2):
                            for cc in range(8):
                                kb.mm(po[dh][:], WT[:, cc * 128:(cc + 1) * 128], Vv[:, eq * 8 + cc, dh * 512:(dh + 1) * 512],
                                      (eq == 0 and cc == 0), (eq == 3 and cc == 7), ["WT", "Vv"], ["po%d" % dh], inc=(cc == 7))
                    for dh in range(2):
                        kb.tt(ho[:, dh * 512:(dh + 1) * 512], h1t[b][:, dh * 512:(dh + 1) * 512], po[dh][:], ALU.add,
                              ["h1t%d" % b, "po%d" % dh], ["ho"])
                    if final:
                        _rstd(kb, ho[:], junk[:], ss[:], D, ["ho"], "f")
                        kb.stt(ho[:], ho[:], ss[:, 0:1], gf[:], ALU.mult, ALU.mult, ["ho", "fss", "gf"], ["ho"])
                    kb.dma("sp", hdst[rs, :], ho[:], reads=["ho"], writes=["hdst"])
                phase_end()
    return nc


def forward_fused(x, meta_tokens, attn_norm, w_in, attn_sinks, gla_gate_w2, gla_gate_b, swa_out_norm,
                  sb_out_norm, gla_out_norm, w_out, ffn_norm, peer_w_q, peer_sub_keys, peer_u, peer_v, final_norm, runner=None):
    f32 = lambda a: np.asarray(a, np.float32)
    x = f32(x)
    B, SEQ, _ = x.shape
    depth = attn_norm.shape[0]
    L = SEQ + 128
    Lp = ((L + 511) // 512) * 512
    TQ = Lp // 4
    assert B * 4 == NCORES
    nc = _prog_fused(Lp, depth)
    w_in, w_out = f32(w_in), f32(w_out)
    shared = {
        "g_attn": _c(np.stack([_gk(attn_norm[i]) for i in range(depth)])),
        "gffn": _c(np.stack([_gk(ffn_norm[i]) for i in range(depth)])),
        "wq": _c(f32(peer_w_q)),
        "ksub": _c(np.stack([np.transpose(f32(peer_sub_keys[i]).reshape(16, 64, 128), (2, 0, 1)) for i in range(depth)])),
        "uT": _c(np.transpose(f32(peer_u), (0, 2, 1))), "v": _c(f32(peer_v)),
        "gfin": _c(np.broadcast_to(f32(final_norm)[None, :], (128, D))),
    }
    per_j = []
    for j in range(4):
        kv = j // 2
        cols = np.concatenate([np.arange(128 * j, 128 * j + 128), np.arange(512 + 64 * kv, 512 + 64 * kv + 64),
                               np.arange(512 + 64 * kv, 512 + 64 * kv + 64), np.arange(768 + 64 * j, 768 + 64 * j + 64),
                               np.arange(1024 + 64 * j, 1024 + 64 * j + 64), np.arange(1536 + 32 * j, 1536 + 32 * j + 32),
                               np.arange(1664 + 32 * j, 1664 + 32 * j + 32), np.arange(2048, 2064),
                               np.arange(640 + 64 * kv, 640 + 64 * kv + 64), np.arange(1280 + 64 * j, 1280 + 64 * j + 64),
                               np.arange(1792 + 64 * j, 1792 + 64 * j + 64), np.arange(2064 + 64 * j, 2064 + 64 * j + 64)])
        assert len(cols) == CJ
        rows = np.concatenate([np.arange(128 * j, 128 * j + 128), np.arange(512 + 64 * j, 512 + 64 * j + 64),
                               np.arange(768 + 64 * j, 768 + 64 * j + 64)])
        gm = np.stack([np.concatenate([f32(swa_out_norm[i])[128 * j:128 * j + 128], f32(sb_out_norm[i])[64 * j:64 * j + 64],
                                       f32(gla_out_norm[i])[64 * j:64 * j + 64]]) for i in range(depth)])
        per_j.append({
            "w_in": _c(w_in[:, :, cols]),
            "w2": _c(f32(gla_gate_w2)[:, :, 32 * j:32 * j + 32]),
            "gb": _c(f32(gla_gate_b)[:, 32 * j:32 * j + 32].reshape(depth, 32, 1)),
            "sinks": _c(np.broadcast_to(f32(attn_sinks)[:, None, 2 * j:2 * j + 2], (depth, 128, 2))),
            "gmix": _c(np.broadcast_to(gm[:, None, :], (depth, 128, 256))),
            "wout": _c(np.transpose(w_out[:, rows, :].reshape(depth, 2, 128, D), (0, 2, 1, 3))),
        })
    maps = []
    for c in range(NCORES):
        b, r = c // 4, c % 4
        hp = np.zeros((Lp, D), np.float32)
        hp[112:128] = f32(meta_tokens)
        hp[128:L] = x[b]
        maps.append(dict(shared, **per_j[r], h0=_c(hp[r * TQ:(r + 1) * TQ])))
    res = (runner or _run)(nc, maps)
    out = np.zeros((B, SEQ, D), np.float32)
    for b in range(B):
        full = np.concatenate([np.asarray(res[b * 4 + r]["out"]) for r in range(4)], 0)
        out[b] = full[128:L]
    return out


def _prog_fused(Lp, depth):
    key = ("fused", Lp, depth)
    if key not in _CACHE:
        _CACHE[key] = build_fused(Lp, depth)
    return _CACHE[key]
```

```python
import numpy as np
from contextlib import ExitStack
import ml_dtypes
import concourse.bass as bass
import concourse.mybir as mybir
from concourse.bass_utils import run_bass_kernel_spmd

F32 = mybir.dt.float32
BF16 = mybir.dt.bfloat16
AF = mybir.ActivationFunctionType
ALU = mybir.AluOpType
AX = mybir.AxisListType

D = 1024
IN_COLS = 2320
EPS = 1e-6
NEG = -30000.0
NCORES = 8


class KB:
    ENGS = ("pe", "act", "dve", "pool", "sp")
    NDMA = 8

    def __init__(self, nc, stack):
        self.nc = nc
        self._stack = stack
        self.q = {e: [] for e in self.ENGS}
        self.cnt = {e: 0 for e in self.ENGS}
        self.sem = {e: stack.enter_context(nc.semaphore("s_" + e)) for e in self.ENGS}
        self.dsem = {e: [stack.enter_context(nc.semaphore("d_%s%d" % (e, i))) for i in range(self.NDMA)]
                     for e in ("sp", "pool")}
        self.dcnt = {e: [0] * self.NDMA for e in self.dsem}
        self.drot = {e: 0 for e in self.dsem}
        self.seen = {e: {} for e in self.ENGS}
        self.lastw = {}
        self.readers = {}
        self.pending_noinc = {e: False for e in self.ENGS}
        self.excl = set()

    def _deps(self, eng, reads, writes):
        toks = []
        for k in list(reads) + list(writes):
            t = self.lastw.get(k)
            if t is not None:
                toks.append(t)
        for k in writes:
            toks.extend(self.readers.get(k, {}).values())
        waits = {}
        for (sem, val, src) in toks:
            if src == "pe" and eng == "pe":
                continue
            sid = id(sem)
            if self.seen[eng].get(sid, 0) >= val:
                continue
            if sid not in waits or waits[sid][1] < val:
                waits[sid] = (sem, val)
        for sid, (sem, val) in waits.items():
            self.seen[eng][sid] = val
        return list(waits.values())

    def _record(self, rkey, tok, reads, writes):
        for k in writes:
            self.lastw[k] = tok
            self.readers[k] = {}
        for k in reads:
            self.readers.setdefault(k, {})[rkey] = tok

    def op(self, eng, fn, reads=(), writes=(), inc=True):
        ex = [k for k in reads if k in self.excl and k not in writes]
        if ex:
            writes = list(writes) + ex
        waits = self._deps(eng, reads, writes)
        sem = self.sem[eng]
        if inc:
            self.cnt[eng] += 1
            tok = (sem, self.cnt[eng], eng)
            self.pending_noinc[eng] = False
        else:
            tok = (sem, self.cnt[eng] + 1, eng)
            self.pending_noinc[eng] = True
        self.q[eng].append((waits, fn, (sem, 1) if inc else None))
        self._record(eng, tok, reads, writes)

    def dma(self, eng, out, in_, reads=(), writes=()):
        waits = self._deps(eng, reads, writes)
        r = self.drot[eng]
        self.drot[eng] = (r + 1) % self.NDMA
        sem = self.dsem[eng][r]
        prev = self.dcnt[eng][r]
        if prev > 0 and self.seen[eng].get(id(sem), 0) < prev:
            waits.append((sem, prev))
            self.seen[eng][id(sem)] = prev
        self.dcnt[eng][r] += 16
        tok = (sem, self.dcnt[eng][r], "dma")
        self.q[eng].append((waits, lambda e, o=out, i=in_: e.dma_start(out=o, in_=i), (sem, 16)))
        self._record(("dma", id(sem)), tok, reads, writes)

    def coll(self, kind, op, groups, in_, out, reads, writes):
        waits = self._deps("pool", reads, writes)
        if not hasattr(self, "csem"):
            self.csem = self._stack.enter_context(self.nc.semaphore("s_cc"))
            self.ccnt = 0
        if self.ccnt > 0 and self.seen["pool"].get(id(self.csem), 0) < self.ccnt:
            waits.append((self.csem, self.ccnt))
            self.seen["pool"][id(self.csem)] = self.ccnt
        self.ccnt += 1
        tok = (self.csem, self.ccnt, "dma")
        self.q["pool"].append((waits, lambda e, k=kind, o=op, g=groups, i=in_, u=out:
                               e.collective_compute(k, o, replica_groups=g, ins=[i.opt()], outs=[u.opt()]), (self.csem, 1)))
        self._record(("dma", id(self.csem)), tok, reads, writes)

    def wait_all(self, eng, keys):
        waits = self._deps(eng, keys, ())
        self.q[eng].append((waits, None, None))

    def barrier(self):
        for e in self.ENGS:
            waits = []
            for f in self.ENGS:
                if f != e and self.cnt[f] > 0 and self.seen[e].get(id(self.sem[f]), 0) < self.cnt[f]:
                    waits.append((self.sem[f], self.cnt[f]))
                    self.seen[e][id(self.sem[f])] = self.cnt[f]
            for q in self.dsem:
                for r in range(self.NDMA):
                    s, v = self.dsem[q][r], self.dcnt[q][r]
                    if v > 0 and self.seen[e].get(id(s), 0) < v:
                        waits.append((s, v))
                        self.seen[e][id(s)] = v
            if hasattr(self, "csem") and self.ccnt > 0 and self.seen[e].get(id(self.csem), 0) < self.ccnt:
                waits.append((self.csem, self.ccnt))
                self.seen[e][id(self.csem)] = self.ccnt
            self.q[e].append((waits, None, None))

    def emit(self):
        nc = self.nc
        for e in self.ENGS:
            assert not self.pending_noinc[e], "trailing non-inc op on " + e
        qs = self.q
        self.q = {e: [] for e in self.ENGS}
        with nc.Block() as block:
            def run(engname):
                def body(e):
                    for waits, fn, inc in qs[engname]:
                        for sem, val in waits:
                            e.wait_ge(sem, val)
                        if fn is None:
                            continue
                        ins = fn(e)
                        if inc is not None:
                            ins.then_inc(inc[0], inc[1])
                return body
            block.tensor(run("pe"))
            block.scalar(run("act"))
            block.vector(run("dve"))
            block.gpsimd(run("pool"))
            block.sync(run("sp"))

    def act(self, out, in_, func, r, w, eng="act", **kw):
        self.op(eng, lambda e, o=out, i=in_, f=func, k=kw: e.activation(out=o, in_=i, func=f, **k), r, w)

    def tt(self, out, in0, in1, op, r, w, eng="dve"):
        self.op(eng, lambda e, o=out, a=in0, b=in1, p=op: e.tensor_tensor(out=o, in0=a, in1=b, op=p), r, w)

    def ts(self, out, in0, s1, op0, r, w, s2=None, op1=None, eng="dve"):
        if op1 is None:
            self.op(eng, lambda e, o=out, a=in0, x=s1, p=op0: e.tensor_scalar(out=o, in0=a, scalar1=x, scalar2=None, op0=p), r, w)
        else:
            self.op(eng, lambda e, o=out, a=in0, x=s1, y=s2, p=op0, q=op1: e.tensor_scalar(out=o, in0=a, scalar1=x, scalar2=y, op0=p, op1=q), r, w)

    def stt(self, out, in0, scalar, in1, op0, op1, r, w, **kw):
        self.op("dve", lambda e, o=out, a=in0, s=scalar, b=in1, p=op0, q=op1, k=kw:
                e.scalar_tensor_tensor(out=o, in0=a, scalar=s, in1=b, op0=p, op1=q, **k), r, w)

    def cp(self, out, in_, r, w, eng="dve"):
        if eng == "act":
            self.op("act", lambda e, o=out, i=in_: e.activation(out=o, in_=i, func=AF.Copy), r, w)
        else:
            self.op(eng, lambda e, o=out, i=in_: e.tensor_copy(out=o, in_=i), r, w)

    def mm(self, out, lhsT, rhs, start, stop, r, w, inc=True):
        self.op("pe", lambda e, o=out, l=lhsT, x=rhs, s=start, t=stop: e.matmul(o, lhsT=l, rhs=x, start=s, stop=t), r, w, inc=inc)

    def tr(self, out, in_, ident, r, w, inc=True):
        self.op("pe", lambda e, o=out, i=in_, d=ident: e.transpose(o, i, d), r, w, inc=inc)

    def memset(self, ap, val, w, eng="pool"):
        self.op(eng, lambda e, a=ap, v=val: e.memset(a, v), (), w)

    def asel(self, ap, cmp, fill, base, cm, pattern, key):
        self.op("pool", lambda e, a=ap, c=cmp, f=fill, b=base, m=cm, p=pattern:
                e.affine_select(out=a, in_=a, compare_op=c, fill=f, base=b, pattern=p, channel_multiplier=m), [key], [key])


def _ident(kb, T, name="ident"):
    idf = T(name + "_f", [128, 128], F32)
    idb = T(name + "_b", [128, 128], BF16)
    kb.memset(idf[:], 0.0, [name + "_f"])
    kb.asel(idf[:], ALU.not_equal, 1.0, 0, 1, [[-1, 128]], name + "_f")
    kb.cp(idb[:], idf[:], [name + "_f"], [name + "_b"], eng="pool")
    return idf, idb


def _rstd(kb, src, junk, ss, n, rkeys, pfx):
    kb.act(junk, src, AF.Square, rkeys, [pfx + "junk", pfx + "ss"], accum_out=ss)
    kb.ts(ss, ss, 1.0 / n, ALU.mult, [pfx + "ss"], [pfx + "ss"], s2=EPS, op1=ALU.add)
    kb.act(ss, ss, AF.Sqrt, [pfx + "ss"], [pfx + "ss"])
    kb.op("dve", lambda e, a=ss: e.reciprocal(out=a, in_=a), [pfx + "ss"], [pfx + "ss"])


def build_p1(NT):
    nc = bass.Bass("TRN2", target_bir_lowering=False)
    h = nc.dram_tensor("h", [NT * 128, D], F32, kind="ExternalInput").ap()
    w = nc.dram_tensor("w", [D, IN_COLS], F32, kind="ExternalInput").ap()
    g = nc.dram_tensor("g", [128, 8], F32, kind="ExternalInput").ap()
    proj = nc.dram_tensor("proj", [NT * 128, IN_COLS], BF16, kind="ExternalOutput").ap()
    with ExitStack() as st:
        kb = KB(nc, st)
        T = lambda name, shape, dt=F32: st.enter_context(nc.sbuf_tensor(name, shape, dt))
        ps = [st.enter_context(nc.psum_tensor("ps%d" % i, [128, 512], F32)) for i in range(8)]
        idf, idb = _ident(kb, T)
        gt = T("gt", [128, 8])
        kb.dma("sp", gt[:], g, writes=["gt"])
        Wg = T("Wg", [128, 8, IN_COLS], BF16)
        stage = [T("stage%d" % i, [128, IN_COLS]) for i in range(2)]
        for k in range(8):
            sk = "stage%d" % (k % 2)
            kb.dma("sp", stage[k % 2][:], w[k * 128:(k + 1) * 128, :], writes=[sk])
            kb.ts(Wg[:, k, :], stage[k % 2][:], gt[:, k:k + 1], ALU.mult, [sk, "gt"], ["Wg%d" % k])
        ht = [T("ht%d" % i, [128, D]) for i in range(2)]
        junk = T("junk", [128, D], BF16)
        ss = T("ss", [128, 1])
        xn = T("xn", [128, D], BF16)
        xnT = T("xnT", [128, D], BF16)
        pr = [T("pr%d" % i, [128, IN_COLS], BF16) for i in range(2)]
        pT = ps[0][:].bitcast(BF16)
        cgs = [(c0, min(512, IN_COLS - c0)) for c0 in range(0, IN_COLS, 512)]
        wkeys = ["Wg%d" % k for k in range(8)]
        for i in range(NT):
            hk = "ht%d" % (i % 2)
            hb = ht[i % 2]
            kb.dma("sp", hb[:], h[i * 128:(i + 1) * 128, :], writes=[hk])
            _rstd(kb, hb[:], junk[:], ss[:], D, [hk], "a")
            kb.ts(xn[:], hb[:], ss[:, 0:1], ALU.mult, [hk, "ass"], ["xn"])
            for k in range(8):
                kb.tr(pT[:, k * 128:(k + 1) * 128], xn[:, k * 128:(k + 1) * 128], idb[:], ["xn", "ident_b"], ["pT"], inc=(k == 7))
            kb.cp(xnT[:], pT, ["pT"], ["xnT"], eng="act")
            prk = "pr%d" % (i % 2)
            for ci, (c0, cw) in enumerate(cgs):
                pk = "pp%d" % (ci % 2)
                pp = ps[1 + ci % 2]
                for k in range(8):
                    kb.mm(pp[:, 0:cw], xnT[:, k * 128:(k + 1) * 128], Wg[:, k, c0:c0 + cw], k == 0, k == 7,
                          ["xnT", wkeys[k]], [pk], inc=(k == 7))
                kb.cp(pr[i % 2][:, c0:c0 + cw], pp[:, 0:cw], [pk], [prk], eng=("act" if ci % 2 else "dve"))
            kb.dma("pool", proj[i * 128:(i + 1) * 128, :], pr[i % 2][:], reads=[prk], writes=["out"])
        kb.wait_all("pool", ["out"])
        kb.emit()
    return nc


def build_p2(Lp, parts=(1, 1, 1)):
    NCH = Lp // 512
    NB = Lp // 128
    nc = bass.Bass("TRN2", target_bir_lowering=False)
    IN = lambda n, s, dt=BF16: nc.dram_tensor(n, s, dt, kind="ExternalInput").ap()
    qaT = IN("qaT", [128, Lp]); kaT = IN("kaT", [128, Lp]); va = IN("va", [Lp, 64])
    qbT = IN("qbT", [64, Lp]); kbT = IN("kbT", [64, Lp]); vb = IN("vb", [Lp, 64])
    qcT = IN("qcT", [32, Lp]); kcT = IN("kcT", [32, Lp]); vc = IN("vc", [Lp, 64])
    glrT = IN("glrT", [16, Lp])
    w2 = IN("w2", [16, 32], F32); gb = IN("gb", [32, 1], F32); sinks = IN("sinks", [128, 2], F32)
    oa = nc.dram_tensor("oa", [Lp, 128], F32, kind="ExternalOutput").ap()
    obT = nc.dram_tensor("obT", [64, Lp], F32, kind="ExternalOutput").ap()
    oc = nc.dram_tensor("oc", [Lp, 64], F32, kind="ExternalOutput").ap()
    with ExitStack() as st:
        kb = KB(nc, st)
        T = lambda name, shape, dt=F32: st.enter_context(nc.sbuf_tensor(name, shape, dt))
        ps = [st.enter_context(nc.psum_tensor("ps%d" % i, [128, 512], F32)) for i in range(8)]
        idf, idb = _ident(kb, T)
        m_gen = T("m_gen", [128, 256]); m_n0 = T("m_n0", [128, 256]); m_n1 = T("m_n1", [128, 256])
        for m, nm, extra in ((m_gen, "m_gen", None), (m_n0, "m_n0", -240), (m_n1, "m_n1", -112)):
            kb.memset(m[:], 0.0, [nm])
            kb.asel(m[:], ALU.is_ge, NEG, -1, -1, [[1, 256]], nm)
            kb.asel(m[:], ALU.is_ge, NEG, 128, 1, [[-1, 256]], nm)
            if extra is not None:
                kb.asel(m[:], ALU.is_ge, NEG, extra, 0, [[1, 256]], nm)
        sbm_f = T("sbm_f", [128, 512])
        sbm = [T("sbm%d" % d, [128, 512], BF16) for d in range(4)]
        for d in range(4):
            kb.memset(sbm_f[:], 1.0, ["sbm_f"])
            kb.asel(sbm_f[:], ALU.is_ge, 0.0, -1 - 128 * d, -1, [[1, 512]], "sbm_f")
            kb.cp(sbm[d][:], sbm_f[:], ["sbm_f"], ["sbm%d" % d], eng="pool")
        ntri_f = T("ntri_f", [128, 128]); ntri = T("ntri", [128, 128], BF16); nones = T("nones", [128, 128], BF16)
        kb.memset(ntri_f[:], -1.0, ["ntri_f"])
        kb.asel(ntri_f[:], ALU.is_ge, 0.0, 0, 1, [[-1, 128]], "ntri_f")
        kb.cp(ntri[:], ntri_f[:], ["ntri_f"], ["ntri"], eng="pool")
        kb.memset(nones[:], -1.0, ["nones"])
        mle = T("mle", [128, 128])
        kb.memset(mle[:], 1.0, ["mle"])
        kb.asel(mle[:], ALU.is_ge, 0.0, 0, -1, [[1, 128]], "mle")
        rmask = T("rmask", [32, 512])
        kb.memset(rmask[:], 1.0, ["rmask"])
        for b in range(4):
            kb.memset(rmask[:, b * 128:b * 128 + 1], 0.0, ["rmask"])
        w2f = T("w2f", [16, 32]); w2b = T("w2b", [16, 32], BF16); gbt = T("gbt", [32, 1]); sk_t = T("sk_t", [128, 2])
        kb.dma("sp", w2f[:], w2, writes=["w2f"]); kb.dma("sp", gbt[:], gb, writes=["gbt"]); kb.dma("sp", sk_t[:], sinks, writes=["sk"])
        kb.cp(w2b[:], w2f[:], ["w2f"], ["w2b"])
        kb.ts(gbt[:], gbt[:], -1.0, ALU.mult, ["gbt"], ["gbt"])
        KbT = T("KbT", [64, Lp], BF16); Vb = T("Vb", [128, NB, 64], BF16)
        kb.dma("sp", KbT[:], kbT, writes=["KbT"])
        for n0 in range(0, NB, 16):
            n1 = min(NB, n0 + 16)
            kb.dma("sp", Vb[:, n0:n1, :], vb[n0 * 128:n1 * 128, :].rearrange("(n p) d -> p n d", p=128), writes=["Vb"])
        S = T("S", [32, 64]); Sb = T("Sb", [32, 64], BF16)
        kb.memset(S[:], 0.0, ["S"]); kb.memset(Sb[:], 0.0, ["Sb"])
        qa_c = [T("qa_c%d" % i, [128, 512], BF16) for i in range(2)]
        ka_c = [T("ka_c%d" % i, [128, 640], BF16) for i in range(2)]
        va_c = [T("va_c%d" % i, [128, 5, 64], BF16) for i in range(2)]
        qb_c = [T("qb_c%d" % i, [64, 512], BF16) for i in range(2)]
        qs_c = [T("qs_c%d" % i, [64, 512], BF16) for i in range(2)]
        qc_c = [T("qc_c%d" % i, [32, 512], BF16) for i in range(2)]
        kc_c = [T("kc_c%d" % i, [32, 512], BF16) for i in range(2)]
        vc_c = [T("vc_c%d" % i, [128, 4, 64], BF16) for i in range(2)]
        gl_c = [T("gl_c%d" % i, [16, 512], BF16) for i in range(2)]
        sm = T("sm", [128, 256]); pexp = T("pexp", [128, 256], BF16); pTs = T("pTs", [128, 256], BF16)
        st8 = T("st8", [128, 8]); oa_t = [T("oa_t%d" % i, [128, 128]) for i in range(2)]
        ge = T("ge", [32, 512]); gsp = T("gsp", [32, 512]); gcs = T("gcs", [32, 512])
        geq = T("geq", [32, 512]); gek = T("gek", [32, 512])
        qt = T("qt", [32, 512], BF16); kt = T("kt", [32, 512], BF16)
        scb = T("scb", [128, 128], BF16); ktm = T("ktm", [128, 32], BF16); oc_t = [T("oc_t%d" % i, [128, 64]) for i in range(2)]
        stmp = T("stmp", [32, 64])
        e_t = [T("e_t%d" % i, [128, 512]) for i in range(2)]
        sp_t = [T("sp_t%d" % i, [128, 512], BF16) for i in range(2)]
        a_t = [T("a_t%d" % i, [128, 512], BF16) for i in range(2)]
        R = T("R", [128, 512], BF16)
        ob_t = [T("ob_t%d" % i, [64, 512]) for i in range(2)]
        pz = [ps[0], ps[1]]; pc = [ps[2], ps[3]]; pO = ps[4]
        pA = ps[5]; pX = ps[6]; pB = ps[7]
        pT_bf = pB[:].bitcast(BF16)

        def load_chunk(c):
            i = c % 2
            c0 = c * 512
            kb.dma("sp", qa_c[i][:], qaT[:, c0:c0 + 512], writes=["qa_c%d" % i])
            if c == 0:
                kb.memset(ka_c[i][:, 0:128], 0.0, ["ka_c%d" % i])
                kb.memset(va_c[i][:, 0, :], 0.0, ["va_c%d" % i])
                kb.dma("sp", ka_c[i][:, 128:640], kaT[:, 0:512], writes=["ka_c%d" % i])
                kb.dma("sp", va_c[i][:, 1:5, :], va[0:512, :].rearrange("(n p) d -> p n d", p=128), writes=["va_c%d" % i])
            else:
                kb.dma("sp", ka_c[i][:], kaT[:, c0 - 128:c0 + 512], writes=["ka_c%d" % i])
                kb.dma("sp", va_c[i][:], va[c0 - 128:c0 + 512, :].rearrange("(n p) d -> p n d", p=128), writes=["va_c%d" % i])
            kb.dma("sp", qb_c[i][:], qbT[:, c0:c0 + 512], writes=["qb_c%d" % i])
            kb.dma("sp", qc_c[i][:], qcT[:, c0:c0 + 512], writes=["qc_c%d" % i])
            kb.dma("sp", kc_c[i][:], kcT[:, c0:c0 + 512], writes=["kc_c%d" % i])
            kb.dma("sp", vc_c[i][:], vc[c0:c0 + 512, :].rearrange("(n p) d -> p n d", p=128), writes=["vc_c%d" % i])
            kb.dma("sp", gl_c[i][:], glrT[:, c0:c0 + 512], writes=["gl_c%d" % i])

        def _gla(c, i):
            kb.mm(pX[0:32, :], w2b[:], gl_c[i][:], True, True, ["w2b", "gl_c%d" % i], ["pX"])
            kb.act(ge[:], pX[0:32, :], AF.Exp, ["pX", "gbt"], ["ge"], bias=gbt[:, 0:1], scale=-1.0)
            kb.act(gsp[:], ge[:], AF.Ln, ["ge"], ["gsp"], bias=1.0, scale=1.0)
            kb.op("dve", lambda e: e.tensor_tensor_scan(out=gcs[:], data0=rmask[:], data1=gsp[:], initial=0.0, op0=ALU.mult, op1=ALU.add),
                  ["rmask", "gsp"], ["gcs"])
            kb.act(geq[:], gcs[:], AF.Exp, ["gcs"], ["geq"], scale=-1.0 / 16.0)
            kb.act(gek[:], gcs[:], AF.Exp, ["gcs"], ["gek"], scale=1.0 / 16.0)
            kb.stt(qt[:], qc_c[i][:], 32.0 ** -0.5, geq[:], ALU.mult, ALU.mult, ["qc_c%d" % i, "geq"], ["qt"])
            kb.tt(kt[:], kc_c[i][:], gek[:], ALU.mult, ["kc_c%d" % i, "gek"], ["kt"])
            for blk in range(4):
                n = 4 * c + blk
                bs = slice(blk * 128, (blk + 1) * 128)
                kb.mm(pA[:, 384:512], kt[:, bs], qt[:, bs], True, True, ["kt", "qt"], ["pA"])
                kb.tt(scb[:], pA[:, 384:512], mle[:], ALU.mult, ["pA", "mle"], ["scb"])
                kb.tr(pT_bf[:, 512:544], kt[:, bs], idb[0:32, 0:32], ["kt", "ident_b"], ["pB"])
                kb.cp(ktm[:], pT_bf[:, 512:544], ["pB"], ["ktm"], eng="act")
                kb.mm(pA[:, 320:384], scb[:], vc_c[i][:, blk, :], True, False, ["scb", "vc_c%d" % i], ["pA"], inc=False)
                kb.mm(pA[:, 320:384], qt[:, bs], Sb[:], False, True, ["qt", "Sb"], ["pA"])
                ock = "oc_t%d" % (n % 2)
                kb.cp(oc_t[n % 2][:], pA[:, 320:384], ["pA"], [ock], eng="act")
                kb.dma("pool", oc[n * 128:(n + 1) * 128, :], oc_t[n % 2][:], reads=[ock], writes=["oc"])
                kb.mm(pB[0:32, 384:448], ktm[:], vc_c[i][:, blk, :], True, True, ["ktm", "vc_c%d" % i], ["pB"])
                kb.tt(stmp[:], pB[0:32, 384:448], S[:], ALU.add, ["pB", "S"], ["stmp"])
                kb.ts(S[:], stmp[:], geq[:, blk * 128 + 127:blk * 128 + 128], ALU.mult, ["stmp", "geq"], ["S"])
                kb.cp(Sb[:], S[:], ["S"], ["Sb"])

        def _sb(c, i):
            kb.ts(qs_c[i][:], qb_c[i][:], 0.125, ALU.mult, ["qb_c%d" % i], ["qs_c%d" % i])
            nkb = 4 * c + 4
            for it in range(nkb):
                kblk = nkb - 1 - it
                j = it % 2
                dg = kblk - 4 * c
                first, last = (it == 0), (kblk == 0)
                ksl = KbT[:, kblk * 128:(kblk + 1) * 128]
                kb.mm(pz[j][:], ksl, qs_c[i][:], True, True, ["KbT", "qs_c%d" % i], ["pz%d" % j])
                kb.act(e_t[j][:], pz[j][:], AF.Exp, ["pz%d" % j], ["e_t%d" % j])
                kb.act(sp_t[j][:], e_t[j][:], AF.Ln, ["e_t%d" % j], ["sp_t%d" % j], bias=1.0, scale=1.0)
                if dg >= 0:
                    kb.tt(sp_t[j][:], sp_t[j][:], sbm[dg][:], ALU.mult, ["sp_t%d" % j, "sbm%d" % dg], ["sp_t%d" % j])
                kb.mm(pc[j][:], ntri[:], sp_t[j][:], True, False, ["ntri", "sp_t%d" % j], ["pc%d" % j], inc=False)
                if not first:
                    kb.mm(pc[j][:], nones[:], R[:], False, False, ["nones", "R"], ["pc%d" % j], inc=False)
                kb.mm(pc[j][:], ksl, qs_c[i][:], False, True, ["KbT", "qs_c%d" % i], ["pc%d" % j])
                if not last:
                    if first:
                        kb.cp(R[:], sp_t[j][:], ["sp_t%d" % j], ["R"], eng="pool")
                    else:
                        kb.tt(R[:], R[:], sp_t[j][:], ALU.add, ["R", "sp_t%d" % j], ["R"], eng="pool")
                kb.act(a_t[j][:], pc[j][:], AF.Exp, ["pc%d" % j], ["a_t%d" % j])
                if dg >= 0:
                    kb.tt(a_t[j][:], a_t[j][:], sbm[dg][:], ALU.mult, ["a_t%d" % j, "sbm%d" % dg], ["a_t%d" % j])
                kb.mm(pO[0:64, :], Vb[:, kblk, :], a_t[j][:], first, last, ["Vb", "a_t%d" % j], ["pO"], inc=True)
            kb.cp(ob_t[i][:], pO[0:64, :], ["pO"], ["ob_t%d" % i])
            kb.dma("pool", obT[:, c * 512:(c + 1) * 512], ob_t[i][:], reads=["ob_t%d" % i], writes=["ob"])

        load_chunk(0)
        for c in range(NCH):
            i = c % 2
            if c + 1 < NCH:
                load_chunk(c + 1)
            for blk in (range(4) if parts[0] else []):
                n = 4 * c + blk
                msk = m_n0 if n == 0 else (m_n1 if n == 1 else m_gen)
                mk = "m_n0" if n == 0 else ("m_n1" if n == 1 else "m_gen")
                ok = "oa_t%d" % (n % 2)
                for hh in range(2):
                    hs = slice(hh * 64, (hh + 1) * 64)
                    kb.mm(pA[:, 0:256], qa_c[i][hs, blk * 128:(blk + 1) * 128], ka_c[i][hs, blk * 128:blk * 128 + 256],
                          True, True, ["qa_c%d" % i, "ka_c%d" % i], ["pA"])
                    kb.stt(sm[:], pA[:, 0:256], 0.125, msk[:], ALU.mult, ALU.add, ["pA", mk], ["sm"])
                    kb.op("dve", lambda e: e.tensor_reduce(out=st8[:, 0:1], in_=sm[:], axis=AX.X, op=ALU.max), ["sm"], ["st8"])
                    kb.tt(st8[:, 0:1], st8[:, 0:1], sk_t[:, hh:hh + 1], ALU.max, ["st8", "sk"], ["st8"])
                    kb.ts(st8[:, 1:2], st8[:, 0:1], -1.0, ALU.mult, ["st8"], ["st8"])
                    kb.act(pexp[:], sm[:], AF.Exp, ["sm", "st8"], ["pexp", "st8"], bias=st8[:, 1:2], scale=1.0, accum_out=st8[:, 2:3])
                    kb.act(st8[:, 3:4], sk_t[:, hh:hh + 1], AF.Exp, ["sk", "st8"], ["st8"], bias=st8[:, 1:2], scale=1.0)
                    kb.tt(st8[:, 4:5], st8[:, 2:3], st8[:, 3:4], ALU.add, ["st8"], ["st8"])
                    kb.op("dve", lambda e: e.reciprocal(out=st8[:, 5:6], in_=st8[:, 4:5]), ["st8"], ["st8"])
                    kb.tr(pT_bf[:, 0:128], pexp[:, 0:128], idb[:], ["pexp", "ident_b"], ["pB"], inc=False)
                    kb.tr(pT_bf[:, 128:256], pexp[:, 128:256], idb[:], ["pexp", "ident_b"], ["pB"])
                    kb.cp(pTs[:], pT_bf[:, 0:256], ["pB"], ["pTs"], eng="act")
                    kb.mm(pA[:, 256:320], pTs[:, 0:128], va_c[i][:, blk, :], True, False, ["pTs", "va_c%d" % i], ["pA"], inc=False)
                    kb.mm(pA[:, 256:320], pTs[:, 128:256], va_c[i][:, blk + 1, :], False, True, ["pTs", "va_c%d" % i], ["pA"])
                    kb.ts(oa_t[n % 2][:, hs], pA[:, 256:320], st8[:, 5:6], ALU.mult, ["pA", "st8"], [ok])
                kb.dma("pool", oa[n * 128:(n + 1) * 128, :], oa_t[n % 2][:], reads=[ok], writes=["oa"])
            if parts[1]:
              _gla(c, i)
            if parts[2]:
              _sb(c, i)
        kb.wait_all("pool", ["oa", "oc", "ob"])
        kb.emit()
    return nc


def build_p3(NT, final):
    nc = bass.Bass("TRN2", target_bir_lowering=False)
    IN = lambda n, s, dt=F32: nc.dram_tensor(n, s, dt, kind="ExternalInput").ap()
    h = IN("h", [NT * 128, D]); o = IN("o", [NT * 128, D]); rc = IN("rc", [NT * 128, 256], BF16)
    gmix = IN("gmix", [128, D]); wout = IN("wout", [D, D]); gffn = IN("gffn", [128, 8])
    wq = IN("wq", [D, 2048]); ksub = IN("ksub", [128, 16, 64]); uT = IN("uT", [D, 4096]); v = IN("v", [4096, D])
    gfin = IN("gfin", [128, D])
    hout = nc.dram_tensor("hout", [NT * 128, D], F32, kind="ExternalOutput").ap()
    h1_d = nc.dram_tensor("h1_d", [NT * 128, D], F32, kind="Internal").ap()
    sc_d = nc.dram_tensor("sc_d", [NT * 128, D], F32, kind="Internal").ap()
    tb_d = nc.dram_tensor("tb_d", [NT * 128, 16], F32, kind="Internal").ap()
    xT_d = nc.dram_tensor("xT_d", [NT * 128, D], BF16, kind="Internal").ap()
    with ExitStack() as st:
        kb = KB(nc, st)
        ps = [st.enter_context(nc.psum_tensor("ps%d" % i, [128, 512], F32)) for i in range(8)]
        T0 = lambda name, shape, dt=F32: st.enter_context(nc.sbuf_tensor(name, shape, dt))
        idf, idb = _ident(kb, T0)
        gft = T0("gft", [128, 8])
        kb.dma("sp", gft[:], gffn, writes=["gft"])
        stage = [T0("stage%d" % i, [128, 1024]) for i in range(2)]
        scnt = [0]

        def load_w(dst, src, rows_k, cols, key, scale_col=None):
            for k in range(rows_k):
                for c0 in range(0, cols, 1024):
                    cw = min(1024, cols - c0)
                    si = scnt[0] % 2
                    scnt[0] += 1
                    sk = "stage%d" % si
                    kb.dma("sp", stage[si][:, 0:cw], src[k * 128:(k + 1) * 128, c0:c0 + cw], writes=[sk])
                    if scale_col:
                        kb.ts(dst[:, k, c0:c0 + cw], stage[si][:, 0:cw], gft[:, k:k + 1], ALU.mult, [sk, "gft"], [key])
                    else:
                        kb.cp(dst[:, k, c0:c0 + cw], stage[si][:, 0:cw], [sk], [key], eng="pool")

        with ExitStack() as sa:
            T = lambda name, shape, dt=F32: sa.enter_context(nc.sbuf_tensor(name, shape, dt))
            Wo = T("Wo", [128, 8, D], BF16); Wq = T("Wq", [128, 8, 2048], BF16); Ks = T("Ks", [128, 16, 64], BF16)
            gm = T("gm", [128, D]); ksf = T("ksf", [128, 16, 64])
            kb.dma("sp", gm[:], gmix, writes=["gm"])
            kb.dma("sp", ksf[:], ksub, writes=["ksf"])
            kb.cp(Ks[:], ksf[:], ["ksf"], ["Ks"])
            load_w(Wo, wout, 8, D, "Wo")
            load_w(Wq, wq, 8, 2048, "Wq", scale_col=True)
            ht = [T("ht%d" % i, [128, D]) for i in range(2)]
            ot = [T("ot%d" % i, [128, D]) for i in range(2)]
            rct = [T("rct%d" % i, [128, 256], BF16) for i in range(2)]
            sq = T("sq", [128, D]); m1 = T("m1", [128, D]); sil = T("sil", [128, 256])
            s16 = T("s16", [128, 16]); mix = T("mix", [128, D], BF16); mixT = T("mixT", [128, D], BF16)
            h1 = T("h1", [128, D]); junk = T("junk", [128, D], BF16); ss = T("ss", [128, 1])
            xn = T("xn", [128, D], BF16); xnT = T("xnT", [128, D], BF16)
            qT = T("qT", [128, 16, 128], BF16); sct = T("sct", [128, D])
            t1 = T("t1", [128, 8, 16]); t2 = T("t2", [128, 8, 16]); wk = T("wk", [128, 256])
            cand = T("cand", [128, 8, 256]); c8a = T("c8a", [128, 8, 8]); c8b = T("c8b", [128, 8, 8])
            csh = T("csh", [128, 8, 256]); ec = T("ec", [128, 8, 256]); mk8 = T("mk8", [128, 8, 256])
            Z = T("Z", [128, 8]); tb = T("tb", [128, 16])
            pT = ps[0][:].bitcast(BF16)
            for i in range(NT):
                b = i % 2
                rs = slice(i * 128, (i + 1) * 128)
                kb.dma("sp", ht[b][:], h[rs, :], writes=["ht%d" % b])
                kb.dma("sp", ot[b][:], o[rs, :], writes=["ot%d" % b])
                kb.dma("sp", rct[b][:], rc[rs, :], writes=["rct%d" % b])
                kb.act(sq[:], ot[b][:], AF.Square, ["ot%d" % b], ["sq"])
                kb.op("dve", lambda e: e.tensor_reduce(out=s16[:], in_=sq[:].rearrange("p (h d) -> p h d", d=64), axis=AX.X, op=ALU.add),
                      ["sq"], ["s16"])
                kb.ts(s16[:], s16[:], 1.0 / 64, ALU.mult, ["s16"], ["s16"], s2=EPS, op1=ALU.add)
                kb.act(s16[:], s16[:], AF.Sqrt, ["s16"], ["s16"])
                kb.op("dve", lambda e: e.reciprocal(out=s16[:], in_=s16[:]), ["s16"], ["s16"])
                kb.tt(m1[:].rearrange("p (h d) -> p h d", d=64), ot[b][:].rearrange("p (h d) -> p h d", d=64),
                      s16[:].unsqueeze(2).to_broadcast([128, 16, 64]), ALU.mult, ["ot%d" % b, "s16"], ["m1"])
                kb.act(sil[:], rct[b][:], AF.Silu, ["rct%d" % b], ["sil"])
                kb.tt(m1[:, 768:1024], m1[:, 768:1024], sil[:], ALU.mult, ["m1", "sil"], ["m1"])
                kb.tt(mix[:], m1[:], gm[:], ALU.mult, ["m1", "gm"], ["mix"])
                for k in range(8):
                    kb.tr(pT[:, k * 128:(k + 1) * 128], mix[:, k * 128:(k + 1) * 128], idb[:], ["mix", "ident_b"], ["pT"], inc=(k == 7))
                kb.cp(mixT[:], pT, ["pT"], ["mixT"], eng="act")
                for dh in range(2):
                    for k in range(8):
                        kb.mm(ps[1 + dh][:], mixT[:, k * 128:(k + 1) * 128], Wo[:, k, dh * 512:(dh + 1) * 512], k == 0, k == 7,
                              ["mixT", "Wo"], ["pd%d" % dh], inc=(k == 7))
                    kb.tt(h1[:, dh * 512:(dh + 1) * 512], ht[b][:, dh * 512:(dh + 1) * 512], ps[1 + dh][:], ALU.add,
                          ["ht%d" % b, "pd%d" % dh], ["h1"])
                kb.dma("pool", h1_d[rs, :], h1[:], reads=["h1"], writes=["h1_d"])
                _rstd(kb, h1[:], junk[:], ss[:], D, ["h1"], "b")
                kb.ts(xn[:], h1[:], ss[:, 0:1], ALU.mult, ["h1", "bss"], ["xn"])
                for k in range(8):
                    kb.tr(pT[:, k * 128:(k + 1) * 128], xn[:, k * 128:(k + 1) * 128], idb[:], ["xn", "ident_b"], ["pT"], inc=(k == 7))
                kb.cp(xnT[:], pT, ["pT"], ["xnT"], eng="act")
                kb.dma("pool", xT_d[rs, :], xnT[:], reads=["xnT"], writes=["xT_d"])
                for cg in range(4):
                    pq = ps[3 + cg % 2]
                    for cc in range(4):
                        cidx = cg * 4 + cc
                        for k in range(8):
                            kb.mm(pq[:, cc * 128:(cc + 1) * 128], Wq[:, k, cidx * 128:(cidx + 1) * 128], xnT[:, k * 128:(k + 1) * 128],
                                  k == 0, k == 7, ["Wq", "xnT"], ["pq%d" % (cg % 2)], inc=(k == 7 and cc == 3))
                    kb.cp(qT[:, cg * 4:(cg + 1) * 4, :], pq[:].rearrange("p (c t) -> p c t", t=128), ["pq%d" % (cg % 2)], ["qT"],
                          eng=("act" if cg % 2 else "dve"))
                for cidx in range(16):
                    pscb = ps[5 + cidx // 8]
                    kb.mm(pscb[:, (cidx % 8) * 64:(cidx % 8 + 1) * 64], qT[:, cidx, :], Ks[:, cidx, :], True, True, ["qT", "Ks"],
                          ["psc%d" % (cidx // 8)], inc=(cidx % 8 == 7))
                kb.cp(sct[:, 0:512], ps[5][:], ["psc0"], ["sct"], eng="act")
                kb.cp(sct[:, 512:1024], ps[6][:], ["psc1"], ["sct"], eng="dve")
                kb.dma("pool", sc_d[rs, :], sct[:], reads=["sct"], writes=["sc_d"])
                for hd in range(8):
                    for side, tt_ in ((0, t1), (1, t2)):
                        sv = sct[:, hd * 128 + side * 64:hd * 128 + side * 64 + 64]
                        kb.op("dve", lambda e, o_=tt_[:, hd, 0:8], i_=sv: e.max(out=o_, in_=i_), ["sct"], ["tt"])
                        kb.op("dve", lambda e, o_=wk[:, 0:64], r_=tt_[:, hd, 0:8], i_=sv: e.match_replace(out=o_, in_to_replace=r_, in_values=i_, imm_value=-1e30),
                              ["sct", "tt"], ["wk"])
                        kb.op("dve", lambda e, o_=tt_[:, hd, 8:16], i_=wk[:, 0:64]: e.max(out=o_, in_=i_), ["wk"], ["tt"])
                kb.tt(cand[:].rearrange("p h (a b) -> p h a b", b=16), t1[:].unsqueeze(3).to_broadcast([128, 8, 16, 16]),
                      t2[:].unsqueeze(2).to_broadcast([128, 8, 16, 16]), ALU.add, ["tt"], ["cand"])
                for hd in range(8):
                    kb.op("dve", lambda e, o_=c8a[:, hd, :], i_=cand[:, hd, :]: e.max(out=o_, in_=i_), ["cand"], ["c8"])
                    kb.op("dve", lambda e, o_=wk[:], r_=c8a[:, hd, :], i_=cand[:, hd, :]: e.match_replace(out=o_, in_to_replace=r_, in_values=i_, imm_value=-1e30),
                          ["cand", "c8"], ["wk"])
                    kb.op("dve", lambda e, o_=c8b[:, hd, :], i_=wk[:]: e.max(out=o_, in_=i_), ["wk"], ["c8"])
                kb.tt(csh[:], cand[:], c8a[:, :, 0:1].to_broadcast([128, 8, 256]), ALU.subtract, ["cand", "c8"], ["csh"])
                kb.act(ec[:], csh[:], AF.Exp, ["csh"], ["ec"])
                kb.tt(mk8[:], cand[:], c8b[:, :, 7:8].to_broadcast([128, 8, 256]), ALU.is_ge, ["cand", "c8"], ["mk8"])
                kb.tt(ec[:], ec[:], mk8[:], ALU.mult, ["ec", "mk8"], ["ec"])
                kb.op("dve", lambda e: e.tensor_reduce(out=Z[:], in_=ec[:], axis=AX.X, op=ALU.add), ["ec"], ["Z"])
                kb.act(Z[:], Z[:], AF.Ln, ["Z"], ["Z"])
                kb.cp(tb[:, 0:8], c8b[:, :, 7], ["c8"], ["tb"])
                kb.stt(tb[:, 8:16], c8a[:, :, 0], -1.0, Z[:], ALU.mult, ALU.subtract, ["c8", "Z"], ["tb"])
                kb.dma("pool", tb_d[rs, :], tb[:], reads=["tb"], writes=["tb_d"])
            kb.barrier()
            kb.emit()
        with ExitStack() as sb_:
            T = lambda name, shape, dt=F32: sb_.enter_context(nc.sbuf_tensor(name, shape, dt))
            Ub = T("Ub", [128, 8, 4096], BF16); Vv = T("Vv", [128, 32, D], BF16)
            load_w(Ub, uT, 8, 4096, "Ub", scale_col=True)
            load_w(Vv, v, 32, D, "Vv")
            gf = T("gf", [128, D])
            if final:
                kb.dma("sp", gf[:], gfin, writes=["gf"])
            h1t = [T("h1t%d" % i, [128, D]) for i in range(2)]
            sct = [T("sctb%d" % i, [128, D]) for i in range(2)]
            xT = [T("xTb%d" % i, [128, D], BF16) for i in range(2)]
            tbt = [T("tbt%d" % i, [128, 16]) for i in range(2)]
            Sg = [T("Sg%d" % i, [128, 16, 64]) for i in range(2)]
            Eg = [T("Eg%d" % i, [128, 1024], BF16) for i in range(2)]
            Gh = T("Gh", [128, 1024], BF16); G = T("G", [128, 1024])
            gl = [T("gl%d" % i, [128, 512]) for i in range(2)]
            Wb = T("Wb", [128, 1024], BF16); WT = T("WT", [128, 1024], BF16)
            ho = T("ho", [128, D]); junk = T("junkb", [128, D], BF16); ss = T("ssb", [128, 1])
            pH = [ps[0], ps[1]]; ptr = ps[2][:].bitcast(BF16); po = [ps[3], ps[4]]

            def loadB(i):
                b = i % 2
                rs = slice(i * 128, (i + 1) * 128)
                kb.dma("sp", h1t[b][:], h1_d[rs, :], reads=["h1_d"], writes=["h1t%d" % b])
                kb.dma("sp", sct[b][:], sc_d[rs, :], reads=["sc_d"], writes=["sctb%d" % b])
                kb.dma("sp", xT[b][:], xT_d[rs, :], reads=["xT_d"], writes=["xTb%d" % b])
                kb.dma("sp", tbt[b][:], tb_d[rs, :], reads=["tb_d"], writes=["tbt%d" % b])

            loadB(0)
            cnt = 0
            for i in range(NT):
                b = i % 2
                rs = slice(i * 128, (i + 1) * 128)
                if i + 1 < NT:
                    loadB(i + 1)
                for eq in range(4):
                    for hd in range(8):
                        j = cnt % 2
                        cnt += 1
                        s1 = sct[b][:, hd * 128 + 16 * eq:hd * 128 + 16 * eq + 16]
                        s2 = sct[b][:, hd * 128 + 64:hd * 128 + 128]
                        kb.tt(Sg[j][:], s1.unsqueeze(2).to_broadcast([128, 16, 64]), s2.unsqueeze(1).to_broadcast([128, 16, 64]), ALU.add,
                              ["sctb%d" % b], ["Sg%d" % j], eng="pool")
                        Sf = Sg[j][:].rearrange("p a b -> p (a b)")
                        kb.act(Eg[j][:], Sf, AF.Exp, ["Sg%d" % j, "tbt%d" % b], ["Eg%d" % j], bias=tbt[b][:, 8 + hd:9 + hd], scale=1.0)
                        if hd == 0:
                            kb.stt(G[:], Sf, tbt[b][:, hd:hd + 1], Eg[j][:], ALU.is_ge, ALU.mult, ["Sg%d" % j, "Eg%d" % j, "tbt%d" % b], ["G"])
                        else:
                            kb.stt(Gh[:], Sf, tbt[b][:, hd:hd + 1], Eg[j][:], ALU.is_ge, ALU.mult, ["Sg%d" % j, "Eg%d" % j, "tbt%d" % b], ["Gh"])
                            kb.tt(G[:], G[:], Gh[:], ALU.add, ["G", "Gh"], ["G"])
                    for g2 in range(2):
                        e0 = eq * 1024 + g2 * 512
                        for k in range(8):
                            kb.mm(pH[g2][:], xT[b][:, k * 128:(k + 1) * 128], Ub[:, k, e0:e0 + 512], k == 0, k == 7,
                                  ["xTb%d" % b, "Ub"], ["pH%d" % g2], inc=(k == 7))
                        kb.act(gl[g2][:], pH[g2][:], AF.Gelu, ["pH%d" % g2], ["gl%d" % g2])
                        kb.tt(Wb[:, g2 * 512:(g2 + 1) * 512], gl[g2][:], G[:, g2 * 512:(g2 + 1) * 512], ALU.mult, ["gl%d" % g2, "G"], ["Wb"])
                    for cc in range(8):
                        kb.tr(ptr[:, cc * 128:(cc + 1) * 128], Wb[:, cc * 128:(cc + 1) * 128], idb[:], ["Wb", "ident_b"], ["ptr"], inc=(cc == 7))
                    kb.cp(WT[:], ptr, ["ptr"], ["WT"], eng="act")
                    for dh in range(2):
                        for cc in range(8):
                            kb.mm(po[dh][:], WT[:, cc * 128:(cc + 1) * 128], Vv[:, eq * 8 + cc, dh * 512:(dh + 1) * 512],
                                  (eq == 0 and cc == 0), (eq == 3 and cc == 7), ["WT", "Vv"], ["po%d" % dh], inc=(cc == 7))
                for dh in range(2):
                    kb.tt(ho[:, dh * 512:(dh + 1) * 512], h1t[b][:, dh * 512:(dh + 1) * 512], po[dh][:], ALU.add,
                          ["h1t%d" % b, "po%d" % dh], ["ho"])
                if final:
                    _rstd(kb, ho[:], junk[:], ss[:], D, ["ho"], "f")
                    kb.stt(ho[:], ho[:], ss[:, 0:1], gf[:], ALU.mult, ALU.mult, ["ho", "fss", "gf"], ["ho"])
                kb.dma("sp", hout[rs, :], ho[:], reads=["ho"], writes=["hout"])
            kb.wait_all("sp", ["hout"])
            kb.emit()
    return nc


_CACHE = {}


def _prog(name, *args):
    key = (name,) + args
    if key not in _CACHE:
        _CACHE[key] = {"p1": build_p1, "p2": build_p2, "p3": build_p3}[name](*args)
    return _CACHE[key]


def _run(nc, in_maps):
    res = run_bass_kernel_spmd(nc, in_maps, core_ids=list(range(NCORES)))
    return res.results


def _c(a):
    return np.ascontiguousarray(a)


def _gk(g):
    return _c(np.asarray(g, np.float32).reshape(8, 128).T)


def forward(x, meta_tokens, attn_norm, w_in, attn_sinks, gla_gate_w2, gla_gate_b, swa_out_norm,
            sb_out_norm, gla_out_norm, w_out, ffn_norm, peer_w_q, peer_sub_keys, peer_u, peer_v, final_norm):
    x = np.asarray(x, np.float32)
    B, SEQ, _ = x.shape
    depth = attn_norm.shape[0]
    L = SEQ + 128
    Lp = ((L + 511) // 512) * 512
    T = B * L
    NT = (T // 128 + NCORES - 1) // NCORES
    Tp = NT * 128 * NCORES
    hfull = np.zeros((Tp, D), np.float32)
    for b in range(B):
        hfull[b * L + 112:b * L + 128] = meta_tokens
        hfull[b * L + 128:(b + 1) * L] = x[b]
    p1 = _prog("p1", NT)
    p2 = _prog("p2", Lp)
    tsl = [slice(c * NT * 128, (c + 1) * NT * 128) for c in range(NCORES)]
    bf = ml_dtypes.bfloat16
    for i in range(depth):
        w_i = _c(w_in[i]); g_i = _gk(attn_norm[i])
        r = _run(p1, [{"h": hfull[tsl[c]], "w": w_i, "g": g_i} for c in range(NCORES)])
        proj = np.concatenate([np.asarray(r[c]["proj"]).view(bf) if np.asarray(r[c]["proj"]).dtype != bf else np.asarray(r[c]["proj"])
                               for c in range(NCORES)], axis=0)
        maps = []
        for c in range(NCORES):
            b, j = c // 4, c % 4
            pb = np.zeros((Lp, IN_COLS), bf)
            pb[:L] = proj[b * L:(b + 1) * L]
            kv = j // 2
            ka = pb[:, 512 + 64 * kv:512 + 64 * kv + 64].T
            maps.append({
                "qaT": _c(pb[:, 128 * j:128 * j + 128].T), "kaT": _c(np.concatenate([ka, ka], 0)),
                "va": _c(pb[:, 640 + 64 * kv:640 + 64 * kv + 64]),
                "qbT": _c(pb[:, 768 + 64 * j:768 + 64 * j + 64].T), "kbT": _c(pb[:, 1024 + 64 * j:1024 + 64 * j + 64].T),
                "vb": _c(pb[:, 1280 + 64 * j:1280 + 64 * j + 64]),
                "qcT": _c(pb[:, 1536 + 32 * j:1536 + 32 * j + 32].T), "kcT": _c(pb[:, 1664 + 32 * j:1664 + 32 * j + 32].T),
                "vc": _c(pb[:, 1792 + 64 * j:1792 + 64 * j + 64]), "glrT": _c(pb[:, 2048:2064].T),
                "w2": _c(np.asarray(gla_gate_w2[i], np.float32)[:, 32 * j:32 * j + 32]),
                "gb": _c(np.asarray(gla_gate_b[i], np.float32)[32 * j:32 * j + 32].reshape(32, 1)),
                "sinks": _c(np.broadcast_to(np.asarray(attn_sinks[i], np.float32)[2 * j:2 * j + 2][None, :], (128, 2))),
            })
        r = _run(p2, maps)
        ofull = np.zeros((Tp, D), np.float32)
        for c in range(NCORES):
            b, j = c // 4, c % 4
            ofull[b * L:(b + 1) * L, 128 * j:128 * j + 128] = np.asarray(r[c]["oa"])[:L]
            ofull[b * L:(b + 1) * L, 512 + 64 * j:512 + 64 * j + 64] = np.asarray(r[c]["obT"]).T[:L]
            ofull[b * L:(b + 1) * L, 768 + 64 * j:768 + 64 * j + 64] = np.asarray(r[c]["oc"])[:L]
        rcfull = np.zeros((Tp, 256), bf)
        rcfull[:T] = proj[:T, 2064:2320]
        fin = (i == depth - 1)
        p3 = _prog("p3", NT, fin)
        gmix = np.concatenate([swa_out_norm[i], sb_out_norm[i], gla_out_norm[i]]).astype(np.float32)
        shared = {
            "gmix": _c(np.broadcast_to(gmix[None, :], (128, D))), "wout": _c(w_out[i]), "gffn": _gk(ffn_norm[i]),
            "wq": _c(peer_w_q[i]), "ksub": _c(np.transpose(np.asarray(peer_sub_keys[i], np.float32).reshape(16, 64, 128), (2, 0, 1))),
            "uT": _c(np.asarray(peer_u[i], np.float32).T), "v": _c(peer_v[i]),
            "gfin": _c(np.broadcast_to(np.asarray(final_norm, np.float32)[None, :], (128, D))),
        }
        r = _run(p3, [dict(shared, h=hfull[tsl[c]], o=ofull[tsl[c]], rc=rcfull[tsl[c]]) for c in range(NCORES)])
        hfull = np.concatenate([np.asarray(r[c]["hout"]) for c in range(NCORES)], axis=0)
    out = np.stack([hfull[b * L + 128:(b + 1) * L] for b in range(B)], 0)
    return np.ascontiguousarray(out.astype(np.float32))


def kernel(**inputs):
    return forward_fused(**{k: np.asarray(v) for k, v in inputs.items()})


import os as _os
_STOP = _os.environ.get("FUSED_STOP", "")
_POOL_HEADS = tuple(int(c_) for c_ in _os.environ.get("POOL_HEADS", "01234567"))
CJ = 720
RG = [[0, 1, 2, 3], [4, 5, 6, 7]]


def build_fused(Lp, depth):
    TQ = Lp // 4
    NT = TQ // 128
    NCH = Lp // 512
    NB = Lp // 128
    nc = bass.Bass("TRN2", target_bir_lowering=False)
    IN = lambda n, s, dt=F32: nc.dram_tensor(n, s, dt, kind="ExternalInput").ap()
    h0 = IN("h0", [TQ, D])
    w_in = IN("w_in", [depth, D, CJ]); g_attn = IN("g_attn", [depth, 128, 8])
    w2_i = IN("w2", [depth, 16, 32]); gb_i = IN("gb", [depth, 32, 1]); sinks_i = IN("sinks", [depth, 128, 2])
    gmix_i = IN("gmix", [depth, 128, 256]); wout_i = IN("wout", [depth, 128, 2, D])
    gffn_i = IN("gffn", [depth, 128, 8]); wq_i = IN("wq", [depth, D, 2048]); ksub_i = IN("ksub", [depth, 128, 16, 64])
    uT_i = IN("uT", [depth, D, 4096]); v_i = IN("v", [depth, 4096, D]); gfin = IN("gfin", [128, D])
    out = nc.dram_tensor("out", [TQ, D], F32, kind="ExternalOutput").ap()
    DT = lambda n, s, dt=F32: nc.dram_tensor(n, s, dt, kind="Internal").ap()
    GC = 128 * max(d for d in range(1, 5) if NT % d == 0)
    NG = TQ // GC
    xT_loc = [DT("xT_loc%d" % g, [D, GC], BF16) for g in range(NG)]
    xT_all = [DT("xT_all%d" % g, [4 * D, GC], BF16) for g in range(NG)]
    part_loc = DT("part_loc", [Lp, D]); delta_loc = DT("delta_loc", [TQ, D])
    hl = [DT("hl0", [TQ, D]), DT("hl1", [TQ, D])]
    h1_d = DT("h1_d", [TQ, D]); sc_d = DT("sc_d", [TQ, D]); tb_d = DT("tb_d", [TQ, 16]); xT_d = DT("xT_d", [TQ, D], BF16)

    with ExitStack() as st:
        kb = KB(nc, st)
        kb.excl.update(["pT", "pX", "pA", "pB", "pO", "pz0", "pz1", "pc0", "pc1", "pq0", "pq1", "psc0", "psc1",
                        "pH0", "pH1", "ptr", "po0", "po1"])
        ps = [st.enter_context(nc.psum_tensor("ps%d" % i, [128, 512], F32)) for i in range(8)]
        T0 = lambda name, shape, dt=F32: st.enter_context(nc.sbuf_tensor(name, shape, dt))
        idf, idb = _ident(kb, T0)
        stage = [T0("stage%d" % i, [128, 1024]) for i in range(2)]
        scnt = [0]

        def load_w(dst, src, rows_k, cols, key, gt=None):
            for k in range(rows_k):
                for c0 in range(0, cols, 1024):
                    cw = min(1024, cols - c0)
                    si = scnt[0] % 2
                    scnt[0] += 1
                    sk = "stage%d" % si
                    kb.dma("sp", stage[si][:, 0:cw], src[k * 128:(k + 1) * 128, c0:c0 + cw], writes=[sk])
                    if gt is not None:
                        kb.ts(dst[:, k, c0:c0 + cw], stage[si][:, 0:cw], gt[:, k:k + 1], ALU.mult, [sk, "gt"], [key])
                    else:
                        kb.cp(dst[:, k, c0:c0 + cw], stage[si][:, 0:cw], [sk], [key], eng="pool")

        def phase_end():
            kb.barrier()
            kb.emit()

        for li in range(depth):
            hsrc = h0 if li == 0 else hl[(li - 1) % 2]
            hdst = out if li == depth - 1 else hl[li % 2]
            final = (li == depth - 1)
            with ExitStack() as sa:
                T = lambda name, shape, dt=F32, _p="L%dA_" % li: sa.enter_context(nc.sbuf_tensor(_p + name, shape, dt))
                ht = [T("ht%d" % i, [128, D]) for i in range(2)]
                junk = T("junk", [128, D], BF16); ss = T("ss", [128, 1])
                xn = T("xn", [128, D], BF16); xnT = [T("xnT%d" % i, [128, D], BF16) for i in range(2)]
                pT = ps[0][:].bitcast(BF16)
                xv = [x_.rearrange("(k p) t -> p k t", p=128) for x_ in xT_loc]
                for i in range(NT):
                    b = i % 2
                    kb.dma("sp", ht[b][:], hsrc[i * 128:(i + 1) * 128, :], writes=["ht%d" % b])
                    _rstd(kb, ht[b][:], junk[:], ss[:], D, ["ht%d" % b], "a")
                    kb.ts(xn[:], ht[b][:], ss[:, 0:1], ALU.mult, ["ht%d" % b, "ass"], ["xn"])
                    for k in range(8):
                        kb.tr(pT[:, k * 128:(k + 1) * 128], xn[:, k * 128:(k + 1) * 128], idb[:], ["xn", "ident_b"], ["pT"], inc=(k == 7))
                    kb.cp(xnT[b][:], pT, ["pT"], ["xnT%d" % b], eng="act")
                    g_, o_ = (i * 128) // GC, (i * 128) % GC
                    kb.dma("pool", xv[g_][:, :, o_:o_ + 128], xnT[b][:].rearrange("p (k t) -> p k t", t=128),
                           reads=["xnT%d" % b], writes=["xT_loc"])
                kb.barrier()
                for g_ in range(NG):
                    kb.coll("AllGather", ALU.bypass, RG, xT_loc[g_], xT_all[g_], ["xT_loc"], ["xT_all"])
                phase_end()
            if _STOP == "A":
                break
            with ExitStack() as sb_:
                T = lambda name, shape, dt=F32, _p="L%dB_" % li: sb_.enter_context(nc.sbuf_tensor(_p + name, shape, dt))
                gt = T("gt", [128, 8])
                kb.dma("sp", gt[:], g_attn[li], writes=["gt"])
                Wj = T("Wj", [128, 8, 768], BF16)
                load_w(Wj, w_in[li], 8, CJ, "Wj", gt=gt)
                Wo = T("Wo", [128, 2, D], BF16)
                for kc in range(2):
                    si = scnt[0] % 2; scnt[0] += 1
                    kb.dma("sp", stage[si][:], wout_i[li][:, kc, :], writes=["stage%d" % si])
                    kb.cp(Wo[:, kc, :], stage[si][:], ["stage%d" % si], ["Wo"], eng="pool")
                gm = T("gm", [128, 256]); kb.dma("sp", gm[:], gmix_i[li], writes=["gm"])
                m_gen = T("m_gen", [128, 256]); m_n0 = T("m_n0", [128, 256]); m_n1 = T("m_n1", [128, 256])
                for m, nm, extra in ((m_gen, "m_gen", None), (m_n0, "m_n0", -240), (m_n1, "m_n1", -112)):
                    kb.memset(m[:], 0.0, [nm])
                    kb.asel(m[:], ALU.is_ge, NEG, -1, -1, [[1, 256]], nm)
                    kb.asel(m[:], ALU.is_ge, NEG, 128, 1, [[-1, 256]], nm)
                    if extra is not None:
                        kb.asel(m[:], ALU.is_ge, NEG, extra, 0, [[1, 256]], nm)
                sbm_f = T("sbm_f", [128, 512])
                sbm = [T("sbm%d" % d, [128, 512], BF16) for d in range(4)]
                for d in range(4):
                    kb.memset(sbm_f[:], 1.0, ["sbm_f"])
                    kb.asel(sbm_f[:], ALU.is_ge, 0.0, -1 - 128 * d, -1, [[1, 512]], "sbm_f")
                    kb.cp(sbm[d][:], sbm_f[:], ["sbm_f"], ["sbm%d" % d], eng="pool")
                ntri_f = T("ntri_f", [128, 128]); ntri = T("ntri", [128, 128], BF16); nones = T("nones", [128, 128], BF16)
                kb.memset(ntri_f[:], -1.0, ["ntri_f"])
                kb.asel(ntri_f[:], ALU.is_ge, 0.0, 0, 1, [[-1, 128]], "ntri_f")
                kb.cp(ntri[:], ntri_f[:], ["ntri_f"], ["ntri"], eng="pool")
                kb.memset(nones[:], -1.0, ["nones"])
                mle = T("mle", [128, 128])
                kb.memset(mle[:], 1.0, ["mle"])
                kb.asel(mle[:], ALU.is_ge, 0.0, 0, -1, [[1, 128]], "mle")
                rmask = T("rmask", [32, 512])
                kb.memset(rmask[:], 1.0, ["rmask"])
                for b4 in range(4):
                    kb.memset(rmask[:, b4 * 128:b4 * 128 + 1], 0.0, ["rmask"])
                w2f = T("w2f", [16, 32]); w2b = T("w2b", [16, 32], BF16); gbt = T("gbt", [32, 1]); sk_t = T("sk_t", [128, 2])
                kb.dma("sp", w2f[:], w2_i[li], writes=["w2f"]); kb.dma("sp", gbt[:], gb_i[li], writes=["gbt"])
                kb.dma("sp", sk_t[:], sinks_i[li], writes=["sk"])
                kb.cp(w2b[:], w2f[:], ["w2f"], ["w2b"])
                kb.ts(gbt[:], gbt[:], -1.0, ALU.mult, ["gbt"], ["gbt"])
                KbT = T("KbT", [64, Lp], BF16); Vb = T("Vb", [128, NB, 64], BF16)
                S = T("S", [32, 64]); Sb = T("Sb", [32, 64], BF16)
                kb.memset(S[:], 0.0, ["S"]); kb.memset(Sb[:], 0.0, ["Sb"])
                xc = [T("xc%d" % i, [128, 8, 512], BF16) for i in range(2)]
                qa_c = [T("qa_c%d" % i, [128, 512], BF16) for i in range(2)]
                ka_c = [T("ka_c%d" % i, [128, 640], BF16) for i in range(2)]
                va_c = [T("va_c%d" % i, [128, 5, 64], BF16) for i in range(2)]
                qs_c = [T("qs_c%d" % i, [64, 512], BF16) for i in range(2)]
                qc_c = [T("qc_c%d" % i, [32, 512], BF16) for i in range(2)]
                kc_c = [T("kc_c%d" % i, [32, 512], BF16) for i in range(2)]
                vc_c = [T("vc_c%d" % i, [128, 4, 64], BF16) for i in range(2)]
                rc_c = [T("rc_c%d" % i, [128, 4, 64]) for i in range(2)]
                gl_c = [T("gl_c%d" % i, [16, 512], BF16) for i in range(2)]
                mo = [T("mo%d" % i, [128, 4, 256]) for i in range(2)]
                sm = T("sm", [128, 256]); pexp = T("pexp", [128, 256], BF16); pTs = T("pTs", [128, 256], BF16)
                st8 = T("st8", [128, 8])
                ge = T("ge", [32, 512]); gsp = T("gsp", [32, 512]); gcs = T("gcs", [32, 512])
                geq = T("geq", [32, 512]); gek = T("gek", [32, 512])
                qt = T("qt", [32, 512], BF16); kt = T("kt", [32, 512], BF16)
                scb = T("scb", [128, 128], BF16); ktm = T("ktm", [128, 32], BF16)
                stmp = T("stmp", [32, 64])
                e_t = [T("e_t%d" % i, [128, 512]) for i in range(2)]
                sp_t = [[T("sp_t%d_%d" % (pp_, i), [128, 512], BF16) for i in range(2)] for pp_ in range(2)]
                a_t = [[T("a_t%d_%d" % (pp_, i), [128, 512], BF16) for i in range(2)] for pp_ in range(2)]
                R = T("R", [128, 512], BF16)
                ob_t = T("ob_t", [64, 512])
                sq4 = T("sq4", [128, 1024]); s16 = T("s16", [128, 16]); m14 = T("m14", [128, 1024]); sil4 = T("sil4", [128, 4, 64])
                mixb4 = T("mixb4", [128, 1024], BF16); mixT4 = T("mixT4", [128, 1024], BF16)
                pt = [T("pt%d" % i, [128, D]) for i in range(2)]
                pz = [ps[0], ps[1]]; pc = [ps[2], ps[3]]; pO = ps[4]
                pA = ps[5]; pX = ps[6]; pB = ps[7]
                pT_bf = pB[:].bitcast(BF16)

                def pieces(c0, n):
                    t = c0
                    while t < c0 + n:
                        q = t // TQ; tl = t % TQ; g_ = tl // GC; o_ = tl % GC; m = min(c0 + n - t, GC - o_)
                        yield q, g_, o_, t - c0, m
                        t += m

                def load_xc(c):
                    i = c % 2
                    for q, g_, o_, off, m in pieces(c * 512, 512):
                        kb.dma("sp", xc[i][:, :, off:off + m],
                               xT_all[g_][q * D:(q + 1) * D, o_:o_ + m].rearrange("(k p) t -> p k t", p=128), writes=["xc%d" % i])

                def inproj(c):
                    i = c % 2
                    xk = "xc%d" % i
                    if c == 0:
                        kb.memset(ka_c[i][:, 0:128], 0.0, ["ka_c%d" % i])
                        kb.memset(va_c[i][:, 0, :], 0.0, ["va_c%d" % i])
                    else:
                        kb.cp(ka_c[i][:, 0:128], ka_c[1 - i][:, 512:640], ["ka_c%d" % (1 - i)], ["ka_c%d" % i], eng="pool")
                        kb.cp(va_c[i][:, 0, :], va_c[1 - i][:, 4, :], ["va_c%d" % (1 - i)], ["va_c%d" % i], eng="pool")
                    groups = [(0, 128, qa_c[i][:], "qa_c%d" % i, None), (128, 128, ka_c[i][:, 128:640], "ka_c%d" % i, None),
                              (256, 64, qs_c[i][:], "qs_c%d" % i, 0.125), (320, 64, KbT[:, c * 512:(c + 1) * 512], "KbT", None),
                              (384, 32, qc_c[i][:], "qc_c%d" % i, None), (416, 32, kc_c[i][:], "kc_c%d" % i, None),
                              (448, 16, gl_c[i][:], "gl_c%d" % i, None)]
                    for gi, (c0, rows, dst, dk, scale) in enumerate(groups):
                        mr = max(rows, 32)
                        pI, pIk = ((pX, "pX"), (pA, "pA"))[gi % 2]
                        for k in range(8):
                            kb.mm(pI[0:mr, :], Wj[:, k, c0:c0 + mr], xc[i][:, k, :], k == 0, k == 7, ["Wj", xk], [pIk], inc=(k == 7))
                        if scale is not None:
                            kb.ts(dst, pI[0:rows, :], scale, ALU.mult, [pIk], [dk])
                        elif gi % 2:
                            kb.cp(dst, pI[0:rows, :], [pIk], [dk], eng="act")
                        else:
                            kb.cp(dst, pI[0:rows, :], [pIk], [dk])
                    for blk in (range(4) if _STOP != "B1f" else []):
                        n = 4 * c + blk
                        pI, pIk = ((pA, "pA"), (pX, "pX"))[blk % 2]
                        for k in range(8):
                            kb.mm(pI[:, 0:256], xc[i][:, k, blk * 128:(blk + 1) * 128], Wj[:, k, 464:720], k == 0, k == 7, ["Wj", xk], [pIk], inc=(k == 7))
                        ev = "act" if blk % 2 else "dve"
                        kb.cp(va_c[i][:, blk + 1, :], pI[:, 0:64], [pIk], ["va_c%d" % i], eng=ev)
                        kb.cp(Vb[:, n, :], pI[:, 64:128], [pIk], ["Vb"], eng=ev)
                        kb.cp(vc_c[i][:, blk, :], pI[:, 128:192], [pIk], ["vc_c%d" % i], eng=ev)
                        kb.cp(rc_c[i][:, blk, :], pI[:, 192:256], [pIk], ["rc_c%d" % i], eng=ev)

                def swa(c):
                    i = c % 2
                    mk_ = "mo%d" % i
                    for blk in range(4):
                        n = 4 * c + blk
                        msk = m_n0 if n == 0 else (m_n1 if n == 1 else m_gen)
                        mk = "m_n0" if n == 0 else ("m_n1" if n == 1 else "m_gen")
                        for hh in range(2):
                            hs = slice(hh * 64, (hh + 1) * 64)
                            kb.mm(pA[:, 0:256], qa_c[i][hs, blk * 128:(blk + 1) * 128], ka_c[i][hs, blk * 128:blk * 128 + 256],
                                  True, True, ["qa_c%d" % i, "ka_c%d" % i], ["pA"])
                            kb.stt(sm[:], pA[:, 0:256], 0.125, msk[:], ALU.mult, ALU.add, ["pA", mk], ["sm"])
                            kb.op("dve", lambda e: e.tensor_reduce(out=st8[:, 0:1], in_=sm[:], axis=AX.X, op=ALU.max), ["sm"], ["st8"])
                            kb.tt(st8[:, 0:1], st8[:, 0:1], sk_t[:, hh:hh + 1], ALU.max, ["st8", "sk"], ["st8"])
                            kb.ts(st8[:, 1:2], st8[:, 0:1], -1.0, ALU.mult, ["st8"], ["st8"])
                            kb.act(pexp[:], sm[:], AF.Exp, ["sm", "st8"], ["pexp", "st8"], bias=st8[:, 1:2], scale=1.0, accum_out=st8[:, 2:3])
                            kb.act(st8[:, 3:4], sk_t[:, hh:hh + 1], AF.Exp, ["sk", "st8"], ["st8"], bias=st8[:, 1:2], scale=1.0)
                            kb.tt(st8[:, 4:5], st8[:, 2:3], st8[:, 3:4], ALU.add, ["st8"], ["st8"])
                            kb.op("dve", lambda e: e.reciprocal(out=st8[:, 5:6], in_=st8[:, 4:5]), ["st8"], ["st8"])
                            kb.tr(pT_bf[:, 0:128], pexp[:, 0:128], idb[:], ["pexp", "ident_b"], ["pB"], inc=False)
                            kb.tr(pT_bf[:, 128:256], pexp[:, 128:256], idb[:], ["pexp", "ident_b"], ["pB"])
                            kb.cp(pTs[:], pT_bf[:, 0:256], ["pB"], ["pTs"], eng="act")
                            kb.mm(pA[:, 256:320], pTs[:, 0:128], va_c[i][:, blk, :], True, False, ["pTs", "va_c%d" % i], ["pA"], inc=False)
                            kb.mm(pA[:, 256:320], pTs[:, 128:256], va_c[i][:, blk + 1, :], False, True, ["pTs", "va_c%d" % i], ["pA"])
                            kb.ts(mo[i][:, blk, hh * 64:(hh + 1) * 64], pA[:, 256:320], st8[:, 5:6], ALU.mult, ["pA", "st8"], [mk_])

                def gla(c):
                    i = c % 2
                    kb.mm(pX[0:32, :], w2b[:], gl_c[i][:], True, True, ["w2b", "gl_c%d" % i], ["pX"])
                    kb.act(ge[:], pX[0:32, :], AF.Exp, ["pX", "gbt"], ["ge"], bias=gbt[:, 0:1], scale=-1.0)
                    kb.act(gsp[:], ge[:], AF.Ln, ["ge"], ["gsp"], bias=1.0, scale=1.0)
                    kb.op("dve", lambda e: e.tensor_tensor_scan(out=gcs[:], data0=rmask[:], data1=gsp[:], initial=0.0, op0=ALU.mult, op1=ALU.add),
                          ["rmask", "gsp"], ["gcs"])
                    kb.act(geq[:], gcs[:], AF.Exp, ["gcs"], ["geq"], scale=-1.0 / 16.0)
                    kb.act(gek[:], gcs[:], AF.Exp, ["gcs"], ["gek"], scale=1.0 / 16.0)
                    kb.stt(qt[:], qc_c[i][:], 32.0 ** -0.5, geq[:], ALU.mult, ALU.mult, ["qc_c%d" % i, "geq"], ["qt"])
                    kb.tt(kt[:], kc_c[i][:], gek[:], ALU.mult, ["kc_c%d" % i, "gek"], ["kt"])
                    for blk in range(4):
                        bs = slice(blk * 128, (blk + 1) * 128)
                        kb.mm(pA[:, 384:512], kt[:, bs], qt[:, bs], True, True, ["kt", "qt"], ["pA"])
                        kb.tt(scb[:], pA[:, 384:512], mle[:], ALU.mult, ["pA", "mle"], ["scb"])
                        kb.tr(pT_bf[:, 512:544], kt[:, bs], idb[0:32, 0:32], ["kt", "ident_b"], ["pB"])
                        kb.cp(ktm[:], pT_bf[:, 512:544], ["pB"], ["ktm"], eng="act")
                        kb.mm(pA[:, 320:384], scb[:], vc_c[i][:, blk, :], True, False, ["scb", "vc_c%d" % i], ["pA"], inc=False)
                        kb.mm(pA[:, 320:384], qt[:, bs], Sb[:], False, True, ["qt", "Sb"], ["pA"])
                        kb.cp(mo[i][:, blk, 192:256], pA[:, 320:384], ["pA"], ["mo%d" % i], eng="act")
                        kb.mm(pB[0:32, 384:448], ktm[:], vc_c[i][:, blk, :], True, True, ["ktm", "vc_c%d" % i], ["pB"])
                        kb.tt(stmp[:], pB[0:32, 384:448], S[:], ALU.add, ["pB", "S"], ["stmp"])
                        kb.ts(S[:], stmp[:], geq[:, blk * 128 + 127:blk * 128 + 128], ALU.mult, ["stmp", "geq"], ["S"])
                        kb.cp(Sb[:], S[:], ["S"], ["Sb"])

                def sbk(c):
                    i = c % 2
                    nkb = 4 * c + 4
                    npairs = nkb // 2
                    qk = "qs_c%d" % i

                    def kof(p, j):
                        return nkb - 1 - (2 * p + j)

                    def zmm(p):
                        for j in range(2):
                            kblk = kof(p, j)
                            kb.mm(pz[j][:], KbT[:, kblk * 128:(kblk + 1) * 128], qs_c[i][:], True, True, ["KbT", qk], ["pz%d" % j])

                    zmm(0)
                    for p in range(npairs + 2):
                        pp = p % 2
                        if p < npairs:
                            for j in range(2):
                                kb.act(e_t[j][:], pz[j][:], AF.Exp, ["pz%d" % j], ["e_t%d" % j])
                            for j in range(2):
                                kb.act(sp_t[pp][j][:], e_t[j][:], AF.Ln, ["e_t%d" % j], ["sp_t%d_%d" % (pp, j)], bias=1.0, scale=1.0)
                            for j in range(2):
                                dg = kof(p, j) - 4 * c
                                if dg >= 0:
                                    kb.tt(sp_t[pp][j][:], sp_t[pp][j][:], sbm[dg][:], ALU.mult, ["sp_t%d_%d" % (pp, j), "sbm%d" % dg], ["sp_t%d_%d" % (pp, j)])
                        back = 1 <= p <= npairs
                        if back:
                            q = p - 1
                            qq = q % 2
                            first, lastp = (q == 0), (q == npairs - 1)
                            for j in range(2):
                                kblk = kof(q, j)
                                sk_ = "sp_t%d_%d" % (qq, j)
                                kb.mm(pc[j][:], ntri[:], sp_t[qq][j][:], True, False, ["ntri", sk_], ["pc%d" % j], inc=False)
                                if j == 1:
                                    kb.mm(pc[j][:], nones[:], sp_t[qq][0][:], False, False, ["nones", "sp_t%d_0" % qq], ["pc%d" % j], inc=False)
                                if not first:
                                    kb.mm(pc[j][:], nones[:], R[:], False, False, ["nones", "R"], ["pc%d" % j], inc=False)
                                kb.mm(pc[j][:], KbT[:, kblk * 128:(kblk + 1) * 128], qs_c[i][:], False, True, ["KbT", qk], ["pc%d" % j])
                        if p + 1 < npairs:
                            zmm(p + 1)
                        if p >= 2:
                            r_ = p - 2
                            rr = r_ % 2
                            for j in range(2):
                                kblk = kof(r_, j)
                                kb.mm(pO[0:64, :], Vb[:, kblk, :], a_t[rr][j][:], (r_ == 0 and j == 0), (kblk == 0), ["Vb", "a_t%d_%d" % (rr, j)], ["pO"])
                        if back:
                            if not lastp:
                                if first:
                                    kb.tt(R[:], sp_t[qq][0][:], sp_t[qq][1][:], ALU.add, ["sp_t%d_0" % qq, "sp_t%d_1" % qq], ["R"])
                                else:
                                    kb.tt(R[:], R[:], sp_t[qq][0][:], ALU.add, ["R", "sp_t%d_0" % qq], ["R"])
                                    kb.tt(R[:], R[:], sp_t[qq][1][:], ALU.add, ["R", "sp_t%d_1" % qq], ["R"])
                            for j in range(2):
                                kb.act(a_t[qq][j][:], pc[j][:], AF.Exp, ["pc%d" % j], ["a_t%d_%d" % (qq, j)])
                            for j in range(2):
                                dg = kof(q, j) - 4 * c
                                if dg >= 0:
                                    kb.tt(a_t[qq][j][:], a_t[qq][j][:], sbm[dg][:], ALU.mult, ["a_t%d_%d" % (qq, j), "sbm%d" % dg], ["a_t%d_%d" % (qq, j)])
                    kb.cp(ob_t[:], pO[0:64, :], ["pO"], ["ob_t"])

                def post(c):
                    i = c % 2
                    mk_ = "mo%d" % i
                    for blk in range(4):
                        kb.tr(pA[:, blk * 64:(blk + 1) * 64], ob_t[:, blk * 128:(blk + 1) * 128], idf[0:64, 0:64], ["ob_t", "ident_f"], ["pA"], inc=(blk == 3))
                    kb.cp(mo[i][:, :, 128:192], pA[:, 0:256].rearrange("p (b d) -> p b d", d=64), ["pA"], [mk_], eng="act")
                    mof = mo[i][:].rearrange("p b c -> p (b c)")
                    kb.act(sq4[:], mof, AF.Square, [mk_], ["sq4"])
                    kb.op("dve", lambda e: e.tensor_reduce(out=s16[:], in_=sq4[:].rearrange("p (h d) -> p h d", d=64), axis=AX.X, op=ALU.add),
                          ["sq4"], ["s16"])
                    kb.ts(s16[:], s16[:], 1.0 / 64, ALU.mult, ["s16"], ["s16"], s2=EPS, op1=ALU.add)
                    kb.act(s16[:], s16[:], AF.Sqrt, ["s16"], ["s16"])
                    kb.op("dve", lambda e: e.reciprocal(out=s16[:], in_=s16[:]), ["s16"], ["s16"])
                    kb.tt(m14[:].rearrange("p (h d) -> p h d", d=64), mof.rearrange("p (h d) -> p h d", d=64),
                          s16[:].unsqueeze(2).to_broadcast([128, 16, 64]), ALU.mult, [mk_, "s16"], ["m14"])
                    kb.act(sil4[:], rc_c[i][:], AF.Silu, ["rc_c%d" % i], ["sil4"])
                    m14v = m14[:].rearrange("p (b c) -> p b c", c=256)
                    kb.tt(m14v[:, :, 192:256], m14v[:, :, 192:256], sil4[:], ALU.mult, ["m14", "sil4"], ["m14"])
                    kb.tt(mixb4[:].rearrange("p (b c) -> p b c", c=256), m14v, gm[:].unsqueeze(1).to_broadcast([128, 4, 256]), ALU.mult,
                          ["m14", "gm"], ["mixb4"])
                    for t8 in range(8):
                        kb.tr(pT_bf[:, t8 * 128:(t8 + 1) * 128], mixb4[:, t8 * 128:(t8 + 1) * 128], idb[:], ["mixb4", "ident_b"], ["pB"], inc=(t8 == 7))
                    kb.cp(mixT4[:], pT_bf, ["pB"], ["mixT4"], eng="act")
                    for blk in range(4):
                        n = 4 * c + blk
                        pk = "pt%d" % (n % 2)
                        for dh in range(2):
                            pbank, pkey = (pA, "pA") if dh == 0 else (pX, "pX")
                            for kc in range(2):
                                kb.mm(pbank[:], mixT4[:, (2 * blk + kc) * 128:(2 * blk + kc + 1) * 128], Wo[:, kc, dh * 512:(dh + 1) * 512], kc == 0, kc == 1,
                                      ["mixT4", "Wo"], [pkey], inc=(kc == 1))
                            kb.cp(pt[n % 2][:, dh * 512:(dh + 1) * 512], pbank[:], [pkey], [pk], eng=("act" if dh else "dve"))
                        kb.dma("pool", part_loc[n * 128:(n + 1) * 128, :], pt[n % 2][:], reads=[pk], writes=["part_loc"])

                load_xc(0)
                for c in range(NCH):
                    if _STOP == "B0":
                        continue
                    if c + 1 < NCH:
                        load_xc(c + 1)
                    if _STOP == "B0x":
                        continue
                    inproj(c)
                    if _STOP in ("B1", "B1f"):
                        continue
                    swa(c)
                    gla(c)
                    sbk(c)
                    if _STOP == "B2":
                        continue
                    post(c)
                kb.barrier()
                if _STOP not in ("B0", "B0x", "B1", "B1f", "B2", "B3"):
                    kb.coll("ReduceScatter", ALU.add, RG, part_loc, delta_loc, ["part_loc"], ["delta_loc"])
                phase_end()
            if _STOP in ("B", "B0", "B0x", "B1", "B1f", "B2", "B3"):
                break
            with ExitStack() as sc_:
                T = lambda name, shape, dt=F32, _p="L%dC_" % li: sc_.enter_context(nc.sbuf_tensor(_p + name, shape, dt))
                gft = T("gft", [128, 8])
                kb.dma("sp", gft[:], gffn_i[li], writes=["gt"])
                Wq = T("Wq", [128, 8, 2048], BF16); Ks = T("Ks", [128, 16, 64], BF16); ksf = T("ksf", [128, 16, 64])
                kb.dma("sp", ksf[:], ksub_i[li], writes=["ksf"])
                kb.cp(Ks[:], ksf[:], ["ksf"], ["Ks"])
                load_w(Wq, wq_i[li], 8, 2048, "Wq", gt=gft)
                ht = [T("ht%d" % i, [128, D]) for i in range(2)]
                dt_ = [T("dt%d" % i, [128, D]) for i in range(2)]
                h1 = T("h1", [128, D]); junk = T("junk", [128, D], BF16); ss = T("ss", [128, 1])
                xn = T("xn", [128, D], BF16); xnT = T("xnT", [128, D], BF16)
                qT = T("qT", [128, 16, 128], BF16); sct = T("sct", [128, D])
                t1 = T("t1", [128, 8, 16]); t2 = T("t2", [128, 8, 16]); wk1 = T("wk1", [128, 16, 64]); wk2 = T("wk2", [128, 8, 256])
                cand = T("cand", [128, 8, 256]); c8a = T("c8a", [128, 8, 8]); c8b = T("c8b", [128, 8, 8])
                csh = T("csh", [128, 8, 256]); ec = T("ec", [128, 8, 256]); mk8 = T("mk8", [128, 8, 256])
                Z = T("Z", [128, 8]); tb = T("tb", [128, 16])
                pT = ps[0][:].bitcast(BF16)
                for i in range(NT):
                    b = i % 2
                    rs = slice(i * 128, (i + 1) * 128)
                    kb.dma("sp", ht[b][:], hsrc[rs, :], writes=["ht%d" % b])
                    kb.dma("sp", dt_[b][:], delta_loc[rs, :], writes=["dt%d" % b])
                    kb.tt(h1[:], ht[b][:], dt_[b][:], ALU.add, ["ht%d" % b, "dt%d" % b], ["h1"])
                    kb.dma("pool", h1_d[rs, :], h1[:], reads=["h1"], writes=["h1_d"])
                    _rstd(kb, h1[:], junk[:], ss[:], D, ["h1"], "b")
                    kb.ts(xn[:], h1[:], ss[:, 0:1], ALU.mult, ["h1", "bss"], ["xn"])
                    for k in range(8):
                        kb.tr(pT[:, k * 128:(k + 1) * 128], xn[:, k * 128:(k + 1) * 128], idb[:], ["xn", "ident_b"], ["pT"], inc=(k == 7))
                    kb.cp(xnT[:], pT, ["pT"], ["xnT"], eng="act")
                    kb.dma("pool", xT_d[rs, :], xnT[:], reads=["xnT"], writes=["xT_d"])
                    for cg in range(4):
                        pq = ps[3 + cg % 2]
                        for cc in range(4):
                            cidx = cg * 4 + cc
                            for k in range(8):
                                kb.mm(pq[:, cc * 128:(cc + 1) * 128], Wq[:, k, cidx * 128:(cidx + 1) * 128], xnT[:, k * 128:(k + 1) * 128],
                                      k == 0, k == 7, ["Wq", "xnT"], ["pq%d" % (cg % 2)], inc=(k == 7 and cc == 3))
                        kb.cp(qT[:, cg * 4:(cg + 1) * 4, :], pq[:].rearrange("p (c t) -> p c t", t=128), ["pq%d" % (cg % 2)], ["qT"],
                              eng=("act" if cg % 2 else "dve"))
                    for cidx in range(16):
                        pscb = ps[5 + cidx // 8]
                        kb.mm(pscb[:, (cidx % 8) * 64:(cidx % 8 + 1) * 64], qT[:, cidx, :], Ks[:, cidx, :], True, True, ["qT", "Ks"],
                              ["psc%d" % (cidx // 8)], inc=(cidx % 8 == 7))
                    kb.cp(sct[:, 0:512], ps[5][:], ["psc0"], ["sct"], eng="act")
                    kb.cp(sct[:, 512:1024], ps[6][:], ["psc1"], ["sct"], eng="dve")
                    kb.dma("pool", sc_d[rs, :], sct[:], reads=["sct"], writes=["sc_d"])
                    chains = [(hd, side, (t1, t2)[side]) for hd in range(8) for side in range(2)]
                    tkeys = ["tt%d_%d" % (side, hd) for hd, side, _ in chains]
                    for ci_, (hd, side, tt_) in enumerate(chains):
                        sv = sct[:, hd * 128 + side * 64:hd * 128 + side * 64 + 64]
                        kb.op("dve", lambda e, o_=tt_[:, hd, 0:8], i_=sv: e.max(out=o_, in_=i_), ["sct"], [tkeys[ci_]])
                    for ci_, (hd, side, tt_) in enumerate(chains):
                        sv = sct[:, hd * 128 + side * 64:hd * 128 + side * 64 + 64]
                        kb.op("dve", lambda e, o_=wk1[:, ci_, :], r_=tt_[:, hd, 0:8], i_=sv: e.match_replace(out=o_, in_to_replace=r_, in_values=i_, imm_value=-1e30),
                              ["sct", tkeys[ci_]], ["wk1_%d" % ci_])
                    for ci_, (hd, side, tt_) in enumerate(chains):
                        kb.op("dve", lambda e, o_=tt_[:, hd, 8:16], i_=wk1[:, ci_, :]: e.max(out=o_, in_=i_), ["wk1_%d" % ci_], [tkeys[ci_]])
                    kb.tt(cand[:].rearrange("p h (a b) -> p h a b", b=16), t1[:].unsqueeze(3).to_broadcast([128, 8, 16, 16]),
                          t2[:].unsqueeze(2).to_broadcast([128, 8, 16, 16]), ALU.add, tkeys, ["cand"])
                    ckeys = ["c8_%d" % hd for hd in range(8)]
                    for hd in range(8):
                        kb.op("dve", lambda e, o_=c8a[:, hd, :], i_=cand[:, hd, :]: e.max(out=o_, in_=i_), ["cand"], [ckeys[hd]])
                    for hd in range(8):
                        kb.op("dve", lambda e, o_=wk2[:, hd, :], r_=c8a[:, hd, :], i_=cand[:, hd, :]: e.match_replace(out=o_, in_to_replace=r_, in_values=i_, imm_value=-1e30),
                              ["cand", ckeys[hd]], ["wk2_%d" % hd])
                    for hd in range(8):
                        kb.op("dve", lambda e, o_=c8b[:, hd, :], i_=wk2[:, hd, :]: e.max(out=o_, in_=i_), ["wk2_%d" % hd], [ckeys[hd]])
                    kb.tt(csh[:], cand[:], c8a[:, :, 0:1].to_broadcast([128, 8, 256]), ALU.subtract, ["cand"] + ckeys, ["csh"])
                    kb.act(ec[:], csh[:], AF.Exp, ["csh"], ["ec"])
                    kb.tt(mk8[:], cand[:], c8b[:, :, 7:8].to_broadcast([128, 8, 256]), ALU.is_ge, ["cand"] + ckeys, ["mk8"])
                    kb.tt(ec[:], ec[:], mk8[:], ALU.mult, ["ec", "mk8"], ["ec"])
                    kb.op("dve", lambda e: e.tensor_reduce(out=Z[:], in_=ec[:], axis=AX.X, op=ALU.add), ["ec"], ["Z"])
                    kb.act(Z[:], Z[:], AF.Ln, ["Z"], ["Z"])
                    kb.cp(tb[:, 0:8], c8b[:, :, 7], ckeys, ["tb"])
                    kb.stt(tb[:, 8:16], c8a[:, :, 0], -1.0, Z[:], ALU.mult, ALU.subtract, ckeys + ["Z"], ["tb"])
                    kb.dma("pool", tb_d[rs, :], tb[:], reads=["tb"], writes=["tb_d"])
                phase_end()
            if _STOP == "C":
                break
            with ExitStack() as sd_:
                T = lambda name, shape, dt=F32, _p="L%dD_" % li: sd_.enter_context(nc.sbuf_tensor(_p + name, shape, dt))
                gft = T("gft", [128, 8])
                kb.dma("sp", gft[:], gffn_i[li], writes=["gt"])
                Ub = T("Ub", [128, 8, 4096], BF16); Vv = T("Vv", [128, 32, D], BF16)
                load_w(Ub, uT_i[li], 8, 4096, "Ub", gt=gft)
                load_w(Vv, v_i[li], 32, D, "Vv")
                gf = T("gf", [128, D])
                if final:
                    kb.dma("sp", gf[:], gfin, writes=["gf"])
                h1t = [T("h1t%d" % i, [128, D]) for i in range(2)]
                sct = [T("sctb%d" % i, [128, D]) for i in range(2)]
                xT = [T("xTb%d" % i, [128, D], BF16) for i in range(2)]
                tbt = [T("tbt%d" % i, [128, 16]) for i in range(2)]
                NBG = 4
                Sg = [T("Sg%d" % i, [128, 16, 64]) for i in range(NBG)]
                Eg = [T("Eg%d" % i, [128, 1024], BF16) for i in range(NBG)]
                Gh = [T("Gh%d" % i, [128, 1024], BF16) for i in range(2)]; G = T("G", [128, 1024], BF16)
                gl = [T("gl%d" % i, [128, 512]) for i in range(2)]
                Wb = T("Wb", [128, 1024], BF16); WT = T("WT", [128, 1024], BF16)
                ho = T("ho", [128, D]); junk = T("junkb", [128, D], BF16); ss = T("ssb", [128, 1])
                pH = [ps[0], ps[1]]; ptr = ps[2][:].bitcast(BF16); po = [ps[3], ps[4]]

                def loadB(i):
                    b = i % 2
                    rs = slice(i * 128, (i + 1) * 128)
                    kb.dma("sp", h1t[b][:], h1_d[rs, :], writes=["h1t%d" % b])
                    kb.dma("sp", sct[b][:], sc_d[rs, :], writes=["sctb%d" % b])
                    kb.dma("sp", xT[b][:], xT_d[rs, :], writes=["xTb%d" % b])
                    kb.dma("sp", tbt[b][:], tb_d[rs, :], writes=["tbt%d" % b])

                loadB(0)
                cnt = 0
                for i in range(NT):
                    b = i % 2
                    rs = slice(i * 128, (i + 1) * 128)
                    if i + 1 < NT:
                        loadB(i + 1)
                    for eq in range(4):
                        deferred = None
                        for hd in range(8):
                            j = cnt % NBG
                            gj = cnt % 2
                            cnt += 1
                            s1 = sct[b][:, hd * 128 + 16 * eq:hd * 128 + 16 * eq + 16]
                            s2 = sct[b][:, hd * 128 + 64:hd * 128 + 128]
                            kb.tt(Sg[j][:], s1.unsqueeze(2).to_broadcast([128, 16, 64]), s2.unsqueeze(1).to_broadcast([128, 16, 64]), ALU.add,
                                  ["sctb%d" % b], ["Sg%d" % j], eng=("pool" if hd in _POOL_HEADS else "dve"))
                            Sf = Sg[j][:].rearrange("p a b -> p (a b)")
                            kb.act(Eg[j][:], Sf, AF.Exp, ["Sg%d" % j, "tbt%d" % b], ["Eg%d" % j], bias=tbt[b][:, 8 + hd:9 + hd], scale=1.0)
                            if hd == 0:
                                kb.stt(G[:], Sf, tbt[b][:, hd:hd + 1], Eg[j][:], ALU.is_ge, ALU.mult, ["Sg%d" % j, "Eg%d" % j, "tbt%d" % b], ["G"])
                            else:
                                kb.stt(Gh[gj][:], Sf, tbt[b][:, hd:hd + 1], Eg[j][:], ALU.is_ge, ALU.mult, ["Sg%d" % j, "Eg%d" % j, "tbt%d" % b], ["Gh%d" % gj])
                                if deferred is not None:
                                    deferred()
                                deferred = (lambda gj=gj: kb.tt(G[:], G[:], Gh[gj][:], ALU.add, ["G", "Gh%d" % gj], ["G"]))
                        if deferred is not None:
                            deferred()
                        for g2 in range(2):
                            e0 = eq * 1024 + g2 * 512
                            for k in range(8):
                                kb.mm(pH[g2][:], xT[b][:, k * 128:(k + 1) * 128], Ub[:, k, e0:e0 + 512], k == 0, k == 7,
                                      ["xTb%d" % b, "Ub"], ["pH%d" % g2], inc=(k == 7))
                            kb.act(gl[g2][:], pH[g2][:], AF.Gelu, ["pH%d" % g2], ["gl%d" % g2])
                            kb.tt(Wb[:, g2 * 512:(g2 + 1) * 512], gl[g2][:], G[:, g2 * 512:(g2 + 1) * 512], ALU.mult, ["gl%d" % g2, "G"], ["Wb"],
                                  eng=_os.environ.get("WMULT_ENG", "pool"))
                        for cc in range(8):
                            kb.tr(ptr[:, cc * 128:(cc + 1) * 128], Wb[:, cc * 128:(cc + 1) * 128], idb[:], ["Wb", "ident_b"], ["ptr"], inc=(cc == 7))
                        kb.cp(WT[:], ptr, ["ptr"], ["WT"], eng="act")
                        for dh in range(2):
                            for cc in range(8):
                                kb.mm(po[dh][:], WT[:, cc * 128:(cc + 1) * 128], Vv[:, eq * 8 + cc, dh * 512:(dh + 1) * 512],
                                      (eq == 0 and cc == 0), (eq == 3 and cc == 7), ["WT", "Vv"], ["po%d" % dh], inc=(cc == 7))
                    for dh in range(2):
                        kb.tt(ho[:, dh * 512:(dh + 1) * 512], h1t[b][:, dh * 512:(dh + 1) * 512], po[dh][:], ALU.add,
                              ["h1t%d" % b, "po%d" % dh], ["ho"])
                    if final:
                        _rstd(kb, ho[:], junk[:], ss[:], D, ["ho"], "f")
                        kb.stt(ho[:], ho[:], ss[:, 0:1], gf[:], ALU.mult, ALU.mult, ["ho", "fss", "gf"], ["ho"])
                    kb.dma("sp", hdst[rs, :], ho[:], reads=["ho"], writes=["hdst"])
                phase_end()
    return nc


def forward_fused(x, meta_tokens, attn_norm, w_in, attn_sinks, gla_gate_w2, gla_gate_b, swa_out_norm,
                  sb_out_norm, gla_out_norm, w_out, ffn_norm, peer_w_q, peer_sub_keys, peer_u, peer_v, final_norm, runner=None):
    f32 = lambda a: np.asarray(a, np.float32)
    x = f32(x)
    B, SEQ, _ = x.shape
    depth = attn_norm.shape[0]
    L = SEQ + 128
    Lp = ((L + 511) // 512) * 512
    TQ = Lp // 4
    assert B * 4 == NCORES
    nc = _prog_fused(Lp, depth)
    w_in, w_out = f32(w_in), f32(w_out)
    shared = {
        "g_attn": _c(np.stack([_gk(attn_norm[i]) for i in range(depth)])),
        "gffn": _c(np.stack([_gk(ffn_norm[i]) for i in range(depth)])),
        "wq": _c(f32(peer_w_q)),
        "ksub": _c(np.stack([np.transpose(f32(peer_sub_keys[i]).reshape(16, 64, 128), (2, 0, 1)) for i in range(depth)])),
        "uT": _c(np.transpose(f32(peer_u), (0, 2, 1))), "v": _c(f32(peer_v)),
        "gfin": _c(np.broadcast_to(f32(final_norm)[None, :], (128, D))),
    }
    per_j = []
    for j in range(4):
        kv = j // 2
        cols = np.concatenate([np.arange(128 * j, 128 * j + 128), np.arange(512 + 64 * kv, 512 + 64 * kv + 64),
                               np.arange(512 + 64 * kv, 512 + 64 * kv + 64), np.arange(768 + 64 * j, 768 + 64 * j + 64),
                               np.arange(1024 + 64 * j, 1024 + 64 * j + 64), np.arange(1536 + 32 * j, 1536 + 32 * j + 32),
                               np.arange(1664 + 32 * j, 1664 + 32 * j + 32), np.arange(2048, 2064),
                               np.arange(640 + 64 * kv, 640 + 64 * kv + 64), np.arange(1280 + 64 * j, 1280 + 64 * j + 64),
                               np.arange(1792 + 64 * j, 1792 + 64 * j + 64), np.arange(2064 + 64 * j, 2064 + 64 * j + 64)])
        assert len(cols) == CJ
        rows = np.concatenate([np.arange(128 * j, 128 * j + 128), np.arange(512 + 64 * j, 512 + 64 * j + 64),
                               np.arange(768 + 64 * j, 768 + 64 * j + 64)])
        gm = np.stack([np.concatenate([f32(swa_out_norm[i])[128 * j:128 * j + 128], f32(sb_out_norm[i])[64 * j:64 * j + 64],
                                       f32(gla_out_norm[i])[64 * j:64 * j + 64]]) for i in range(depth)])
        per_j.append({
            "w_in": _c(w_in[:, :, cols]),
            "w2": _c(f32(gla_gate_w2)[:, :, 32 * j:32 * j + 32]),
            "gb": _c(f32(gla_gate_b)[:, 32 * j:32 * j + 32].reshape(depth, 32, 1)),
            "sinks": _c(np.broadcast_to(f32(attn_sinks)[:, None, 2 * j:2 * j + 2], (depth, 128, 2))),
            "gmix": _c(np.broadcast_to(gm[:, None, :], (depth, 128, 256))),
            "wout": _c(np.transpose(w_out[:, rows, :].reshape(depth, 2, 128, D), (0, 2, 1, 3))),
        })
    maps = []
    for c in range(NCORES):
        b, r = c // 4, c % 4
        hp = np.zeros((Lp, D), np.float32)
        hp[112:128] = f32(meta_tokens)
        hp[128:L] = x[b]
        maps.append(dict(shared, **per_j[r], h0=_c(hp[r * TQ:(r + 1) * TQ])))
    res = (runner or _run)(nc, maps)
    out = np.zeros((B, SEQ, D), np.float32)
    for b in range(B):
        full = np.concatenate([np.asarray(res[b * 4 + r]["out"]) for r in range(4)], 0)
        out[b] = full[128:L]
    return out


def _prog_fused(Lp, depth):
    key = ("fused", Lp, depth)
    if key not in _CACHE:
        _CACHE[key] = build_fused(Lp, depth)
    return _CACHE[key]
```

```python
import numpy as np
from contextlib import ExitStack
import ml_dtypes
import concourse.bass as bass
import concourse.mybir as mybir
from concourse.bass_utils import run_bass_kernel_spmd

F32 = mybir.dt.float32
BF16 = mybir.dt.bfloat16
AF = mybir.ActivationFunctionType
ALU = mybir.AluOpType
AX = mybir.AxisListType

D = 1024
IN_COLS = 2320
EPS = 1e-6
NEG = -30000.0
NCORES = 8


class KB:
    ENGS = ("pe", "act", "dve", "pool", "sp")
    NDMA = 8

    def __init__(self, nc, stack):
        self.nc = nc
        self._stack = stack
        self.q = {e: [] for e in self.ENGS}
        self.cnt = {e: 0 for e in self.ENGS}
        self.sem = {e: stack.enter_context(nc.semaphore("s_" + e)) for e in self.ENGS}
        self.dsem = {e: [stack.enter_context(nc.semaphore("d_%s%d" % (e, i))) for i in range(self.NDMA)]
                     for e in ("sp", "pool")}
        self.dcnt = {e: [0] * self.NDMA for e in self.dsem}
        self.drot = {e: 0 for e in self.dsem}
        self.seen = {e: {} for e in self.ENGS}
        self.lastw = {}
        self.readers = {}
        self.pending_noinc = {e: False for e in self.ENGS}
        self.excl = set()

    def _deps(self, eng, reads, writes):
        toks = []
        for k in list(reads) + list(writes):
            t = self.lastw.get(k)
            if t is not None:
                toks.append(t)
        for k in writes:
            toks.extend(self.readers.get(k, {}).values())
        waits = {}
        for (sem, val, src) in toks:
            if src == "pe" and eng == "pe":
                continue
            sid = id(sem)
            if self.seen[eng].get(sid, 0) >= val:
                continue
            if sid not in waits or waits[sid][1] < val:
                waits[sid] = (sem, val)
        for sid, (sem, val) in waits.items():
            self.seen[eng][sid] = val
        return list(waits.values())

    def _record(self, rkey, tok, reads, writes):
        for k in writes:
            self.lastw[k] = tok
            self.readers[k] = {}
        for k in reads:
            self.readers.setdefault(k, {})[rkey] = tok

    def op(self, eng, fn, reads=(), writes=(), inc=True):
        ex = [k for k in reads if k in self.excl and k not in writes]
        if ex:
            writes = list(writes) + ex
        waits = self._deps(eng, reads, writes)
        sem = self.sem[eng]
        if inc:
            self.cnt[eng] += 1
            tok = (sem, self.cnt[eng], eng)
            self.pending_noinc[eng] = False
        else:
            tok = (sem, self.cnt[eng] + 1, eng)
            self.pending_noinc[eng] = True
        self.q[eng].append((waits, fn, (sem, 1) if inc else None))
        self._record(eng, tok, reads, writes)

    def dma(self, eng, out, in_, reads=(), writes=()):
        waits = self._deps(eng, reads, writes)
        r = self.drot[eng]
        self.drot[eng] = (r + 1) % self.NDMA
        sem = self.dsem[eng][r]
        prev = self.dcnt[eng][r]
        if prev > 0 and self.seen[eng].get(id(sem), 0) < prev:
            waits.append((sem, prev))
            self.seen[eng][id(sem)] = prev
        self.dcnt[eng][r] += 16
        tok = (sem, self.dcnt[eng][r], "dma")
        self.q[eng].append((waits, lambda e, o=out, i=in_: e.dma_start(out=o, in_=i), (sem, 16)))
        self._record(("dma", id(sem)), tok, reads, writes)

    def coll(self, kind, op, groups, in_, out, reads, writes):
        waits = self._deps("pool", reads, writes)
        if not hasattr(self, "csem"):
            self.csem = self._stack.enter_context(self.nc.semaphore("s_cc"))
            self.ccnt = 0
        if self.ccnt > 0 and self.seen["pool"].get(id(self.csem), 0) < self.ccnt:
            waits.append((self.csem, self.ccnt))
            self.seen["pool"][id(self.csem)] = self.ccnt
        self.ccnt += 1
        tok = (self.csem, self.ccnt, "dma")
        self.q["pool"].append((waits, lambda e, k=kind, o=op, g=groups, i=in_, u=out:
                               e.collective_compute(k, o, replica_groups=g, ins=[i.opt()], outs=[u.opt()]), (self.csem, 1)))
        self._record(("dma", id(self.csem)), tok, reads, writes)

    def wait_all(self, eng, keys):
        waits = self._deps(eng, keys, ())
        self.q[eng].append((waits, None, None))

    def barrier(self):
        for e in self.ENGS:
            waits = []
            for f in self.ENGS:
                if f != e and self.cnt[f] > 0 and self.seen[e].get(id(self.sem[f]), 0) < self.cnt[f]:
                    waits.append((self.sem[f], self.cnt[f]))
                    self.seen[e][id(self.sem[f])] = self.cnt[f]
            for q in self.dsem:
                for r in range(self.NDMA):
                    s, v = self.dsem[q][r], self.dcnt[q][r]
                    if v > 0 and self.seen[e].get(id(s), 0) < v:
                        waits.append((s, v))
                        self.seen[e][id(s)] = v
            if hasattr(self, "csem") and self.ccnt > 0 and self.seen[e].get(id(self.csem), 0) < self.ccnt:
                waits.append((self.csem, self.ccnt))
                self.seen[e][id(self.csem)] = self.ccnt
            self.q[e].append((waits, None, None))

    def emit(self):
        nc = self.nc
        for e in self.ENGS:
            assert not self.pending_noinc[e], "trailing non-inc op on " + e
        qs = self.q
        self.q = {e: [] for e in self.ENGS}
        with nc.Block() as block:
            def run(engname):
                def body(e):
                    for waits, fn, inc in qs[engname]:
                        for sem, val in waits:
                            e.wait_ge(sem, val)
                        if fn is None:
                            continue
                        ins = fn(e)
                        if inc is not None:
                            ins.then_inc(inc[0], inc[1])
                return body
            block.tensor(run("pe"))
            block.scalar(run("act"))
            block.vector(run("dve"))
            block.gpsimd(run("pool"))
            block.sync(run("sp"))

    def act(self, out, in_, func, r, w, eng="act", **kw):
        self.op(eng, lambda e, o=out, i=in_, f=func, k=kw: e.activation(out=o, in_=i, func=f, **k), r, w)

    def tt(self, out, in0, in1, op, r, w, eng="dve"):
        self.op(eng, lambda e, o=out, a=in0, b=in1, p=op: e.tensor_tensor(out=o, in0=a, in1=b, op=p), r, w)

    def ts(self, out, in0, s1, op0, r, w, s2=None, op1=None, eng="dve"):
        if op1 is None:
            self.op(eng, lambda e, o=out, a=in0, x=s1, p=op0: e.tensor_scalar(out=o, in0=a, scalar1=x, scalar2=None, op0=p), r, w)
        else:
            self.op(eng, lambda e, o=out, a=in0, x=s1, y=s2, p=op0, q=op1: e.tensor_scalar(out=o, in0=a, scalar1=x, scalar2=y, op0=p, op1=q), r, w)

    def stt(self, out, in0, scalar, in1, op0, op1, r, w, **kw):
        self.op("dve", lambda e, o=out, a=in0, s=scalar, b=in1, p=op0, q=op1, k=kw:
                e.scalar_tensor_tensor(out=o, in0=a, scalar=s, in1=b, op0=p, op1=q, **k), r, w)

    def cp(self, out, in_, r, w, eng="dve"):
        if eng == "act":
            self.op("act", lambda e, o=out, i=in_: e.activation(out=o, in_=i, func=AF.Copy), r, w)
        else:
            self.op(eng, lambda e, o=out, i=in_: e.tensor_copy(out=o, in_=i), r, w)

    def mm(self, out, lhsT, rhs, start, stop, r, w, inc=True):
        self.op("pe", lambda e, o=out, l=lhsT, x=rhs, s=start, t=stop: e.matmul(o, lhsT=l, rhs=x, start=s, stop=t), r, w, inc=inc)

    def tr(self, out, in_, ident, r, w, inc=True):
        self.op("pe", lambda e, o=out, i=in_, d=ident: e.transpose(o, i, d), r, w, inc=inc)

    def memset(self, ap, val, w, eng="pool"):
        self.op(eng, lambda e, a=ap, v=val: e.memset(a, v), (), w)

    def asel(self, ap, cmp, fill, base, cm, pattern, key):
        self.op("pool", lambda e, a=ap, c=cmp, f=fill, b=base, m=cm, p=pattern:
                e.affine_select(out=a, in_=a, compare_op=c, fill=f, base=b, pattern=p, channel_multiplier=m), [key], [key])


def _ident(kb, T, name="ident"):
    idf = T(name + "_f", [128, 128], F32)
    idb = T(name + "_b", [128, 128], BF16)
    kb.memset(idf[:], 0.0, [name + "_f"])
    kb.asel(idf[:], ALU.not_equal, 1.0, 0, 1, [[-1, 128]], name + "_f")
    kb.cp(idb[:], idf[:], [name + "_f"], [name + "_b"], eng="pool")
    return idf, idb


def _rstd(kb, src, junk, ss, n, rkeys, pfx):
    kb.act(junk, src, AF.Square, rkeys, [pfx + "junk", pfx + "ss"], accum_out=ss)
    kb.ts(ss, ss, 1.0 / n, ALU.mult, [pfx + "ss"], [pfx + "ss"], s2=EPS, op1=ALU.add)
    kb.act(ss, ss, AF.Sqrt, [pfx + "ss"], [pfx + "ss"])
    kb.op("dve", lambda e, a=ss: e.reciprocal(out=a, in_=a), [pfx + "ss"], [pfx + "ss"])


def build_p1(NT):
    nc = bass.Bass("TRN2", target_bir_lowering=False)
    h = nc.dram_tensor("h", [NT * 128, D], F32, kind="ExternalInput").ap()
    w = nc.dram_tensor("w", [D, IN_COLS], F32, kind="ExternalInput").ap()
    g = nc.dram_tensor("g", [128, 8], F32, kind="ExternalInput").ap()
    proj = nc.dram_tensor("proj", [NT * 128, IN_COLS], BF16, kind="ExternalOutput").ap()
    with ExitStack() as st:
        kb = KB(nc, st)
        T = lambda name, shape, dt=F32: st.enter_context(nc.sbuf_tensor(name, shape, dt))
        ps = [st.enter_context(nc.psum_tensor("ps%d" % i, [128, 512], F32)) for i in range(8)]
        idf, idb = _ident(kb, T)
        gt = T("gt", [128, 8])
        kb.dma("sp", gt[:], g, writes=["gt"])
        Wg = T("Wg", [128, 8, IN_COLS], BF16)
        stage = [T("stage%d" % i, [128, IN_COLS]) for i in range(2)]
        for k in range(8):
            sk = "stage%d" % (k % 2)
            kb.dma("sp", stage[k % 2][:], w[k * 128:(k + 1) * 128, :], writes=[sk])
            kb.ts(Wg[:, k, :], stage[k % 2][:], gt[:, k:k + 1], ALU.mult, [sk, "gt"], ["Wg%d" % k])
        ht = [T("ht%d" % i, [128, D]) for i in range(2)]
        junk = T("junk", [128, D], BF16)
        ss = T("ss", [128, 1])
        xn = T("xn", [128, D], BF16)
        xnT = T("xnT", [128, D], BF16)
        pr = [T("pr%d" % i, [128, IN_COLS], BF16) for i in range(2)]
        pT = ps[0][:].bitcast(BF16)
        cgs = [(c0, min(512, IN_COLS - c0)) for c0 in range(0, IN_COLS, 512)]
        wkeys = ["Wg%d" % k for k in range(8)]
        for i in range(NT):
            hk = "ht%d" % (i % 2)
            hb = ht[i % 2]
            kb.dma("sp", hb[:], h[i * 128:(i + 1) * 128, :], writes=[hk])
            _rstd(kb, hb[:], junk[:], ss[:], D, [hk], "a")
            kb.ts(xn[:], hb[:], ss[:, 0:1], ALU.mult, [hk, "ass"], ["xn"])
            for k in range(8):
                kb.tr(pT[:, k * 128:(k + 1) * 128], xn[:, k * 128:(k + 1) * 128], idb[:], ["xn", "ident_b"], ["pT"], inc=(k == 7))
            kb.cp(xnT[:], pT, ["pT"], ["xnT"], eng="act")
            prk = "pr%d" % (i % 2)
            for ci, (c0, cw) in enumerate(cgs):
                pk = "pp%d" % (ci % 2)
                pp = ps[1 + ci % 2]
                for k in range(8):
                    kb.mm(pp[:, 0:cw], xnT[:, k * 128:(k + 1) * 128], Wg[:, k, c0:c0 + cw], k == 0, k == 7,
                          ["xnT", wkeys[k]], [pk], inc=(k == 7))
                kb.cp(pr[i % 2][:, c0:c0 + cw], pp[:, 0:cw], [pk], [prk], eng=("act" if ci % 2 else "dve"))
            kb.dma("pool", proj[i * 128:(i + 1) * 128, :], pr[i % 2][:], reads=[prk], writes=["out"])
        kb.wait_all("pool", ["out"])
        kb.emit()
    return nc


def build_p2(Lp, parts=(1, 1, 1)):
    NCH = Lp // 512
    NB = Lp // 128
    nc = bass.Bass("TRN2", target_bir_lowering=False)
    IN = lambda n, s, dt=BF16: nc.dram_tensor(n, s, dt, kind="ExternalInput").ap()
    qaT = IN("qaT", [128, Lp]); kaT = IN("kaT", [128, Lp]); va = IN("va", [Lp, 64])
    qbT = IN("qbT", [64, Lp]); kbT = IN("kbT", [64, Lp]); vb = IN("vb", [Lp, 64])
    qcT = IN("qcT", [32, Lp]); kcT = IN("kcT", [32, Lp]); vc = IN("vc", [Lp, 64])
    glrT = IN("glrT", [16, Lp])
    w2 = IN("w2", [16, 32], F32); gb = IN("gb", [32, 1], F32); sinks = IN("sinks", [128, 2], F32)
    oa = nc.dram_tensor("oa", [Lp, 128], F32, kind="ExternalOutput").ap()
    obT = nc.dram_tensor("obT", [64, Lp], F32, kind="ExternalOutput").ap()
    oc = nc.dram_tensor("oc", [Lp, 64], F32, kind="ExternalOutput").ap()
    with ExitStack() as st:
        kb = KB(nc, st)
        T = lambda name, shape, dt=F32: st.enter_context(nc.sbuf_tensor(name, shape, dt))
        ps = [st.enter_context(nc.psum_tensor("ps%d" % i, [128, 512], F32)) for i in range(8)]
        idf, idb = _ident(kb, T)
        m_gen = T("m_gen", [128, 256]); m_n0 = T("m_n0", [128, 256]); m_n1 = T("m_n1", [128, 256])
        for m, nm, extra in ((m_gen, "m_gen", None), (m_n0, "m_n0", -240), (m_n1, "m_n1", -112)):
            kb.memset(m[:], 0.0, [nm])
            kb.asel(m[:], ALU.is_ge, NEG, -1, -1, [[1, 256]], nm)
            kb.asel(m[:], ALU.is_ge, NEG, 128, 1, [[-1, 256]], nm)
            if extra is not None:
                kb.asel(m[:], ALU.is_ge, NEG, extra, 0, [[1, 256]], nm)
        sbm_f = T("sbm_f", [128, 512])
        sbm = [T("sbm%d" % d, [128, 512], BF16) for d in range(4)]
        for d in range(4):
            kb.memset(sbm_f[:], 1.0, ["sbm_f"])
            kb.asel(sbm_f[:], ALU.is_ge, 0.0, -1 - 128 * d, -1, [[1, 512]], "sbm_f")
            kb.cp(sbm[d][:], sbm_f[:], ["sbm_f"], ["sbm%d" % d], eng="pool")
        ntri_f = T("ntri_f", [128, 128]); ntri = T("ntri", [128, 128], BF16); nones = T("nones", [128, 128], BF16)
        kb.memset(ntri_f[:], -1.0, ["ntri_f"])
        kb.asel(ntri_f[:], ALU.is_ge, 0.0, 0, 1, [[-1, 128]], "ntri_f")
        kb.cp(ntri[:], ntri_f[:], ["ntri_f"], ["ntri"], eng="pool")
        kb.memset(nones[:], -1.0, ["nones"])
        mle = T("mle", [128, 128])
        kb.memset(mle[:], 1.0, ["mle"])
        kb.asel(mle[:], ALU.is_ge, 0.0, 0, -1, [[1, 128]], "mle")
        rmask = T("rmask", [32, 512])
        kb.memset(rmask[:], 1.0, ["rmask"])
        for b in range(4):
            kb.memset(rmask[:, b * 128:b * 128 + 1], 0.0, ["rmask"])
        w2f = T("w2f", [16, 32]); w2b = T("w2b", [16, 32], BF16); gbt = T("gbt", [32, 1]); sk_t = T("sk_t", [128, 2])
        kb.dma("sp", w2f[:], w2, writes=["w2f"]); kb.dma("sp", gbt[:], gb, writes=["gbt"]); kb.dma("sp", sk_t[:], sinks, writes=["sk"])
        kb.cp(w2b[:], w2f[:], ["w2f"], ["w2b"])
        kb.ts(gbt[:], gbt[:], -1.0, ALU.mult, ["gbt"], ["gbt"])
        KbT = T("KbT", [64, Lp], BF16); Vb = T("Vb", [128, NB, 64], BF16)
        kb.dma("sp", KbT[:], kbT, writes=["KbT"])
        for n0 in range(0, NB, 16):
            n1 = min(NB, n0 + 16)
            kb.dma("sp", Vb[:, n0:n1, :], vb[n0 * 128:n1 * 128, :].rearrange("(n p) d -> p n d", p=128), writes=["Vb"])
        S = T("S", [32, 64]); Sb = T("Sb", [32, 64], BF16)
        kb.memset(S[:], 0.0, ["S"]); kb.memset(Sb[:], 0.0, ["Sb"])
        qa_c = [T("qa_c%d" % i, [128, 512], BF16) for i in range(2)]
        ka_c = [T("ka_c%d" % i, [128, 640], BF16) for i in range(2)]
        va_c = [T("va_c%d" % i, [128, 5, 64], BF16) for i in range(2)]
        qb_c = [T("qb_c%d" % i, [64, 512], BF16) for i in range(2)]
        qs_c = [T("qs_c%d" % i, [64, 512], BF16) for i in range(2)]
        qc_c = [T("qc_c%d" % i, [32, 512], BF16) for i in range(2)]
        kc_c = [T("kc_c%d" % i, [32, 512], BF16) for i in range(2)]
        vc_c = [T("vc_c%d" % i, [128, 4, 64], BF16) for i in range(2)]
        gl_c = [T("gl_c%d" % i, [16, 512], BF16) for i in range(2)]
        sm = T("sm", [128, 256]); pexp = T("pexp", [128, 256], BF16); pTs = T("pTs", [128, 256], BF16)
        st8 = T("st8", [128, 8]); oa_t = [T("oa_t%d" % i, [128, 128]) for i in range(2)]
        ge = T("ge", [32, 512]); gsp = T("gsp", [32, 512]); gcs = T("gcs", [32, 512])
        geq = T("geq", [32, 512]); gek = T("gek", [32, 512])
        qt = T("qt", [32, 512], BF16); kt = T("kt", [32, 512], BF16)
        scb = T("scb", [128, 128], BF16); ktm = T("ktm", [128, 32], BF16); oc_t = [T("oc_t%d" % i, [128, 64]) for i in range(2)]
        stmp = T("stmp", [32, 64])
        e_t = [T("e_t%d" % i, [128, 512]) for i in range(2)]
        sp_t = [T("sp_t%d" % i, [128, 512], BF16) for i in range(2)]
        a_t = [T("a_t%d" % i, [128, 512], BF16) for i in range(2)]
        R = T("R", [128, 512], BF16)
        ob_t = [T("ob_t%d" % i, [64, 512]) for i in range(2)]
        pz = [ps[0], ps[1]]; pc = [ps[2], ps[3]]; pO = ps[4]
        pA = ps[5]; pX = ps[6]; pB = ps[7]
        pT_bf = pB[:].bitcast(BF16)

        def load_chunk(c):
            i = c % 2
            c0 = c * 512
            kb.dma("sp", qa_c[i][:], qaT[:, c0:c0 + 512], writes=["qa_c%d" % i])
            if c == 0:
                kb.memset(ka_c[i][:, 0:128], 0.0, ["ka_c%d" % i])
                kb.memset(va_c[i][:, 0, :], 0.0, ["va_c%d" % i])
                kb.dma("sp", ka_c[i][:, 128:640], kaT[:, 0:512], writes=["ka_c%d" % i])
                kb.dma("sp", va_c[i][:, 1:5, :], va[0:512, :].rearrange("(n p) d -> p n d", p=128), writes=["va_c%d" % i])
            else:
                kb.dma("sp", ka_c[i][:], kaT[:, c0 - 128:c0 + 512], writes=["ka_c%d" % i])
                kb.dma("sp", va_c[i][:], va[c0 - 128:c0 + 512, :].rearrange("(n p) d -> p n d", p=128), writes=["va_c%d" % i])
            kb.dma("sp", qb_c[i][:], qbT[:, c0:c0 + 512], writes=["qb_c%d" % i])
            kb.dma("sp", qc_c[i][:], qcT[:, c0:c0 + 512], writes=["qc_c%d" % i])
            kb.dma("sp", kc_c[i][:], kcT[:, c0:c0 + 512], writes=["kc_c%d" % i])
            kb.dma("sp", vc_c[i][:], vc[c0:c0 + 512, :].rearrange("(n p) d -> p n d", p=128), writes=["vc_c%d" % i])
            kb.dma("sp", gl_c[i][:], glrT[:, c0:c0 + 512], writes=["gl_c%d" % i])

        def _gla(c, i):
            kb.mm(pX[0:32, :], w2b[:], gl_c[i][:], True, True, ["w2b", "gl_c%d" % i], ["pX"])
            kb.act(ge[:], pX[0:32, :], AF.Exp, ["pX", "gbt"], ["ge"], bias=gbt[:, 0:1], scale=-1.0)
            kb.act(gsp[:], ge[:], AF.Ln, ["ge"], ["gsp"], bias=1.0, scale=1.0)
            kb.op("dve", lambda e: e.tensor_tensor_scan(out=gcs[:], data0=rmask[:], data1=gsp[:], initial=0.0, op0=ALU.mult, op1=ALU.add),
                  ["rmask", "gsp"], ["gcs"])
            kb.act(geq[:], gcs[:], AF.Exp, ["gcs"], ["geq"], scale=-1.0 / 16.0)
            kb.act(gek[:], gcs[:], AF.Exp, ["gcs"], ["gek"], scale=1.0 / 16.0)
            kb.stt(qt[:], qc_c[i][:], 32.0 ** -0.5, geq[:], ALU.mult, ALU.mult, ["qc_c%d" % i, "geq"], ["qt"])
            kb.tt(kt[:], kc_c[i][:], gek[:], ALU.mult, ["kc_c%d" % i, "gek"], ["kt"])
            for blk in range(4):
                n = 4 * c + blk
                bs = slice(blk * 128, (blk + 1) * 128)
                kb.mm(pA[:, 384:512], kt[:, bs], qt[:, bs], True, True, ["kt", "qt"], ["pA"])
                kb.tt(scb[:], pA[:, 384:512], mle[:], ALU.mult, ["pA", "mle"], ["scb"])
                kb.tr(pT_bf[:, 512:544], kt[:, bs], idb[0:32, 0:32], ["kt", "ident_b"], ["pB"])
                kb.cp(ktm[:], pT_bf[:, 512:544], ["pB"], ["ktm"], eng="act")
                kb.mm(pA[:, 320:384], scb[:], vc_c[i][:, blk, :], True, False, ["scb", "vc_c%d" % i], ["pA"], inc=False)
                kb.mm(pA[:, 320:384], qt[:, bs], Sb[:], False, True, ["qt", "Sb"], ["pA"])
                ock = "oc_t%d" % (n % 2)
                kb.cp(oc_t[n % 2][:], pA[:, 320:384], ["pA"], [ock], eng="act")
                kb.dma("pool", oc[n * 128:(n + 1) * 128, :], oc_t[n % 2][:], reads=[ock], writes=["oc"])
                kb.mm(pB[0:32, 384:448], ktm[:], vc_c[i][:, blk, :], True, True, ["ktm", "vc_c%d" % i], ["pB"])
                kb.tt(stmp[:], pB[0:32, 384:448], S[:], ALU.add, ["pB", "S"], ["stmp"])
                kb.ts(S[:], stmp[:], geq[:, blk * 128 + 127:blk * 128 + 128], ALU.mult, ["stmp", "geq"], ["S"])
                kb.cp(Sb[:], S[:], ["S"], ["Sb"])

        def _sb(c, i):
            kb.ts(qs_c[i][:], qb_c[i][:], 0.125, ALU.mult, ["qb_c%d" % i], ["qs_c%d" % i])
            nkb = 4 * c + 4
            for it in range(nkb):
                kblk = nkb - 1 - it
                j = it % 2
                dg = kblk - 4 * c
                first, last = (it == 0), (kblk == 0)
                ksl = KbT[:, kblk * 128:(kblk + 1) * 128]
                kb.mm(pz[j][:], ksl, qs_c[i][:], True, True, ["KbT", "qs_c%d" % i], ["pz%d" % j])
                kb.act(e_t[j][:], pz[j][:], AF.Exp, ["pz%d" % j], ["e_t%d" % j])
                kb.act(sp_t[j][:], e_t[j][:], AF.Ln, ["e_t%d" % j], ["sp_t%d" % j], bias=1.0, scale=1.0)
                if dg >= 0:
                    kb.tt(sp_t[j][:], sp_t[j][:], sbm[dg][:], ALU.mult, ["sp_t%d" % j, "sbm%d" % dg], ["sp_t%d" % j])
                kb.mm(pc[j][:], ntri[:], sp_t[j][:], True, False, ["ntri", "sp_t%d" % j], ["pc%d" % j], inc=False)
                if not first:
                    kb.mm(pc[j][:], nones[:], R[:], False, False, ["nones", "R"], ["pc%d" % j], inc=False)
                kb.mm(pc[j][:], ksl, qs_c[i][:], False, True, ["KbT", "qs_c%d" % i], ["pc%d" % j])
                if not last:
                    if first:
                        kb.cp(R[:], sp_t[j][:], ["sp_t%d" % j], ["R"], eng="pool")
                    else:
                        kb.tt(R[:], R[:], sp_t[j][:], ALU.add, ["R", "sp_t%d" % j], ["R"], eng="pool")
                kb.act(a_t[j][:], pc[j][:], AF.Exp, ["pc%d" % j], ["a_t%d" % j])
                if dg >= 0:
                    kb.tt(a_t[j][:], a_t[j][:], sbm[dg][:], ALU.mult, ["a_t%d" % j, "sbm%d" % dg], ["a_t%d" % j])
                kb.mm(pO[0:64, :], Vb[:, kblk, :], a_t[j][:], first, last, ["Vb", "a_t%d" % j], ["pO"], inc=True)
            kb.cp(ob_t[i][:], pO[0:64, :], ["pO"], ["ob_t%d" % i])
            kb.dma("pool", obT[:, c * 512:(c + 1) * 512], ob_t[i][:], reads=["ob_t%d" % i], writes=["ob"])

        load_chunk(0)
        for c in range(NCH):
            i = c % 2
            if c + 1 < NCH:
                load_chunk(c + 1)
            for blk in (range(4) if parts[0] else []):
                n = 4 * c + blk
                msk = m_n0 if n == 0 else (m_n1 if n == 1 else m_gen)
                mk = "m_n0" if n == 0 else ("m_n1" if n == 1 else "m_gen")
                ok = "oa_t%d" % (n % 2)
                for hh in range(2):
                    hs = slice(hh * 64, (hh + 1) * 64)
                    kb.mm(pA[:, 0:256], qa_c[i][hs, blk * 128:(blk + 1) * 128], ka_c[i][hs, blk * 128:blk * 128 + 256],
                          True, True, ["qa_c%d" % i, "ka_c%d" % i], ["pA"])
                    kb.stt(sm[:], pA[:, 0:256], 0.125, msk[:], ALU.mult, ALU.add, ["pA", mk], ["sm"])
                    kb.op("dve", lambda e: e.tensor_reduce(out=st8[:, 0:1], in_=sm[:], axis=AX.X, op=ALU.max), ["sm"], ["st8"])
                    kb.tt(st8[:, 0:1], st8[:, 0:1], sk_t[:, hh:hh + 1], ALU.max, ["st8", "sk"], ["st8"])
                    kb.ts(st8[:, 1:2], st8[:, 0:1], -1.0, ALU.mult, ["st8"], ["st8"])
                    kb.act(pexp[:], sm[:], AF.Exp, ["sm", "st8"], ["pexp", "st8"], bias=st8[:, 1:2], scale=1.0, accum_out=st8[:, 2:3])
                    kb.act(st8[:, 3:4], sk_t[:, hh:hh + 1], AF.Exp, ["sk", "st8"], ["st8"], bias=st8[:, 1:2], scale=1.0)
                    kb.tt(st8[:, 4:5], st8[:, 2:3], st8[:, 3:4], ALU.add, ["st8"], ["st8"])
                    kb.op("dve", lambda e: e.reciprocal(out=st8[:, 5:6], in_=st8[:, 4:5]), ["st8"], ["st8"])
                    kb.tr(pT_bf[:, 0:128], pexp[:, 0:128], idb[:], ["pexp", "ident_b"], ["pB"], inc=False)
                    kb.tr(pT_bf[:, 128:256], pexp[:, 128:256], idb[:], ["pexp", "ident_b"], ["pB"])
                    kb.cp(pTs[:], pT_bf[:, 0:256], ["pB"], ["pTs"], eng="act")
                    kb.mm(pA[:, 256:320], pTs[:, 0:128], va_c[i][:, blk, :], True, False, ["pTs", "va_c%d" % i], ["pA"], inc=False)
                    kb.mm(pA[:, 256:320], pTs[:, 128:256], va_c[i][:, blk + 1, :], False, True, ["pTs", "va_c%d" % i], ["pA"])
                    kb.ts(oa_t[n % 2][:, hs], pA[:, 256:320], st8[:, 5:6], ALU.mult, ["pA", "st8"], [ok])
                kb.dma("pool", oa[n * 128:(n + 1) * 128, :], oa_t[n % 2][:], reads=[ok], writes=["oa"])
            if parts[1]:
              _gla(c, i)
            if parts[2]:
              _sb(c, i)
        kb.wait_all("pool", ["oa", "oc", "ob"])
        kb.emit()
    return nc


def build_p3(NT, final):
    nc = bass.Bass("TRN2", target_bir_lowering=False)
    IN = lambda n, s, dt=F32: nc.dram_tensor(n, s, dt, kind="ExternalInput").ap()
    h = IN("h", [NT * 128, D]); o = IN("o", [NT * 128, D]); rc = IN("rc", [NT * 128, 256], BF16)
    gmix = IN("gmix", [128, D]); wout = IN("wout", [D, D]); gffn = IN("gffn", [128, 8])
    wq = IN("wq", [D, 2048]); ksub = IN("ksub", [128, 16, 64]); uT = IN("uT", [D, 4096]); v = IN("v", [4096, D])
    gfin = IN("gfin", [128, D])
    hout = nc.dram_tensor("hout", [NT * 128, D], F32, kind="ExternalOutput").ap()
    h1_d = nc.dram_tensor("h1_d", [NT * 128, D], F32, kind="Internal").ap()
    sc_d = nc.dram_tensor("sc_d", [NT * 128, D], F32, kind="Internal").ap()
    tb_d = nc.dram_tensor("tb_d", [NT * 128, 16], F32, kind="Internal").ap()
    xT_d = nc.dram_tensor("xT_d", [NT * 128, D], BF16, kind="Internal").ap()
    with ExitStack() as st:
        kb = KB(nc, st)
        ps = [st.enter_context(nc.psum_tensor("ps%d" % i, [128, 512], F32)) for i in range(8)]
        T0 = lambda name, shape, dt=F32: st.enter_context(nc.sbuf_tensor(name, shape, dt))
        idf, idb = _ident(kb, T0)
        gft = T0("gft", [128, 8])
        kb.dma("sp", gft[:], gffn, writes=["gft"])
        stage = [T0("stage%d" % i, [128, 1024]) for i in range(2)]
        scnt = [0]

        def load_w(dst, src, rows_k, cols, key, scale_col=None):
            for k in range(rows_k):
                for c0 in range(0, cols, 1024):
                    cw = min(1024, cols - c0)
                    si = scnt[0] % 2
                    scnt[0] += 1
                    sk = "stage%d" % si
                    kb.dma("sp", stage[si][:, 0:cw], src[k * 128:(k + 1) * 128, c0:c0 + cw], writes=[sk])
                    if scale_col:
                        kb.ts(dst[:, k, c0:c0 + cw], stage[si][:, 0:cw], gft[:, k:k + 1], ALU.mult, [sk, "gft"], [key])
                    else:
                        kb.cp(dst[:, k, c0:c0 + cw], stage[si][:, 0:cw], [sk], [key], eng="pool")

        with ExitStack() as sa:
            T = lambda name, shape, dt=F32: sa.enter_context(nc.sbuf_tensor(name, shape, dt))
            Wo = T("Wo", [128, 8, D], BF16); Wq = T("Wq", [128, 8, 2048], BF16); Ks = T("Ks", [128, 16, 64], BF16)
            gm = T("gm", [128, D]); ksf = T("ksf", [128, 16, 64])
            kb.dma("sp", gm[:], gmix, writes=["gm"])
            kb.dma("sp", ksf[:], ksub, writes=["ksf"])
            kb.cp(Ks[:], ksf[:], ["ksf"], ["Ks"])
            load_w(Wo, wout, 8, D, "Wo")
            load_w(Wq, wq, 8, 2048, "Wq", scale_col=True)
            ht = [T("ht%d" % i, [128, D]) for i in range(2)]
            ot = [T("ot%d" % i, [128, D]) for i in range(2)]
            rct = [T("rct%d" % i, [128, 256], BF16) for i in range(2)]
            sq = T("sq", [128, D]); m1 = T("m1", [128, D]); sil = T("sil", [128, 256])
            s16 = T("s16", [128, 16]); mix = T("mix", [128, D], BF16); mixT = T("mixT", [128, D], BF16)
            h1 = T("h1", [128, D]); junk = T("junk", [128, D], BF16); ss = T("ss", [128, 1])
            xn = T("xn", [128, D], BF16); xnT = T("xnT", [128, D], BF16)
            qT = T("qT", [128, 16, 128], BF16); sct = T("sct", [128, D])
            t1 = T("t1", [128, 8, 16]); t2 = T("t2", [128, 8, 16]); wk = T("wk", [128, 256])
            cand = T("cand", [128, 8, 256]); c8a = T("c8a", [128, 8, 8]); c8b = T("c8b", [128, 8, 8])
            csh = T("csh", [128, 8, 256]); ec = T("ec", [128, 8, 256]); mk8 = T("mk8", [128, 8, 256])
            Z = T("Z", [128, 8]); tb = T("tb", [128, 16])
            pT = ps[0][:].bitcast(BF16)
            for i in range(NT):
                b = i % 2
                rs = slice(i * 128, (i + 1) * 128)
                kb.dma("sp", ht[b][:], h[rs, :], writes=["ht%d" % b])
                kb.dma("sp", ot[b][:], o[rs, :], writes=["ot%d" % b])
                kb.dma("sp", rct[b][:], rc[rs, :], writes=["rct%d" % b])
                kb.act(sq[:], ot[b][:], AF.Square, ["ot%d" % b], ["sq"])
                kb.op("dve", lambda e: e.tensor_reduce(out=s16[:], in_=sq[:].rearrange("p (h d) -> p h d", d=64), axis=AX.X, op=ALU.add),
                      ["sq"], ["s16"])
                kb.ts(s16[:], s16[:], 1.0 / 64, ALU.mult, ["s16"], ["s16"], s2=EPS, op1=ALU.add)
                kb.act(s16[:], s16[:], AF.Sqrt, ["s16"], ["s16"])
                kb.op("dve", lambda e: e.reciprocal(out=s16[:], in_=s16[:]), ["s16"], ["s16"])
                kb.tt(m1[:].rearrange("p (h d) -> p h d", d=64), ot[b][:].rearrange("p (h d) -> p h d", d=64),
                      s16[:].unsqueeze(2).to_broadcast([128, 16, 64]), ALU.mult, ["ot%d" % b, "s16"], ["m1"])
                kb.act(sil[:], rct[b][:], AF.Silu, ["rct%d" % b], ["sil"])
                kb.tt(m1[:, 768:1024], m1[:, 768:1024], sil[:], ALU.mult, ["m1", "sil"], ["m1"])
                kb.tt(mix[:], m1[:], gm[:], ALU.mult, ["m1", "gm"], ["mix"])
                for k in range(8):
                    kb.tr(pT[:, k * 128:(k + 1) * 128], mix[:, k * 128:(k + 1) * 128], idb[:], ["mix", "ident_b"], ["pT"], inc=(k == 7))
                kb.cp(mixT[:], pT, ["pT"], ["mixT"], eng="act")
                for dh in range(2):
                    for k in range(8):
                        kb.mm(ps[1 + dh][:], mixT[:, k * 128:(k + 1) * 128], Wo[:, k, dh * 512:(dh + 1) * 512], k == 0, k == 7,
                              ["mixT", "Wo"], ["pd%d" % dh], inc=(k == 7))
                    kb.tt(h1[:, dh * 512:(dh + 1) * 512], ht[b][:, dh * 512:(dh + 1) * 512], ps[1 + dh][:], ALU.add,
                          ["ht%d" % b, "pd%d" % dh], ["h1"])
                kb.dma("pool", h1_d[rs, :], h1[:], reads=["h1"], writes=["h1_d"])
                _rstd(kb, h1[:], junk[:], ss[:], D, ["h1"], "b")
                kb.ts(xn[:], h1[:], ss[:, 0:1], ALU.mult, ["h1", "bss"], ["xn"])
                for k in range(8):
                    kb.tr(pT[:, k * 128:(k + 1) * 128], xn[:, k * 128:(k + 1) * 128], idb[:], ["xn", "ident_b"], ["pT"], inc=(k == 7))
                kb.cp(xnT[:], pT, ["pT"], ["xnT"], eng="act")
                kb.dma("pool", xT_d[rs, :], xnT[:], reads=["xnT"], writes=["xT_d"])
                for cg in range(4):
                    pq = ps[3 + cg % 2]
                    for cc in range(4):
                        cidx = cg * 4 + cc
                        for k in range(8):
                            kb.mm(pq[:, cc * 128:(cc + 1) * 128], Wq[:, k, cidx * 128:(cidx + 1) * 128], xnT[:, k * 128:(k + 1) * 128],
                                  k == 0, k == 7, ["Wq", "xnT"], ["pq%d" % (cg % 2)], inc=(k == 7 and cc == 3))
                    kb.cp(qT[:, cg * 4:(cg + 1) * 4, :], pq[:].rearrange("p (c t) -> p c t", t=128), ["pq%d" % (cg % 2)], ["qT"],
                          eng=("act" if cg % 2 else "dve"))
                for cidx in range(16):
                    pscb = ps[5 + cidx // 8]
                    kb.mm(pscb[:, (cidx % 8) * 64:(cidx % 8 + 1) * 64], qT[:, cidx, :], Ks[:, cidx, :], True, True, ["qT", "Ks"],
                          ["psc%d" % (cidx // 8)], inc=(cidx % 8 == 7))
                kb.cp(sct[:, 0:512], ps[5][:], ["psc0"], ["sct"], eng="act")
                kb.cp(sct[:, 512:1024], ps[6][:], ["psc1"], ["sct"], eng="dve")
                kb.dma("pool", sc_d[rs, :], sct[:], reads=["sct"], writes=["sc_d"])
                for hd in range(8):
                    for side, tt_ in ((0, t1), (1, t2)):
                        sv = sct[:, hd * 128 + side * 64:hd * 128 + side * 64 + 64]
                        kb.op("dve", lambda e, o_=tt_[:, hd, 0:8], i_=sv: e.max(out=o_, in_=i_), ["sct"], ["tt"])
                        kb.op("dve", lambda e, o_=wk[:, 0:64], r_=tt_[:, hd, 0:8], i_=sv: e.match_replace(out=o_, in_to_replace=r_, in_values=i_, imm_value=-1e30),
                              ["sct", "tt"], ["wk"])
                        kb.op("dve", lambda e, o_=tt_[:, hd, 8:16], i_=wk[:, 0:64]: e.max(out=o_, in_=i_), ["wk"], ["tt"])
                kb.tt(cand[:].rearrange("p h (a b) -> p h a b", b=16), t1[:].unsqueeze(3).to_broadcast([128, 8, 16, 16]),
                      t2[:].unsqueeze(2).to_broadcast([128, 8, 16, 16]), ALU.add, ["tt"], ["cand"])
                for hd in range(8):
                    kb.op("dve", lambda e, o_=c8a[:, hd, :], i_=cand[:, hd, :]: e.max(out=o_, in_=i_), ["cand"], ["c8"])
                    kb.op("dve", lambda e, o_=wk[:], r_=c8a[:, hd, :], i_=cand[:, hd, :]: e.match_replace(out=o_, in_to_replace=r_, in_values=i_, imm_value=-1e30),
                          ["cand", "c8"], ["wk"])
                    kb.op("dve", lambda e, o_=c8b[:, hd, :], i_=wk[:]: e.max(out=o_, in_=i_), ["wk"], ["c8"])
                kb.tt(csh[:], cand[:], c8a[:, :, 0:1].to_broadcast([128, 8, 256]), ALU.subtract, ["cand", "c8"], ["csh"])
                kb.act(ec[:], csh[:], AF.Exp, ["csh"], ["ec"])
                kb.tt(mk8[:], cand[:], c8b[:, :, 7:8].to_broadcast([128, 8, 256]), ALU.is_ge, ["cand", "c8"], ["mk8"])
                kb.tt(ec[:], ec[:], mk8[:], ALU.mult, ["ec", "mk8"], ["ec"])
                kb.op("dve", lambda e: e.tensor_reduce(out=Z[:], in_=ec[:], axis=AX.X, op=ALU.add), ["ec"], ["Z"])
                kb.act(Z[:], Z[:], AF.Ln, ["Z"], ["Z"])
                kb.cp(tb[:, 0:8], c8b[:, :, 7], ["c8"], ["tb"])
                kb.stt(tb[:, 8:16], c8a[:, :, 0], -1.0, Z[:], ALU.mult, ALU.subtract, ["c8", "Z"], ["tb"])
                kb.dma("pool", tb_d[rs, :], tb[:], reads=["tb"], writes=["tb_d"])
            kb.barrier()
            kb.emit()
        with ExitStack() as sb_:
            T = lambda name, shape, dt=F32: sb_.enter_context(nc.sbuf_tensor(name, shape, dt))
            Ub = T("Ub", [128, 8, 4096], BF16); Vv = T("Vv", [128, 32, D], BF16)
            load_w(Ub, uT, 8, 4096, "Ub", scale_col=True)
            load_w(Vv, v, 32, D, "Vv")
            gf = T("gf", [128, D])
            if final:
                kb.dma("sp", gf[:], gfin, writes=["gf"])
            h1t = [T("h1t%d" % i, [128, D]) for i in range(2)]
            sct = [T("sctb%d" % i, [128, D]) for i in range(2)]
            xT = [T("xTb%d" % i, [128, D], BF16) for i in range(2)]
            tbt = [T("tbt%d" % i, [128, 16]) for i in range(2)]
            Sg = [T("Sg%d" % i, [128, 16, 64]) for i in range(2)]
            Eg = [T("Eg%d" % i, [128, 1024], BF16) for i in range(2)]
            Gh = T("Gh", [128, 1024], BF16); G = T("G", [128, 1024])
            gl = [T("gl%d" % i, [128, 512]) for i in range(2)]
            Wb = T("Wb", [128, 1024], BF16); WT = T("WT", [128, 1024], BF16)
            ho = T("ho", [128, D]); junk = T("junkb", [128, D], BF16); ss = T("ssb", [128, 1])
            pH = [ps[0], ps[1]]; ptr = ps[2][:].bitcast(BF16); po = [ps[3], ps[4]]

            def loadB(i):
                b = i % 2
                rs = slice(i * 128, (i + 1) * 128)
                kb.dma("sp", h1t[b][:], h1_d[rs, :], reads=["h1_d"], writes=["h1t%d" % b])
                kb.dma("sp", sct[b][:], sc_d[rs, :], reads=["sc_d"], writes=["sctb%d" % b])
                kb.dma("sp", xT[b][:], xT_d[rs, :], reads=["xT_d"], writes=["xTb%d" % b])
                kb.dma("sp", tbt[b][:], tb_d[rs, :], reads=["tb_d"], writes=["tbt%d" % b])

            loadB(0)
            cnt = 0
            for i in range(NT):
                b = i % 2
                rs = slice(i * 128, (i + 1) * 128)
                if i + 1 < NT:
                    loadB(i + 1)
                for eq in range(4):
                    for hd in range(8):
                        j = cnt % 2
                        cnt += 1
                        s1 = sct[b][:, hd * 128 + 16 * eq:hd * 128 + 16 * eq + 16]
                        s2 = sct[b][:, hd * 128 + 64:hd * 128 + 128]
                        kb.tt(Sg[j][:], s1.unsqueeze(2).to_broadcast([128, 16, 64]), s2.unsqueeze(1).to_broadcast([128, 16, 64]), ALU.add,
                              ["sctb%d" % b], ["Sg%d" % j], eng="pool")
                        Sf = Sg[j][:].rearrange("p a b -> p (a b)")
                        kb.act(Eg[j][:], Sf, AF.Exp, ["Sg%d" % j, "tbt%d" % b], ["Eg%d" % j], bias=tbt[b][:, 8 + hd:9 + hd], scale=1.0)
                        if hd == 0:
                            kb.stt(G[:], Sf, tbt[b][:, hd:hd + 1], Eg[j][:], ALU.is_ge, ALU.mult, ["Sg%d" % j, "Eg%d" % j, "tbt%d" % b], ["G"])
                        else:
                            kb.stt(Gh[:], Sf, tbt[b][:, hd:hd + 1], Eg[j][:], ALU.is_ge, ALU.mult, ["Sg%d" % j, "Eg%d" % j, "tbt%d" % b], ["Gh"])
                            kb.tt(G[:], G[:], Gh[:], ALU.add, ["G", "Gh"], ["G"])
                    for g2 in range(2):
                        e0 = eq * 1024 + g2 * 512
                        for k in range(8):
                            kb.mm(pH[g2][:], xT[b][:, k * 128:(k + 1) * 128], Ub[:, k, e0:e0 + 512], k == 0, k == 7,
                                  ["xTb%d" % b, "Ub"], ["pH%d" % g2], inc=(k == 7))
                        kb.act(gl[g2][:], pH[g2][:], AF.Gelu, ["pH%d" % g2], ["gl%d" % g2])
                        kb.tt(Wb[:, g2 * 512:(g2 + 1) * 512], gl[g2][:], G[:, g2 * 512:(g2 + 1) * 512], ALU.mult, ["gl%d" % g2, "G"], ["Wb"])
                    for cc in range(8):
                        kb.tr(ptr[:, cc * 128:(cc + 1) * 128], Wb[:, cc * 128:(cc + 1) * 128], idb[:], ["Wb", "ident_b"], ["ptr"], inc=(cc == 7))
                    kb.cp(WT[:], ptr, ["ptr"], ["WT"], eng="act")
                    for dh in range(2):
                        for cc in range(8):
                            kb.mm(po[dh][:], WT[:, cc * 128:(cc + 1) * 128], Vv[:, eq * 8 + cc, dh * 512:(dh + 1) * 512],
                                  (eq == 0 and cc == 0), (eq == 3 and cc == 7), ["WT", "Vv"], ["po%d" % dh], inc=(cc == 7))
                for dh in range(2):
                    kb.tt(ho[:, dh * 512:(dh + 1) * 512], h1t[b][:, dh * 512:(dh + 1) * 512], po[dh][:], ALU.add,
                          ["h1t%d" % b, "po%d" % dh], ["ho"])
                if final:
                    _rstd(kb, ho[:], junk[:], ss[:], D, ["ho"], "f")
                    kb.stt(ho[:], ho[:], ss[:, 0:1], gf[:], ALU.mult, ALU.mult, ["ho", "fss", "gf"], ["ho"])
                kb.dma("sp", hout[rs, :], ho[:], reads=["ho"], writes=["hout"])
            kb.wait_all("sp", ["hout"])
            kb.emit()
    return nc


_CACHE = {}


def _prog(name, *args):
    key = (name,) + args
    if key not in _CACHE:
        _CACHE[key] = {"p1": build_p1, "p2": build_p2, "p3": build_p3}[name](*args)
    return _CACHE[key]


def _run(nc, in_maps):
    res = run_bass_kernel_spmd(nc, in_maps, core_ids=list(range(NCORES)))
    return res.results


def _c(a):
    return np.ascontiguousarray(a)


def _gk(g):
    return _c(np.asarray(g, np.float32).reshape(8, 128).T)


def forward(x, meta_tokens, attn_norm, w_in, attn_sinks, gla_gate_w2, gla_gate_b, swa_out_norm,
            sb_out_norm, gla_out_norm, w_out, ffn_norm, peer_w_q, peer_sub_keys, peer_u, peer_v, final_norm):
    x = np.asarray(x, np.float32)
    B, SEQ, _ = x.shape
    depth = attn_norm.shape[0]
    L = SEQ + 128
    Lp = ((L + 511) // 512) * 512
    T = B * L
    NT = (T // 128 + NCORES - 1) // NCORES
    Tp = NT * 128 * NCORES
    hfull = np.zeros((Tp, D), np.float32)
    for b in range(B):
        hfull[b * L + 112:b * L + 128] = meta_tokens
        hfull[b * L + 128:(b + 1) * L] = x[b]
    p1 = _prog("p1", NT)
    p2 = _prog("p2", Lp)
    tsl = [slice(c * NT * 128, (c + 1) * NT * 128) for c in range(NCORES)]
    bf = ml_dtypes.bfloat16
    for i in range(depth):
        w_i = _c(w_in[i]); g_i = _gk(attn_norm[i])
        r = _run(p1, [{"h": hfull[tsl[c]], "w": w_i, "g": g_i} for c in range(NCORES)])
        proj = np.concatenate([np.asarray(r[c]["proj"]).view(bf) if np.asarray(r[c]["proj"]).dtype != bf else np.asarray(r[c]["proj"])
                               for c in range(NCORES)], axis=0)
        maps = []
        for c in range(NCORES):
            b, j = c // 4, c % 4
            pb = np.zeros((Lp, IN_COLS), bf)
            pb[:L] = proj[b * L:(b + 1) * L]
            kv = j // 2
            ka = pb[:, 512 + 64 * kv:512 + 64 * kv + 64].T
            maps.append({
                "qaT": _c(pb[:, 128 * j:128 * j + 128].T), "kaT": _c(np.concatenate([ka, ka], 0)),
                "va": _c(pb[:, 640 + 64 * kv:640 + 64 * kv + 64]),
                "qbT": _c(pb[:, 768 + 64 * j:768 + 64 * j + 64].T), "kbT": _c(pb[:, 1024 + 64 * j:1024 + 64 * j + 64].T),
                "vb": _c(pb[:, 1280 + 64 * j:1280 + 64 * j + 64]),
                "qcT": _c(pb[:, 1536 + 32 * j:1536 + 32 * j + 32].T), "kcT": _c(pb[:, 1664 + 32 * j:1664 + 32 * j + 32].T),
                "vc": _c(pb[:, 1792 + 64 * j:1792 + 64 * j + 64]), "glrT": _c(pb[:, 2048:2064].T),
                "w2": _c(np.asarray(gla_gate_w2[i], np.float32)[:, 32 * j:32 * j + 32]),
                "gb": _c(np.asarray(gla_gate_b[i], np.float32)[32 * j:32 * j + 32].reshape(32, 1)),
                "sinks": _c(np.broadcast_to(np.asarray(attn_sinks[i], np.float32)[2 * j:2 * j + 2][None, :], (128, 2))),
            })
        r = _run(p2, maps)
        ofull = np.zeros((Tp, D), np.float32)
        for c in range(NCORES):
            b, j = c // 4, c % 4
            ofull[b * L:(b + 1) * L, 128 * j:128 * j + 128] = np.asarray(r[c]["oa"])[:L]
            ofull[b * L:(b + 1) * L, 512 + 64 * j:512 + 64 * j + 64] = np.asarray(r[c]["obT"]).T[:L]
            ofull[b * L:(b + 1) * L, 768 + 64 * j:768 + 64 * j + 64] = np.asarray(r[c]["oc"])[:L]
        rcfull = np.zeros((Tp, 256), bf)
        rcfull[:T] = proj[:T, 2064:2320]
        fin = (i == depth - 1)
        p3 = _prog("p3", NT, fin)
        gmix = np.concatenate([swa_out_norm[i], sb_out_norm[i], gla_out_norm[i]]).astype(np.float32)
        shared = {
            "gmix": _c(np.broadcast_to(gmix[None, :], (128, D))), "wout": _c(w_out[i]), "gffn": _gk(ffn_norm[i]),
            "wq": _c(peer_w_q[i]), "ksub": _c(np.transpose(np.asarray(peer_sub_keys[i], np.float32).reshape(16, 64, 128), (2, 0, 1))),
            "uT": _c(np.asarray(peer_u[i], np.float32).T), "v": _c(peer_v[i]),
            "gfin": _c(np.broadcast_to(np.asarray(final_norm, np.float32)[None, :], (128, D))),
        }
        r = _run(p3, [dict(shared, h=hfull[tsl[c]], o=ofull[tsl[c]], rc=rcfull[tsl[c]]) for c in range(NCORES)])
        hfull = np.concatenate([np.asarray(r[c]["hout"]) for c in range(NCORES)], axis=0)
    out = np.stack([hfull[b * L + 128:(b + 1) * L] for b in range(B)], 0)
    return np.ascontiguousarray(out.astype(np.float32))


def kernel(**inputs):
    return forward_fused(**{k: np.asarray(v) for k, v in inputs.items()})


import os as _os
_STOP = _os.environ.get("FUSED_STOP", "")
_POOL_HEADS = tuple(int(c_) for c_ in _os.environ.get("POOL_HEADS", "01234567"))
CJ = 720
RG = [[0, 1, 2, 3], [4, 5, 6, 7]]


def build_fused(Lp, depth):
    TQ = Lp // 4
    NT = TQ // 128
    NCH = Lp // 512
    NB = Lp // 128
    nc = bass.Bass("TRN2", target_bir_lowering=False)
    IN = lambda n, s, dt=F32: nc.dram_tensor(n, s, dt, kind="ExternalInput").ap()
    h0 = IN("h0", [TQ, D])
    w_in = IN("w_in", [depth, D, CJ]); g_attn = IN("g_attn", [depth, 128, 8])
    w2_i = IN("w2", [depth, 16, 32]); gb_i = IN("gb", [depth, 32, 1]); sinks_i = IN("sinks", [depth, 128, 2])
    gmix_i = IN("gmix", [depth, 128, 256]); wout_i = IN("wout", [depth, 128, 2, D])
    gffn_i = IN("gffn", [depth, 128, 8]); wq_i = IN("wq", [depth, D, 2048]); ksub_i = IN("ksub", [depth, 128, 16, 64])
    uT_i = IN("uT", [depth, D, 4096]); v_i = IN("v", [depth, 4096, D]); gfin = IN("gfin", [128, D])
    out = nc.dram_tensor("out", [TQ, D], F32, kind="ExternalOutput").ap()
    DT = lambda n, s, dt=F32: nc.dram_tensor(n, s, dt, kind="Internal").ap()
    GC = 128 * max(d for d in range(1, 5) if NT % d == 0)
    NG = TQ // GC
    xT_loc = [DT("xT_loc%d" % g, [D, GC], BF16) for g in range(NG)]
    xT_all = [DT("xT_all%d" % g, [4 * D, GC], BF16) for g in range(NG)]
    part_loc = DT("part_loc", [Lp, D]); delta_loc = DT("delta_loc", [TQ, D])
    hl = [DT("hl0", [TQ, D]), DT("hl1", [TQ, D])]
    h1_d = DT("h1_d", [TQ, D]); sc_d = DT("sc_d", [TQ, D]); tb_d = DT("tb_d", [TQ, 16]); xT_d = DT("xT_d", [TQ, D], BF16)

    with ExitStack() as st:
        kb = KB(nc, st)
        kb.excl.update(["pT", "pX", "pA", "pB", "pO", "pz0", "pz1", "pc0", "pc1", "pq0", "pq1", "psc0", "psc1",
                        "pH0", "pH1", "ptr", "po0", "po1"])
        ps = [st.enter_context(nc.psum_tensor("ps%d" % i, [128, 512], F32)) for i in range(8)]
        T0 = lambda name, shape, dt=F32: st.enter_context(nc.sbuf_tensor(name, shape, dt))
        idf, idb = _ident(kb, T0)
        stage = [T0("stage%d" % i, [128, 1024]) for i in range(2)]
        scnt = [0]

        def load_w(dst, src, rows_k, cols, key, gt=None):
            for k in range(rows_k):
                for c0 in range(0, cols, 1024):
                    cw = min(1024, cols - c0)
                    si = scnt[0] % 2
                    scnt[0] += 1
                    sk = "stage%d" % si
                    kb.dma("sp", stage[si][:, 0:cw], src[k * 128:(k + 1) * 128, c0:c0 + cw], writes=[sk])
                    if gt is not None:
                        kb.ts(dst[:, k, c0:c0 + cw], stage[si][:, 0:cw], gt[:, k:k + 1], ALU.mult, [sk, "gt"], [key])
                    else:
                        kb.cp(dst[:, k, c0:c0 + cw], stage[si][:, 0:cw], [sk], [key], eng="pool")

        def phase_end():
            kb.barrier()
            kb.emit()

        for li in range(depth):
            hsrc = h0 if li == 0 else hl[(li - 1) % 2]
            hdst = out if li == depth - 1 else hl[li % 2]
            final = (li == depth - 1)
            with ExitStack() as sa:
                T = lambda name, shape, dt=F32, _p="L%dA_" % li: sa.enter_context(nc.sbuf_tensor(_p + name, shape, dt))
                ht = [T("ht%d" % i, [128, D]) for i in range(2)]
                junk = T("junk", [128, D], BF16); ss = T("ss", [128, 1])
                xn = T("xn", [128, D], BF16); xnT = [T("xnT%d" % i, [128, D], BF16) for i in range(2)]
                pT = ps[0][:].bitcast(BF16)
                xv = [x_.rearrange("(k p) t -> p k t", p=128) for x_ in xT_loc]
                for i in range(NT):
                    b = i % 2
                    kb.dma("sp", ht[b][:], hsrc[i * 128:(i + 1) * 128, :], writes=["ht%d" % b])
                    _rstd(kb, ht[b][:], junk[:], ss[:], D, ["ht%d" % b], "a")
                    kb.ts(xn[:], ht[b][:], ss[:, 0:1], ALU.mult, ["ht%d" % b, "ass"], ["xn"])
                    for k in range(8):
                        kb.tr(pT[:, k * 128:(k + 1) * 128], xn[:, k * 128:(k + 1) * 128], idb[:], ["xn", "ident_b"], ["pT"], inc=(k == 7))
                    kb.cp(xnT[b][:], pT, ["pT"], ["xnT%d" % b], eng="act")
                    g_, o_ = (i * 128) // GC, (i * 128) % GC
                    kb.dma("pool", xv[g_][:, :, o_:o_ + 128], xnT[b][:].rearrange("p (k t) -> p k t", t=128),
                           reads=["xnT%d" % b], writes=["xT_loc"])
                kb.barrier()
                for g_ in range(NG):
                    kb.coll("AllGather", ALU.bypass, RG, xT_loc[g_], xT_all[g_], ["xT_loc"], ["xT_all"])
                phase_end()
            if _STOP == "A":
                break
            with ExitStack() as sb_:
                T = lambda name, shape, dt=F32, _p="L%dB_" % li: sb_.enter_context(nc.sbuf_tensor(_p + name, shape, dt))
                gt = T("gt", [128, 8])
                kb.dma("sp", gt[:], g_attn[li], writes=["gt"])
                Wj = T("Wj", [128, 8, 768], BF16)
                load_w(Wj, w_in[li], 8, CJ, "Wj", gt=gt)
                Wo = T("Wo", [128, 2, D], BF16)
                for kc in range(2):
                    si = scnt[0] % 2; scnt[0] += 1
                    kb.dma("sp", stage[si][:], wout_i[li][:, kc, :], writes=["stage%d" % si])
                    kb.cp(Wo[:, kc, :], stage[si][:], ["stage%d" % si], ["Wo"], eng="pool")
                gm = T("gm", [128, 256]); kb.dma("sp", gm[:], gmix_i[li], writes=["gm"])
                m_gen = T("m_gen", [128, 256]); m_n0 = T("m_n0", [128, 256]); m_n1 = T("m_n1", [128, 256])
                for m, nm, extra in ((m_gen, "m_gen", None), (m_n0, "m_n0", -240), (m_n1, "m_n1", -112)):
                    kb.memset(m[:], 0.0, [nm])
                    kb.asel(m[:], ALU.is_ge, NEG, -1, -1, [[1, 256]], nm)
                    kb.asel(m[:], ALU.is_ge, NEG, 128, 1, [[-1, 256]], nm)
                    if extra is not None:
                        kb.asel(m[:], ALU.is_ge, NEG, extra, 0, [[1, 256]], nm)
                sbm_f = T("sbm_f", [128, 512])
                sbm = [T("sbm%d" % d, [128, 512], BF16) for d in range(4)]
                for d in range(4):
                    kb.memset(sbm_f[:], 1.0, ["sbm_f"])
                    kb.asel(sbm_f[:], ALU.is_ge, 0.0, -1 - 128 * d, -1, [[1, 512]], "sbm_f")
                    kb.cp(sbm[d][:], sbm_f[:], ["sbm_f"], ["sbm%d" % d], eng="pool")
                ntri_f = T("ntri_f", [128, 128]); ntri = T("ntri", [128, 128], BF16); nones = T("nones", [128, 128], BF16)
                kb.memset(ntri_f[:], -1.0, ["ntri_f"])
                kb.asel(ntri_f[:], ALU.is_ge, 0.0, 0, 1, [[-1, 128]], "ntri_f")
                kb.cp(ntri[:], ntri_f[:], ["ntri_f"], ["ntri"], eng="pool")
                kb.memset(nones[:], -1.0, ["nones"])
                mle = T("mle", [128, 128])
                kb.memset(mle[:], 1.0, ["mle"])
                kb.asel(mle[:], ALU.is_ge, 0.0, 0, -1, [[1, 128]], "mle")
                rmask = T("rmask", [32, 512])
                kb.memset(rmask[:], 1.0, ["rmask"])
                for b4 in range(4):
                    kb.memset(rmask[:, b4 * 128:b4 * 128 + 1], 0.0, ["rmask"])
                w2f = T("w2f", [16, 32]); w2b = T("w2b", [16, 32], BF16); gbt = T("gbt", [32, 1]); sk_t = T("sk_t", [128, 2])
                kb.dma("sp", w2f[:], w2_i[li], writes=["w2f"]); kb.dma("sp", gbt[:], gb_i[li], writes=["gbt"])
                kb.dma("sp", sk_t[:], sinks_i[li], writes=["sk"])
                kb.cp(w2b[:], w2f[:], ["w2f"], ["w2b"])
                kb.ts(gbt[:], gbt[:], -1.0, ALU.mult, ["gbt"], ["gbt"])
                KbT = T("KbT", [64, Lp], BF16); Vb = T("Vb", [128, NB, 64], BF16)
                S = T("S", [32, 64]); Sb = T("Sb", [32, 64], BF16)
                kb.memset(S[:], 0.0, ["S"]); kb.memset(Sb[:], 0.0, ["Sb"])
                xc = [T("xc%d" % i, [128, 8, 512], BF16) for i in range(2)]
                qa_c = [T("qa_c%d" % i, [128, 512], BF16) for i in range(2)]
                ka_c = [T("ka_c%d" % i, [128, 640], BF16) for i in range(2)]
                va_c = [T("va_c%d" % i, [128, 5, 64], BF16) for i in range(2)]
                qs_c = [T("qs_c%d" % i, [64, 512], BF16) for i in range(2)]
                qc_c = [T("qc_c%d" % i, [32, 512], BF16) for i in range(2)]
                kc_c = [T("kc_c%d" % i, [32, 512], BF16) for i in range(2)]
                vc_c = [T("vc_c%d" % i, [128, 4, 64], BF16) for i in range(2)]
                rc_c = [T("rc_c%d" % i, [128, 4, 64]) for i in range(2)]
                gl_c = [T("gl_c%d" % i, [16, 512], BF16) for i in range(2)]
                mo = [T("mo%d" % i, [128, 4, 256]) for i in range(2)]
                sm = T("sm", [128, 256]); pexp = T("pexp", [128, 256], BF16); pTs = T("pTs", [128, 256], BF16)
                st8 = T("st8", [128, 8])
                ge = T("ge", [32, 512]); gsp = T("gsp", [32, 512]); gcs = T("gcs", [32, 512])
                geq = T("geq", [32, 512]); gek = T("gek", [32, 512])
                qt = T("qt", [32, 512], BF16); kt = T("kt", [32, 512], BF16)
                scb = T("scb", [128, 128], BF16); ktm = T("ktm", [128, 32], BF16)
                stmp = T("stmp", [32, 64])
                e_t = [T("e_t%d" % i, [128, 512]) for i in range(2)]
                sp_t = [[T("sp_t%d_%d" % (pp_, i), [128, 512], BF16) for i in range(2)] for pp_ in range(2)]
                a_t = [[T("a_t%d_%d" % (pp_, i), [128, 512], BF16) for i in range(2)] for pp_ in range(2)]
                R = T("R", [128, 512], BF16); Rp = T("Rp", [128, 512], BF16)
                ob_t = T("ob_t", [64, 512])
                sq4 = T("sq4", [128, 1024]); s16 = T("s16", [128, 16]); m14 = T("m14", [128, 1024]); sil4 = T("sil4", [128, 4, 64])
                mixb4 = T("mixb4", [128, 1024], BF16); mixT4 = T("mixT4", [128, 1024], BF16)
                pt = [T("pt%d" % i, [128, D]) for i in range(2)]
                pz = [ps[0], ps[1]]; pc = [ps[2], ps[3]]; pO = ps[4]
                pA = ps[5]; pX = ps[6]; pB = ps[7]
                pT_bf = pB[:].bitcast(BF16)

                def pieces(c0, n):
                    t = c0
                    while t < c0 + n:
                        q = t // TQ; tl = t % TQ; g_ = tl // GC; o_ = tl % GC; m = min(c0 + n - t, GC - o_)
                        yield q, g_, o_, t - c0, m
                        t += m

                def load_xc(c):
                    i = c % 2
                    for q, g_, o_, off, m in pieces(c * 512, 512):
                        kb.dma("sp", xc[i][:, :, off:off + m],
                               xT_all[g_][q * D:(q + 1) * D, o_:o_ + m].rearrange("(k p) t -> p k t", p=128), writes=["xc%d" % i])

                def inproj(c):
                    i = c % 2
                    xk = "xc%d" % i
                    if c == 0:
                        kb.memset(ka_c[i][:, 0:128], 0.0, ["ka_c%d" % i])
                        kb.memset(va_c[i][:, 0, :], 0.0, ["va_c%d" % i])
                    else:
                        kb.cp(ka_c[i][:, 0:128], ka_c[1 - i][:, 512:640], ["ka_c%d" % (1 - i)], ["ka_c%d" % i], eng="pool")
                        kb.cp(va_c[i][:, 0, :], va_c[1 - i][:, 4, :], ["va_c%d" % (1 - i)], ["va_c%d" % i], eng="pool")
                    groups = [(0, 128, qa_c[i][:], "qa_c%d" % i, None), (128, 128, ka_c[i][:, 128:640], "ka_c%d" % i, None),
                              (256, 64, qs_c[i][:], "qs_c%d" % i, 0.125), (320, 64, KbT[:, c * 512:(c + 1) * 512], "KbT", None),
                              (384, 32, qc_c[i][:], "qc_c%d" % i, None), (416, 32, kc_c[i][:], "kc_c%d" % i, None),
                              (448, 16, gl_c[i][:], "gl_c%d" % i, None)]
                    for gi, (c0, rows, dst, dk, scale) in enumerate(groups):
                        mr = max(rows, 32)
                        pI, pIk = ((pX, "pX"), (pA, "pA"))[gi % 2]
                        for k in range(8):
                            kb.mm(pI[0:mr, :], Wj[:, k, c0:c0 + mr], xc[i][:, k, :], k == 0, k == 7, ["Wj", xk], [pIk], inc=(k == 7))
                        if scale is not None:
                            kb.ts(dst, pI[0:rows, :], scale, ALU.mult, [pIk], [dk])
                        elif gi % 2:
                            kb.cp(dst, pI[0:rows, :], [pIk], [dk], eng="act")
                        else:
                            kb.cp(dst, pI[0:rows, :], [pIk], [dk])
                    for blk in (range(4) if _STOP != "B1f" else []):
                        n = 4 * c + blk
                        pI, pIk = ((pA, "pA"), (pX, "pX"))[blk % 2]
                        for k in range(8):
                            kb.mm(pI[:, 0:256], xc[i][:, k, blk * 128:(blk + 1) * 128], Wj[:, k, 464:720], k == 0, k == 7, ["Wj", xk], [pIk], inc=(k == 7))
                        ev = "act" if blk % 2 else "dve"
                        kb.cp(va_c[i][:, blk + 1, :], pI[:, 0:64], [pIk], ["va_c%d" % i], eng=ev)
                        kb.cp(Vb[:, n, :], pI[:, 64:128], [pIk], ["Vb"], eng=ev)
                        kb.cp(vc_c[i][:, blk, :], pI[:, 128:192], [pIk], ["vc_c%d" % i], eng=ev)
                        kb.cp(rc_c[i][:, blk, :], pI[:, 192:256], [pIk], ["rc_c%d" % i], eng=ev)

                def swa(c):
                    i = c % 2
                    mk_ = "mo%d" % i
                    for blk in range(4):
                        n = 4 * c + blk
                        msk = m_n0 if n == 0 else (m_n1 if n == 1 else m_gen)
                        mk = "m_n0" if n == 0 else ("m_n1" if n == 1 else "m_gen")
                        for hh in range(2):
                            hs = slice(hh * 64, (hh + 1) * 64)
                            kb.mm(pA[:, 0:256], qa_c[i][hs, blk * 128:(blk + 1) * 128], ka_c[i][hs, blk * 128:blk * 128 + 256],
                                  True, True, ["qa_c%d" % i, "ka_c%d" % i], ["pA"])
                            kb.stt(sm[:], pA[:, 0:256], 0.125, msk[:], ALU.mult, ALU.add, ["pA", mk], ["sm"])
                            kb.op("dve", lambda e: e.tensor_reduce(out=st8[:, 0:1], in_=sm[:], axis=AX.X, op=ALU.max), ["sm"], ["st8"])
                            kb.tt(st8[:, 0:1], st8[:, 0:1], sk_t[:, hh:hh + 1], ALU.max, ["st8", "sk"], ["st8"])
                            kb.ts(st8[:, 1:2], st8[:, 0:1], -1.0, ALU.mult, ["st8"], ["st8"])
                            kb.act(pexp[:], sm[:], AF.Exp, ["sm", "st8"], ["pexp", "st8"], bias=st8[:, 1:2], scale=1.0, accum_out=st8[:, 2:3])
                            kb.act(st8[:, 3:4], sk_t[:, hh:hh + 1], AF.Exp, ["sk", "st8"], ["st8"], bias=st8[:, 1:2], scale=1.0)
                            kb.tt(st8[:, 4:5], st8[:, 2:3], st8[:, 3:4], ALU.add, ["st8"], ["st8"])
                            kb.op("dve", lambda e: e.reciprocal(out=st8[:, 5:6], in_=st8[:, 4:5]), ["st8"], ["st8"])
                            kb.tr(pT_bf[:, 0:128], pexp[:, 0:128], idb[:], ["pexp", "ident_b"], ["pB"], inc=False)
                            kb.tr(pT_bf[:, 128:256], pexp[:, 128:256], idb[:], ["pexp", "ident_b"], ["pB"])
                            kb.cp(pTs[:], pT_bf[:, 0:256], ["pB"], ["pTs"], eng="act")
                            kb.mm(pA[:, 256:320], pTs[:, 0:128], va_c[i][:, blk, :], True, False, ["pTs", "va_c%d" % i], ["pA"], inc=False)
                            kb.mm(pA[:, 256:320], pTs[:, 128:256], va_c[i][:, blk + 1, :], False, True, ["pTs", "va_c%d" % i], ["pA"])
                            kb.ts(mo[i][:, blk, hh * 64:(hh + 1) * 64], pA[:, 256:320], st8[:, 5:6], ALU.mult, ["pA", "st8"], [mk_])

                def gla(c):
                    i = c % 2
                    kb.mm(pX[0:32, :], w2b[:], gl_c[i][:], True, True, ["w2b", "gl_c%d" % i], ["pX"])
                    kb.act(ge[:], pX[0:32, :], AF.Exp, ["pX", "gbt"], ["ge"], bias=gbt[:, 0:1], scale=-1.0)
                    kb.act(gsp[:], ge[:], AF.Ln, ["ge"], ["gsp"], bias=1.0, scale=1.0)
                    kb.op("dve", lambda e: e.tensor_tensor_scan(out=gcs[:], data0=rmask[:], data1=gsp[:], initial=0.0, op0=ALU.mult, op1=ALU.add),
                          ["rmask", "gsp"], ["gcs"])
                    kb.act(geq[:], gcs[:], AF.Exp, ["gcs"], ["geq"], scale=-1.0 / 16.0)
                    kb.act(gek[:], gcs[:], AF.Exp, ["gcs"], ["gek"], scale=1.0 / 16.0)
                    kb.stt(qt[:], qc_c[i][:], 32.0 ** -0.5, geq[:], ALU.mult, ALU.mult, ["qc_c%d" % i, "geq"], ["qt"])
                    kb.tt(kt[:], kc_c[i][:], gek[:], ALU.mult, ["kc_c%d" % i, "gek"], ["kt"])
                    for blk in range(4):
                        bs = slice(blk * 128, (blk + 1) * 128)
                        kb.mm(pA[:, 384:512], kt[:, bs], qt[:, bs], True, True, ["kt", "qt"], ["pA"])
                        kb.tt(scb[:], pA[:, 384:512], mle[:], ALU.mult, ["pA", "mle"], ["scb"])
                        kb.tr(pT_bf[:, 512:544], kt[:, bs], idb[0:32, 0:32], ["kt", "ident_b"], ["pB"])
                        kb.cp(ktm[:], pT_bf[:, 512:544], ["pB"], ["ktm"], eng="act")
                        kb.mm(pA[:, 320:384], scb[:], vc_c[i][:, blk, :], True, False, ["scb", "vc_c%d" % i], ["pA"], inc=False)
                        kb.mm(pA[:, 320:384], qt[:, bs], Sb[:], False, True, ["qt", "Sb"], ["pA"])
                        kb.cp(mo[i][:, blk, 192:256], pA[:, 320:384], ["pA"], ["mo%d" % i], eng="act")
                        kb.mm(pB[0:32, 384:448], ktm[:], vc_c[i][:, blk, :], True, True, ["ktm", "vc_c%d" % i], ["pB"])
                        kb.tt(stmp[:], pB[0:32, 384:448], S[:], ALU.add, ["pB", "S"], ["stmp"])
                        kb.ts(S[:], stmp[:], geq[:, blk * 128 + 127:blk * 128 + 128], ALU.mult, ["stmp", "geq"], ["S"])
                        kb.cp(Sb[:], S[:], ["S"], ["Sb"])

                def sbk(c):
                    i = c % 2
                    nkb = 4 * c + 4
                    npairs = nkb // 2
                    qk = "qs_c%d" % i

                    def kof(p, j):
                        return nkb - 1 - (2 * p + j)

                    def zmm(p):
                        for j in range(2):
                            kblk = kof(p, j)
                            kb.mm(pz[j][:], KbT[:, kblk * 128:(kblk + 1) * 128], qs_c[i][:], True, True, ["KbT", qk], ["pz%d" % j])

                    zmm(0)
                    for p in range(npairs + 2):
                        pp = p % 2
                        if p < npairs:
                            for j in range(2):
                                kb.act(e_t[j][:], pz[j][:], AF.Exp, ["pz%d" % j], ["e_t%d" % j])
                            for j in range(2):
                                kb.act(sp_t[pp][j][:], e_t[j][:], AF.Ln, ["e_t%d" % j], ["sp_t%d_%d" % (pp, j)], bias=1.0, scale=1.0)
                            for j in range(2):
                                dg = kof(p, j) - 4 * c
                                if dg >= 0:
                                    kb.tt(sp_t[pp][j][:], sp_t[pp][j][:], sbm[dg][:], ALU.mult, ["sp_t%d_%d" % (pp, j), "sbm%d" % dg], ["sp_t%d_%d" % (pp, j)])
                        back = 1 <= p <= npairs
                        if back:
                            q = p - 1
                            qq = q % 2
                            first, lastp = (q == 0), (q == npairs - 1)
                            for j in range(2):
                                kblk = kof(q, j)
                                sk_ = "sp_t%d_%d" % (qq, j)
                                kb.mm(pc[j][:], ntri[:], sp_t[qq][j][:], True, False, ["ntri", sk_], ["pc%d" % j], inc=False)
                                if j == 0:
                                    if not first:
                                        kb.mm(pc[j][:], nones[:], R[:], False, False, ["nones", "R"], ["pc%d" % j], inc=False)
                                        kb.tt(Rp[:], R[:], sp_t[qq][0][:], ALU.add, ["R", "sp_t%d_0" % qq], ["Rp"])
                                elif first:
                                    kb.mm(pc[j][:], nones[:], sp_t[qq][0][:], False, False, ["nones", "sp_t%d_0" % qq], ["pc%d" % j], inc=False)
                                else:
                                    kb.mm(pc[j][:], nones[:], Rp[:], False, False, ["nones", "Rp"], ["pc%d" % j], inc=False)
                                kb.mm(pc[j][:], KbT[:, kblk * 128:(kblk + 1) * 128], qs_c[i][:], False, True, ["KbT", qk], ["pc%d" % j])
                        if p + 1 < npairs:
                            zmm(p + 1)
                        if p >= 2:
                            r_ = p - 2
                            rr = r_ % 2
                            for j in range(2):
                                kblk = kof(r_, j)
                                kb.mm(pO[0:64, :], Vb[:, kblk, :], a_t[rr][j][:], (r_ == 0 and j == 0), (kblk == 0), ["Vb", "a_t%d_%d" % (rr, j)], ["pO"])
                        if back:
                            if not lastp:
                                if first:
                                    kb.tt(R[:], sp_t[qq][0][:], sp_t[qq][1][:], ALU.add, ["sp_t%d_0" % qq, "sp_t%d_1" % qq], ["R"])
                                else:
                                    kb.tt(R[:], Rp[:], sp_t[qq][1][:], ALU.add, ["Rp", "sp_t%d_1" % qq], ["R"])
                            for j in range(2):
                                kb.act(a_t[qq][j][:], pc[j][:], AF.Exp, ["pc%d" % j], ["a_t%d_%d" % (qq, j)])
                            for j in range(2):
                                dg = kof(q, j) - 4 * c
                                if dg >= 0:
                                    kb.tt(a_t[qq][j][:], a_t[qq][j][:], sbm[dg][:], ALU.mult, ["a_t%d_%d" % (qq, j), "sbm%d" % dg], ["a_t%d_%d" % (qq, j)])
                    kb.cp(ob_t[:], pO[0:64, :], ["pO"], ["ob_t"])

                def post(c):
                    i = c % 2
                    mk_ = "mo%d" % i
                    for blk in range(4):
                        kb.tr(pA[:, blk * 64:(blk + 1) * 64], ob_t[:, blk * 128:(blk + 1) * 128], idf[0:64, 0:64], ["ob_t", "ident_f"], ["pA"], inc=(blk == 3))
                    kb.cp(mo[i][:, :, 128:192], pA[:, 0:256].rearrange("p (b d) -> p b d", d=64), ["pA"], [mk_], eng="act")
                    mof = mo[i][:].rearrange("p b c -> p (b c)")
                    kb.act(sq4[:], mof, AF.Square, [mk_], ["sq4"])
                    kb.op("dve", lambda e: e.tensor_reduce(out=s16[:], in_=sq4[:].rearrange("p (h d) -> p h d", d=64), axis=AX.X, op=ALU.add),
                          ["sq4"], ["s16"])
                    kb.ts(s16[:], s16[:], 1.0 / 64, ALU.mult, ["s16"], ["s16"], s2=EPS, op1=ALU.add)
                    kb.act(s16[:], s16[:], AF.Sqrt, ["s16"], ["s16"])
                    kb.op("dve", lambda e: e.reciprocal(out=s16[:], in_=s16[:]), ["s16"], ["s16"])
                    kb.tt(m14[:].rearrange("p (h d) -> p h d", d=64), mof.rearrange("p (h d) -> p h d", d=64),
                          s16[:].unsqueeze(2).to_broadcast([128, 16, 64]), ALU.mult, [mk_, "s16"], ["m14"])
                    kb.act(sil4[:], rc_c[i][:], AF.Silu, ["rc_c%d" % i], ["sil4"])
                    m14v = m14[:].rearrange("p (b c) -> p b c", c=256)
                    kb.tt(m14v[:, :, 192:256], m14v[:, :, 192:256], sil4[:], ALU.mult, ["m14", "sil4"], ["m14"])
                    kb.tt(mixb4[:].rearrange("p (b c) -> p b c", c=256), m14v, gm[:].unsqueeze(1).to_broadcast([128, 4, 256]), ALU.mult,
                          ["m14", "gm"], ["mixb4"])
                    for t8 in range(8):
                        kb.tr(pT_bf[:, t8 * 128:(t8 + 1) * 128], mixb4[:, t8 * 128:(t8 + 1) * 128], idb[:], ["mixb4", "ident_b"], ["pB"], inc=(t8 == 7))
                    kb.cp(mixT4[:], pT_bf, ["pB"], ["mixT4"], eng="act")
                    for blk in range(4):
                        n = 4 * c + blk
                        pk = "pt%d" % (n % 2)
                        for dh in range(2):
                            pbank, pkey = (pA, "pA") if dh == 0 else (pX, "pX")
                            for kc in range(2):
                                kb.mm(pbank[:], mixT4[:, (2 * blk + kc) * 128:(2 * blk + kc + 1) * 128], Wo[:, kc, dh * 512:(dh + 1) * 512], kc == 0, kc == 1,
                                      ["mixT4", "Wo"], [pkey], inc=(kc == 1))
                            kb.cp(pt[n % 2][:, dh * 512:(dh + 1) * 512], pbank[:], [pkey], [pk], eng=("act" if dh else "dve"))
                        kb.dma("pool", part_loc[n * 128:(n + 1) * 128, :], pt[n % 2][:], reads=[pk], writes=["part_loc"])

                load_xc(0)
                for c in range(NCH):
                    if _STOP == "B0":
                        continue
                    if c + 1 < NCH:
                        load_xc(c + 1)
                    if _STOP == "B0x":
                        continue
                    inproj(c)
                    if _STOP in ("B1", "B1f"):
                        continue
                    swa(c)
                    gla(c)
                    sbk(c)
                    if _STOP == "B2":
                        continue
                    post(c)
                kb.barrier()
                if _STOP not in ("B0", "B0x", "B1", "B1f", "B2", "B3"):
                    kb.coll("ReduceScatter", ALU.add, RG, part_loc, delta_loc, ["part_loc"], ["delta_loc"])
                phase_end()
            if _STOP in ("B", "B0", "B0x", "B1", "B1f", "B2", "B3"):
                break
            with ExitStack() as sc_:
                T = lambda name, shape, dt=F32, _p="L%dC_" % li: sc_.enter_context(nc.sbuf_tensor(_p + name, shape, dt))
                gft = T("gft", [128, 8])
                kb.dma("sp", gft[:], gffn_i[li], writes=["gt"])
                Wq = T("Wq", [128, 8, 2048], BF16); Ks = T("Ks", [128, 16, 64], BF16); ksf = T("ksf", [128, 16, 64])
                kb.dma("sp", ksf[:], ksub_i[li], writes=["ksf"])
                kb.cp(Ks[:], ksf[:], ["ksf"], ["Ks"])
                load_w(Wq, wq_i[li], 8, 2048, "Wq", gt=gft)
                ht = [T("ht%d" % i, [128, D]) for i in range(2)]
                dt_ = [T("dt%d" % i, [128, D]) for i in range(2)]
                h1 = T("h1", [128, D]); junk = T("junk", [128, D], BF16); ss = T("ss", [128, 1])
                xn = T("xn", [128, D], BF16); xnT = T("xnT", [128, D], BF16)
                qT = T("qT", [128, 16, 128], BF16); sct = T("sct", [128, D])
                t1 = T("t1", [128, 8, 16]); t2 = T("t2", [128, 8, 16]); wk1 = T("wk1", [128, 16, 64]); wk2 = T("wk2", [128, 8, 256])
                cand = T("cand", [128, 8, 256]); c8a = T("c8a", [128, 8, 8]); c8b = T("c8b", [128, 8, 8])
                csh = T("csh", [128, 8, 256]); ec = T("ec", [128, 8, 256]); mk8 = T("mk8", [128, 8, 256])
                Z = T("Z", [128, 8]); tb = T("tb", [128, 16])
                pT = ps[0][:].bitcast(BF16)
                for i in range(NT):
                    b = i % 2
                    rs = slice(i * 128, (i + 1) * 128)
                    kb.dma("sp", ht[b][:], hsrc[rs, :], writes=["ht%d" % b])
                    kb.dma("sp", dt_[b][:], delta_loc[rs, :], writes=["dt%d" % b])
                    kb.tt(h1[:], ht[b][:], dt_[b][:], ALU.add, ["ht%d" % b, "dt%d" % b], ["h1"])
                    kb.dma("pool", h1_d[rs, :], h1[:], reads=["h1"], writes=["h1_d"])
                    _rstd(kb, h1[:], junk[:], ss[:], D, ["h1"], "b")
                    kb.ts(xn[:], h1[:], ss[:, 0:1], ALU.mult, ["h1", "bss"], ["xn"])
                    for k in range(8):
                        kb.tr(pT[:, k * 128:(k + 1) * 128], xn[:, k * 128:(k + 1) * 128], idb[:], ["xn", "ident_b"], ["pT"], inc=(k == 7))
                    kb.cp(xnT[:], pT, ["pT"], ["xnT"], eng="act")
                    kb.dma("pool", xT_d[rs, :], xnT[:], reads=["xnT"], writes=["xT_d"])
                    for cg in range(4):
                        pq = ps[3 + cg % 2]
                        for cc in range(4):
                            cidx = cg * 4 + cc
                            for k in range(8):
                                kb.mm(pq[:, cc * 128:(cc + 1) * 128], Wq[:, k, cidx * 128:(cidx + 1) * 128], xnT[:, k * 128:(k + 1) * 128],
                                      k == 0, k == 7, ["Wq", "xnT"], ["pq%d" % (cg % 2)], inc=(k == 7 and cc == 3))
                        kb.cp(qT[:, cg * 4:(cg + 1) * 4, :], pq[:].rearrange("p (c t) -> p c t", t=128), ["pq%d" % (cg % 2)], ["qT"],
                              eng=("act" if cg % 2 else "dve"))
                    for cidx in range(16):
                        pscb = ps[5 + cidx // 8]
                        kb.mm(pscb[:, (cidx % 8) * 64:(cidx % 8 + 1) * 64], qT[:, cidx, :], Ks[:, cidx, :], True, True, ["qT", "Ks"],
                              ["psc%d" % (cidx // 8)], inc=(cidx % 8 == 7))
                    kb.cp(sct[:, 0:512], ps[5][:], ["psc0"], ["sct"], eng="act")
                    kb.cp(sct[:, 512:1024], ps[6][:], ["psc1"], ["sct"], eng="dve")
                    kb.dma("pool", sc_d[rs, :], sct[:], reads=["sct"], writes=["sc_d"])
                    chains = [(hd, side, (t1, t2)[side]) for hd in range(8) for side in range(2)]
                    tkeys = ["tt%d_%d" % (side, hd) for hd, side, _ in chains]
                    for ci_, (hd, side, tt_) in enumerate(chains):
                        sv = sct[:, hd * 128 + side * 64:hd * 128 + side * 64 + 64]
                        kb.op("dve", lambda e, o_=tt_[:, hd, 0:8], i_=sv: e.max(out=o_, in_=i_), ["sct"], [tkeys[ci_]])
                    for ci_, (hd, side, tt_) in enumerate(chains):
                        sv = sct[:, hd * 128 + side * 64:hd * 128 + side * 64 + 64]
                        kb.op("dve", lambda e, o_=wk1[:, ci_, :], r_=tt_[:, hd, 0:8], i_=sv: e.match_replace(out=o_, in_to_replace=r_, in_values=i_, imm_value=-1e30),
                              ["sct", tkeys[ci_]], ["wk1_%d" % ci_])
                    for ci_, (hd, side, tt_) in enumerate(chains):
                        kb.op("dve", lambda e, o_=tt_[:, hd, 8:16], i_=wk1[:, ci_, :]: e.max(out=o_, in_=i_), ["wk1_%d" % ci_], [tkeys[ci_]])
                    kb.tt(cand[:].rearrange("p h (a b) -> p h a b", b=16), t1[:].unsqueeze(3).to_broadcast([128, 8, 16, 16]),
                          t2[:].unsqueeze(2).to_broadcast([128, 8, 16, 16]), ALU.add, tkeys, ["cand"])
                    ckeys = ["c8_%d" % hd for hd in range(8)]
                    for hd in range(8):
                        kb.op("dve", lambda e, o_=c8a[:, hd, :], i_=cand[:, hd, :]: e.max(out=o_, in_=i_), ["cand"], [ckeys[hd]])
                    for hd in range(8):
                        kb.op("dve", lambda e, o_=wk2[:, hd, :], r_=c8a[:, hd, :], i_=cand[:, hd, :]: e.match_replace(out=o_, in_to_replace=r_, in_values=i_, imm_value=-1e30),
                              ["cand", ckeys[hd]], ["wk2_%d" % hd])
                    for hd in range(8):
                        kb.op("dve", lambda e, o_=c8b[:, hd, :], i_=wk2[:, hd, :]: e.max(out=o_, in_=i_), ["wk2_%d" % hd], [ckeys[hd]])
                    kb.tt(csh[:], cand[:], c8a[:, :, 0:1].to_broadcast([128, 8, 256]), ALU.subtract, ["cand"] + ckeys, ["csh"])
                    kb.act(ec[:], csh[:], AF.Exp, ["csh"], ["ec"])
                    kb.tt(mk8[:], cand[:], c8b[:, :, 7:8].to_broadcast([128, 8, 256]), ALU.is_ge, ["cand"] + ckeys, ["mk8"])
                    kb.tt(ec[:], ec[:], mk8[:], ALU.mult, ["ec", "mk8"], ["ec"])
                    kb.op("dve", lambda e: e.tensor_reduce(out=Z[:], in_=ec[:], axis=AX.X, op=ALU.add), ["ec"], ["Z"])
                    kb.act(Z[:], Z[:], AF.Ln, ["Z"], ["Z"])
                    kb.cp(tb[:, 0:8], c8b[:, :, 7], ckeys, ["tb"])
                    kb.stt(tb[:, 8:16], c8a[:, :, 0], -1.0, Z[:], ALU.mult, ALU.subtract, ckeys + ["Z"], ["tb"])
                    kb.dma("pool", tb_d[rs, :], tb[:], reads=["tb"], writes=["tb_d"])
                phase_end()
            if _STOP == "C":
                break
            with ExitStack() as sd_:
                T = lambda name, shape, dt=F32, _p="L%dD_" % li: sd_.enter_context(nc.sbuf_tensor(_p + name, shape, dt))
                gft = T("gft", [128, 8])
                kb.dma("sp", gft[:], gffn_i[li], writes=["gt"])
                Ub = T("Ub", [128, 8, 4096], BF16); Vv = T("Vv", [128, 32, D], BF16)
                load_w(Ub, uT_i[li], 8, 4096, "Ub", gt=gft)
                load_w(Vv, v_i[li], 32, D, "Vv")
                gf = T("gf", [128, D])
                if final:
                    kb.dma("sp", gf[:], gfin, writes=["gf"])
                h1t = [T("h1t%d" % i, [128, D]) for i in range(2)]
                sct = [T("sctb%d" % i, [128, D]) for i in range(2)]
                xT = [T("xTb%d" % i, [128, D], BF16) for i in range(2)]
                tbt = [T("tbt%d" % i, [128, 16]) for i in range(2)]
                NBG = 4
                Sg = [T("Sg%d" % i, [128, 16, 64]) for i in range(NBG)]
                Eg = [T("Eg%d" % i, [128, 1024], BF16) for i in range(NBG)]
                Gh = [T("Gh%d" % i, [128, 1024], BF16) for i in range(2)]; G = T("G", [128, 1024], BF16)
                gl = [T("gl%d" % i, [128, 512]) for i in range(2)]
                Wb = T("Wb", [128, 1024], BF16); WT = T("WT", [128, 1024], BF16)
                ho = T("ho", [128, D]); junk = T("junkb", [128, D], BF16); ss = T("ssb", [128, 1])
                pH = [ps[0], ps[1]]; ptr = ps[2][:].bitcast(BF16); po = [ps[3], ps[4]]

                def loadB(i):
                    b = i % 2
                    rs = slice(i * 128, (i + 1) * 128)
                    kb.dma("sp", h1t[b][:], h1_d[rs, :], writes=["h1t%d" % b])
                    kb.dma("sp", sct[b][:], sc_d[rs, :], writes=["sctb%d" % b])
                    kb.dma("sp", xT[b][:], xT_d[rs, :], writes=["xTb%d" % b])
                    kb.dma("sp", tbt[b][:], tb_d[rs, :], writes=["tbt%d" % b])

                loadB(0)
                cnt = 0
                for i in range(NT):
                    b = i % 2
                    rs = slice(i * 128, (i + 1) * 128)
                    if i + 1 < NT:
                        loadB(i + 1)
                    for eq in range(4):
                        deferred = None
                        for hd in range(8):
                            j = cnt % NBG
                            gj = cnt % 2
                            cnt += 1
                            s1 = sct[b][:, hd * 128 + 16 * eq:hd * 128 + 16 * eq + 16]
                            s2 = sct[b][:, hd * 128 + 64:hd * 128 + 128]
                            kb.tt(Sg[j][:], s1.unsqueeze(2).to_broadcast([128, 16, 64]), s2.unsqueeze(1).to_broadcast([128, 16, 64]), ALU.add,
                                  ["sctb%d" % b], ["Sg%d" % j], eng=("pool" if hd in _POOL_HEADS else "dve"))
                            Sf = Sg[j][:].rearrange("p a b -> p (a b)")
                            kb.act(Eg[j][:], Sf, AF.Exp, ["Sg%d" % j, "tbt%d" % b], ["Eg%d" % j], bias=tbt[b][:, 8 + hd:9 + hd], scale=1.0)
                            if hd == 0:
                                kb.stt(G[:], Sf, tbt[b][:, hd:hd + 1], Eg[j][:], ALU.is_ge, ALU.mult, ["Sg%d" % j, "Eg%d" % j, "tbt%d" % b], ["G"])
                            else:
                                kb.stt(Gh[gj][:], Sf, tbt[b][:, hd:hd + 1], Eg[j][:], ALU.is_ge, ALU.mult, ["Sg%d" % j, "Eg%d" % j, "tbt%d" % b], ["Gh%d" % gj])
                                if deferred is not None:
                                    deferred()
                                deferred = (lambda gj=gj: kb.tt(G[:], G[:], Gh[gj][:], ALU.add, ["G", "Gh%d" % gj], ["G"]))
                        if deferred is not None:
                            deferred()
                        for g2 in range(2):
                            e0 = eq * 1024 + g2 * 512
                            for k in range(8):
                                kb.mm(pH[g2][:], xT[b][:, k * 128:(k + 1) * 128], Ub[:, k, e0:e0 + 512], k == 0, k == 7,
                                      ["xTb%d" % b, "Ub"], ["pH%d" % g2], inc=(k == 7))
                            kb.act(gl[g2][:], pH[g2][:], AF.Gelu, ["pH%d" % g2], ["gl%d" % g2])
                            kb.tt(Wb[:, g2 * 512:(g2 + 1) * 512], gl[g2][:], G[:, g2 * 512:(g2 + 1) * 512], ALU.mult, ["gl%d" % g2, "G"], ["Wb"],
                                  eng=_os.environ.get("WMULT_ENG", "pool"))
                        for cc in range(8):
                            kb.tr(ptr[:, cc * 128:(cc + 1) * 128], Wb[:, cc * 128:(cc + 1) * 128], idb[:], ["Wb", "ident_b"], ["ptr"], inc=(cc == 7))
                        kb.cp(WT[:], ptr, ["ptr"], ["WT"], eng="act")
                        for dh in range(2):
                            for cc in range(8):
                                kb.mm(po[dh][:], WT[:, cc * 128:(cc + 1) * 128], Vv[:, eq * 8 + cc, dh * 512:(dh + 1) * 512],
                                      (eq == 0 and cc == 0), (eq == 3 and cc == 7), ["WT", "Vv"], ["po%d" % dh], inc=(cc == 7))
                    for dh in range(2):
                        kb.tt(ho[:, dh * 512:(dh + 1) * 512], h1t[b][:, dh * 512:(dh + 1) * 512], po[dh][:], ALU.add,
                              ["h1t%d" % b, "po%d" % dh], ["ho"])
                    if final:
                        _rstd(kb, ho[:], junk[:], ss[:], D, ["ho"], "f")
                        kb.stt(ho[:], ho[:], ss[:, 0:1], gf[:], ALU.mult, ALU.mult, ["ho", "fss", "gf"], ["ho"])
                    kb.dma("sp", hdst[rs, :], ho[:], reads=["ho"], writes=["hdst"])
                phase_end()
    return nc


def forward_fused(x, meta_tokens, attn_norm, w_in, attn_sinks, gla_gate_w2, gla_gate_b, swa_out_norm,
                  sb_out_norm, gla_out_norm, w_out, ffn_norm, peer_w_q, peer_sub_keys, peer_u, peer_v, final_norm, runner=None):
    f32 = lambda a: np.asarray(a, np.float32)
    x = f32(x)
    B, SEQ, _ = x.shape
    depth = attn_norm.shape[0]
    L = SEQ + 128
    Lp = ((L + 511) // 512) * 512
    TQ = Lp // 4
    assert B * 4 == NCORES
    nc = _prog_fused(Lp, depth)
    w_in, w_out = f32(w_in), f32(w_out)
    shared = {
        "g_attn": _c(np.stack([_gk(attn_norm[i]) for i in range(depth)])),
        "gffn": _c(np.stack([_gk(ffn_norm[i]) for i in range(depth)])),
        "wq": _c(f32(peer_w_q)),
        "ksub": _c(np.stack([np.transpose(f32(peer_sub_keys[i]).reshape(16, 64, 128), (2, 0, 1)) for i in range(depth)])),
        "uT": _c(np.transpose(f32(peer_u), (0, 2, 1))), "v": _c(f32(peer_v)),
        "gfin": _c(np.broadcast_to(f32(final_norm)[None, :], (128, D))),
    }
    per_j = []
    for j in range(4):
        kv = j // 2
        cols = np.concatenate([np.arange(128 * j, 128 * j + 128), np.arange(512 + 64 * kv, 512 + 64 * kv + 64),
                               np.arange(512 + 64 * kv, 512 + 64 * kv + 64), np.arange(768 + 64 * j, 768 + 64 * j + 64),
                               np.arange(1024 + 64 * j, 1024 + 64 * j + 64), np.arange(1536 + 32 * j, 1536 + 32 * j + 32),
                               np.arange(1664 + 32 * j, 1664 + 32 * j + 32), np.arange(2048, 2064),
                               np.arange(640 + 64 * kv, 640 + 64 * kv + 64), np.arange(1280 + 64 * j, 1280 + 64 * j + 64),
                               np.arange(1792 + 64 * j, 1792 + 64 * j + 64), np.arange(2064 + 64 * j, 2064 + 64 * j + 64)])
        assert len(cols) == CJ
        rows = np.concatenate([np.arange(128 * j, 128 * j + 128), np.arange(512 + 64 * j, 512 + 64 * j + 64),
                               np.arange(768 + 64 * j, 768 + 64 * j + 64)])
        gm = np.stack([np.concatenate([f32(swa_out_norm[i])[128 * j:128 * j + 128], f32(sb_out_norm[i])[64 * j:64 * j + 64],
                                       f32(gla_out_norm[i])[64 * j:64 * j + 64]]) for i in range(depth)])
        per_j.append({
            "w_in": _c(w_in[:, :, cols]),
            "w2": _c(f32(gla_gate_w2)[:, :, 32 * j:32 * j + 32]),
            "gb": _c(f32(gla_gate_b)[:, 32 * j:32 * j + 32].reshape(depth, 32, 1)),
            "sinks": _c(np.broadcast_to(f32(attn_sinks)[:, None, 2 * j:2 * j + 2], (depth, 128, 2))),
            "gmix": _c(np.broadcast_to(gm[:, None, :], (depth, 128, 256))),
            "wout": _c(np.transpose(w_out[:, rows, :].reshape(depth, 2, 128, D), (0, 2, 1, 3))),
        })
    maps = []
    for c in range(NCORES):
        b, r = c // 4, c % 4
        hp = np.zeros((Lp, D), np.float32)
        hp[112:128] = f32(meta_tokens)
        hp[128:L] = x[b]
        maps.append(dict(shared, **per_j[r], h0=_c(hp[r * TQ:(r + 1) * TQ])))
    res = (runner or _run)(nc, maps)
    out = np.zeros((B, SEQ, D), np.float32)
    for b in range(B):
        full = np.concatenate([np.asarray(res[b * 4 + r]["out"]) for r in range(4)], 0)
        out[b] = full[128:L]
    return out


def _prog_fused(Lp, depth):
    key = ("fused", Lp, depth)
    if key not in _CACHE:
        _CACHE[key] = build_fused(Lp, depth)
    return _CACHE[key]
```
